# Optimizing a Trainium2 kernel written in Bass

```python
import math
import jax, jax.numpy as jnp
from jax import lax
import numpy as np

D_MODEL = 1024
BATCH = 8
SEQ = 4096
DEPTH = 2

N_MIXERS = 2
EPS = 1e-6

HYENA_ORDER = 2
HYENA_EMB_DIM = 33
HYENA_BANDS = (HYENA_EMB_DIM - 1) // 2
HYENA_FILTER_HIDDEN = 64
HYENA_SHORT_CONV = 3
HYENA_FAST_DECAY_PCT = 0.3
HYENA_SLOW_DECAY_PCT = 1.5
HYENA_DECAY_TARGET = 1e-2
HYENA_N_DIRS = 2

MLA_HEADS = 8
QK_NOPE = 128
QK_ROPE = 64
V_HEAD = 128
Q_LORA = 256
KV_LORA = 128
ROPE_THETA = 10000.0
Q_BLOCK = 128
SOFTMAX_SCALE = 1.0 / math.sqrt(QK_NOPE + QK_ROPE)

N_EXPERTS = 16
EXPERT_FF = 1024
EC_CAPACITY_FACTOR = 2

kernel_name = 'hybrid_hyena_mla_ecmoe_encoder'


def rmsnorm(x, g):
    xf = x.astype(jnp.float32)
    y = xf * lax.rsqrt(jnp.mean(xf * xf, axis=-1, keepdims=True) + EPS)
    return (y * g.astype(jnp.float32)).astype(x.dtype)


def modulate(h, shift, scale):
    return h * (1 + scale[:, None, :]) + shift[:, None, :]


def short_conv(u, w, b):
    up = jnp.pad(u, ((0, 0), (1, 1), (0, 0)))
    return w[0] * up[:, :-2] + w[1] * up[:, 1:-1] + w[2] * up[:, 2:] + b


def hyena_pos_features(L):
    t = jnp.linspace(0.0, 1.0, L, dtype=jnp.float32)[:, None]
    w = 2.0 * math.pi * jnp.arange(L, dtype=jnp.float32)[:, None] / L
    f = jnp.linspace(1e-4, HYENA_BANDS - 1, HYENA_BANDS, dtype=jnp.float32)[None, :]
    ang = f * w
    return jnp.concatenate([t, jnp.cos(ang), -jnp.sin(ang)], axis=-1)


def hyena_filters(L, w1, b1, w2, b2, w3, freq):
    z = hyena_pos_features(L)
    fr = freq.astype(jnp.float32)
    h = jnp.sin(fr * (z @ w1.astype(jnp.float32) + b1.astype(jnp.float32)))
    h = jnp.sin(fr * (h @ w2.astype(jnp.float32) + b2.astype(jnp.float32)))
    h = (h @ w3.astype(jnp.float32)).reshape(L, HYENA_ORDER - 1, HYENA_N_DIRS, D_MODEL)
    t = jnp.linspace(0.0, 1.0, L, dtype=jnp.float32)[:, None]
    min_decay = math.log(HYENA_DECAY_TARGET) / HYENA_FAST_DECAY_PCT
    max_decay = math.log(HYENA_DECAY_TARGET) / HYENA_SLOW_DECAY_PCT
    deltas = jnp.linspace(min_decay, max_decay, D_MODEL, dtype=jnp.float32)[None, :]
    decay = jnp.exp(-t * jnp.abs(deltas))
    return h * decay[:, None, None, :]


def bidir_long_conv(z, h_fwd, h_bwd, bias):
    L = z.shape[1]
    k = jnp.concatenate([h_fwd, jnp.zeros_like(h_fwd[:1]), h_bwd[:0:-1]], axis=0)
    kf = jnp.fft.rfft(k, n=2 * L, axis=0)
    zf = jnp.fft.rfft(z.astype(jnp.float32), n=2 * L, axis=1)
    y = jnp.fft.irfft(zf * kf[None], n=2 * L, axis=1)[:, :L]
    return (y + z.astype(jnp.float32) * bias.astype(jnp.float32)).astype(z.dtype)


def hyena_mixer(h, w_in, b_in, conv_w, conv_b, f_w1, f_b1, f_w2, f_b2, f_w3, f_freq, f_bias, w_out, b_out):
    L = h.shape[1]
    u = short_conv(h @ w_in + b_in, conv_w, conv_b)
    parts = jnp.split(u, HYENA_ORDER + 1, axis=-1)
    filt = hyena_filters(L, f_w1, f_b1, f_w2, f_b2, f_w3, f_freq)
    z = parts[-1]
    for o in range(HYENA_ORDER - 1):
        z = z * parts[HYENA_ORDER - 1 - o]
        z = bidir_long_conv(z, filt[:, o, 0], filt[:, o, 1], f_bias[o])
    y = z * parts[0]
    return y @ w_out + b_out


def apply_rope(x, cos, sin):
    half = x.shape[-1] // 2
    x1, x2 = x[..., :half], x[..., half:]
    return jnp.concatenate([x1 * cos - x2 * sin, x1 * sin + x2 * cos], axis=-1).astype(x.dtype)


def mla_mixer(h, positions, w_in, q_norm_g, w_qb, kv_norm_g, w_kvb, w_out):
    B, S, _ = h.shape
    a = h @ w_in
    cq, ckv, k_pe = jnp.split(a, [Q_LORA, Q_LORA + KV_LORA], axis=-1)
    q = (rmsnorm(cq, q_norm_g) @ w_qb).reshape(B, S, MLA_HEADS, QK_NOPE + QK_ROPE)
    q_nope, q_pe = q[..., :QK_NOPE], q[..., QK_NOPE:]
    kv = (rmsnorm(ckv, kv_norm_g) @ w_kvb).reshape(B, S, MLA_HEADS, QK_NOPE + V_HEAD)
    k_nope, v = kv[..., :QK_NOPE], kv[..., QK_NOPE:]
    inv_freq = ROPE_THETA ** (-jnp.arange(0, QK_ROPE, 2, dtype=jnp.float32) / QK_ROPE)
    ang = positions.astype(jnp.float32)[..., None] * inv_freq
    cos, sin = jnp.cos(ang), jnp.sin(ang)
    q_pe = apply_rope(q_pe, cos[:, :, None, :], sin[:, :, None, :])
    k_pe = apply_rope(k_pe, cos, sin)
    nb = S // Q_BLOCK
    qn_b = q_nope.reshape(B, nb, Q_BLOCK, MLA_HEADS, QK_NOPE).swapaxes(0, 1)
    qp_b = q_pe.reshape(B, nb, Q_BLOCK, MLA_HEADS, QK_ROPE).swapaxes(0, 1)

    def attend(blk):
        qn, qp = blk
        s = jnp.einsum('bqhd,bkhd->bhqk', qn, k_nope) + jnp.einsum('bqhr,bkr->bhqk', qp, k_pe)
        p = jax.nn.softmax(s.astype(jnp.float32) * SOFTMAX_SCALE, axis=-1).astype(v.dtype)
        return jnp.einsum('bhqk,bkhd->bqhd', p, v)

    o = lax.map(attend, (qn_b, qp_b))
    o = o.swapaxes(0, 1).reshape(B, S, MLA_HEADS * V_HEAD)
    return o @ w_out


def ec_moe(h, w_router, w_gate, w_up, w_down):
    B, T, D = h.shape
    C = EC_CAPACITY_FACTOR * T // N_EXPERTS
    aff = jax.nn.softmax(jnp.einsum('btd,de->bte', h, w_router).astype(jnp.float32), axis=-1)
    g, idx = lax.top_k(aff.swapaxes(1, 2), C)
    xs = jax.vmap(lambda hb, ib: hb[ib])(h, idx)
    hid = jax.nn.silu(jnp.einsum('becd,edf->becf', xs, w_gate)) * jnp.einsum('becd,edf->becf', xs, w_up)
    ye = jnp.einsum('becf,efd->becd', hid, w_down) * g[..., None].astype(h.dtype)
    return jax.vmap(lambda yb, ib: jnp.zeros((T, D), yb.dtype).at[ib.reshape(-1)].add(yb.reshape(-1, D)))(ye, idx)


def setup_inputs(seed: int = 0) -> dict:
    key = jax.random.key(seed)
    ks = jax.random.split(key, 40)
    D = D_MODEL
    nh = (DEPTH + 1) // 2
    nm = DEPTH // 2
    nrm = lambda k, shape, s: jax.random.normal(k, shape, jnp.float32) * s
    HID = HYENA_FILTER_HIDDEN
    return {
        'x': nrm(ks[0], (BATCH, SEQ, D), 1.0),
        'c': nrm(ks[1], (BATCH, D), 1.0),
        'positions': jnp.broadcast_to(jnp.arange(SEQ, dtype=jnp.int32), (BATCH, SEQ)),
        'ada_w': nrm(ks[2], (DEPTH, D, 6 * D), 0.5 * D ** -0.5),
        'ada_b': nrm(ks[3], (DEPTH, 6 * D), 0.02),
        'norm_mix_g': 1.0 + nrm(ks[4], (DEPTH, D), 0.02),
        'norm_ffn_g': 1.0 + nrm(ks[5], (DEPTH, D), 0.02),
        'hy_w_in': nrm(ks[6], (nh, D, (HYENA_ORDER + 1) * D), D ** -0.5),
        'hy_b_in': nrm(ks[7], (nh, (HYENA_ORDER + 1) * D), 0.02),
        'hy_conv_w': nrm(ks[8], (nh, HYENA_SHORT_CONV, (HYENA_ORDER + 1) * D), HYENA_SHORT_CONV ** -0.5),
        'hy_conv_b': nrm(ks[9], (nh, (HYENA_ORDER + 1) * D), 0.02),
        'hy_f_w1': nrm(ks[10], (nh, HYENA_EMB_DIM, HID), HYENA_EMB_DIM ** -0.5),
        'hy_f_b1': nrm(ks[11], (nh, HID), 0.1),
        'hy_f_w2': nrm(ks[12], (nh, HID, HID), HID ** -0.5),
        'hy_f_b2': nrm(ks[13], (nh, HID), 0.1),
        'hy_f_w3': nrm(ks[14], (nh, HID, (HYENA_ORDER - 1) * HYENA_N_DIRS * D), 0.02 * HID ** -0.5),
        'hy_f_freq': 1.0 + nrm(ks[15], (nh, HID), 0.02),
        'hy_f_bias': nrm(ks[16], (nh, HYENA_ORDER - 1, D), 0.1),
        'hy_w_out': nrm(ks[17], (nh, D, D), D ** -0.5),
        'hy_b_out': nrm(ks[18], (nh, D), 0.02),
        'mla_w_in': nrm(ks[19], (nm, D, Q_LORA + KV_LORA + QK_ROPE), D ** -0.5),
        'mla_q_norm_g': 1.0 + nrm(ks[20], (nm, Q_LORA), 0.02),
        'mla_w_qb': nrm(ks[21], (nm, Q_LORA, MLA_HEADS * (QK_NOPE + QK_ROPE)), Q_LORA ** -0.5),
        'mla_kv_norm_g': 1.0 + nrm(ks[22], (nm, KV_LORA), 0.02),
        'mla_w_kvb': nrm(ks[23], (nm, KV_LORA, MLA_HEADS * (QK_NOPE + V_HEAD)), KV_LORA ** -0.5),
        'mla_w_out': nrm(ks[24], (nm, MLA_HEADS * V_HEAD, D), (MLA_HEADS * V_HEAD) ** -0.5),
        'moe_w_router': nrm(ks[25], (DEPTH, D, N_EXPERTS), D ** -0.5),
        'moe_w_gate': nrm(ks[26], (DEPTH, N_EXPERTS, D, EXPERT_FF), D ** -0.5),
        'moe_w_up': nrm(ks[27], (DEPTH, N_EXPERTS, D, EXPERT_FF), D ** -0.5),
        'moe_w_down': nrm(ks[28], (DEPTH, N_EXPERTS, EXPERT_FF, D), EXPERT_FF ** -0.5),
        'final_norm_g': 1.0 + nrm(ks[29], (D,), 0.02),
    }


def reference(x, c, positions, ada_w, ada_b, norm_mix_g, norm_ffn_g,
              hy_w_in, hy_b_in, hy_conv_w, hy_conv_b, hy_f_w1, hy_f_b1, hy_f_w2, hy_f_b2,
              hy_f_w3, hy_f_freq, hy_f_bias, hy_w_out, hy_b_out,
              mla_w_in, mla_q_norm_g, mla_w_qb, mla_kv_norm_g, mla_w_kvb, mla_w_out,
              moe_w_router, moe_w_gate, moe_w_up, moe_w_down, final_norm_g):
    cs = jax.nn.silu(c)
    for i in range(DEPTH):
        mod = cs @ ada_w[i] + ada_b[i]
        sh1, sc1, g1, sh2, sc2, g2 = jnp.split(mod, 6, axis=-1)
        hm = modulate(rmsnorm(x, norm_mix_g[i]), sh1, sc1)
        j = i // N_MIXERS
        if i % N_MIXERS == 0:
            y = hyena_mixer(hm, hy_w_in[j], hy_b_in[j], hy_conv_w[j], hy_conv_b[j],
                            hy_f_w1[j], hy_f_b1[j], hy_f_w2[j], hy_f_b2[j], hy_f_w3[j],
                            hy_f_freq[j], hy_f_bias[j], hy_w_out[j], hy_b_out[j])
        else:
            y = mla_mixer(hm, positions, mla_w_in[j], mla_q_norm_g[j], mla_w_qb[j],
                          mla_kv_norm_g[j], mla_w_kvb[j], mla_w_out[j])
        x = x + g1[:, None, :] * y
        hf = modulate(rmsnorm(x, norm_ffn_g[i]), sh2, sc2)
        x = x + g2[:, None, :] * ec_moe(hf, moe_w_router[i], moe_w_gate[i], moe_w_up[i], moe_w_down[i])
    return rmsnorm(x, final_norm_g)
```

```python
import math
from contextlib import ExitStack

import ml_dtypes
import numpy as np

import concourse.bass as bass
import concourse.mybir as mybir
from concourse.bass_utils import run_bass_kernel_spmd

F32 = mybir.dt.float32
BF16 = mybir.dt.bfloat16
I32 = mybir.dt.int32
U32 = mybir.dt.uint32
AF = mybir.ActivationFunctionType
ALU = mybir.AluOpType
AX = mybir.AxisListType

S = 4096
D = 1024
NT = S // 128
EPS = 1e-6
NEXP = 16
CAP = 512
TWO_PI = 2.0 * math.pi


class Buf:
    __slots__ = ("w", "r")

    def __init__(self):
        self.w = None
        self.r = []


class Prog:
    NQ = 6

    def __init__(self, nc, es):
        self.nc = nc
        self.es = es
        self.eng = {"pe": nc.tensor, "act": nc.scalar, "dve": nc.vector,
                    "pool": nc.gpsimd, "sp": nc.sync}
        self.sem = {}
        self.cnt = {}
        self.nsem = 0
        for e in ("pe", "act", "dve", "pool"):
            self.sem[e] = self._newsem()
            self.cnt[e] = 0
        self.waited = {e: {} for e in self.eng}
        self.dq = {}
        self.dqi = {}
        for q in ("sp", "act", "pool"):
            self.dq[q] = [[self._newsem(), 0] for _ in range(16 if q == "pool" else self.NQ)]
            self.dqi[q] = 0

    def _newsem(self):
        self.nsem += 1
        return self.es.enter_context(self.nc.semaphore(f"sm{self.nsem}"))

    def _wait(self, e, tok):
        sem, val, src = tok
        if src == "pe" and e == "pe":
            return
        key = id(sem)
        if self.waited[e].get(key, 0) >= val:
            return
        self.eng[e].wait_ge(sem, val)
        self.waited[e][key] = val

    def _deps(self, e, reads, writes):
        for b in reads:
            if b.w is not None:
                self._wait(e, b.w)
        for b in writes:
            if b.w is not None:
                self._wait(e, b.w)
            for t in b.r:
                self._wait(e, t)

    def _record(self, tok, reads, writes):
        for b in reads:
            b.r = [t for t in b.r if t[0] is not tok[0]]
            b.r.append(tok)
        for b in writes:
            b.w = tok
            b.r = []

    def op(self, e, fn, reads=(), writes=()):
        self._deps(e, reads, writes)
        ins = fn(self.eng[e])
        self.cnt[e] += 1
        ins.then_inc(self.sem[e], 1)
        tok = (self.sem[e], self.cnt[e], e)
        self._record(tok, reads, writes)
        return tok

    def dma(self, q, out, in_, reads=(), writes=(), fn=None, **kw):
        slot = self.dq[q][self.dqi[q] % len(self.dq[q])]
        self.dqi[q] += 1
        sem, c = slot
        if c > 0:
            self._wait(q, (sem, c, None))
        self._deps(q, reads, writes)
        if fn is None:
            ins = self.eng[q].dma_start(out=out, in_=in_, **kw)
        else:
            ins = fn(self.eng[q])
        ins.then_inc(sem, 16)
        slot[1] = c + 16
        tok = (sem, c + 16, None)
        self._record(tok, reads, writes)
        return tok

    def barrier(self):
        toks = [(self.sem[e], self.cnt[e], None) for e in self.sem if self.cnt[e] > 0]
        for q in self.dq:
            for sem, c in self.dq[q]:
                if c > 0:
                    toks.append((sem, c, None))
        for e in self.eng:
            for t in toks:
                self._wait(e, t)
        for e in self.sem:
            if self.cnt[e] > 6000:
                self.sem[e] = self._newsem()
                self.cnt[e] = 0
        for q in self.dq:
            for slot in self.dq[q]:
                if slot[1] > 6000:
                    slot[0] = self._newsem()
                    slot[1] = 0


def _consts():
    c = {}
    c["ident"] = np.eye(128, dtype=np.float32)
    L = S
    t = np.linspace(0.0, 1.0, L, dtype=np.float32)[:, None]
    w = (2.0 * math.pi * np.arange(L, dtype=np.float32)[:, None] / L).astype(np.float32)
    f = np.linspace(1e-4, 15.0, 16, dtype=np.float32)[None, :]
    ang = (f * w).astype(np.float32)
    z = np.concatenate([t, np.cos(ang), -np.sin(ang)], axis=-1).astype(np.float32)
    c["zfT"] = np.ascontiguousarray(z.T)
    c["tneg"] = np.ascontiguousarray((-t[:, 0]).reshape(NT, 128).T)
    mind = math.log(1e-2) / 0.3
    maxd = math.log(1e-2) / 1.5
    deltas = np.linspace(mind, maxd, D, dtype=np.float32)
    c["absd"] = np.abs(deltas).astype(np.float32)
    fi = np.arange(4096, dtype=np.int64)
    ti = np.arange(4096, dtype=np.int64)
    m = ((2 * fi[None, :] + 1) * ti[:, None]) % 16384
    angm = m.astype(np.float64) * (math.pi / 8192.0)
    cosm = np.cos(angm)
    sinm = np.sin(angm)
    fw = np.stack([cosm, -sinm], axis=0)
    fw = fw.reshape(2, NT, 128, 32, 128)
    c["FWD"] = np.ascontiguousarray(fw.transpose(3, 2, 0, 1, 4)).astype(ml_dtypes.bfloat16)
    sc = 2.0 / 8192.0
    iv = np.stack([cosm * sc, -sinm * sc], axis=0)
    iv = iv.reshape(2, NT, 128, 32, 128)
    c["INV"] = np.ascontiguousarray(iv.transpose(1, 4, 3, 0, 2)).astype(ml_dtypes.bfloat16)
    invf = (10000.0 ** (-np.arange(0, 64, 2, dtype=np.float32) / 64)).astype(np.float32)
    c["invf"] = invf
    return c


_CONSTS = None


def build(stage="all", debug=False):
    nc = bass.Bass("TRN2", target_bir_lowering=False)
    T = {}

    def din(name, shape, dt=F32):
        T[name] = nc.dram_tensor(name, list(shape), dt, kind="ExternalInput").ap()
        return T[name]

    def dscr(name, shape, dt=F32):
        T[name] = nc.dram_tensor(name, list(shape), dt).ap()
        return T[name]

    din("x", [S, D]); din("c", [1, D]); din("pos", [S], I32)
    din("ada_w", [2, D, 6 * D]); din("ada_b", [2, 6 * D])
    din("norm_mix_g", [2, D]); din("norm_ffn_g", [2, D])
    din("hy_w_in", [1, D, 3 * D]); din("hy_b_in", [1, 3 * D]); din("hy_conv_w", [1, 3, 3 * D])
    din("hy_conv_b", [1, 3 * D]); din("hy_f_w1", [1, 33, 64]); din("hy_f_b1", [1, 64])
    din("hy_f_w2", [1, 64, 64]); din("hy_f_b2", [1, 64]); din("hy_f_w3", [1, 64, 2 * D])
    din("hy_f_freq", [1, 64]); din("hy_f_bias", [1, 1, D]); din("hy_w_out", [1, D, D]); din("hy_b_out", [1, D])
    din("mla_w_in", [1, D, 448]); din("mla_q_norm_g", [1, 256]); din("mla_w_qb", [1, 256, 1536])
    din("mla_kv_norm_g", [1, 128]); din("mla_w_kvb", [1, 128, 2048]); din("mla_w_out", [1, D, D])
    din("moe_w_router", [2, D, NEXP]); din("moe_w_gate", [2, NEXP, D, D]); din("moe_w_up", [2, NEXP, D, D])
    din("moe_w_down", [2, NEXP, D, D]); din("final_norm_g", [D])
    din("ident", [128, 128]); din("zfT", [33, S]); din("tneg", [128, NT]); din("absd", [D])
    din("FWD", [32, 128, 2, 32, 128], BF16); din("INV", [32, 128, 32, 2, 128], BF16); din("invf", [32])
    OUT = nc.dram_tensor("out", [S, D], F32, kind="ExternalOutput").ap()

    dscr("MOD", [2, 6 * D])
    dscr("ZT", [S, D], BF16)
    dscr("X0C", [D, S], BF16)
    dscr("KS", [2, S, D])
    dscr("KT", [2, S, D], BF16)
    dscr("YS", [32, 128, 2, D], BF16)
    dscr("XA", [S, D]); dscr("XB", [S, D]); dscr("XC", [S, D])
    dscr("HF", [S, D], BF16); dscr("ACC", [S, D])
    dscr("QN", [8, 128, S], BF16); dscr("KN", [8, 128, S], BF16); dscr("QP", [8, 64, S], BF16)
    dscr("KP", [64, S], BF16); dscr("V", [S, D], BF16)

    es = ExitStack()
    with es:
        P = Prog(nc, es)
        ps = [es.enter_context(nc.psum_tensor(f"ps{i}", [128, 512], F32)) for i in range(8)]
        psB = [Buf() for _ in range(8)]
        psi = [0]

        def bank():
            i = psi[0] % 8
            psi[0] += 1
            return ps[i], psB[i]

        sbn = [0]

        def sb(st, name, shape, dt):
            sbn[0] += 1
            return st.enter_context(nc.sbuf_tensor(f"{name}_{sbn[0]}", list(shape), dt))

        identf = sb(es, "identf", [128, 128], F32); identfB = Buf()
        identb = sb(es, "identb", [128, 128], BF16); identbB = Buf()
        P.dma("sp", identf[:], T["ident"][:, :], writes=[identfB])
        P.dma("pool", identb[:], T["ident"][:, :], writes=[identbB])

        def bcast_load(q, dst, dstB, src1d):
            return P.dma(q, dst, src1d.partition_broadcast(128), writes=[dstB])

        def phase_mod():
            with ExitStack() as st:
                ccol = sb(st, "ccol", [128, 8], F32); ccolB = Buf()
                cs = sb(st, "cs", [128, 8], F32); csB_ = Buf()
                csb = sb(st, "csb", [128, 8, 128], F32); csbB = Buf()
                adb = sb(st, "adb", [128, 6 * D], F32); adbB = Buf()
                modt = sb(st, "modt", [128, 6 * D], F32); modtB = Buf()
                wb = [sb(st, f"adw{i}", [128, 8, 512], F32) for i in range(2)]
                wbB = [Buf(), Buf()]
                P.dma("sp", ccol[:], T["c"].rearrange("o (kc p) -> p (o kc)", p=128), writes=[ccolB],
                      allow_slow_non_contiguous=True)
                P.op("act", lambda e: e.activation(out=cs[:], in_=ccol[:], func=AF.Silu), [ccolB], [csB_])
                for kc in range(8):
                    P.op("dve", lambda e, kc=kc: e.tensor_copy(out=csb[:, kc, :], in_=cs[:, kc:kc + 1].to_broadcast([128, 128])),
                         [csB_], [csbB])
                n = 0
                for i in range(2):
                    bcast_load("sp", adb[:], adbB, T["ada_b"][i, :])
                    wv = T["ada_w"][i].rearrange("(kc p) n -> p kc n", p=128)
                    for q in range(12):
                        w, wB = wb[n % 2], wbB[n % 2]
                        n += 1
                        P.dma("sp", w[:], wv[:, :, q * 512:(q + 1) * 512], writes=[wB])
                        pt, pB = bank()
                        for kc in range(8):
                            P.op("pe", lambda e, kc=kc, w=w, pt=pt: e.matmul(pt[:], lhsT=csb[:, kc, :], rhs=w[:, kc, :],
                                                                              start=(kc == 0), stop=(kc == 7)),
                                 [csbB, wB], [pB])
                        P.op("dve", lambda e, q=q, pt=pt: e.tensor_tensor(out=modt[:, q * 512:(q + 1) * 512], in0=pt[:],
                                                                         in1=adb[:, q * 512:(q + 1) * 512], op=ALU.add),
                             [pB, adbB], [modtB])
                    P.dma("sp", T["MOD"][i:i + 1, :], modt[0:1, :], reads=[modtB], writes=[MODB])
                P.barrier()

        MODB = Buf()

        def load_mod_tiles(st, layer, which, gname):
            base = 0 if which == 0 else 3
            A = sb(st, f"A{layer}{which}", [128, D], F32); AB = Buf()
            Bt = sb(st, f"B{layer}{which}", [128, D], F32); BB = Buf()
            G = sb(st, f"G{layer}{which}", [128, D], F32); GB = Buf()
            gt = sb(st, f"g{layer}{which}", [128, D], F32); gB = Buf()
            P.dma("sp", Bt[:], T["MOD"][layer, (base + 0) * D:(base + 1) * D].partition_broadcast(128), reads=[MODB], writes=[BB])
            P.dma("sp", A[:], T["MOD"][layer, (base + 1) * D:(base + 2) * D].partition_broadcast(128), reads=[MODB], writes=[AB])
            P.dma("sp", G[:], T["MOD"][layer, (base + 2) * D:(base + 3) * D].partition_broadcast(128), reads=[MODB], writes=[GB])
            bcast_load("sp", gt[:], gB, T[gname][layer, :])
            P.op("dve", lambda e: e.scalar_tensor_tensor(out=A[:], in0=A[:], scalar=1.0, in1=gt[:], op0=ALU.add, op1=ALU.mult),
                 [gB, AB], [AB])
            return (A, AB), (Bt, BB), (G, GB)

        class NormCtx:
            def __init__(self, st, tag):
                self.junk = sb(st, f"junk{tag}", [128, D], F32); self.junkB = Buf()
                self.ss = [sb(st, f"ss{tag}{i}", [128, 4], F32) for i in range(2)]; self.ssB = [Buf(), Buf()]
                self.tmp = [sb(st, f"nt{tag}{i}", [128, D], F32) for i in range(2)]; self.tmpB = [Buf(), Buf()]
                self.n = 0

        def norm_mod(nx, xt, xtB, A, AB, Bt, BB, out, outB, tmp_unused=None, tmpB_unused=None):
            k = nx.n % 2
            nx.n += 1
            ss, ssB = nx.ss[k], nx.ssB[k]
            tmp, tmpB = nx.tmp[k][:], nx.tmpB[k]
            P.op("act", lambda e: e.activation(out=nx.junk[:], in_=xt, func=AF.Square, accum_out=ss[:, 0:1]),
                 [xtB], [nx.junkB, ssB])
            P.op("dve", lambda e: e.tensor_scalar(out=ss[:, 1:2], in0=ss[:, 0:1], scalar1=1.0 / D, scalar2=EPS,
                                                  op0=ALU.mult, op1=ALU.add), [ssB], [ssB])
            P.op("act", lambda e: e.activation(out=ss[:, 2:3], in_=ss[:, 1:2], func=AF.Sqrt), [ssB], [ssB])
            P.op("dve", lambda e: e.reciprocal(out=ss[:, 3:4], in_=ss[:, 2:3]), [ssB], [ssB])
            if Bt is None:
                P.op("dve", lambda e: e.scalar_tensor_tensor(out=out, in0=xt, scalar=ss[:, 3:4], in1=A[:],
                                                             op0=ALU.mult, op1=ALU.mult), [xtB, ssB, AB], [outB])
                return
            P.op("dve", lambda e: e.scalar_tensor_tensor(out=tmp, in0=xt, scalar=ss[:, 3:4], in1=A[:],
                                                         op0=ALU.mult, op1=ALU.mult), [xtB, ssB, AB], [tmpB])
            P.op("pool", lambda e: e.tensor_tensor(out=out, in0=tmp, in1=Bt[:], op=ALU.add), [tmpB, BB], [outB])

        def fwd_dft(st, A, AB_, Bm, BB_, consume):
            fwb = [sb(st, f"fw{i}", [128, 2, 32, 128], BF16) for i in range(2)]
            fwB = [Buf(), Buf()]
            def load_fw(fc):
                fw, fB = fwb[fc % 2], fwB[fc % 2]
                P.dma("sp", fw[:, 0], T["FWD"][fc, :, 0], writes=[fB])
                P.dma("sp", fw[:, 1], T["FWD"][fc, :, 1], writes=[fB])

            load_fw(0)
            for fc in range(32):
                fw, fB = fwb[fc % 2], fwB[fc % 2]
                if fc + 1 < 32:
                    load_fw(fc + 1)
                res = []
                for h in range(2):
                    for cs_ in range(2):
                        src, sB = (A, AB_) if cs_ == 0 else (Bm, BB_)
                        pt, pB = bank()
                        for tt in range(32):
                            P.op("pe", lambda e, pt=pt, fw=fw, cs_=cs_, tt=tt, src=src, h=h:
                                 e.matmul(pt[:], lhsT=fw[:, cs_, tt, :], rhs=src[:, tt, h * 512:(h + 1) * 512],
                                          start=(tt == 0), stop=(tt == 31)), [fB, sB], [pB])
                        res.append((pt, pB))
                consume(fc, res)

        def phase_filter():
            with ExitStack() as st:
                KTB = Buf()
                with ExitStack() as s2:
                    kab = [sb(s2, f"kab{i}", [128, 2, D], BF16) for i in range(2)]; kabB = [Buf(), Buf()]
                    zf = sb(s2, "zf", [33, S], F32); zfB = Buf()
                    h1 = sb(s2, "h1", [64, S], F32); h1B = Buf()
                    h2 = sb(s2, "h2", [64, S], F32); h2B = Buf()
                    w1 = sb(s2, "fw1", [33, 64], F32); w1B = Buf()
                    w2 = sb(s2, "fw2", [64, 64], F32); w2B = Buf()
                    w3 = sb(s2, "fw3", [64, 2 * D], F32); w3B = Buf()
                    col = sb(s2, "fcol", [64, 8], F32); colB = Buf()
                    pre = sb(s2, "fpre", [64, 512], F32); preB = Buf()
                    ki = sb(s2, "fki", [64, 512], I32); kiB = Buf()
                    kf = sb(s2, "fkf", [64, 512], F32); kfB = Buf()
                    absd = sb(s2, "absd", [128, D], F32); absdB = Buf()
                    tneg = sb(s2, "tneg", [128, NT], F32); tnegB = Buf()
                    dec = sb(s2, "dec", [128, D], F32); decB = Buf()
                    kfw = sb(s2, "kfw", [128, D], F32); kfwB = Buf()
                    kbw = sb(s2, "kbw", [128, D], F32); kbwB = Buf()
                    fbias = sb(s2, "fbias", [1, D], F32); fbiasB = Buf()
                    P.dma("sp", zf[:], T["zfT"][:, :], writes=[zfB])
                    P.dma("sp", w1[:], T["hy_f_w1"][0], writes=[w1B])
                    P.dma("sp", w2[:], T["hy_f_w2"][0], writes=[w2B])
                    P.dma("sp", w3[:], T["hy_f_w3"][0], writes=[w3B])
                    P.dma("sp", col[:, 0:1], T["hy_f_b1"][0].rearrange("(p o) -> p o", o=1), writes=[colB])
                    P.dma("sp", col[:, 1:2], T["hy_f_b2"][0].rearrange("(p o) -> p o", o=1), writes=[colB])
                    P.dma("sp", col[:, 2:3], T["hy_f_freq"][0].rearrange("(p o) -> p o", o=1), writes=[colB])
                    P.dma("sp", tneg[:], T["tneg"][:, :], writes=[tnegB])
                    P.dma("sp", fbias[:], T["hy_f_bias"][0], writes=[fbiasB])
                    bcast_load("sp", absd[:], absdB, T["absd"])
                    P.op("dve", lambda e: e.tensor_tensor(out=col[:, 3:4], in0=col[:, 0:1], in1=col[:, 2:3], op=ALU.mult), [colB], [colB])
                    P.op("dve", lambda e: e.tensor_tensor(out=col[:, 4:5], in0=col[:, 1:2], in1=col[:, 2:3], op=ALU.mult), [colB], [colB])

                    def sin_layer(w, wB, src, srcB, K, bcol, dst, dstB):
                        for tch in range(8):
                            pt, pB = bank()
                            P.op("pe", lambda e, pt=pt, tch=tch: e.matmul(pt[0:64, :], lhsT=w[0:K, :], rhs=src[0:K, tch * 512:(tch + 1) * 512],
                                                                          start=True, stop=True), [wB, srcB], [pB])
                            P.op("dve", lambda e, pt=pt: e.tensor_scalar(out=pre[:], in0=pt[0:64, :], scalar1=col[:, 2:3], scalar2=col[:, bcol:bcol + 1],
                                                                         op0=ALU.mult, op1=ALU.add), [pB, colB], [preB])
                            P.op("dve", lambda e: e.tensor_scalar(out=ki[:], in0=pre[:], scalar1=1.0 / TWO_PI, scalar2=None, op0=ALU.mult), [preB], [kiB])
                            P.op("dve", lambda e: e.tensor_copy(out=kf[:], in_=ki[:]), [kiB], [kfB])
                            P.op("dve", lambda e: e.scalar_tensor_tensor(out=pre[:], in0=kf[:], scalar=-TWO_PI, in1=pre[:], op0=ALU.mult, op1=ALU.add),
                                 [kfB, preB], [preB])
                            P.op("dve", lambda e: e.tensor_scalar(out=pre[:], in0=pre[:], scalar1=math.pi, scalar2=-math.pi, op0=ALU.min, op1=ALU.max), [preB], [preB])
                            P.op("act", lambda e, tch=tch: e.activation(out=dst[:, tch * 512:(tch + 1) * 512], in_=pre[:], func=AF.Sin), [preB], [dstB])

                    sin_layer(w1, w1B, zf, zfB, 33, 3, h1, h1B)
                    sin_layer(w2, w2B, h1, h1B, 64, 4, h2, h2B)
                    for tt in range(NT):
                        P.op("act", lambda e, tt=tt: e.activation(out=dec[:], in_=absd[:], func=AF.Exp, scale=tneg[:, tt:tt + 1]), [absdB, tnegB], [decB])
                        for q in range(4):
                            pt, pB = bank()
                            P.op("pe", lambda e, pt=pt, tt=tt, q=q: e.matmul(pt[:], lhsT=h2[:, tt * 128:(tt + 1) * 128], rhs=w3[:, q * 512:(q + 1) * 512],
                                                                             start=True, stop=True), [h2B, w3B], [pB])
                            dst, dB = (kfw, kfwB) if q < 2 else (kbw, kbwB)
                            P.op("dve", lambda e, pt=pt, q=q, dst=dst: e.tensor_tensor(out=dst[:, (q % 2) * 512:(q % 2 + 1) * 512], in0=pt[:],
                                                                                       in1=dec[:, (q % 2) * 512:(q % 2 + 1) * 512], op=ALU.mult),
                                 [pB, decB], [dB])
                        if tt == 0:
                            P.op("dve", lambda e: e.tensor_tensor(out=kfw[0:1, :], in0=kfw[0:1, :], in1=fbias[0:1, :], op=ALU.add), [kfwB, fbiasB], [kfwB])
                            P.op("dve", lambda e: e.memset(kbw[0:1, :], 0.0), [], [kbwB])
                        ka, kaB = kab[tt % 2], kabB[tt % 2]
                        P.op("pool", lambda e, ka=ka: e.tensor_tensor(out=ka[:, 0, :], in0=kfw[:], in1=kbw[:], op=ALU.add), [kfwB, kbwB], [kaB])
                        P.op("dve", lambda e, ka=ka: e.tensor_tensor(out=ka[:, 1, :], in0=kfw[:], in1=kbw[:], op=ALU.subtract), [kfwB, kbwB], [kaB])
                        for j in range(2):
                            P.dma("sp", T["KT"][j, tt * 128:(tt + 1) * 128, :], ka[:, j, :], reads=[kaB], writes=[KTB])
                    P.barrier()
                with ExitStack() as s3:
                    KA = sb(s3, "KA", [128, NT, D], BF16); KAB = Buf()
                    KBm = sb(s3, "KBm", [128, NT, D], BF16); KBB = Buf()
                    for j, (kt, ktB) in enumerate(((KA, KAB), (KBm, KBB))):
                        kv = T["KT"][j].rearrange("(tt p) c -> p tt c", p=128)
                        for q in range(4):
                            P.dma("sp", kt[:, q * 8:(q + 1) * 8, :], kv[:, q * 8:(q + 1) * 8, :], reads=[KTB], writes=[ktB])
                    stg = [sb(s3, f"kst{i}", [128, 2, D], F32) for i in range(2)]
                    stgB = [Buf(), Buf()]

                    def store(fc, res):
                        sg, sB = stg[fc % 2], stgB[fc % 2]
                        for h in range(2):
                            for cs_ in range(2):
                                pt, pB = res[h * 2 + cs_]
                                P.op("act", lambda e, pt=pt, h=h, cs_=cs_, sg=sg: e.activation(out=sg[:, cs_, h * 512:(h + 1) * 512], in_=pt[:], func=AF.Identity),
                                     [pB], [sB])
                        for cs_ in range(2):
                            P.dma("sp", T["KS"][cs_, fc * 128:(fc + 1) * 128, :], sg[:, cs_, :], reads=[sB], writes=[KSB])

                    fwd_dft(s3, KA, KAB, KBm, KBB, store)
                    P.barrier()

        KSB = Buf()
        ZTB = Buf(); X0CB = Buf(); YSB = Buf(); XAB = Buf()

        def phase_hyena_in():
            with ExitStack() as st:
                hmT = sb(st, "hmT", [128, 8, S], BF16); hmTB = Buf()
                with ExitStack() as s2:
                    (A, AB), (Bt, BB), _ = load_mod_tiles(s2, 0, 0, "norm_mix_g")
                    nx = NormCtx(s2, "h")
                    xb = [sb(s2, f"hx{i}", [128, D], F32) for i in range(2)]; xbB = [Buf(), Buf()]
                    tmp = None; tmpB = None
                    hb = [sb(s2, f"hb{i}", [128, D], BF16) for i in range(2)]; hbB = [Buf(), Buf()]
                    for tt in range(NT):
                        xt, xtB = xb[tt % 2], xbB[tt % 2]
                        P.dma("sp", xt[:], T["x"][tt * 128:(tt + 1) * 128, :], writes=[xtB])
                        h_, hB_ = hb[tt % 2], hbB[tt % 2]
                        norm_mod(nx, xt[:], xtB, A, AB, Bt, BB, h_[:], hB_)
                        pt, pB = bank()
                        ptb = pt[:].bitcast(BF16)
                        for kc in range(8):
                            P.op("pe", lambda e, kc=kc, ptb=ptb, h_=h_: e.transpose(out=ptb[:, kc * 128:(kc + 1) * 128], in_=h_[:, kc * 128:(kc + 1) * 128], identity=identb[:]),
                                 [hB_, identbB], [pB])
                        P.op("act", lambda e, tt=tt, ptb=ptb: e.activation(out=hmT[:, :, tt * 128:(tt + 1) * 128], in_=ptb.rearrange("p (k t) -> p k t", k=8), func=AF.Identity),
                             [pB], [hmTB])
                    P.barrier()
                with ExitStack() as s2:
                    bcol = sb(s2, "bcol", [128, 24], F32); bcolB = Buf()
                    cw = sb(s2, "cw", [128, 3, 24], F32); cwB = Buf()
                    cbc = sb(s2, "cbc", [128, 24], F32); cbcB = Buf()
                    P.dma("sp", bcol[:], T["hy_b_in"][0].rearrange("(cc p) -> p cc", p=128), writes=[bcolB], allow_slow_non_contiguous=True)
                    P.dma("sp", cbc[:], T["hy_conv_b"][0].rearrange("(cc p) -> p cc", p=128), writes=[cbcB], allow_slow_non_contiguous=True)
                    for j in range(3):
                        P.dma("sp", cw[:, j, :], T["hy_conv_w"][0, j].rearrange("(cc p) -> p cc", p=128), writes=[cwB], allow_slow_non_contiguous=True)
                    wch = [sb(s2, f"wch{i}", [128, 8, 128], BF16) for i in range(2)]; wchB = [Buf(), Buf()]
                    us = [sb(s2, f"u{i}", [128, S + 2], F32) for i in range(2)]; usB = [Buf(), Buf()]
                    ob = [sb(s2, f"ob{i}", [128, S], F32) for i in range(2)]; obB = [Buf(), Buf()]
                    zb = sb(s2, "zb", [128, S], BF16); zbB = Buf()
                    zst = sb(s2, "zst", [128, NT, 128], BF16); zstB = Buf()
                    for u, uB in zip(us, usB):
                        P.op("dve", lambda e, u=u: e.memset(u[:, 0:1], 0.0), [], [uB])
                        P.op("dve", lambda e, u=u: e.memset(u[:, S + 1:S + 2], 0.0), [], [uB])
                    wv = T["hy_w_in"][0].rearrange("(kc p) n -> p kc n", p=128)
                    order = []
                    for j in range(8):
                        order += [8 + j, 16 + j]
                    order += list(range(8))
                    for n, cc in enumerate(order):
                        w, wB = wch[n % 2], wchB[n % 2]
                        u, uB = us[n % 2], usB[n % 2]
                        P.dma("pool", w[:], wv[:, :, cc * 128:(cc + 1) * 128], writes=[wB])
                        for tch in range(8):
                            pt, pB = bank()
                            for kc in range(8):
                                P.op("pe", lambda e, pt=pt, kc=kc, w=w, tch=tch: e.matmul(pt[:], lhsT=w[:, kc, :], rhs=hmT[:, kc, tch * 512:(tch + 1) * 512],
                                                                                         start=(kc == 0), stop=(kc == 7)), [wB, hmTB], [pB])
                            P.op("act", lambda e, pt=pt, tch=tch, cc=cc, u=u: e.activation(out=u[:, 1 + tch * 512:1 + (tch + 1) * 512], in_=pt[:], func=AF.Identity,
                                                                                    bias=bcol[:, cc:cc + 1]), [pB, bcolB], [uB])
                        o, oB = ob[n % 2], obB[n % 2]
                        P.op("act", lambda e, o=o, cc=cc, u=u: e.activation(out=o[:], in_=u[:, 1:S + 1], func=AF.Identity, scale=cw[:, 1, cc:cc + 1], bias=cbc[:, cc:cc + 1]),
                             [uB, cwB, cbcB], [oB])
                        P.op("dve", lambda e, o=o, cc=cc, u=u: e.scalar_tensor_tensor(out=o[:], in0=u[:, 0:S], scalar=cw[:, 0, cc:cc + 1], in1=o[:], op0=ALU.mult, op1=ALU.add),
                             [uB, cwB, oB], [oB])
                        if cc >= 8:
                            P.op("dve", lambda e, o=o, cc=cc, u=u: e.scalar_tensor_tensor(out=o[:], in0=u[:, 2:S + 2], scalar=cw[:, 2, cc:cc + 1], in1=o[:], op0=ALU.mult, op1=ALU.add),
                                 [uB, cwB, oB], [oB])
                        else:
                            P.op("dve", lambda e, o=o, cc=cc, u=u: e.scalar_tensor_tensor(out=zb[:], in0=u[:, 2:S + 2], scalar=cw[:, 2, cc:cc + 1], in1=o[:], op0=ALU.mult, op1=ALU.add),
                                 [uB, cwB, oB], [zbB])
                            P.dma("sp", T["X0C"][cc * 128:(cc + 1) * 128, :], zb[:], reads=[zbB], writes=[X0CB])
                        if cc >= 16:
                            j = cc - 16
                            o1, o1B = ob[(n - 1) % 2], obB[(n - 1) % 2]
                            P.op("pool", lambda e, o=o, o1=o1: e.tensor_tensor(out=zb[:], in0=o[:], in1=o1[:], op=ALU.mult), [oB, o1B], [zbB])
                            for g4 in range(4):
                                pt, pB = bank()
                                ptb = pt[:].bitcast(BF16)
                                for k in range(8):
                                    tt = g4 * 8 + k
                                    P.op("pe", lambda e, ptb=ptb, k=k, tt=tt: e.transpose(out=ptb[:, k * 128:(k + 1) * 128], in_=zb[:, tt * 128:(tt + 1) * 128], identity=identb[:]),
                                         [zbB, identbB], [pB])
                                P.op("act", lambda e, ptb=ptb, g4=g4: e.activation(out=zst[:, g4 * 8:(g4 + 1) * 8, :], in_=ptb.rearrange("p (k c) -> p k c", k=8), func=AF.Identity),
                                     [pB], [zstB])
                            P.dma("sp", T["ZT"].rearrange("(tt p) c -> p tt c", p=128)[:, :, j * 128:(j + 1) * 128], zst[:], reads=[zstB], writes=[ZTB])
                    P.barrier()

        def phase_hyena_fwd():
            with ExitStack() as st:
                zt = sb(st, "zt", [128, NT, D], BF16); ztB = Buf()
                zv = T["ZT"].rearrange("(tt p) c -> p tt c", p=128)
                for q in range(4):
                    P.dma("sp", zt[:, q * 8:(q + 1) * 8, :], zv[:, q * 8:(q + 1) * 8, :], reads=[ZTB], writes=[ztB])
                kk = [sb(st, f"kk{i}", [128, 2, D], F32) for i in range(2)]; kkB = [Buf(), Buf()]
                yt = [sb(st, f"yt{i}", [128, 2, D], BF16) for i in range(2)]; ytB = [Buf(), Buf()]
                t1 = sb(st, "yt1", [128, 512], F32); t1B = Buf()
                t2 = sb(st, "yt2", [128, 512], F32); t2B = Buf()
                t3 = sb(st, "yt3", [128, 512], F32); t3B = Buf()
                t4 = sb(st, "yt4", [128, 512], F32); t4B = Buf()

                def mulk(fc, res):
                    k, kB = kk[fc % 2], kkB[fc % 2]
                    y, yB = yt[fc % 2], ytB[fc % 2]
                    for cs_ in range(2):
                        P.dma("sp", k[:, cs_, :], T["KS"][cs_, fc * 128:(fc + 1) * 128, :], reads=[KSB], writes=[kB])
                    for h in range(2):
                        zr, zrB = res[h * 2]
                        zi, ziB = res[h * 2 + 1]
                        sl = slice(h * 512, (h + 1) * 512)
                        P.op("dve", lambda e, zr=zr, sl=sl: e.tensor_tensor(out=t1[:], in0=zr[:], in1=k[:, 0, sl], op=ALU.mult), [zrB, kB], [t1B])
                        P.op("dve", lambda e, zi=zi, sl=sl: e.tensor_tensor(out=t2[:], in0=zi[:], in1=k[:, 1, sl], op=ALU.mult), [ziB, kB], [t2B])
                        P.op("dve", lambda e, zr=zr, sl=sl: e.tensor_tensor(out=t3[:], in0=zr[:], in1=k[:, 1, sl], op=ALU.mult), [zrB, kB], [t3B])
                        P.op("dve", lambda e, zi=zi, sl=sl: e.tensor_tensor(out=t4[:], in0=zi[:], in1=k[:, 0, sl], op=ALU.mult), [ziB, kB], [t4B])
                        P.op("pool", lambda e, sl=sl, y=y: e.tensor_tensor(out=y[:, 0, sl], in0=t1[:], in1=t2[:], op=ALU.subtract), [t1B, t2B], [yB])
                        P.op("pool", lambda e, sl=sl, y=y: e.tensor_tensor(out=y[:, 1, sl], in0=t3[:], in1=t4[:], op=ALU.add), [t3B, t4B], [yB])
                    P.dma("sp", T["YS"][fc], y[:], reads=[yB], writes=[YSB])

                fwd_dft(st, zt, ztB, zt, ztB, mulk)
                P.barrier()

        def phase_hyena_out():
            with ExitStack() as st:
                X0 = sb(st, "X0", [128, 8, S], BF16); X0B = Buf()
                xv = T["X0C"].rearrange("(cc p) t -> p cc t", p=128)
                for cc in range(8):
                    P.dma("sp", X0[:, cc, :], xv[:, cc, :], reads=[X0CB], writes=[X0B])
                with ExitStack() as s2:
                    Yh = sb(s2, "Yh", [128, 32, 2, 512], BF16); YhB = Buf()
                    gvb = [sb(s2, f"gv{i}", [128, 32, 2, 128], BF16) for i in range(2)]; gvB = [Buf(), Buf()]
                    ytm = [sb(s2, f"ytm{i}", [128, 512], BF16) for i in range(2)]; ytmB = [Buf(), Buf()]
                    yv = T["YS"].rearrange("fc p cs c -> p fc cs c")
                    prev = None

                    def xpose(h, to, ym, ymB):
                        p2, p2B = bank()
                        p2b = p2[:].bitcast(BF16)
                        for j in range(4):
                            P.op("pe", lambda e, j=j: e.transpose(out=p2b[:, j * 128:(j + 1) * 128], in_=ym[:, j * 128:(j + 1) * 128], identity=identb[:]),
                                 [ymB, identbB], [p2B])
                        P.op("dve", lambda e: e.tensor_tensor(out=X0[:, h * 4:(h + 1) * 4, to * 128:(to + 1) * 128],
                                                              in0=p2b[:, 0:512].rearrange("p (j t) -> p j t", j=4),
                                                              in1=X0[:, h * 4:(h + 1) * 4, to * 128:(to + 1) * 128], op=ALU.mult),
                             [p2B, X0B], [X0B])

                    for h in range(2):
                        for q in range(4):
                            for cs_ in range(2):
                                P.dma("sp", Yh[:, q * 8:(q + 1) * 8, cs_, :], yv[:, q * 8:(q + 1) * 8, cs_, h * 512:(h + 1) * 512], reads=[YSB], writes=[YhB])
                        for to in range(NT):
                            gv, gB = gvb[to % 2], gvB[to % 2]
                            for q in range(2):
                                P.dma("sp", gv[:, q * 16:(q + 1) * 16], T["INV"][to, :, q * 16:(q + 1) * 16], writes=[gB])
                            pt, pB = bank()
                            n = 0
                            for fc in range(32):
                                for cs_ in range(2):
                                    P.op("pe", lambda e, pt=pt, gv=gv, fc=fc, cs_=cs_, n=n: e.matmul(pt[:], lhsT=gv[:, fc, cs_, :], rhs=Yh[:, fc, cs_, :],
                                                                                               start=(n == 0), stop=(n == 63)), [gB, YhB], [pB])
                                    n += 1
                            ym, ymB = ytm[to % 2], ytmB[to % 2]
                            P.op("act", lambda e, pt=pt, ym=ym: e.activation(out=ym[:], in_=pt[:], func=AF.Identity), [pB], [ymB])
                            if prev is not None:
                                xpose(*prev)
                            prev = (h, to, ym, ymB)
                    xpose(*prev)
                    P.barrier()
                with ExitStack() as s2:
                    wo = sb(s2, "wo", [128, 8, D], BF16); woB = Buf()
                    wv = T["hy_w_out"][0].rearrange("(kc p) n -> p kc n", p=128)
                    for q in range(2):
                        P.dma("pool", wo[:, q * 4:(q + 1) * 4, :], wv[:, q * 4:(q + 1) * 4, :], writes=[woB])
                    _, _, (G, GB) = load_mod_tiles(s2, 0, 0, "norm_mix_g")
                    bo = sb(s2, "bo", [128, D], F32); boB = Buf()
                    bcast_load("sp", bo[:], boB, T["hy_b_out"][0, :])
                    xb = [sb(s2, f"ox{i}", [128, D], F32) for i in range(2)]; xbB = [Buf(), Buf()]
                    yo = [sb(s2, f"oy{i}", [128, D], F32) for i in range(2)]; yoB = [Buf(), Buf()]
                    P.dma("sp", xb[0][:], T["x"][0:128, :], writes=[xbB[0]])
                    for tt in range(NT):
                        xt, xtB = xb[tt % 2], xbB[tt % 2]
                        y, yB = yo[tt % 2], yoB[tt % 2]
                        if tt + 1 < NT:
                            P.dma("sp", xb[(tt + 1) % 2][:], T["x"][(tt + 1) * 128:(tt + 2) * 128, :], writes=[xbB[(tt + 1) % 2]])
                        for nh in range(2):
                            pt, pB = bank()
                            for cc in range(8):
                                P.op("pe", lambda e, pt=pt, cc=cc, tt=tt, nh=nh: e.matmul(pt[:], lhsT=X0[:, cc, tt * 128:(tt + 1) * 128], rhs=wo[:, cc, nh * 512:(nh + 1) * 512],
                                                                                         start=(cc == 0), stop=(cc == 7)), [X0B, woB], [pB])
                            sl = slice(nh * 512, (nh + 1) * 512)
                            P.op("dve", lambda e, pt=pt, sl=sl, y=y: e.tensor_tensor(out=y[:, sl], in0=pt[:], in1=bo[:, sl], op=ALU.add), [pB, boB], [yB])
                        P.op("pool", lambda e, y=y: e.tensor_tensor(out=y[:], in0=y[:], in1=G[:], op=ALU.mult), [yB, GB], [yB])
                        P.op("dve", lambda e, y=y, xt=xt: e.tensor_tensor(out=y[:], in0=y[:], in1=xt[:], op=ALU.add), [yB, xtB], [yB])
                        P.dma("sp", T["XA"][tt * 128:(tt + 1) * 128, :], y[:], reads=[yB], writes=[XAB])
                    P.barrier()

        HFB = Buf(); ACCB = Buf()

        def phase_moe(layer, XIN, XINB, XOUT, XOUTB, final):
            with ExitStack() as st:
                IDX = sb(st, "IDX", [128, 4, NEXP], U32); IDXB = Buf()
                GVt = sb(st, "GVt", [128, 4, NEXP], F32); GVB = Buf()
                (A, AB), (Bt, BB), (G, GB) = load_mod_tiles(st, layer, 1, "norm_ffn_g")
                wts = [[sb(st, f"w{n}{i}", [128, 8, D], BF16) for n in "gud"] for i in range(2)]
                wtsB = [[Buf() for _ in range(3)] for i in range(2)]
                wnames = ("moe_w_gate", "moe_w_up", "moe_w_down")

                def issue_wloads(e_):
                    for n in range(3):
                        wv = T[wnames[n]][layer, e_].rearrange("(kc p) n -> p kc n", p=128)
                        w, wB = wts[e_ % 2][n], wtsB[e_ % 2][n]
                        for q in range(2):
                            P.dma("pool", w[:, q * 4:(q + 1) * 4, :], wv[:, q * 4:(q + 1) * 4, :], writes=[wB])

                issue_wloads(0)
                issue_wloads(1)
                with ExitStack() as s1:
                    AFFT = sb(s1, "AFFT", [NEXP, S], F32); AFFTB = Buf()
                    with ExitStack() as s2:
                        nx = NormCtx(s2, "m")
                        wr = sb(s2, "wr", [128, 8, NEXP], F32); wrB = Buf()
                        P.dma("sp", wr[:], T["moe_w_router"][layer].rearrange("(kc p) e -> p kc e", p=128), writes=[wrB])
                        zt_ = sb(s2, "zero", [128, D], F32); ztB_ = Buf()
                        P.op("pool", lambda e: e.memset(zt_[:], 0.0), [], [ztB_])
                        for tt in range(NT):
                            P.dma("sp", T["ACC"][tt * 128:(tt + 1) * 128, :], zt_[:], reads=[ztB_], writes=[ACCB])
                        xb = [sb(s2, f"mx{i}", [128, D], F32) for i in range(2)]; xbB = [Buf(), Buf()]
                        tmp = None; tmpB = None
                        hf = [sb(s2, f"hf{i}", [128, D], F32) for i in range(2)]; hfB = [Buf(), Buf()]
                        hfb = [sb(s2, f"hfb{i}", [128, D], BF16) for i in range(2)]; hfbB = [Buf(), Buf()]
                        hfT = sb(s2, "hfT", [128, 8, 128], F32); hfTB = Buf()
                        sm = sb(s2, "sm", [128, 8], F32); smB = Buf()
                        ex = sb(s2, "ex", [128, NEXP], F32); exB = Buf()
                        aff = sb(s2, "aff", [128, NEXP], F32); affB = Buf()
                        P.dma("sp", xb[0][:], XIN[0:128, :], reads=[XINB], writes=[xbB[0]])
                        for tt in range(NT):
                            xt, xtB = xb[tt % 2], xbB[tt % 2]
                            if tt + 1 < NT:
                                P.dma("sp", xb[(tt + 1) % 2][:], XIN[(tt + 1) * 128:(tt + 2) * 128, :], reads=[XINB], writes=[xbB[(tt + 1) % 2]])
                            h_, hB_ = hf[tt % 2], hfB[tt % 2]
                            hb_, hbB_ = hfb[tt % 2], hfbB[tt % 2]
                            norm_mod(nx, xt[:], xtB, A, AB, Bt, BB, h_[:], hB_)
                            P.op("act", lambda e, h_=h_, hb_=hb_: e.activation(out=hb_[:], in_=h_[:], func=AF.Identity), [hB_], [hbB_])
                            P.dma("sp", T["HF"][tt * 128:(tt + 1) * 128, :], hb_[:], reads=[hbB_], writes=[HFB])
                            for half in range(2):
                                pt, pB = bank()
                                for k in range(4):
                                    kc = half * 4 + k
                                    P.op("pe", lambda e, pt=pt, k=k, kc=kc, h_=h_: e.transpose(out=pt[:, k * 128:(k + 1) * 128], in_=h_[:, kc * 128:(kc + 1) * 128], identity=identf[:]),
                                         [hB_, identfB], [pB])
                                P.op("act", lambda e, pt=pt, half=half: e.activation(out=hfT[:, half * 4:(half + 1) * 4, :], in_=pt[:].rearrange("p (k t) -> p k t", k=4), func=AF.Identity),
                                     [pB], [hfTB])
                            pt, pB = bank()
                            for kc in range(8):
                                P.op("pe", lambda e, pt=pt, kc=kc: e.matmul(pt[:, 0:NEXP], lhsT=hfT[:, kc, :], rhs=wr[:, kc, :], start=(kc == 0), stop=(kc == 7)),
                                     [hfTB, wrB], [pB])
                            P.op("dve", lambda e, pt=pt: e.tensor_reduce(out=sm[:, 0:1], in_=pt[:, 0:NEXP], axis=AX.X, op=ALU.max, negate=True), [pB], [smB])
                            P.op("act", lambda e, pt=pt: e.activation(out=ex[:], in_=pt[:, 0:NEXP], func=AF.Exp, bias=sm[:, 0:1], accum_out=sm[:, 1:2]), [pB, smB], [exB, smB])
                            P.op("dve", lambda e: e.reciprocal(out=sm[:, 2:3], in_=sm[:, 1:2]), [smB], [smB])
                            P.op("dve", lambda e: e.tensor_scalar(out=aff[:], in0=ex[:], scalar1=sm[:, 2:3], scalar2=None, op0=ALU.mult), [exB, smB], [affB])
                            p2, p2B = bank()
                            P.op("pe", lambda e, p2=p2: e.transpose(out=p2[0:NEXP, 0:128], in_=aff[:, 0:NEXP], identity=identf[:]), [affB, identfB], [p2B])
                            P.op("act", lambda e, p2=p2, tt=tt: e.activation(out=AFFT[:, tt * 128:(tt + 1) * 128], in_=p2[0:NEXP, 0:128], func=AF.Identity), [p2B], [AFFTB])
                        P.barrier()
                    with ExitStack() as s2:
                        work = sb(s2, "work", [NEXP, S], F32); workB = Buf()
                        vals = sb(s2, "vals", [NEXP, CAP], F32); valsB = Buf()
                        idxu = sb(s2, "idxu", [NEXP, CAP], U32); idxuB = Buf()
                        idxf = sb(s2, "idxf", [NEXP, CAP], F32); idxfB = Buf()
                        idt = sb(s2, "idt", [128, 4, NEXP], F32); idtB = Buf()
                        P.op("dve", lambda e: e.tensor_copy(out=work[:], in_=AFFT[:]), [AFFTB], [workB])
                        for r in range(CAP // 8):
                            sl = slice(8 * r, 8 * r + 8)
                            P.op("dve", lambda e, sl=sl: e.max(out=vals[:, sl], in_=work[:]), [workB], [valsB])
                            P.op("dve", lambda e, sl=sl: e.max_index(out=idxu[:, sl], in_max=vals[:, sl], in_values=work[:]), [workB, valsB], [idxuB])
                            P.op("dve", lambda e, sl=sl: e.match_replace(out=work[:], in_to_replace=vals[:, sl], in_values=work[:], imm_value=-1.0), [valsB, workB], [workB])
                        P.op("dve", lambda e: e.tensor_copy(out=idxf[:], in_=idxu[:]), [idxuB], [idxfB])
                        for s_ in range(4):
                            pt, pB = bank()
                            P.op("pe", lambda e, pt=pt, s_=s_: e.transpose(out=pt[:, 0:NEXP], in_=idxf[0:NEXP, s_ * 128:(s_ + 1) * 128], identity=identf[0:NEXP, 0:NEXP]),
                                 [idxfB, identfB], [pB])
                            P.op("dve", lambda e, pt=pt, s_=s_: e.tensor_copy(out=idt[:, s_, :], in_=pt[:, 0:NEXP]), [pB], [idtB])
                            P.op("dve", lambda e, s_=s_: e.tensor_copy(out=IDX[:, s_, :], in_=idt[:, s_, :]), [idtB], [IDXB])
                            p2, p2B = bank()
                            P.op("pe", lambda e, p2=p2, s_=s_: e.transpose(out=p2[:, 0:NEXP], in_=vals[0:NEXP, s_ * 128:(s_ + 1) * 128], identity=identf[0:NEXP, 0:NEXP]),
                                 [valsB, identfB], [p2B])
                            P.op("dve", lambda e, p2=p2, s_=s_: e.tensor_copy(out=GVt[:, s_, :], in_=p2[:, 0:NEXP]), [p2B], [GVB])
                        P.barrier()
                with ExitStack() as s1:
                    xs = [sb(s1, f"xs{i}", [128, 4, D], BF16) for i in range(2)]; xsB = [Buf(), Buf()]
                    xsT = sb(s1, "xsT", [128, 8, CAP], BF16); xsTB = Buf()
                    hid = sb(s1, "hid", [128, 8, CAP], BF16); hidB = Buf()
                    sg = [sb(s1, f"sg{i}", [128, 512], F32) for i in range(2)]; sgB = [Buf(), Buf()]
                    ye = [sb(s1, f"ye{i}", [128, D], F32) for i in range(4)]; yeB = [Buf() for _ in range(4)]

                    def issue_gathers(e_):
                        x_, xB_ = xs[e_ % 2], xsB[e_ % 2]
                        for s_ in range(4):
                            P.dma("pool", None, None, reads=[HFB, IDXB], writes=[xB_],
                                  fn=lambda g, s_=s_, x_=x_, e_=e_: g.indirect_dma_start(
                                      out=x_[:, s_, :], out_offset=None, in_=T["HF"][:, :],
                                      in_offset=bass.IndirectOffsetOnAxis(ap=IDX[:, s_, e_:e_ + 1], axis=0)))

                    issue_gathers(0)
                    issue_gathers(1)
                    for e_ in range(NEXP):
                        (wg, wu, wd), (wgB, wuB, wdB) = wts[e_ % 2], wtsB[e_ % 2]
                        x_, xB_ = xs[e_ % 2], xsB[e_ % 2]
                        for s_ in range(4):
                            pt, pB = bank()
                            ptb = pt[:].bitcast(BF16)
                            for kc in range(8):
                                P.op("pe", lambda e, ptb=ptb, kc=kc, s_=s_, x_=x_: e.transpose(out=ptb[:, kc * 128:(kc + 1) * 128], in_=x_[:, s_, kc * 128:(kc + 1) * 128], identity=identb[:]),
                                     [xB_, identbB], [pB])
                            P.op("act", lambda e, ptb=ptb, s_=s_: e.activation(out=xsT[:, :, s_ * 128:(s_ + 1) * 128], in_=ptb.rearrange("p (k t) -> p k t", k=8), func=AF.Identity),
                                 [pB], [xsTB])
                        for fcn in range(8):
                            pg, pgB = bank()
                            pu, puB = bank()
                            for kc in range(8):
                                P.op("pe", lambda e, pg=pg, kc=kc, fcn=fcn, wg=wg: e.matmul(pg[:], lhsT=wg[:, kc, fcn * 128:(fcn + 1) * 128], rhs=xsT[:, kc, :], start=(kc == 0), stop=(kc == 7)),
                                     [wgB, xsTB], [pgB])
                            for kc in range(8):
                                P.op("pe", lambda e, pu=pu, kc=kc, fcn=fcn, wu=wu: e.matmul(pu[:], lhsT=wu[:, kc, fcn * 128:(fcn + 1) * 128], rhs=xsT[:, kc, :], start=(kc == 0), stop=(kc == 7)),
                                     [wuB, xsTB], [puB])
                            s__, sB__ = sg[fcn % 2], sgB[fcn % 2]
                            P.op("act", lambda e, pg=pg, s__=s__: e.activation(out=s__[:], in_=pg[:], func=AF.Silu), [pgB], [sB__])
                            P.op("dve", lambda e, pu=pu, s__=s__, fcn=fcn: e.tensor_tensor(out=hid[:, fcn, :], in0=pu[:], in1=s__[:], op=ALU.mult), [puB, sB__], [hidB])
                        for s_ in range(4):
                            y, yB = ye[s_], yeB[s_]
                            for nh in range(2):
                                pt, pB = bank()
                                for fcn in range(8):
                                    P.op("pe", lambda e, pt=pt, fcn=fcn, s_=s_, nh=nh, wd=wd: e.matmul(pt[:], lhsT=hid[:, fcn, s_ * 128:(s_ + 1) * 128], rhs=wd[:, fcn, nh * 512:(nh + 1) * 512],
                                                                                                 start=(fcn == 0), stop=(fcn == 7)), [hidB, wdB], [pB])
                                P.op("act", lambda e, pt=pt, y=y, nh=nh, s_=s_, e_=e_: e.activation(out=y[:, nh * 512:(nh + 1) * 512], in_=pt[:], func=AF.Identity, scale=GVt[:, s_, e_:e_ + 1]),
                                     [pB, GVB], [yB])
                        if e_ + 2 < NEXP:
                            issue_wloads(e_ + 2)
                        for s_ in range(4):
                            y, yB = ye[s_], yeB[s_]
                            P.dma("pool", None, None, reads=[yB, IDXB], writes=[ACCB],
                                  fn=lambda g, s_=s_, y=y, e_=e_: g.indirect_dma_start(
                                      out=T["ACC"][:, :], out_offset=bass.IndirectOffsetOnAxis(ap=IDX[:, s_, e_:e_ + 1], axis=0),
                                      in_=y[:], in_offset=None, compute_op=ALU.add))
                        if e_ + 2 < NEXP:
                            issue_gathers(e_ + 2)
                    P.barrier()
                with ExitStack() as s1:
                    xb = [sb(s1, f"cx{i}", [128, D], F32) for i in range(2)]; xbB = [Buf(), Buf()]
                    ab = [sb(s1, f"ca{i}", [128, D], F32) for i in range(2)]; abB = [Buf(), Buf()]
                    ob_ = [sb(s1, f"co{i}", [128, D], F32) for i in range(2)]; obB_ = [Buf(), Buf()]
                    if final:
                        nx = NormCtx(s1, "f")
                        fg = sb(s1, "fg", [128, D], F32); fgB = Buf()
                        bcast_load("sp", fg[:], fgB, T["final_norm_g"])
                    def ld_c(tt):
                        P.dma("sp", xb[tt % 2][:], XIN[tt * 128:(tt + 1) * 128, :], reads=[XINB], writes=[xbB[tt % 2]])
                        P.dma("sp", ab[tt % 2][:], T["ACC"][tt * 128:(tt + 1) * 128, :], reads=[ACCB], writes=[abB[tt % 2]])

                    ld_c(0)
                    for tt in range(NT):
                        xt, xtB = xb[tt % 2], xbB[tt % 2]
                        a_, aB_ = ab[tt % 2], abB[tt % 2]
                        if tt + 1 < NT:
                            ld_c(tt + 1)
                        P.op("pool", lambda e, a_=a_: e.tensor_tensor(out=a_[:], in0=a_[:], in1=G[:], op=ALU.mult), [aB_, GB], [aB_])
                        P.op("dve", lambda e, a_=a_, xt=xt: e.tensor_tensor(out=a_[:], in0=a_[:], in1=xt[:], op=ALU.add), [aB_, xtB], [aB_])
                        if final:
                            o_, oB_ = ob_[tt % 2], obB_[tt % 2]
                            norm_mod(nx, a_[:], aB_, fg, fgB, None, None, o_[:], oB_, None, None)
                            P.dma("sp", XOUT[tt * 128:(tt + 1) * 128, :], o_[:], reads=[oB_], writes=[XOUTB])
                        else:
                            P.dma("sp", XOUT[tt * 128:(tt + 1) * 128, :], a_[:], reads=[aB_], writes=[XOUTB])
                    P.barrier()

        def bank_fixed(i):
            return ps[i], psB[i]

        SCALE = 1.0 / math.sqrt(192.0)
        C1 = 6.28125
        C2 = TWO_PI - 6.28125
        QNB = Buf(); QPB = Buf(); KNB = Buf(); KPB = Buf(); VB = Buf()

        def phase_mla_proj(XIN, XINB):
            with ExitStack() as st:
                cqnT = sb(st, "cqnT", [128, 2, S], BF16); cqnTB = Buf()
                ckvT = sb(st, "ckvT", [128, S], BF16); ckvTB = Buf()
                kpT = sb(st, "kpT", [64, S], BF16); kpTB = Buf()
                cosT = sb(st, "cosT", [64, S], F32); cosTB = Buf()
                sinT = sb(st, "sinT", [64, S], F32); sinTB = Buf()
                with ExitStack() as s2:
                    posi = sb(s2, "posi", [64, S], I32); posiB = Buf()
                    ang = sb(s2, "ang", [64, S], F32); angB = Buf()
                    a2 = sb(s2, "a2", [64, S], F32); a2B = Buf()
                    kq = sb(s2, "kq", [64, S], I32); kqB = Buf()
                    kqf = sb(s2, "kqf", [64, S], F32); kqfB = Buf()
                    ivf = sb(s2, "ivf", [64, 1], F32); ivfB = Buf()
                    P.dma("sp", posi[:], T["pos"].partition_broadcast(64), writes=[posiB])
                    P.dma("sp", ivf[0:32, :], T["invf"].rearrange("(p o) -> p o", o=1), writes=[ivfB])
                    P.dma("sp", ivf[32:64, :], T["invf"].rearrange("(p o) -> p o", o=1), writes=[ivfB])
                    P.op("dve", lambda e: e.tensor_copy(out=ang[:], in_=posi[:]), [posiB], [angB])
                    P.op("dve", lambda e: e.tensor_scalar(out=ang[:], in0=ang[:], scalar1=ivf[:, 0:1], scalar2=None, op0=ALU.mult), [angB, ivfB], [angB])
                    for shift, dst, dB in ((0.0, sinT, sinTB), (math.pi / 2.0, cosT, cosTB)):
                        P.op("dve", lambda e, shift=shift: e.tensor_scalar(out=a2[:], in0=ang[:], scalar1=shift, scalar2=None, op0=ALU.add), [angB], [a2B])
                        P.op("dve", lambda e: e.tensor_scalar(out=kq[:], in0=a2[:], scalar1=1.0 / TWO_PI, scalar2=None, op0=ALU.mult), [a2B], [kqB])
                        P.op("dve", lambda e: e.tensor_copy(out=kqf[:], in_=kq[:]), [kqB], [kqfB])
                        P.op("dve", lambda e: e.scalar_tensor_tensor(out=a2[:], in0=kqf[:], scalar=-C1, in1=a2[:], op0=ALU.mult, op1=ALU.add), [kqfB, a2B], [a2B])
                        P.op("dve", lambda e: e.scalar_tensor_tensor(out=a2[:], in0=kqf[:], scalar=-C2, in1=a2[:], op0=ALU.mult, op1=ALU.add), [kqfB, a2B], [a2B])
                        P.op("dve", lambda e: e.tensor_scalar(out=a2[:], in0=a2[:], scalar1=math.pi, scalar2=-math.pi, op0=ALU.min, op1=ALU.max), [a2B], [a2B])
                        P.op("act", lambda e, dst=dst: e.activation(out=dst[:], in_=a2[:], func=AF.Sin), [a2B], [dB])
                    P.barrier()
                with ExitStack() as s2:
                    (A, AB), (Bt, BB), _ = load_mod_tiles(s2, 1, 0, "norm_mix_g")
                    nx = NormCtx(s2, "a")
                    win = sb(s2, "win", [128, 8, 448], BF16); winB = Buf()
                    wrot = sb(s2, "wrot", [128, 8, 64], BF16); wrotB = Buf()
                    P.dma("pool", win[:], T["mla_w_in"][0].rearrange("(kc p) n -> p kc n", p=128), writes=[winB])
                    P.op("dve", lambda e: e.tensor_scalar(out=wrot[:, :, 0:32], in0=win[:, :, 416:448], scalar1=-1.0, scalar2=None, op0=ALU.mult), [winB], [wrotB])
                    P.op("dve", lambda e: e.tensor_copy(out=wrot[:, :, 32:64], in_=win[:, :, 384:416]), [winB], [wrotB])
                    qg = sb(s2, "qg", [128, 256], F32); qgB = Buf()
                    kg = sb(s2, "kg", [128, 128], F32); kgB = Buf()
                    bcast_load("sp", qg[:], qgB, T["mla_q_norm_g"][0, :])
                    bcast_load("sp", kg[:], kgB, T["mla_kv_norm_g"][0, :])
                    xb = [sb(s2, f"ax{i}", [128, D], F32) for i in range(2)]; xbB = [Buf(), Buf()]
                    tmp = None; tmpB = None
                    hb = [sb(s2, f"ahb{i}", [128, D], BF16) for i in range(2)]; hbB = [Buf(), Buf()]
                    hT = [sb(s2, f"ahT{i}", [128, 8, 128], BF16) for i in range(2)]; hTB = [Buf(), Buf()]
                    jq = sb(s2, "jq", [128, 256], F32); jqB = Buf()
                    sq = sb(s2, "sq", [128, 8], F32); sqB = Buf()
                    cn = [sb(s2, f"cn{i}", [128, 384], BF16) for i in range(2)]; cnB = [Buf(), Buf()]
                    r1 = sb(s2, "r1", [64, 128], F32); r1B = Buf()
                    r2 = sb(s2, "r2", [64, 128], F32); r2B = Buf()
                    for tt in range(NT):
                        tsl = slice(tt * 128, (tt + 1) * 128)
                        xt, xtB = xb[tt % 2], xbB[tt % 2]
                        P.dma("sp", xt[:], XIN[tsl, :], reads=[XINB], writes=[xtB])
                        h_, hB_ = hb[tt % 2], hbB[tt % 2]
                        norm_mod(nx, xt[:], xtB, A, AB, Bt, BB, h_[:], hB_)
                        pt, pB = bank()
                        ptb = pt[:].bitcast(BF16)
                        for kc in range(8):
                            P.op("pe", lambda e, kc=kc, ptb=ptb, h_=h_: e.transpose(out=ptb[:, kc * 128:(kc + 1) * 128], in_=h_[:, kc * 128:(kc + 1) * 128], identity=identb[:]),
                                 [hB_, identbB], [pB])
                        ht, htB = hT[tt % 2], hTB[tt % 2]
                        P.op("act", lambda e, ptb=ptb, ht=ht: e.activation(out=ht[:], in_=ptb.rearrange("p (k t) -> p k t", k=8), func=AF.Identity), [pB], [htB])
                        pa, paB = bank()
                        for kc in range(8):
                            P.op("pe", lambda e, pa=pa, kc=kc, ht=ht: e.matmul(pa[:, 0:384], lhsT=ht[:, kc, :], rhs=win[:, kc, 0:384], start=(kc == 0), stop=(kc == 7)),
                                 [htB, winB], [paB])
                        pk1, pk1B = bank()
                        for kc in range(8):
                            P.op("pe", lambda e, pk1=pk1, kc=kc, ht=ht: e.matmul(pk1[0:64, 0:128], lhsT=win[:, kc, 384:448], rhs=ht[:, kc, :], start=(kc == 0), stop=(kc == 7)),
                                 [htB, winB], [pk1B])
                        pk2, pk2B = bank()
                        for kc in range(8):
                            P.op("pe", lambda e, pk2=pk2, kc=kc, ht=ht: e.matmul(pk2[0:64, 0:128], lhsT=wrot[:, kc, :], rhs=ht[:, kc, :], start=(kc == 0), stop=(kc == 7)),
                                 [htB, wrotB], [pk2B])
                        P.op("dve", lambda e, pk1=pk1, tsl=tsl: e.tensor_tensor(out=r1[:], in0=pk1[0:64, 0:128], in1=cosT[:, tsl], op=ALU.mult), [pk1B, cosTB], [r1B])
                        P.op("dve", lambda e, pk2=pk2, tsl=tsl: e.tensor_tensor(out=r2[:], in0=pk2[0:64, 0:128], in1=sinT[:, tsl], op=ALU.mult), [pk2B, sinTB], [r2B])
                        P.op("pool", lambda e, tsl=tsl: e.tensor_tensor(out=kpT[:, tsl], in0=r1[:], in1=r2[:], op=ALU.add), [r1B, r2B], [kpTB])
                        c_, cB_ = cn[tt % 2], cnB[tt % 2]
                        for (lo, hi, gt_, gB_, n_) in ((0, 256, qg, qgB, 256.0), (256, 384, kg, kgB, 128.0)):
                            o4 = 0 if lo == 0 else 4
                            P.op("act", lambda e, pa=pa, lo=lo, hi=hi, o4=o4: e.activation(out=jq[:, 0:hi - lo], in_=pa[:, lo:hi], func=AF.Square, accum_out=sq[:, o4:o4 + 1]),
                                 [paB], [jqB, sqB])
                            P.op("dve", lambda e, o4=o4, n_=n_: e.tensor_scalar(out=sq[:, o4 + 1:o4 + 2], in0=sq[:, o4:o4 + 1], scalar1=1.0 / n_, scalar2=EPS, op0=ALU.mult, op1=ALU.add), [sqB], [sqB])
                            P.op("act", lambda e, o4=o4: e.activation(out=sq[:, o4 + 2:o4 + 3], in_=sq[:, o4 + 1:o4 + 2], func=AF.Sqrt), [sqB], [sqB])
                            P.op("dve", lambda e, o4=o4: e.reciprocal(out=sq[:, o4 + 3:o4 + 4], in_=sq[:, o4 + 2:o4 + 3]), [sqB], [sqB])
                            P.op("dve", lambda e, pa=pa, lo=lo, hi=hi, o4=o4, gt_=gt_, c_=c_: e.scalar_tensor_tensor(out=c_[:, lo:hi], in0=pa[:, lo:hi], scalar=sq[:, o4 + 3:o4 + 4], in1=gt_[:],
                                                                                                              op0=ALU.mult, op1=ALU.mult), [paB, sqB, gB_], [cB_])
                        p3, p3B = bank()
                        p3b = p3[:].bitcast(BF16)
                        for j in range(3):
                            P.op("pe", lambda e, p3b=p3b, j=j, c_=c_: e.transpose(out=p3b[:, j * 128:(j + 1) * 128], in_=c_[:, j * 128:(j + 1) * 128], identity=identb[:]),
                                 [cB_, identbB], [p3B])
                        P.op("act", lambda e, p3b=p3b, tsl=tsl: e.activation(out=cqnT[:, :, tsl], in_=p3b[:, 0:256].rearrange("p (k t) -> p k t", k=2), func=AF.Identity), [p3B], [cqnTB])
                        P.op("act", lambda e, p3b=p3b, tsl=tsl: e.activation(out=ckvT[:, tsl], in_=p3b[:, 256:384], func=AF.Identity), [p3B], [ckvTB])
                    P.dma("sp", T["KP"][:, :], kpT[:], reads=[kpTB], writes=[KPB])
                    P.barrier()
                with ExitStack() as s2:
                    wqn = sb(s2, "wqn", [128, 2, 8, 128], BF16); wqnB = Buf()
                    wqp = sb(s2, "wqp", [128, 2, 8, 64], BF16); wqpB = Buf()
                    wqr = sb(s2, "wqr", [128, 2, 8, 64], BF16); wqrB = Buf()
                    wkk = sb(s2, "wkk", [128, 8, 128], BF16); wkkB = Buf()
                    wkv = sb(s2, "wkv", [128, 8, 128], BF16); wkvB = Buf()
                    qv = T["mla_w_qb"][0].rearrange("(k2 p) (h c) -> p k2 h c", p=128, c=192)
                    for k2 in range(2):
                        P.dma("pool", wqn[:, k2], qv[:, k2, :, 0:128], writes=[wqnB])
                        P.dma("pool", wqp[:, k2], qv[:, k2, :, 128:192], writes=[wqpB])
                    kvv = T["mla_w_kvb"][0].rearrange("k (h c) -> k h c", c=256)
                    P.dma("pool", wkk[:], kvv[:, :, 0:128], writes=[wkkB])
                    P.dma("pool", wkv[:], kvv[:, :, 128:256], writes=[wkvB])
                    P.op("dve", lambda e: e.tensor_scalar(out=wqr[:, :, :, 0:32], in0=wqp[:, :, :, 32:64], scalar1=-1.0, scalar2=None, op0=ALU.mult), [wqpB], [wqrB])
                    P.op("dve", lambda e: e.tensor_copy(out=wqr[:, :, :, 32:64], in_=wqp[:, :, :, 0:32]), [wqpB], [wqrB])
                    qn = [sb(s2, f"qn{i}", [128, S], BF16) for i in range(2)]; qnB = [Buf(), Buf()]
                    kn = [sb(s2, f"kn{i}", [128, S], BF16) for i in range(2)]; knB = [Buf(), Buf()]
                    qp = [sb(s2, f"qp{i}", [64, S], BF16) for i in range(2)]; qpB = [Buf(), Buf()]
                    r1 = sb(s2, "q1", [64, 512], F32); r1B = Buf()
                    r2 = sb(s2, "q2", [64, 512], F32); r2B = Buf()
                    vt = [sb(s2, f"vt{i}", [128, D], BF16) for i in range(2)]; vtB = [Buf(), Buf()]
                    for tt in range(NT):
                        tsl = slice(tt * 128, (tt + 1) * 128)
                        v_, vB_ = vt[tt % 2], vtB[tt % 2]
                        for nh in range(2):
                            pt, pB = bank()
                            P.op("pe", lambda e, pt=pt, tsl=tsl, nh=nh: e.matmul(pt[:], lhsT=ckvT[:, tsl], rhs=wkv[:, nh * 4:(nh + 1) * 4, :], start=True, stop=True),
                                 [ckvTB, wkvB], [pB])
                            P.op("act", lambda e, pt=pt, nh=nh, v_=v_: e.activation(out=v_[:, nh * 512:(nh + 1) * 512], in_=pt[:], func=AF.Identity), [pB], [vB_])
                        P.dma("sp", T["V"][tsl, :], v_[:], reads=[vB_], writes=[VB])
                    for h in range(8):
                        q_, qB_ = qn[h % 2], qnB[h % 2]
                        k_, kB_ = kn[h % 2], knB[h % 2]
                        p_, pB_ = qp[h % 2], qpB[h % 2]
                        for tch in range(8):
                            csl = slice(tch * 512, (tch + 1) * 512)
                            pt, pB = bank()
                            for k2 in range(2):
                                P.op("pe", lambda e, pt=pt, k2=k2, h=h, csl=csl: e.matmul(pt[:], lhsT=wqn[:, k2, h, :], rhs=cqnT[:, k2, csl], start=(k2 == 0), stop=(k2 == 1)),
                                     [wqnB, cqnTB], [pB])
                            P.op("act", lambda e, pt=pt, q_=q_, csl=csl: e.activation(out=q_[:, csl], in_=pt[:], func=AF.Identity), [pB], [qB_])
                            pt2, pB2 = bank()
                            P.op("pe", lambda e, pt2=pt2, h=h, csl=csl: e.matmul(pt2[:], lhsT=wkk[:, h, :], rhs=ckvT[:, csl], start=True, stop=True), [wkkB, ckvTB], [pB2])
                            P.op("act", lambda e, pt2=pt2, k_=k_, csl=csl: e.activation(out=k_[:, csl], in_=pt2[:], func=AF.Identity), [pB2], [kB_])
                            pa, paB = bank()
                            for k2 in range(2):
                                P.op("pe", lambda e, pa=pa, k2=k2, h=h, csl=csl: e.matmul(pa[0:64, :], lhsT=wqp[:, k2, h, :], rhs=cqnT[:, k2, csl], start=(k2 == 0), stop=(k2 == 1)),
                                     [wqpB, cqnTB], [paB])
                            pb_, pbB = bank()
                            for k2 in range(2):
                                P.op("pe", lambda e, pb_=pb_, k2=k2, h=h, csl=csl: e.matmul(pb_[0:64, :], lhsT=wqr[:, k2, h, :], rhs=cqnT[:, k2, csl], start=(k2 == 0), stop=(k2 == 1)),
                                     [wqrB, cqnTB], [pbB])
                            P.op("dve", lambda e, pa=pa, csl=csl: e.tensor_tensor(out=r1[:], in0=pa[0:64, :], in1=cosT[:, csl], op=ALU.mult), [paB, cosTB], [r1B])
                            P.op("dve", lambda e, pb_=pb_, csl=csl: e.tensor_tensor(out=r2[:], in0=pb_[0:64, :], in1=sinT[:, csl], op=ALU.mult), [pbB, sinTB], [r2B])
                            P.op("pool", lambda e, p_=p_, csl=csl: e.tensor_tensor(out=p_[:, csl], in0=r1[:], in1=r2[:], op=ALU.add), [r1B, r2B], [pB_])
                        P.dma("sp", T["QN"][h], q_[:], reads=[qB_], writes=[QNB])
                        P.dma("sp", T["KN"][h], k_[:], reads=[kB_], writes=[KNB])
                        P.dma("sp", T["QP"][h], p_[:], reads=[pB_], writes=[QPB])
                    P.barrier()

        def phase_mla_attn(XIN, XINB, XOUT, XOUTB):
            with ExitStack() as st:
                OT = sb(st, "OT", [128, 8, S], BF16); OTB = Buf()
                with ExitStack() as s2:
                    kp = sb(s2, "kp", [64, S], BF16); kpB = Buf()
                    P.dma("sp", kp[:], T["KP"][:, :], reads=[KPB], writes=[kpB])
                    ones = sb(s2, "ones", [128, 128], BF16); onesB = Buf()
                    P.op("dve", lambda e: e.memset(ones[:], 1.0), [], [onesB])
                    qn = [sb(s2, f"aq{i}", [128, S], BF16) for i in range(2)]; qnB = [Buf(), Buf()]
                    kn = [sb(s2, f"ak{i}", [128, S], BF16) for i in range(2)]; knB = [Buf(), Buf()]
                    qp = [sb(s2, f"ap{i}", [64, S], BF16) for i in range(2)]; qpB = [Buf(), Buf()]
                    vh = [sb(s2, f"av{i}", [128, NT, 128], BF16) for i in range(2)]; vhB = [Buf(), Buf()]
                    pT = [sb(s2, f"pT{i}", [128, 512], BF16) for i in range(3)]; pTB = [Buf(), Buf(), Buf()]
                    rs = sb(s2, "rs", [128, 512], F32); rsB = Buf()
                    npt = 0
                    nst = 0
                    vv = T["V"].rearrange("(tt p) c -> p tt c", p=128)

                    def load_head(h):
                        P.dma("sp", qn[h % 2][:], T["QN"][h], reads=[QNB], writes=[qnB[h % 2]])
                        P.dma("sp", kn[h % 2][:], T["KN"][h], reads=[KNB], writes=[knB[h % 2]])
                        P.dma("sp", qp[h % 2][:], T["QP"][h], reads=[QPB], writes=[qpB[h % 2]])
                        P.dma("sp", vh[h % 2][:], vv[:, :, h * 128:(h + 1) * 128], reads=[VB], writes=[vhB[h % 2]])

                    load_head(0)
                    for h in range(8):
                        if h + 1 < 8:
                            load_head(h + 1)
                        q_, qB_ = qn[h % 2], qnB[h % 2]
                        k_, kB_ = kn[h % 2], knB[h % 2]
                        p_, pB_ = qp[h % 2], qpB[h % 2]
                        v_, vB_ = vh[h % 2], vhB[h % 2]
                        for qc in range(8):
                            csl = slice(qc * 512, (qc + 1) * 512)
                            po, poB = bank_fixed(4 + qc % 2)
                            pz, pzB = bank_fixed(6 + qc % 2)
                            def qk(kt, k_=k_, kB_=kB_, q_=q_, qB_=qB_, p_=p_, pB_=pB_, csl=csl):
                                ksl = slice(kt * 128, (kt + 1) * 128)
                                pst, pstB = bank_fixed(kt % 4)
                                P.op("pe", lambda e: e.matmul(pst[:], lhsT=k_[:, ksl], rhs=q_[:, csl], start=True, stop=False),
                                     [kB_, qB_], [pstB])
                                P.op("pe", lambda e: e.matmul(pst[:], lhsT=kp[:, ksl], rhs=p_[:, csl], start=False, stop=True),
                                     [kpB, pB_], [pstB])
                                return pst, pstB

                            pend = {0: qk(0), 1: qk(1), 2: qk(2)}
                            for kt in range(NT):
                                pst, pstB = pend.pop(kt)
                                t_, tB_ = pT[npt % 3], pTB[npt % 3]
                                npt += 1
                                P.op("act", lambda e, pst=pst, t_=t_: e.activation(out=t_[:], in_=pst[:], func=AF.Exp, scale=SCALE), [pstB], [tB_])
                                if kt + 3 < NT:
                                    pend[kt + 3] = qk(kt + 3)
                                P.op("pe", lambda e, po=po, kt=kt, t_=t_, v_=v_: e.matmul(po[:], lhsT=v_[:, kt, :], rhs=t_[:], start=(kt == 0), stop=(kt == NT - 1)),
                                     [vB_, tB_], [poB])
                                P.op("pe", lambda e, pz=pz, kt=kt, t_=t_: e.matmul(pz[:], lhsT=ones[:], rhs=t_[:], start=(kt == 0), stop=(kt == NT - 1)),
                                     [onesB, tB_], [pzB])
                            P.op("dve", lambda e, pz=pz: e.reciprocal(out=rs[:], in_=pz[:]), [pzB], [rsB])
                            P.op("dve", lambda e, po=po, h=h, csl=csl: e.tensor_tensor(out=OT[:, h, csl], in0=po[:], in1=rs[:], op=ALU.mult), [poB, rsB], [OTB])
                    P.barrier()
                with ExitStack() as s2:
                    wo = sb(s2, "mwo", [128, 8, D], BF16); woB = Buf()
                    wv = T["mla_w_out"][0].rearrange("(kc p) n -> p kc n", p=128)
                    for q in range(2):
                        P.dma("pool", wo[:, q * 4:(q + 1) * 4, :], wv[:, q * 4:(q + 1) * 4, :], writes=[woB])
                    _, _, (G, GB) = load_mod_tiles(s2, 1, 0, "norm_mix_g")
                    xb = [sb(s2, f"bx{i}", [128, D], F32) for i in range(2)]; xbB = [Buf(), Buf()]
                    yo = [sb(s2, f"by{i}", [128, D], F32) for i in range(2)]; yoB = [Buf(), Buf()]
                    P.dma("sp", xb[0][:], XIN[0:128, :], reads=[XINB], writes=[xbB[0]])
                    for tt in range(NT):
                        tsl = slice(tt * 128, (tt + 1) * 128)
                        xt, xtB = xb[tt % 2], xbB[tt % 2]
                        y, yB = yo[tt % 2], yoB[tt % 2]
                        if tt + 1 < NT:
                            P.dma("sp", xb[(tt + 1) % 2][:], XIN[(tt + 1) * 128:(tt + 2) * 128, :], reads=[XINB], writes=[xbB[(tt + 1) % 2]])
                        for nh in range(2):
                            pt, pB = bank()
                            for hh in range(8):
                                P.op("pe", lambda e, pt=pt, hh=hh, tsl=tsl, nh=nh: e.matmul(pt[:], lhsT=OT[:, hh, tsl], rhs=wo[:, hh, nh * 512:(nh + 1) * 512], start=(hh == 0), stop=(hh == 7)),
                                     [OTB, woB], [pB])
                            sl = slice(nh * 512, (nh + 1) * 512)
                            P.op("dve", lambda e, pt=pt, sl=sl, y=y: e.tensor_tensor(out=y[:, sl], in0=pt[:], in1=G[:, sl], op=ALU.mult), [pB, GB], [yB])
                        P.op("pool", lambda e, y=y, xt=xt: e.tensor_tensor(out=y[:], in0=y[:], in1=xt[:], op=ALU.add), [yB, xtB], [yB])
                        P.dma("sp", XOUT[tsl, :], y[:], reads=[yB], writes=[XOUTB])
                    P.barrier()

        def copy_to_out(src, srcB):
            with ExitStack() as st:
                cb = [sb(st, f"cpy{i}", [128, D], F32) for i in range(2)]; cbB = [Buf(), Buf()]
                for tt in range(NT):
                    t_, tB = cb[tt % 2], cbB[tt % 2]
                    P.dma("sp", t_[:], src[tt * 128:(tt + 1) * 128, :], reads=[srcB], writes=[tB])
                    P.dma("sp", OUT[tt * 128:(tt + 1) * 128, :], t_[:], reads=[tB], writes=[OUTB])
                P.barrier()

        OUTB = Buf()

        phase_mod()
        phase_filter()
        phase_hyena_in()
        phase_hyena_fwd()
        phase_hyena_out()
        if stage == "hyena":
            copy_to_out(T["XA"], XAB)
        else:
            XBB = Buf()
            phase_moe(0, T["XA"], XAB, T["XB"], XBB, False)
            if stage == "moe0":
                copy_to_out(T["XB"], XBB)
            else:
                XCB = Buf()
                phase_mla_proj(T["XB"], XBB)
                phase_mla_attn(T["XB"], XBB, T["XC"], XCB)
                if stage == "mla":
                    copy_to_out(T["XC"], XCB)
                else:
                    phase_moe(1, T["XC"], XCB, OUT, OUTB, True)
        P.barrier()
    return nc


def _prep_inputs(inputs):
    global _CONSTS
    if _CONSTS is None:
        _CONSTS = _consts()
    shared = {}
    for k, v in inputs.items():
        if k in ("x", "c", "positions"):
            continue
        shared[k] = np.ascontiguousarray(np.asarray(v))
    shared.update(_CONSTS)
    in_maps = []
    x = np.asarray(inputs["x"]); c = np.asarray(inputs["c"]); pos = np.asarray(inputs["positions"])
    for b in range(x.shape[0]):
        m = dict(shared)
        m["x"] = np.ascontiguousarray(x[b])
        m["c"] = np.ascontiguousarray(c[b:b + 1])
        m["pos"] = np.ascontiguousarray(pos[b].astype(np.int32))
        in_maps.append(m)
    return in_maps


def kernel(**inputs):
    in_maps = _prep_inputs(inputs)
    nc = build("all")
    res = run_bass_kernel_spmd(nc, in_maps, core_ids=list(range(len(in_maps))))
    return np.stack([r["out"] for r in res.results], axis=0).astype(np.float32)
```

```python
import math
from contextlib import ExitStack

import ml_dtypes
import numpy as np

import concourse.bass as bass
import concourse.mybir as mybir
from concourse.bass_utils import run_bass_kernel_spmd

F32 = mybir.dt.float32
BF16 = mybir.dt.bfloat16
I32 = mybir.dt.int32
U32 = mybir.dt.uint32
AF = mybir.ActivationFunctionType
ALU = mybir.AluOpType
AX = mybir.AxisListType

S = 4096
D = 1024
NT = S // 128
EPS = 1e-6
NEXP = 16
CAP = 512
TWO_PI = 2.0 * math.pi


class Buf:
    __slots__ = ("w", "r")

    def __init__(self):
        self.w = None
        self.r = []


class Prog:
    NQ = 6

    def __init__(self, nc, es):
        self.nc = nc
        self.es = es
        self.eng = {"pe": nc.tensor, "act": nc.scalar, "dve": nc.vector,
                    "pool": nc.gpsimd, "sp": nc.sync}
        self.sem = {}
        self.cnt = {}
        self.nsem = 0
        for e in ("pe", "act", "dve", "pool"):
            self.sem[e] = self._newsem()
            self.cnt[e] = 0
        self.waited = {e: {} for e in self.eng}
        self.dq = {}
        self.dqi = {}
        for q in ("sp", "act", "pool"):
            self.dq[q] = [[self._newsem(), 0] for _ in range(16 if q == "pool" else self.NQ)]
            self.dqi[q] = 0

    def _newsem(self):
        self.nsem += 1
        return self.es.enter_context(self.nc.semaphore(f"sm{self.nsem}"))

    def _wait(self, e, tok):
        sem, val, src = tok
        if src == "pe" and e == "pe":
            return
        key = id(sem)
        if self.waited[e].get(key, 0) >= val:
            return
        self.eng[e].wait_ge(sem, val)
        self.waited[e][key] = val

    def _deps(self, e, reads, writes):
        for b in reads:
            if b.w is not None:
                self._wait(e, b.w)
        for b in writes:
            if b.w is not None:
                self._wait(e, b.w)
            for t in b.r:
                self._wait(e, t)

    def _record(self, tok, reads, writes):
        for b in reads:
            b.r = [t for t in b.r if t[0] is not tok[0]]
            b.r.append(tok)
        for b in writes:
            b.w = tok
            b.r = []

    def op(self, e, fn, reads=(), writes=()):
        self._deps(e, reads, writes)
        ins = fn(self.eng[e])
        self.cnt[e] += 1
        ins.then_inc(self.sem[e], 1)
        tok = (self.sem[e], self.cnt[e], e)
        self._record(tok, reads, writes)
        return tok

    def dma(self, q, out, in_, reads=(), writes=(), fn=None, **kw):
        slot = self.dq[q][self.dqi[q] % len(self.dq[q])]
        self.dqi[q] += 1
        sem, c = slot
        if c > 0:
            self._wait(q, (sem, c, None))
        self._deps(q, reads, writes)
        if fn is None:
            ins = self.eng[q].dma_start(out=out, in_=in_, **kw)
        else:
            ins = fn(self.eng[q])
        ins.then_inc(sem, 16)
        slot[1] = c + 16
        tok = (sem, c + 16, None)
        self._record(tok, reads, writes)
        return tok

    def barrier(self):
        toks = [(self.sem[e], self.cnt[e], None) for e in self.sem if self.cnt[e] > 0]
        for q in self.dq:
            for sem, c in self.dq[q]:
                if c > 0:
                    toks.append((sem, c, None))
        for e in self.eng:
            for t in toks:
                self._wait(e, t)
        for e in self.sem:
            if self.cnt[e] > 6000:
                self.sem[e] = self._newsem()
                self.cnt[e] = 0
        for q in self.dq:
            for slot in self.dq[q]:
                if slot[1] > 6000:
                    slot[0] = self._newsem()
                    slot[1] = 0


def _consts():
    c = {}
    c["ident"] = np.eye(128, dtype=np.float32)
    L = S
    t = np.linspace(0.0, 1.0, L, dtype=np.float32)[:, None]
    w = (2.0 * math.pi * np.arange(L, dtype=np.float32)[:, None] / L).astype(np.float32)
    f = np.linspace(1e-4, 15.0, 16, dtype=np.float32)[None, :]
    ang = (f * w).astype(np.float32)
    z = np.concatenate([t, np.cos(ang), -np.sin(ang)], axis=-1).astype(np.float32)
    c["zfT"] = np.ascontiguousarray(z.T)
    c["tneg"] = np.ascontiguousarray((-t[:, 0]).reshape(NT, 128).T)
    mind = math.log(1e-2) / 0.3
    maxd = math.log(1e-2) / 1.5
    deltas = np.linspace(mind, maxd, D, dtype=np.float32)
    c["absd"] = np.abs(deltas).astype(np.float32)
    fi = np.arange(4096, dtype=np.int64)
    ti = np.arange(4096, dtype=np.int64)
    m = ((2 * fi[None, :] + 1) * ti[:, None]) % 16384
    angm = m.astype(np.float64) * (math.pi / 8192.0)
    cosm = np.cos(angm)
    sinm = np.sin(angm)
    fw = np.stack([cosm, -sinm], axis=0)
    fw = fw.reshape(2, NT, 128, 32, 128)
    c["FWD"] = np.ascontiguousarray(fw.transpose(3, 2, 0, 1, 4)).astype(ml_dtypes.bfloat16)
    sc = 2.0 / 8192.0
    iv = np.stack([cosm * sc, -sinm * sc], axis=0)
    iv = iv.reshape(2, NT, 128, 32, 128)
    c["INV"] = np.ascontiguousarray(iv.transpose(1, 4, 3, 0, 2)).astype(ml_dtypes.bfloat16)
    invf = (10000.0 ** (-np.arange(0, 64, 2, dtype=np.float32) / 64)).astype(np.float32)
    c["invf"] = invf
    return c


_CONSTS = None


def build(stage="all", debug=False):
    nc = bass.Bass("TRN2", target_bir_lowering=False)
    T = {}

    def din(name, shape, dt=F32):
        T[name] = nc.dram_tensor(name, list(shape), dt, kind="ExternalInput").ap()
        return T[name]

    def dscr(name, shape, dt=F32):
        T[name] = nc.dram_tensor(name, list(shape), dt).ap()
        return T[name]

    din("x", [S, D]); din("c", [1, D]); din("pos", [S], I32)
    din("ada_w", [2, D, 6 * D]); din("ada_b", [2, 6 * D])
    din("norm_mix_g", [2, D]); din("norm_ffn_g", [2, D])
    din("hy_w_in", [1, D, 3 * D]); din("hy_b_in", [1, 3 * D]); din("hy_conv_w", [1, 3, 3 * D])
    din("hy_conv_b", [1, 3 * D]); din("hy_f_w1", [1, 33, 64]); din("hy_f_b1", [1, 64])
    din("hy_f_w2", [1, 64, 64]); din("hy_f_b2", [1, 64]); din("hy_f_w3", [1, 64, 2 * D])
    din("hy_f_freq", [1, 64]); din("hy_f_bias", [1, 1, D]); din("hy_w_out", [1, D, D]); din("hy_b_out", [1, D])
    din("mla_w_in", [1, D, 448]); din("mla_q_norm_g", [1, 256]); din("mla_w_qb", [1, 256, 1536])
    din("mla_kv_norm_g", [1, 128]); din("mla_w_kvb", [1, 128, 2048]); din("mla_w_out", [1, D, D])
    din("moe_w_router", [2, D, NEXP]); din("moe_w_gate", [2, NEXP, D, D]); din("moe_w_up", [2, NEXP, D, D])
    din("moe_w_down", [2, NEXP, D, D]); din("final_norm_g", [D])
    din("ident", [128, 128]); din("zfT", [33, S]); din("tneg", [128, NT]); din("absd", [D])
    din("FWD", [32, 128, 2, 32, 128], BF16); din("INV", [32, 128, 32, 2, 128], BF16); din("invf", [32])
    OUT = nc.dram_tensor("out", [S, D], F32, kind="ExternalOutput").ap()

    dscr("MOD", [2, 6 * D])
    dscr("ZT", [S, D], BF16)
    dscr("X0C", [D, S], BF16)
    dscr("KS", [2, S, D])
    dscr("KT", [2, S, D], BF16)
    dscr("YS", [32, 128, 2, D], BF16)
    dscr("XA", [S, D]); dscr("XB", [S, D]); dscr("XC", [S, D])
    dscr("HF", [S, D], BF16); dscr("ACC", [S, D])
    dscr("QN", [8, 128, S], BF16); dscr("KN", [8, 128, S], BF16); dscr("QP", [8, 64, S], BF16)
    dscr("KP", [64, S], BF16); dscr("V", [S, D], BF16)

    es = ExitStack()
    with es:
        P = Prog(nc, es)
        ps = [es.enter_context(nc.psum_tensor(f"ps{i}", [128, 512], F32)) for i in range(8)]
        psB = [Buf() for _ in range(8)]
        psi = [0]

        def bank():
            i = psi[0] % 8
            psi[0] += 1
            return ps[i], psB[i]

        sbn = [0]

        def sb(st, name, shape, dt):
            sbn[0] += 1
            return st.enter_context(nc.sbuf_tensor(f"{name}_{sbn[0]}", list(shape), dt))

        identf = sb(es, "identf", [128, 128], F32); identfB = Buf()
        identb = sb(es, "identb", [128, 128], BF16); identbB = Buf()
        P.dma("sp", identf[:], T["ident"][:, :], writes=[identfB])
        P.dma("pool", identb[:], T["ident"][:, :], writes=[identbB])

        def bcast_load(q, dst, dstB, src1d):
            return P.dma(q, dst, src1d.partition_broadcast(128), writes=[dstB])

        def phase_mod():
            with ExitStack() as st:
                ccol = sb(st, "ccol", [128, 8], F32); ccolB = Buf()
                cs = sb(st, "cs", [128, 8], F32); csB_ = Buf()
                csb = sb(st, "csb", [128, 8, 128], F32); csbB = Buf()
                adb = sb(st, "adb", [128, 6 * D], F32); adbB = Buf()
                modt = sb(st, "modt", [128, 6 * D], F32); modtB = Buf()
                wb = [sb(st, f"adw{i}", [128, 8, 512], F32) for i in range(2)]
                wbB = [Buf(), Buf()]
                P.dma("sp", ccol[:], T["c"].rearrange("o (kc p) -> p (o kc)", p=128), writes=[ccolB],
                      allow_slow_non_contiguous=True)
                P.op("act", lambda e: e.activation(out=cs[:], in_=ccol[:], func=AF.Silu), [ccolB], [csB_])
                for kc in range(8):
                    P.op("dve", lambda e, kc=kc: e.tensor_copy(out=csb[:, kc, :], in_=cs[:, kc:kc + 1].to_broadcast([128, 128])),
                         [csB_], [csbB])
                n = 0
                for i in range(2):
                    bcast_load("sp", adb[:], adbB, T["ada_b"][i, :])
                    wv = T["ada_w"][i].rearrange("(kc p) n -> p kc n", p=128)
                    for q in range(12):
                        w, wB = wb[n % 2], wbB[n % 2]
                        n += 1
                        P.dma("sp", w[:], wv[:, :, q * 512:(q + 1) * 512], writes=[wB])
                        pt, pB = bank()
                        for kc in range(8):
                            P.op("pe", lambda e, kc=kc, w=w, pt=pt: e.matmul(pt[:], lhsT=csb[:, kc, :], rhs=w[:, kc, :],
                                                                              start=(kc == 0), stop=(kc == 7)),
                                 [csbB, wB], [pB])
                        P.op("dve", lambda e, q=q, pt=pt: e.tensor_tensor(out=modt[:, q * 512:(q + 1) * 512], in0=pt[:],
                                                                         in1=adb[:, q * 512:(q + 1) * 512], op=ALU.add),
                             [pB, adbB], [modtB])
                    P.dma("sp", T["MOD"][i:i + 1, :], modt[0:1, :], reads=[modtB], writes=[MODB])
                P.barrier()

        MODB = Buf()

        def load_mod_tiles(st, layer, which, gname):
            base = 0 if which == 0 else 3
            A = sb(st, f"A{layer}{which}", [128, D], F32); AB = Buf()
            Bt = sb(st, f"B{layer}{which}", [128, D], F32); BB = Buf()
            G = sb(st, f"G{layer}{which}", [128, D], F32); GB = Buf()
            gt = sb(st, f"g{layer}{which}", [128, D], F32); gB = Buf()
            P.dma("sp", Bt[:], T["MOD"][layer, (base + 0) * D:(base + 1) * D].partition_broadcast(128), reads=[MODB], writes=[BB])
            P.dma("sp", A[:], T["MOD"][layer, (base + 1) * D:(base + 2) * D].partition_broadcast(128), reads=[MODB], writes=[AB])
            P.dma("sp", G[:], T["MOD"][layer, (base + 2) * D:(base + 3) * D].partition_broadcast(128), reads=[MODB], writes=[GB])
            bcast_load("sp", gt[:], gB, T[gname][layer, :])
            P.op("dve", lambda e: e.scalar_tensor_tensor(out=A[:], in0=A[:], scalar=1.0, in1=gt[:], op0=ALU.add, op1=ALU.mult),
                 [gB, AB], [AB])
            return (A, AB), (Bt, BB), (G, GB)

        class NormCtx:
            def __init__(self, st, tag):
                self.junk = sb(st, f"junk{tag}", [128, D], F32); self.junkB = Buf()
                self.ss = [sb(st, f"ss{tag}{i}", [128, 4], F32) for i in range(2)]; self.ssB = [Buf(), Buf()]
                self.tmp = [sb(st, f"nt{tag}{i}", [128, D], F32) for i in range(2)]; self.tmpB = [Buf(), Buf()]
                self.n = 0

        def norm_mod(nx, xt, xtB, A, AB, Bt, BB, out, outB, tmp_unused=None, tmpB_unused=None):
            k = nx.n % 2
            nx.n += 1
            ss, ssB = nx.ss[k], nx.ssB[k]
            tmp, tmpB = nx.tmp[k][:], nx.tmpB[k]
            P.op("act", lambda e: e.activation(out=nx.junk[:], in_=xt, func=AF.Square, accum_out=ss[:, 0:1]),
                 [xtB], [nx.junkB, ssB])
            P.op("dve", lambda e: e.tensor_scalar(out=ss[:, 1:2], in0=ss[:, 0:1], scalar1=1.0 / D, scalar2=EPS,
                                                  op0=ALU.mult, op1=ALU.add), [ssB], [ssB])
            P.op("act", lambda e: e.activation(out=ss[:, 2:3], in_=ss[:, 1:2], func=AF.Sqrt), [ssB], [ssB])
            P.op("dve", lambda e: e.reciprocal(out=ss[:, 3:4], in_=ss[:, 2:3]), [ssB], [ssB])
            if Bt is None:
                P.op("dve", lambda e: e.scalar_tensor_tensor(out=out, in0=xt, scalar=ss[:, 3:4], in1=A[:],
                                                             op0=ALU.mult, op1=ALU.mult), [xtB, ssB, AB], [outB])
                return
            P.op("dve", lambda e: e.scalar_tensor_tensor(out=tmp, in0=xt, scalar=ss[:, 3:4], in1=A[:],
                                                         op0=ALU.mult, op1=ALU.mult), [xtB, ssB, AB], [tmpB])
            P.op("pool", lambda e: e.tensor_tensor(out=out, in0=tmp, in1=Bt[:], op=ALU.add), [tmpB, BB], [outB])

        def fwd_dft(st, A, AB_, Bm, BB_, consume):
            fwb = [sb(st, f"fw{i}", [128, 2, 32, 128], BF16) for i in range(2)]
            fwB = [Buf(), Buf()]
            def load_fw(fc):
                fw, fB = fwb[fc % 2], fwB[fc % 2]
                P.dma("sp", fw[:, 0], T["FWD"][fc, :, 0], writes=[fB])
                P.dma("sp", fw[:, 1], T["FWD"][fc, :, 1], writes=[fB])

            load_fw(0)
            for fc in range(32):
                fw, fB = fwb[fc % 2], fwB[fc % 2]
                if fc + 1 < 32:
                    load_fw(fc + 1)
                res = []
                for h in range(2):
                    for cs_ in range(2):
                        src, sB = (A, AB_) if cs_ == 0 else (Bm, BB_)
                        pt, pB = bank()
                        for tt in range(32):
                            P.op("pe", lambda e, pt=pt, fw=fw, cs_=cs_, tt=tt, src=src, h=h:
                                 e.matmul(pt[:], lhsT=fw[:, cs_, tt, :], rhs=src[:, tt, h * 512:(h + 1) * 512],
                                          start=(tt == 0), stop=(tt == 31)), [fB, sB], [pB])
                        res.append((pt, pB))
                consume(fc, res)

        def phase_filter():
            with ExitStack() as st:
                KTB = Buf()
                with ExitStack() as s2:
                    kab = [sb(s2, f"kab{i}", [128, 2, D], BF16) for i in range(2)]; kabB = [Buf(), Buf()]
                    zf = sb(s2, "zf", [33, S], F32); zfB = Buf()
                    h1 = sb(s2, "h1", [64, S], F32); h1B = Buf()
                    h2 = sb(s2, "h2", [64, S], F32); h2B = Buf()
                    w1 = sb(s2, "fw1", [33, 64], F32); w1B = Buf()
                    w2 = sb(s2, "fw2", [64, 64], F32); w2B = Buf()
                    w3 = sb(s2, "fw3", [64, 2 * D], F32); w3B = Buf()
                    col = sb(s2, "fcol", [64, 8], F32); colB = Buf()
                    pre = sb(s2, "fpre", [64, 512], F32); preB = Buf()
                    ki = sb(s2, "fki", [64, 512], I32); kiB = Buf()
                    kf = sb(s2, "fkf", [64, 512], F32); kfB = Buf()
                    absd = sb(s2, "absd", [128, D], F32); absdB = Buf()
                    tneg = sb(s2, "tneg", [128, NT], F32); tnegB = Buf()
                    dec = sb(s2, "dec", [128, D], F32); decB = Buf()
                    kfw = sb(s2, "kfw", [128, D], F32); kfwB = Buf()
                    kbw = sb(s2, "kbw", [128, D], F32); kbwB = Buf()
                    fbias = sb(s2, "fbias", [1, D], F32); fbiasB = Buf()
                    P.dma("sp", zf[:], T["zfT"][:, :], writes=[zfB])
                    P.dma("sp", w1[:], T["hy_f_w1"][0], writes=[w1B])
                    P.dma("sp", w2[:], T["hy_f_w2"][0], writes=[w2B])
                    P.dma("sp", w3[:], T["hy_f_w3"][0], writes=[w3B])
                    P.dma("sp", col[:, 0:1], T["hy_f_b1"][0].rearrange("(p o) -> p o", o=1), writes=[colB])
                    P.dma("sp", col[:, 1:2], T["hy_f_b2"][0].rearrange("(p o) -> p o", o=1), writes=[colB])
                    P.dma("sp", col[:, 2:3], T["hy_f_freq"][0].rearrange("(p o) -> p o", o=1), writes=[colB])
                    P.dma("sp", tneg[:], T["tneg"][:, :], writes=[tnegB])
                    P.dma("sp", fbias[:], T["hy_f_bias"][0], writes=[fbiasB])
                    bcast_load("sp", absd[:], absdB, T["absd"])
                    P.op("dve", lambda e: e.tensor_tensor(out=col[:, 3:4], in0=col[:, 0:1], in1=col[:, 2:3], op=ALU.mult), [colB], [colB])
                    P.op("dve", lambda e: e.tensor_tensor(out=col[:, 4:5], in0=col[:, 1:2], in1=col[:, 2:3], op=ALU.mult), [colB], [colB])

                    def sin_layer(w, wB, src, srcB, K, bcol, dst, dstB):
                        for tch in range(8):
                            pt, pB = bank()
                            P.op("pe", lambda e, pt=pt, tch=tch: e.matmul(pt[0:64, :], lhsT=w[0:K, :], rhs=src[0:K, tch * 512:(tch + 1) * 512],
                                                                          start=True, stop=True), [wB, srcB], [pB])
                            P.op("dve", lambda e, pt=pt: e.tensor_scalar(out=pre[:], in0=pt[0:64, :], scalar1=col[:, 2:3], scalar2=col[:, bcol:bcol + 1],
                                                                         op0=ALU.mult, op1=ALU.add), [pB, colB], [preB])
                            P.op("dve", lambda e: e.tensor_scalar(out=ki[:], in0=pre[:], scalar1=1.0 / TWO_PI, scalar2=None, op0=ALU.mult), [preB], [kiB])
                            P.op("dve", lambda e: e.tensor_copy(out=kf[:], in_=ki[:]), [kiB], [kfB])
                            P.op("dve", lambda e: e.scalar_tensor_tensor(out=pre[:], in0=kf[:], scalar=-TWO_PI, in1=pre[:], op0=ALU.mult, op1=ALU.add),
                                 [kfB, preB], [preB])
                            P.op("dve", lambda e: e.tensor_scalar(out=pre[:], in0=pre[:], scalar1=math.pi, scalar2=-math.pi, op0=ALU.min, op1=ALU.max), [preB], [preB])
                            P.op("act", lambda e, tch=tch: e.activation(out=dst[:, tch * 512:(tch + 1) * 512], in_=pre[:], func=AF.Sin), [preB], [dstB])

                    sin_layer(w1, w1B, zf, zfB, 33, 3, h1, h1B)
                    sin_layer(w2, w2B, h1, h1B, 64, 4, h2, h2B)
                    for tt in range(NT):
                        P.op("act", lambda e, tt=tt: e.activation(out=dec[:], in_=absd[:], func=AF.Exp, scale=tneg[:, tt:tt + 1]), [absdB, tnegB], [decB])
                        for q in range(4):
                            pt, pB = bank()
                            P.op("pe", lambda e, pt=pt, tt=tt, q=q: e.matmul(pt[:], lhsT=h2[:, tt * 128:(tt + 1) * 128], rhs=w3[:, q * 512:(q + 1) * 512],
                                                                             start=True, stop=True), [h2B, w3B], [pB])
                            dst, dB = (kfw, kfwB) if q < 2 else (kbw, kbwB)
                            P.op("dve", lambda e, pt=pt, q=q, dst=dst: e.tensor_tensor(out=dst[:, (q % 2) * 512:(q % 2 + 1) * 512], in0=pt[:],
                                                                                       in1=dec[:, (q % 2) * 512:(q % 2 + 1) * 512], op=ALU.mult),
                                 [pB, decB], [dB])
                        if tt == 0:
                            P.op("dve", lambda e: e.tensor_tensor(out=kfw[0:1, :], in0=kfw[0:1, :], in1=fbias[0:1, :], op=ALU.add), [kfwB, fbiasB], [kfwB])
                            P.op("dve", lambda e: e.memset(kbw[0:1, :], 0.0), [], [kbwB])
                        ka, kaB = kab[tt % 2], kabB[tt % 2]
                        P.op("pool", lambda e, ka=ka: e.tensor_tensor(out=ka[:, 0, :], in0=kfw[:], in1=kbw[:], op=ALU.add), [kfwB, kbwB], [kaB])
                        P.op("dve", lambda e, ka=ka: e.tensor_tensor(out=ka[:, 1, :], in0=kfw[:], in1=kbw[:], op=ALU.subtract), [kfwB, kbwB], [kaB])
                        for j in range(2):
                            P.dma("sp", T["KT"][j, tt * 128:(tt + 1) * 128, :], ka[:, j, :], reads=[kaB], writes=[KTB])
                    P.barrier()
                with ExitStack() as s3:
                    KA = sb(s3, "KA", [128, NT, D], BF16); KAB = Buf()
                    KBm = sb(s3, "KBm", [128, NT, D], BF16); KBB = Buf()
                    for j, (kt, ktB) in enumerate(((KA, KAB), (KBm, KBB))):
                        kv = T["KT"][j].rearrange("(tt p) c -> p tt c", p=128)
                        for q in range(4):
                            P.dma("sp", kt[:, q * 8:(q + 1) * 8, :], kv[:, q * 8:(q + 1) * 8, :], reads=[KTB], writes=[ktB])
                    stg = [sb(s3, f"kst{i}", [128, 2, D], F32) for i in range(2)]
                    stgB = [Buf(), Buf()]

                    def store(fc, res):
                        sg, sB = stg[fc % 2], stgB[fc % 2]
                        for h in range(2):
                            for cs_ in range(2):
                                pt, pB = res[h * 2 + cs_]
                                P.op("act", lambda e, pt=pt, h=h, cs_=cs_, sg=sg: e.activation(out=sg[:, cs_, h * 512:(h + 1) * 512], in_=pt[:], func=AF.Identity),
                                     [pB], [sB])
                        for cs_ in range(2):
                            P.dma("sp", T["KS"][cs_, fc * 128:(fc + 1) * 128, :], sg[:, cs_, :], reads=[sB], writes=[KSB])

                    fwd_dft(s3, KA, KAB, KBm, KBB, store)
                    P.barrier()

        KSB = Buf()
        ZTB = Buf(); X0CB = Buf(); YSB = Buf(); XAB = Buf()

        def phase_hyena_in():
            with ExitStack() as st:
                hmT = sb(st, "hmT", [128, 8, S], BF16); hmTB = Buf()
                with ExitStack() as s2:
                    (A, AB), (Bt, BB), _ = load_mod_tiles(s2, 0, 0, "norm_mix_g")
                    nx = NormCtx(s2, "h")
                    xb = [sb(s2, f"hx{i}", [128, D], F32) for i in range(2)]; xbB = [Buf(), Buf()]
                    tmp = None; tmpB = None
                    hb = [sb(s2, f"hb{i}", [128, D], BF16) for i in range(2)]; hbB = [Buf(), Buf()]
                    for tt in range(NT):
                        xt, xtB = xb[tt % 2], xbB[tt % 2]
                        P.dma("sp", xt[:], T["x"][tt * 128:(tt + 1) * 128, :], writes=[xtB])
                        h_, hB_ = hb[tt % 2], hbB[tt % 2]
                        norm_mod(nx, xt[:], xtB, A, AB, Bt, BB, h_[:], hB_)
                        pt, pB = bank()
                        ptb = pt[:].bitcast(BF16)
                        for kc in range(8):
                            P.op("pe", lambda e, kc=kc, ptb=ptb, h_=h_: e.transpose(out=ptb[:, kc * 128:(kc + 1) * 128], in_=h_[:, kc * 128:(kc + 1) * 128], identity=identb[:]),
                                 [hB_, identbB], [pB])
                        P.op("act", lambda e, tt=tt, ptb=ptb: e.activation(out=hmT[:, :, tt * 128:(tt + 1) * 128], in_=ptb.rearrange("p (k t) -> p k t", k=8), func=AF.Identity),
                             [pB], [hmTB])
                    P.barrier()
                with ExitStack() as s2:
                    bcol = sb(s2, "bcol", [128, 24], F32); bcolB = Buf()
                    cw = sb(s2, "cw", [128, 3, 24], F32); cwB = Buf()
                    cbc = sb(s2, "cbc", [128, 24], F32); cbcB = Buf()
                    P.dma("sp", bcol[:], T["hy_b_in"][0].rearrange("(cc p) -> p cc", p=128), writes=[bcolB], allow_slow_non_contiguous=True)
                    P.dma("sp", cbc[:], T["hy_conv_b"][0].rearrange("(cc p) -> p cc", p=128), writes=[cbcB], allow_slow_non_contiguous=True)
                    for j in range(3):
                        P.dma("sp", cw[:, j, :], T["hy_conv_w"][0, j].rearrange("(cc p) -> p cc", p=128), writes=[cwB], allow_slow_non_contiguous=True)
                    wch = [sb(s2, f"wch{i}", [128, 8, 128], BF16) for i in range(2)]; wchB = [Buf(), Buf()]
                    us = [sb(s2, f"u{i}", [128, S + 2], F32) for i in range(2)]; usB = [Buf(), Buf()]
                    ob = [sb(s2, f"ob{i}", [128, S], F32) for i in range(2)]; obB = [Buf(), Buf()]
                    zb = sb(s2, "zb", [128, S], BF16); zbB = Buf()
                    zst = sb(s2, "zst", [128, NT, 128], BF16); zstB = Buf()
                    for u, uB in zip(us, usB):
                        P.op("dve", lambda e, u=u: e.memset(u[:, 0:1], 0.0), [], [uB])
                        P.op("dve", lambda e, u=u: e.memset(u[:, S + 1:S + 2], 0.0), [], [uB])
                    wv = T["hy_w_in"][0].rearrange("(kc p) n -> p kc n", p=128)
                    order = []
                    for j in range(8):
                        order += [8 + j, 16 + j]
                    order += list(range(8))
                    for n, cc in enumerate(order):
                        w, wB = wch[n % 2], wchB[n % 2]
                        u, uB = us[n % 2], usB[n % 2]
                        P.dma("pool", w[:], wv[:, :, cc * 128:(cc + 1) * 128], writes=[wB])
                        for tch in range(8):
                            pt, pB = bank()
                            for kc in range(8):
                                P.op("pe", lambda e, pt=pt, kc=kc, w=w, tch=tch: e.matmul(pt[:], lhsT=w[:, kc, :], rhs=hmT[:, kc, tch * 512:(tch + 1) * 512],
                                                                                         start=(kc == 0), stop=(kc == 7)), [wB, hmTB], [pB])
                            P.op("act", lambda e, pt=pt, tch=tch, cc=cc, u=u: e.activation(out=u[:, 1 + tch * 512:1 + (tch + 1) * 512], in_=pt[:], func=AF.Identity,
                                                                                    bias=bcol[:, cc:cc + 1]), [pB, bcolB], [uB])
                        o, oB = ob[n % 2], obB[n % 2]
                        P.op("act", lambda e, o=o, cc=cc, u=u: e.activation(out=o[:], in_=u[:, 1:S + 1], func=AF.Identity, scale=cw[:, 1, cc:cc + 1], bias=cbc[:, cc:cc + 1]),
                             [uB, cwB, cbcB], [oB])
                        P.op("dve", lambda e, o=o, cc=cc, u=u: e.scalar_tensor_tensor(out=o[:], in0=u[:, 0:S], scalar=cw[:, 0, cc:cc + 1], in1=o[:], op0=ALU.mult, op1=ALU.add),
                             [uB, cwB, oB], [oB])
                        if cc >= 8:
                            P.op("dve", lambda e, o=o, cc=cc, u=u: e.scalar_tensor_tensor(out=o[:], in0=u[:, 2:S + 2], scalar=cw[:, 2, cc:cc + 1], in1=o[:], op0=ALU.mult, op1=ALU.add),
                                 [uB, cwB, oB], [oB])
                        else:
                            P.op("dve", lambda e, o=o, cc=cc, u=u: e.scalar_tensor_tensor(out=zb[:], in0=u[:, 2:S + 2], scalar=cw[:, 2, cc:cc + 1], in1=o[:], op0=ALU.mult, op1=ALU.add),
                                 [uB, cwB, oB], [zbB])
                            P.dma("sp", T["X0C"][cc * 128:(cc + 1) * 128, :], zb[:], reads=[zbB], writes=[X0CB])
                        if cc >= 16:
                            j = cc - 16
                            o1, o1B = ob[(n - 1) % 2], obB[(n - 1) % 2]
                            P.op("pool", lambda e, o=o, o1=o1: e.tensor_tensor(out=zb[:], in0=o[:], in1=o1[:], op=ALU.mult), [oB, o1B], [zbB])
                            for g4 in range(4):
                                pt, pB = bank()
                                ptb = pt[:].bitcast(BF16)
                                for k in range(8):
                                    tt = g4 * 8 + k
                                    P.op("pe", lambda e, ptb=ptb, k=k, tt=tt: e.transpose(out=ptb[:, k * 128:(k + 1) * 128], in_=zb[:, tt * 128:(tt + 1) * 128], identity=identb[:]),
                                         [zbB, identbB], [pB])
                                P.op("act", lambda e, ptb=ptb, g4=g4: e.activation(out=zst[:, g4 * 8:(g4 + 1) * 8, :], in_=ptb.rearrange("p (k c) -> p k c", k=8), func=AF.Identity),
                                     [pB], [zstB])
                            P.dma("sp", T["ZT"].rearrange("(tt p) c -> p tt c", p=128)[:, :, j * 128:(j + 1) * 128], zst[:], reads=[zstB], writes=[ZTB])
                    P.barrier()

        def phase_hyena_fwd():
            with ExitStack() as st:
                zt = sb(st, "zt", [128, NT, D], BF16); ztB = Buf()
                zv = T["ZT"].rearrange("(tt p) c -> p tt c", p=128)
                for q in range(4):
                    P.dma("sp", zt[:, q * 8:(q + 1) * 8, :], zv[:, q * 8:(q + 1) * 8, :], reads=[ZTB], writes=[ztB])
                kk = [sb(st, f"kk{i}", [128, 2, D], F32) for i in range(2)]; kkB = [Buf(), Buf()]
                yt = [sb(st, f"yt{i}", [128, 2, D], BF16) for i in range(2)]; ytB = [Buf(), Buf()]
                t1 = sb(st, "yt1", [128, 512], F32); t1B = Buf()
                t2 = sb(st, "yt2", [128, 512], F32); t2B = Buf()
                t3 = sb(st, "yt3", [128, 512], F32); t3B = Buf()
                t4 = sb(st, "yt4", [128, 512], F32); t4B = Buf()

                def mulk(fc, res):
                    k, kB = kk[fc % 2], kkB[fc % 2]
                    y, yB = yt[fc % 2], ytB[fc % 2]
                    for cs_ in range(2):
                        P.dma("sp", k[:, cs_, :], T["KS"][cs_, fc * 128:(fc + 1) * 128, :], reads=[KSB], writes=[kB])
                    for h in range(2):
                        zr, zrB = res[h * 2]
                        zi, ziB = res[h * 2 + 1]
                        sl = slice(h * 512, (h + 1) * 512)
                        P.op("dve", lambda e, zr=zr, sl=sl: e.tensor_tensor(out=t1[:], in0=zr[:], in1=k[:, 0, sl], op=ALU.mult), [zrB, kB], [t1B])
                        P.op("dve", lambda e, zi=zi, sl=sl: e.tensor_tensor(out=t2[:], in0=zi[:], in1=k[:, 1, sl], op=ALU.mult), [ziB, kB], [t2B])
                        P.op("dve", lambda e, zr=zr, sl=sl: e.tensor_tensor(out=t3[:], in0=zr[:], in1=k[:, 1, sl], op=ALU.mult), [zrB, kB], [t3B])
                        P.op("dve", lambda e, zi=zi, sl=sl: e.tensor_tensor(out=t4[:], in0=zi[:], in1=k[:, 0, sl], op=ALU.mult), [ziB, kB], [t4B])
                        P.op("pool", lambda e, sl=sl, y=y: e.tensor_tensor(out=y[:, 0, sl], in0=t1[:], in1=t2[:], op=ALU.subtract), [t1B, t2B], [yB])
                        P.op("pool", lambda e, sl=sl, y=y: e.tensor_tensor(out=y[:, 1, sl], in0=t3[:], in1=t4[:], op=ALU.add), [t3B, t4B], [yB])
                    P.dma("sp", T["YS"][fc], y[:], reads=[yB], writes=[YSB])

                fwd_dft(st, zt, ztB, zt, ztB, mulk)
                P.barrier()

        def phase_hyena_out():
            with ExitStack() as st:
                X0 = sb(st, "X0", [128, 8, S], BF16); X0B = Buf()
                xv = T["X0C"].rearrange("(cc p) t -> p cc t", p=128)
                for cc in range(8):
                    P.dma("sp", X0[:, cc, :], xv[:, cc, :], reads=[X0CB], writes=[X0B])
                with ExitStack() as s2:
                    Yh = sb(s2, "Yh", [128, 32, 2, 512], BF16); YhB = Buf()
                    gvb = [sb(s2, f"gv{i}", [128, 32, 2, 128], BF16) for i in range(2)]; gvB = [Buf(), Buf()]
                    ytm = [sb(s2, f"ytm{i}", [128, 512], BF16) for i in range(2)]; ytmB = [Buf(), Buf()]
                    yv = T["YS"].rearrange("fc p cs c -> p fc cs c")
                    prev = None

                    def xpose(h, to, ym, ymB):
                        p2, p2B = bank()
                        p2b = p2[:].bitcast(BF16)
                        for j in range(4):
                            P.op("pe", lambda e, j=j: e.transpose(out=p2b[:, j * 128:(j + 1) * 128], in_=ym[:, j * 128:(j + 1) * 128], identity=identb[:]),
                                 [ymB, identbB], [p2B])
                        P.op("dve", lambda e: e.tensor_tensor(out=X0[:, h * 4:(h + 1) * 4, to * 128:(to + 1) * 128],
                                                              in0=p2b[:, 0:512].rearrange("p (j t) -> p j t", j=4),
                                                              in1=X0[:, h * 4:(h + 1) * 4, to * 128:(to + 1) * 128], op=ALU.mult),
                             [p2B, X0B], [X0B])

                    for h in range(2):
                        for q in range(4):
                            for cs_ in range(2):
                                P.dma("sp", Yh[:, q * 8:(q + 1) * 8, cs_, :], yv[:, q * 8:(q + 1) * 8, cs_, h * 512:(h + 1) * 512], reads=[YSB], writes=[YhB])
                        for to in range(NT):
                            gv, gB = gvb[to % 2], gvB[to % 2]
                            for q in range(2):
                                P.dma("sp", gv[:, q * 16:(q + 1) * 16], T["INV"][to, :, q * 16:(q + 1) * 16], writes=[gB])
                            pt, pB = bank()
                            n = 0
                            for fc in range(32):
                                for cs_ in range(2):
                                    P.op("pe", lambda e, pt=pt, gv=gv, fc=fc, cs_=cs_, n=n: e.matmul(pt[:], lhsT=gv[:, fc, cs_, :], rhs=Yh[:, fc, cs_, :],
                                                                                               start=(n == 0), stop=(n == 63)), [gB, YhB], [pB])
                                    n += 1
                            ym, ymB = ytm[to % 2], ytmB[to % 2]
                            P.op("act", lambda e, pt=pt, ym=ym: e.activation(out=ym[:], in_=pt[:], func=AF.Identity), [pB], [ymB])
                            if prev is not None:
                                xpose(*prev)
                            prev = (h, to, ym, ymB)
                    xpose(*prev)
                    P.barrier()
                with ExitStack() as s2:
                    wo = sb(s2, "wo", [128, 8, D], BF16); woB = Buf()
                    wv = T["hy_w_out"][0].rearrange("(kc p) n -> p kc n", p=128)
                    for q in range(2):
                        P.dma("pool", wo[:, q * 4:(q + 1) * 4, :], wv[:, q * 4:(q + 1) * 4, :], writes=[woB])
                    _, _, (G, GB) = load_mod_tiles(s2, 0, 0, "norm_mix_g")
                    bo = sb(s2, "bo", [128, D], F32); boB = Buf()
                    bcast_load("sp", bo[:], boB, T["hy_b_out"][0, :])
                    xb = [sb(s2, f"ox{i}", [128, D], F32) for i in range(2)]; xbB = [Buf(), Buf()]
                    yo = [sb(s2, f"oy{i}", [128, D], F32) for i in range(2)]; yoB = [Buf(), Buf()]
                    P.dma("sp", xb[0][:], T["x"][0:128, :], writes=[xbB[0]])
                    for tt in range(NT):
                        xt, xtB = xb[tt % 2], xbB[tt % 2]
                        y, yB = yo[tt % 2], yoB[tt % 2]
                        if tt + 1 < NT:
                            P.dma("sp", xb[(tt + 1) % 2][:], T["x"][(tt + 1) * 128:(tt + 2) * 128, :], writes=[xbB[(tt + 1) % 2]])
                        for nh in range(2):
                            pt, pB = bank()
                            for cc in range(8):
                                P.op("pe", lambda e, pt=pt, cc=cc, tt=tt, nh=nh: e.matmul(pt[:], lhsT=X0[:, cc, tt * 128:(tt + 1) * 128], rhs=wo[:, cc, nh * 512:(nh + 1) * 512],
                                                                                         start=(cc == 0), stop=(cc == 7)), [X0B, woB], [pB])
                            sl = slice(nh * 512, (nh + 1) * 512)
                            P.op("dve", lambda e, pt=pt, sl=sl, y=y: e.tensor_tensor(out=y[:, sl], in0=pt[:], in1=bo[:, sl], op=ALU.add), [pB, boB], [yB])
                        P.op("pool", lambda e, y=y: e.tensor_tensor(out=y[:], in0=y[:], in1=G[:], op=ALU.mult), [yB, GB], [yB])
                        P.op("dve", lambda e, y=y, xt=xt: e.tensor_tensor(out=y[:], in0=y[:], in1=xt[:], op=ALU.add), [yB, xtB], [yB])
                        P.dma("sp", T["XA"][tt * 128:(tt + 1) * 128, :], y[:], reads=[yB], writes=[XAB])
                    P.barrier()

        HFB = Buf(); ACCB = Buf()

        def phase_moe(layer, XIN, XINB, XOUT, XOUTB, final):
            with ExitStack() as st:
                IDX = sb(st, "IDX", [128, 4, NEXP], U32); IDXB = Buf()
                GVt = sb(st, "GVt", [128, 4, NEXP], F32); GVB = Buf()
                (A, AB), (Bt, BB), (G, GB) = load_mod_tiles(st, layer, 1, "norm_ffn_g")
                wts = [[sb(st, f"w{n}{i}", [128, 8, D], BF16) for n in "gud"] for i in range(2)]
                wtsB = [[Buf() for _ in range(3)] for i in range(2)]
                wnames = ("moe_w_gate", "moe_w_up", "moe_w_down")

                def issue_wloads(e_):
                    for n in range(3):
                        wv = T[wnames[n]][layer, e_].rearrange("(kc p) n -> p kc n", p=128)
                        w, wB = wts[e_ % 2][n], wtsB[e_ % 2][n]
                        P.dma("pool", w[:], wv[:, :, :], writes=[wB])

                issue_wloads(0)
                issue_wloads(1)
                with ExitStack() as s1:
                    AFFT = sb(s1, "AFFT", [NEXP, S], F32); AFFTB = Buf()
                    with ExitStack() as s2:
                        nx = NormCtx(s2, "m")
                        wr = sb(s2, "wr", [128, 8, NEXP], F32); wrB = Buf()
                        P.dma("sp", wr[:], T["moe_w_router"][layer].rearrange("(kc p) e -> p kc e", p=128), writes=[wrB])
                        zt_ = sb(s2, "zero", [128, D], F32); ztB_ = Buf()
                        P.op("pool", lambda e: e.memset(zt_[:], 0.0), [], [ztB_])
                        for tt in range(NT):
                            P.dma("sp", T["ACC"][tt * 128:(tt + 1) * 128, :], zt_[:], reads=[ztB_], writes=[ACCB])
                        xb = [sb(s2, f"mx{i}", [128, D], F32) for i in range(2)]; xbB = [Buf(), Buf()]
                        tmp = None; tmpB = None
                        hf = [sb(s2, f"hf{i}", [128, D], F32) for i in range(2)]; hfB = [Buf(), Buf()]
                        hfb = [sb(s2, f"hfb{i}", [128, D], BF16) for i in range(2)]; hfbB = [Buf(), Buf()]
                        hfT = sb(s2, "hfT", [128, 8, 128], F32); hfTB = Buf()
                        sm = sb(s2, "sm", [128, 8], F32); smB = Buf()
                        ex = sb(s2, "ex", [128, NEXP], F32); exB = Buf()
                        aff = sb(s2, "aff", [128, NEXP], F32); affB = Buf()
                        P.dma("sp", xb[0][:], XIN[0:128, :], reads=[XINB], writes=[xbB[0]])
                        for tt in range(NT):
                            xt, xtB = xb[tt % 2], xbB[tt % 2]
                            if tt + 1 < NT:
                                P.dma("sp", xb[(tt + 1) % 2][:], XIN[(tt + 1) * 128:(tt + 2) * 128, :], reads=[XINB], writes=[xbB[(tt + 1) % 2]])
                            h_, hB_ = hf[tt % 2], hfB[tt % 2]
                            hb_, hbB_ = hfb[tt % 2], hfbB[tt % 2]
                            norm_mod(nx, xt[:], xtB, A, AB, Bt, BB, h_[:], hB_)
                            P.op("act", lambda e, h_=h_, hb_=hb_: e.activation(out=hb_[:], in_=h_[:], func=AF.Identity), [hB_], [hbB_])
                            P.dma("sp", T["HF"][tt * 128:(tt + 1) * 128, :], hb_[:], reads=[hbB_], writes=[HFB])
                            for half in range(2):
                                pt, pB = bank()
                                for k in range(4):
                                    kc = half * 4 + k
                                    P.op("pe", lambda e, pt=pt, k=k, kc=kc, h_=h_: e.transpose(out=pt[:, k * 128:(k + 1) * 128], in_=h_[:, kc * 128:(kc + 1) * 128], identity=identf[:]),
                                         [hB_, identfB], [pB])
                                P.op("act", lambda e, pt=pt, half=half: e.activation(out=hfT[:, half * 4:(half + 1) * 4, :], in_=pt[:].rearrange("p (k t) -> p k t", k=4), func=AF.Identity),
                                     [pB], [hfTB])
                            pt, pB = bank()
                            for kc in range(8):
                                P.op("pe", lambda e, pt=pt, kc=kc: e.matmul(pt[:, 0:NEXP], lhsT=hfT[:, kc, :], rhs=wr[:, kc, :], start=(kc == 0), stop=(kc == 7)),
                                     [hfTB, wrB], [pB])
                            P.op("dve", lambda e, pt=pt: e.tensor_reduce(out=sm[:, 0:1], in_=pt[:, 0:NEXP], axis=AX.X, op=ALU.max, negate=True), [pB], [smB])
                            P.op("act", lambda e, pt=pt: e.activation(out=ex[:], in_=pt[:, 0:NEXP], func=AF.Exp, bias=sm[:, 0:1], accum_out=sm[:, 1:2]), [pB, smB], [exB, smB])
                            P.op("dve", lambda e: e.reciprocal(out=sm[:, 2:3], in_=sm[:, 1:2]), [smB], [smB])
                            P.op("dve", lambda e: e.tensor_scalar(out=aff[:], in0=ex[:], scalar1=sm[:, 2:3], scalar2=None, op0=ALU.mult), [exB, smB], [affB])
                            p2, p2B = bank()
                            P.op("pe", lambda e, p2=p2: e.transpose(out=p2[0:NEXP, 0:128], in_=aff[:, 0:NEXP], identity=identf[:]), [affB, identfB], [p2B])
                            P.op("act", lambda e, p2=p2, tt=tt: e.activation(out=AFFT[:, tt * 128:(tt + 1) * 128], in_=p2[0:NEXP, 0:128], func=AF.Identity), [p2B], [AFFTB])
                        P.barrier()
                    with ExitStack() as s2:
                        work = sb(s2, "work", [NEXP, S], F32); workB = Buf()
                        vals = sb(s2, "vals", [NEXP, CAP], F32); valsB = Buf()
                        idxu = sb(s2, "idxu", [NEXP, CAP], U32); idxuB = Buf()
                        idxf = sb(s2, "idxf", [NEXP, CAP], F32); idxfB = Buf()
                        idt = sb(s2, "idt", [128, 4, NEXP], F32); idtB = Buf()
                        P.op("dve", lambda e: e.tensor_copy(out=work[:], in_=AFFT[:]), [AFFTB], [workB])
                        for r in range(CAP // 8):
                            sl = slice(8 * r, 8 * r + 8)
                            P.op("dve", lambda e, sl=sl: e.max(out=vals[:, sl], in_=work[:]), [workB], [valsB])
                            P.op("dve", lambda e, sl=sl: e.max_index(out=idxu[:, sl], in_max=vals[:, sl], in_values=work[:]), [workB, valsB], [idxuB])
                            P.op("dve", lambda e, sl=sl: e.match_replace(out=work[:], in_to_replace=vals[:, sl], in_values=work[:], imm_value=-1.0), [valsB, workB], [workB])
                        P.op("dve", lambda e: e.tensor_copy(out=idxf[:], in_=idxu[:]), [idxuB], [idxfB])
                        for s_ in range(4):
                            pt, pB = bank()
                            P.op("pe", lambda e, pt=pt, s_=s_: e.transpose(out=pt[:, 0:NEXP], in_=idxf[0:NEXP, s_ * 128:(s_ + 1) * 128], identity=identf[0:NEXP, 0:NEXP]),
                                 [idxfB, identfB], [pB])
                            P.op("dve", lambda e, pt=pt, s_=s_: e.tensor_copy(out=idt[:, s_, :], in_=pt[:, 0:NEXP]), [pB], [idtB])
                            P.op("dve", lambda e, s_=s_: e.tensor_copy(out=IDX[:, s_, :], in_=idt[:, s_, :]), [idtB], [IDXB])
                            p2, p2B = bank()
                            P.op("pe", lambda e, p2=p2, s_=s_: e.transpose(out=p2[:, 0:NEXP], in_=vals[0:NEXP, s_ * 128:(s_ + 1) * 128], identity=identf[0:NEXP, 0:NEXP]),
                                 [valsB, identfB], [p2B])
                            P.op("dve", lambda e, p2=p2, s_=s_: e.tensor_copy(out=GVt[:, s_, :], in_=p2[:, 0:NEXP]), [p2B], [GVB])
                        P.barrier()
                with ExitStack() as s1:
                    xs = [sb(s1, f"xs{i}", [128, 4, D], BF16) for i in range(2)]; xsB = [[Buf() for _ in range(4)] for _ in range(2)]
                    xsT = sb(s1, "xsT", [128, 8, CAP], BF16); xsTB = Buf()
                    hid = sb(s1, "hid", [128, 8, CAP], BF16); hidB = Buf()
                    sg = [sb(s1, f"sg{i}", [128, 512], F32) for i in range(2)]; sgB = [Buf(), Buf()]
                    ye = [sb(s1, f"ye{i}", [128, D], F32) for i in range(4)]; yeB = [Buf() for _ in range(4)]

                    def issue_gathers(e_):
                        x_ = xs[e_ % 2]
                        for s_ in range(4):
                            P.dma("pool", None, None, reads=[HFB, IDXB], writes=[xsB[e_ % 2][s_]],
                                  fn=lambda g, s_=s_, x_=x_, e_=e_: g.indirect_dma_start(
                                      out=x_[:, s_, :], out_offset=None, in_=T["HF"][:, :],
                                      in_offset=bass.IndirectOffsetOnAxis(ap=IDX[:, s_, e_:e_ + 1], axis=0)))

                    issue_gathers(0)
                    issue_gathers(1)
                    prev_sc = []
                    for e_ in range(NEXP):
                        (wg, wu, wd), (wgB, wuB, wdB) = wts[e_ % 2], wtsB[e_ % 2]
                        x_ = xs[e_ % 2]
                        for s_ in range(4):
                            pt, pB = bank()
                            ptb = pt[:].bitcast(BF16)
                            for kc in range(8):
                                P.op("pe", lambda e, ptb=ptb, kc=kc, s_=s_, x_=x_: e.transpose(out=ptb[:, kc * 128:(kc + 1) * 128], in_=x_[:, s_, kc * 128:(kc + 1) * 128], identity=identb[:]),
                                     [xsB[e_ % 2][s_], identbB], [pB])
                            P.op("act", lambda e, ptb=ptb, s_=s_: e.activation(out=xsT[:, :, s_ * 128:(s_ + 1) * 128], in_=ptb.rearrange("p (k t) -> p k t", k=8), func=AF.Identity),
                                 [pB], [xsTB])
                        for fcn in range(8):
                            pg, pgB = bank()
                            pu, puB = bank()
                            for kc in range(8):
                                P.op("pe", lambda e, pg=pg, kc=kc, fcn=fcn, wg=wg: e.matmul(pg[:], lhsT=wg[:, kc, fcn * 128:(fcn + 1) * 128], rhs=xsT[:, kc, :], start=(kc == 0), stop=(kc == 7)),
                                     [wgB, xsTB], [pgB])
                            for kc in range(8):
                                P.op("pe", lambda e, pu=pu, kc=kc, fcn=fcn, wu=wu: e.matmul(pu[:], lhsT=wu[:, kc, fcn * 128:(fcn + 1) * 128], rhs=xsT[:, kc, :], start=(kc == 0), stop=(kc == 7)),
                                     [wuB, xsTB], [puB])
                            s__, sB__ = sg[fcn % 2], sgB[fcn % 2]
                            P.op("act", lambda e, pg=pg, s__=s__: e.activation(out=s__[:], in_=pg[:], func=AF.Silu), [pgB], [sB__])
                            P.op("dve", lambda e, pu=pu, s__=s__, fcn=fcn: e.tensor_tensor(out=hid[:, fcn, :], in0=pu[:], in1=s__[:], op=ALU.mult), [puB, sB__], [hidB])
                        for s_ in range(4):
                            y, yB = ye[s_], yeB[s_]
                            for nh in range(2):
                                pt, pB = bank()
                                for fcn in range(8):
                                    P.op("pe", lambda e, pt=pt, fcn=fcn, s_=s_, nh=nh, wd=wd: e.matmul(pt[:], lhsT=hid[:, fcn, s_ * 128:(s_ + 1) * 128], rhs=wd[:, fcn, nh * 512:(nh + 1) * 512],
                                                                                                 start=(fcn == 0), stop=(fcn == 7)), [hidB, wdB], [pB])
                                P.op("act", lambda e, pt=pt, y=y, nh=nh, s_=s_, e_=e_: e.activation(out=y[:, nh * 512:(nh + 1) * 512], in_=pt[:], func=AF.Identity, scale=GVt[:, s_, e_:e_ + 1]),
                                     [pB, GVB], [yB])
                        if e_ + 2 < NEXP:
                            issue_wloads(e_ + 2)
                        if ACCB.w is not None:
                            P._wait("pool", ACCB.w)
                        for t_ in prev_sc:
                            P._wait("pool", t_)
                        cur_sc = []
                        for s_ in range(4):
                            y, yB = ye[s_], yeB[s_]
                            cur_sc.append(P.dma("pool", None, None, reads=[yB, IDXB], writes=[ACCB],
                                                fn=lambda g, s_=s_, y=y, e_=e_: g.indirect_dma_start(
                                                    out=T["ACC"][:, :], out_offset=bass.IndirectOffsetOnAxis(ap=IDX[:, s_, e_:e_ + 1], axis=0),
                                                    in_=y[:], in_offset=None, compute_op=ALU.add)))
                        prev_sc[:] = cur_sc
                        if e_ + 2 < NEXP:
                            issue_gathers(e_ + 2)
                    P.barrier()
                with ExitStack() as s1:
                    xb = [sb(s1, f"cx{i}", [128, D], F32) for i in range(2)]; xbB = [Buf(), Buf()]
                    ab = [sb(s1, f"ca{i}", [128, D], F32) for i in range(2)]; abB = [Buf(), Buf()]
                    ob_ = [sb(s1, f"co{i}", [128, D], F32) for i in range(2)]; obB_ = [Buf(), Buf()]
                    if final:
                        nx = NormCtx(s1, "f")
                        fg = sb(s1, "fg", [128, D], F32); fgB = Buf()
                        bcast_load("sp", fg[:], fgB, T["final_norm_g"])
                    def ld_c(tt):
                        P.dma("sp", xb[tt % 2][:], XIN[tt * 128:(tt + 1) * 128, :], reads=[XINB], writes=[xbB[tt % 2]])
                        P.dma("sp", ab[tt % 2][:], T["ACC"][tt * 128:(tt + 1) * 128, :], reads=[ACCB], writes=[abB[tt % 2]])

                    ld_c(0)
                    for tt in range(NT):
                        xt, xtB = xb[tt % 2], xbB[tt % 2]
                        a_, aB_ = ab[tt % 2], abB[tt % 2]
                        if tt + 1 < NT:
                            ld_c(tt + 1)
                        P.op("pool", lambda e, a_=a_: e.tensor_tensor(out=a_[:], in0=a_[:], in1=G[:], op=ALU.mult), [aB_, GB], [aB_])
                        P.op("dve", lambda e, a_=a_, xt=xt: e.tensor_tensor(out=a_[:], in0=a_[:], in1=xt[:], op=ALU.add), [aB_, xtB], [aB_])
                        if final:
                            o_, oB_ = ob_[tt % 2], obB_[tt % 2]
                            norm_mod(nx, a_[:], aB_, fg, fgB, None, None, o_[:], oB_, None, None)
                            P.dma("sp", XOUT[tt * 128:(tt + 1) * 128, :], o_[:], reads=[oB_], writes=[XOUTB])
                        else:
                            P.dma("sp", XOUT[tt * 128:(tt + 1) * 128, :], a_[:], reads=[aB_], writes=[XOUTB])
                    P.barrier()

        def bank_fixed(i):
            return ps[i], psB[i]

        SCALE = 1.0 / math.sqrt(192.0)
        C1 = 6.28125
        C2 = TWO_PI - 6.28125
        QNB = Buf(); QPB = Buf(); KNB = Buf(); KPB = Buf(); VB = Buf()

        def phase_mla_proj(XIN, XINB):
            with ExitStack() as st:
                cqnT = sb(st, "cqnT", [128, 2, S], BF16); cqnTB = Buf()
                ckvT = sb(st, "ckvT", [128, S], BF16); ckvTB = Buf()
                kpT = sb(st, "kpT", [64, S], BF16); kpTB = Buf()
                cosT = sb(st, "cosT", [64, S], F32); cosTB = Buf()
                sinT = sb(st, "sinT", [64, S], F32); sinTB = Buf()
                with ExitStack() as s2:
                    posi = sb(s2, "posi", [64, S], I32); posiB = Buf()
                    ang = sb(s2, "ang", [64, S], F32); angB = Buf()
                    a2 = sb(s2, "a2", [64, S], F32); a2B = Buf()
                    kq = sb(s2, "kq", [64, S], I32); kqB = Buf()
                    kqf = sb(s2, "kqf", [64, S], F32); kqfB = Buf()
                    ivf = sb(s2, "ivf", [64, 1], F32); ivfB = Buf()
                    P.dma("sp", posi[:], T["pos"].partition_broadcast(64), writes=[posiB])
                    P.dma("sp", ivf[0:32, :], T["invf"].rearrange("(p o) -> p o", o=1), writes=[ivfB])
                    P.dma("sp", ivf[32:64, :], T["invf"].rearrange("(p o) -> p o", o=1), writes=[ivfB])
                    P.op("dve", lambda e: e.tensor_copy(out=ang[:], in_=posi[:]), [posiB], [angB])
                    P.op("dve", lambda e: e.tensor_scalar(out=ang[:], in0=ang[:], scalar1=ivf[:, 0:1], scalar2=None, op0=ALU.mult), [angB, ivfB], [angB])
                    for shift, dst, dB in ((0.0, sinT, sinTB), (math.pi / 2.0, cosT, cosTB)):
                        P.op("dve", lambda e, shift=shift: e.tensor_scalar(out=a2[:], in0=ang[:], scalar1=shift, scalar2=None, op0=ALU.add), [angB], [a2B])
                        P.op("dve", lambda e: e.tensor_scalar(out=kq[:], in0=a2[:], scalar1=1.0 / TWO_PI, scalar2=None, op0=ALU.mult), [a2B], [kqB])
                        P.op("dve", lambda e: e.tensor_copy(out=kqf[:], in_=kq[:]), [kqB], [kqfB])
                        P.op("dve", lambda e: e.scalar_tensor_tensor(out=a2[:], in0=kqf[:], scalar=-C1, in1=a2[:], op0=ALU.mult, op1=ALU.add), [kqfB, a2B], [a2B])
                        P.op("dve", lambda e: e.scalar_tensor_tensor(out=a2[:], in0=kqf[:], scalar=-C2, in1=a2[:], op0=ALU.mult, op1=ALU.add), [kqfB, a2B], [a2B])
                        P.op("dve", lambda e: e.tensor_scalar(out=a2[:], in0=a2[:], scalar1=math.pi, scalar2=-math.pi, op0=ALU.min, op1=ALU.max), [a2B], [a2B])
                        P.op("act", lambda e, dst=dst: e.activation(out=dst[:], in_=a2[:], func=AF.Sin), [a2B], [dB])
                    P.barrier()
                with ExitStack() as s2:
                    (A, AB), (Bt, BB), _ = load_mod_tiles(s2, 1, 0, "norm_mix_g")
                    nx = NormCtx(s2, "a")
                    win = sb(s2, "win", [128, 8, 448], BF16); winB = Buf()
                    wrot = sb(s2, "wrot", [128, 8, 64], BF16); wrotB = Buf()
                    P.dma("pool", win[:], T["mla_w_in"][0].rearrange("(kc p) n -> p kc n", p=128), writes=[winB])
                    P.op("dve", lambda e: e.tensor_scalar(out=wrot[:, :, 0:32], in0=win[:, :, 416:448], scalar1=-1.0, scalar2=None, op0=ALU.mult), [winB], [wrotB])
                    P.op("dve", lambda e: e.tensor_copy(out=wrot[:, :, 32:64], in_=win[:, :, 384:416]), [winB], [wrotB])
                    qg = sb(s2, "qg", [128, 256], F32); qgB = Buf()
                    kg = sb(s2, "kg", [128, 128], F32); kgB = Buf()
                    bcast_load("sp", qg[:], qgB, T["mla_q_norm_g"][0, :])
                    bcast_load("sp", kg[:], kgB, T["mla_kv_norm_g"][0, :])
                    xb = [sb(s2, f"ax{i}", [128, D], F32) for i in range(2)]; xbB = [Buf(), Buf()]
                    tmp = None; tmpB = None
                    hb = [sb(s2, f"ahb{i}", [128, D], BF16) for i in range(2)]; hbB = [Buf(), Buf()]
                    hT = [sb(s2, f"ahT{i}", [128, 8, 128], BF16) for i in range(2)]; hTB = [Buf(), Buf()]
                    jq = sb(s2, "jq", [128, 256], F32); jqB = Buf()
                    sq = sb(s2, "sq", [128, 8], F32); sqB = Buf()
                    cn = [sb(s2, f"cn{i}", [128, 384], BF16) for i in range(2)]; cnB = [Buf(), Buf()]
                    r1 = sb(s2, "r1", [64, 128], F32); r1B = Buf()
                    r2 = sb(s2, "r2", [64, 128], F32); r2B = Buf()
                    for tt in range(NT):
                        tsl = slice(tt * 128, (tt + 1) * 128)
                        xt, xtB = xb[tt % 2], xbB[tt % 2]
                        P.dma("sp", xt[:], XIN[tsl, :], reads=[XINB], writes=[xtB])
                        h_, hB_ = hb[tt % 2], hbB[tt % 2]
                        norm_mod(nx, xt[:], xtB, A, AB, Bt, BB, h_[:], hB_)
                        pt, pB = bank()
                        ptb = pt[:].bitcast(BF16)
                        for kc in range(8):
                            P.op("pe", lambda e, kc=kc, ptb=ptb, h_=h_: e.transpose(out=ptb[:, kc * 128:(kc + 1) * 128], in_=h_[:, kc * 128:(kc + 1) * 128], identity=identb[:]),
                                 [hB_, identbB], [pB])
                        ht, htB = hT[tt % 2], hTB[tt % 2]
                        P.op("act", lambda e, ptb=ptb, ht=ht: e.activation(out=ht[:], in_=ptb.rearrange("p (k t) -> p k t", k=8), func=AF.Identity), [pB], [htB])
                        pa, paB = bank()
                        for kc in range(8):
                            P.op("pe", lambda e, pa=pa, kc=kc, ht=ht: e.matmul(pa[:, 0:384], lhsT=ht[:, kc, :], rhs=win[:, kc, 0:384], start=(kc == 0), stop=(kc == 7)),
                                 [htB, winB], [paB])
                        pk1, pk1B = bank()
                        for kc in range(8):
                            P.op("pe", lambda e, pk1=pk1, kc=kc, ht=ht: e.matmul(pk1[0:64, 0:128], lhsT=win[:, kc, 384:448], rhs=ht[:, kc, :], start=(kc == 0), stop=(kc == 7)),
                                 [htB, winB], [pk1B])
                        pk2, pk2B = bank()
                        for kc in range(8):
                            P.op("pe", lambda e, pk2=pk2, kc=kc, ht=ht: e.matmul(pk2[0:64, 0:128], lhsT=wrot[:, kc, :], rhs=ht[:, kc, :], start=(kc == 0), stop=(kc == 7)),
                                 [htB, wrotB], [pk2B])
                        P.op("dve", lambda e, pk1=pk1, tsl=tsl: e.tensor_tensor(out=r1[:], in0=pk1[0:64, 0:128], in1=cosT[:, tsl], op=ALU.mult), [pk1B, cosTB], [r1B])
                        P.op("dve", lambda e, pk2=pk2, tsl=tsl: e.tensor_tensor(out=r2[:], in0=pk2[0:64, 0:128], in1=sinT[:, tsl], op=ALU.mult), [pk2B, sinTB], [r2B])
                        P.op("pool", lambda e, tsl=tsl: e.tensor_tensor(out=kpT[:, tsl], in0=r1[:], in1=r2[:], op=ALU.add), [r1B, r2B], [kpTB])
                        c_, cB_ = cn[tt % 2], cnB[tt % 2]
                        for (lo, hi, gt_, gB_, n_) in ((0, 256, qg, qgB, 256.0), (256, 384, kg, kgB, 128.0)):
                            o4 = 0 if lo == 0 else 4
                            P.op("act", lambda e, pa=pa, lo=lo, hi=hi, o4=o4: e.activation(out=jq[:, 0:hi - lo], in_=pa[:, lo:hi], func=AF.Square, accum_out=sq[:, o4:o4 + 1]),
                                 [paB], [jqB, sqB])
                            P.op("dve", lambda e, o4=o4, n_=n_: e.tensor_scalar(out=sq[:, o4 + 1:o4 + 2], in0=sq[:, o4:o4 + 1], scalar1=1.0 / n_, scalar2=EPS, op0=ALU.mult, op1=ALU.add), [sqB], [sqB])
                            P.op("act", lambda e, o4=o4: e.activation(out=sq[:, o4 + 2:o4 + 3], in_=sq[:, o4 + 1:o4 + 2], func=AF.Sqrt), [sqB], [sqB])
                            P.op("dve", lambda e, o4=o4: e.reciprocal(out=sq[:, o4 + 3:o4 + 4], in_=sq[:, o4 + 2:o4 + 3]), [sqB], [sqB])
                            P.op("dve", lambda e, pa=pa, lo=lo, hi=hi, o4=o4, gt_=gt_, c_=c_: e.scalar_tensor_tensor(out=c_[:, lo:hi], in0=pa[:, lo:hi], scalar=sq[:, o4 + 3:o4 + 4], in1=gt_[:],
                                                                                                              op0=ALU.mult, op1=ALU.mult), [paB, sqB, gB_], [cB_])
                        p3, p3B = bank()
                        p3b = p3[:].bitcast(BF16)
                        for j in range(3):
                            P.op("pe", lambda e, p3b=p3b, j=j, c_=c_: e.transpose(out=p3b[:, j * 128:(j + 1) * 128], in_=c_[:, j * 128:(j + 1) * 128], identity=identb[:]),
                                 [cB_, identbB], [p3B])
                        P.op("act", lambda e, p3b=p3b, tsl=tsl: e.activation(out=cqnT[:, :, tsl], in_=p3b[:, 0:256].rearrange("p (k t) -> p k t", k=2), func=AF.Identity), [p3B], [cqnTB])
                        P.op("act", lambda e, p3b=p3b, tsl=tsl: e.activation(out=ckvT[:, tsl], in_=p3b[:, 256:384], func=AF.Identity), [p3B], [ckvTB])
                    P.dma("sp", T["KP"][:, :], kpT[:], reads=[kpTB], writes=[KPB])
                    P.barrier()
                with ExitStack() as s2:
                    wqn = sb(s2, "wqn", [128, 2, 8, 128], BF16); wqnB = Buf()
                    wqp = sb(s2, "wqp", [128, 2, 8, 64], BF16); wqpB = Buf()
                    wqr = sb(s2, "wqr", [128, 2, 8, 64], BF16); wqrB = Buf()
                    wkk = sb(s2, "wkk", [128, 8, 128], BF16); wkkB = Buf()
                    wkv = sb(s2, "wkv", [128, 8, 128], BF16); wkvB = Buf()
                    qv = T["mla_w_qb"][0].rearrange("(k2 p) (h c) -> p k2 h c", p=128, c=192)
                    for k2 in range(2):
                        P.dma("pool", wqn[:, k2], qv[:, k2, :, 0:128], writes=[wqnB])
                        P.dma("pool", wqp[:, k2], qv[:, k2, :, 128:192], writes=[wqpB])
                    kvv = T["mla_w_kvb"][0].rearrange("k (h c) -> k h c", c=256)
                    P.dma("pool", wkk[:], kvv[:, :, 0:128], writes=[wkkB])
                    P.dma("pool", wkv[:], kvv[:, :, 128:256], writes=[wkvB])
                    P.op("dve", lambda e: e.tensor_scalar(out=wqr[:, :, :, 0:32], in0=wqp[:, :, :, 32:64], scalar1=-1.0, scalar2=None, op0=ALU.mult), [wqpB], [wqrB])
                    P.op("dve", lambda e: e.tensor_copy(out=wqr[:, :, :, 32:64], in_=wqp[:, :, :, 0:32]), [wqpB], [wqrB])
                    qn = [sb(s2, f"qn{i}", [128, S], BF16) for i in range(2)]; qnB = [Buf(), Buf()]
                    kn = [sb(s2, f"kn{i}", [128, S], BF16) for i in range(2)]; knB = [Buf(), Buf()]
                    qp = [sb(s2, f"qp{i}", [64, S], BF16) for i in range(2)]; qpB = [Buf(), Buf()]
                    r1 = sb(s2, "q1", [64, 512], F32); r1B = Buf()
                    r2 = sb(s2, "q2", [64, 512], F32); r2B = Buf()
                    vt = [sb(s2, f"vt{i}", [128, D], BF16) for i in range(2)]; vtB = [Buf(), Buf()]
                    for tt in range(NT):
                        tsl = slice(tt * 128, (tt + 1) * 128)
                        v_, vB_ = vt[tt % 2], vtB[tt % 2]
                        for nh in range(2):
                            pt, pB = bank()
                            P.op("pe", lambda e, pt=pt, tsl=tsl, nh=nh: e.matmul(pt[:], lhsT=ckvT[:, tsl], rhs=wkv[:, nh * 4:(nh + 1) * 4, :], start=True, stop=True),
                                 [ckvTB, wkvB], [pB])
                            P.op("act", lambda e, pt=pt, nh=nh, v_=v_: e.activation(out=v_[:, nh * 512:(nh + 1) * 512], in_=pt[:], func=AF.Identity), [pB], [vB_])
                        P.dma("sp", T["V"][tsl, :], v_[:], reads=[vB_], writes=[VB])
                    for h in range(8):
                        q_, qB_ = qn[h % 2], qnB[h % 2]
                        k_, kB_ = kn[h % 2], knB[h % 2]
                        p_, pB_ = qp[h % 2], qpB[h % 2]
                        for tch in range(8):
                            csl = slice(tch * 512, (tch + 1) * 512)
                            pt, pB = bank()
                            for k2 in range(2):
                                P.op("pe", lambda e, pt=pt, k2=k2, h=h, csl=csl: e.matmul(pt[:], lhsT=wqn[:, k2, h, :], rhs=cqnT[:, k2, csl], start=(k2 == 0), stop=(k2 == 1)),
                                     [wqnB, cqnTB], [pB])
                            P.op("act", lambda e, pt=pt, q_=q_, csl=csl: e.activation(out=q_[:, csl], in_=pt[:], func=AF.Identity), [pB], [qB_])
                            pt2, pB2 = bank()
                            P.op("pe", lambda e, pt2=pt2, h=h, csl=csl: e.matmul(pt2[:], lhsT=wkk[:, h, :], rhs=ckvT[:, csl], start=True, stop=True), [wkkB, ckvTB], [pB2])
                            P.op("act", lambda e, pt2=pt2, k_=k_, csl=csl: e.activation(out=k_[:, csl], in_=pt2[:], func=AF.Identity), [pB2], [kB_])
                            pa, paB = bank()
                            for k2 in range(2):
                                P.op("pe", lambda e, pa=pa, k2=k2, h=h, csl=csl: e.matmul(pa[0:64, :], lhsT=wqp[:, k2, h, :], rhs=cqnT[:, k2, csl], start=(k2 == 0), stop=(k2 == 1)),
                                     [wqpB, cqnTB], [paB])
                            pb_, pbB = bank()
                            for k2 in range(2):
                                P.op("pe", lambda e, pb_=pb_, k2=k2, h=h, csl=csl: e.matmul(pb_[0:64, :], lhsT=wqr[:, k2, h, :], rhs=cqnT[:, k2, csl], start=(k2 == 0), stop=(k2 == 1)),
                                     [wqrB, cqnTB], [pbB])
                            P.op("dve", lambda e, pa=pa, csl=csl: e.tensor_tensor(out=r1[:], in0=pa[0:64, :], in1=cosT[:, csl], op=ALU.mult), [paB, cosTB], [r1B])
                            P.op("dve", lambda e, pb_=pb_, csl=csl: e.tensor_tensor(out=r2[:], in0=pb_[0:64, :], in1=sinT[:, csl], op=ALU.mult), [pbB, sinTB], [r2B])
                            P.op("pool", lambda e, p_=p_, csl=csl: e.tensor_tensor(out=p_[:, csl], in0=r1[:], in1=r2[:], op=ALU.add), [r1B, r2B], [pB_])
                        P.dma("sp", T["QN"][h], q_[:], reads=[qB_], writes=[QNB])
                        P.dma("sp", T["KN"][h], k_[:], reads=[kB_], writes=[KNB])
                        P.dma("sp", T["QP"][h], p_[:], reads=[pB_], writes=[QPB])
                    P.barrier()

        def phase_mla_attn(XIN, XINB, XOUT, XOUTB):
            with ExitStack() as st:
                OT = sb(st, "OT", [128, 8, S], BF16); OTB = Buf()
                with ExitStack() as s2:
                    kp = sb(s2, "kp", [64, S], BF16); kpB = Buf()
                    P.dma("sp", kp[:], T["KP"][:, :], reads=[KPB], writes=[kpB])
                    ones = sb(s2, "ones", [128, 128], BF16); onesB = Buf()
                    P.op("dve", lambda e: e.memset(ones[:], 1.0), [], [onesB])
                    qn = [sb(s2, f"aq{i}", [128, S], BF16) for i in range(2)]; qnB = [Buf(), Buf()]
                    kn = [sb(s2, f"ak{i}", [128, S], BF16) for i in range(2)]; knB = [Buf(), Buf()]
                    qp = [sb(s2, f"ap{i}", [64, S], BF16) for i in range(2)]; qpB = [Buf(), Buf()]
                    vh = [sb(s2, f"av{i}", [128, NT, 128], BF16) for i in range(2)]; vhB = [Buf(), Buf()]
                    pT = [sb(s2, f"pT{i}", [128, 512], BF16) for i in range(3)]; pTB = [Buf(), Buf(), Buf()]
                    rs = sb(s2, "rs", [128, 512], F32); rsB = Buf()
                    npt = 0
                    nst = 0
                    vv = T["V"].rearrange("(tt p) c -> p tt c", p=128)

                    def load_head(h):
                        P.dma("sp", qn[h % 2][:], T["QN"][h], reads=[QNB], writes=[qnB[h % 2]])
                        P.dma("sp", kn[h % 2][:], T["KN"][h], reads=[KNB], writes=[knB[h % 2]])
                        P.dma("sp", qp[h % 2][:], T["QP"][h], reads=[QPB], writes=[qpB[h % 2]])
                        P.dma("sp", vh[h % 2][:], vv[:, :, h * 128:(h + 1) * 128], reads=[VB], writes=[vhB[h % 2]])

                    load_head(0)
                    for h in range(8):
                        if h + 1 < 8:
                            load_head(h + 1)
                        q_, qB_ = qn[h % 2], qnB[h % 2]
                        k_, kB_ = kn[h % 2], knB[h % 2]
                        p_, pB_ = qp[h % 2], qpB[h % 2]
                        v_, vB_ = vh[h % 2], vhB[h % 2]
                        for qc in range(8):
                            csl = slice(qc * 512, (qc + 1) * 512)
                            po, poB = bank_fixed(4 + qc % 2)
                            pz, pzB = bank_fixed(6 + qc % 2)
                            def qk(kt, k_=k_, kB_=kB_, q_=q_, qB_=qB_, p_=p_, pB_=pB_, csl=csl):
                                ksl = slice(kt * 128, (kt + 1) * 128)
                                pst, pstB = bank_fixed(kt % 4)
                                P.op("pe", lambda e: e.matmul(pst[:], lhsT=k_[:, ksl], rhs=q_[:, csl], start=True, stop=False),
                                     [kB_, qB_], [pstB])
                                P.op("pe", lambda e: e.matmul(pst[:], lhsT=kp[:, ksl], rhs=p_[:, csl], start=False, stop=True),
                                     [kpB, pB_], [pstB])
                                return pst, pstB

                            pend = {0: qk(0), 1: qk(1), 2: qk(2)}
                            for kt in range(NT):
                                pst, pstB = pend.pop(kt)
                                t_, tB_ = pT[npt % 3], pTB[npt % 3]
                                npt += 1
                                P.op("act", lambda e, pst=pst, t_=t_: e.activation(out=t_[:], in_=pst[:], func=AF.Exp, scale=SCALE), [pstB], [tB_])
                                if kt + 3 < NT:
                                    pend[kt + 3] = qk(kt + 3)
                                P.op("pe", lambda e, po=po, kt=kt, t_=t_, v_=v_: e.matmul(po[:], lhsT=v_[:, kt, :], rhs=t_[:], start=(kt == 0), stop=(kt == NT - 1)),
                                     [vB_, tB_], [poB])
                                P.op("pe", lambda e, pz=pz, kt=kt, t_=t_: e.matmul(pz[:], lhsT=ones[:], rhs=t_[:], start=(kt == 0), stop=(kt == NT - 1)),
                                     [onesB, tB_], [pzB])
                            P.op("dve", lambda e, pz=pz: e.reciprocal(out=rs[:], in_=pz[:]), [pzB], [rsB])
                            P.op("dve", lambda e, po=po, h=h, csl=csl: e.tensor_tensor(out=OT[:, h, csl], in0=po[:], in1=rs[:], op=ALU.mult), [poB, rsB], [OTB])
                    P.barrier()
                with ExitStack() as s2:
                    wo = sb(s2, "mwo", [128, 8, D], BF16); woB = Buf()
                    wv = T["mla_w_out"][0].rearrange("(kc p) n -> p kc n", p=128)
                    for q in range(2):
                        P.dma("pool", wo[:, q * 4:(q + 1) * 4, :], wv[:, q * 4:(q + 1) * 4, :], writes=[woB])
                    _, _, (G, GB) = load_mod_tiles(s2, 1, 0, "norm_mix_g")
                    xb = [sb(s2, f"bx{i}", [128, D], F32) for i in range(2)]; xbB = [Buf(), Buf()]
                    yo = [sb(s2, f"by{i}", [128, D], F32) for i in range(2)]; yoB = [Buf(), Buf()]
                    P.dma("sp", xb[0][:], XIN[0:128, :], reads=[XINB], writes=[xbB[0]])
                    for tt in range(NT):
                        tsl = slice(tt * 128, (tt + 1) * 128)
                        xt, xtB = xb[tt % 2], xbB[tt % 2]
                        y, yB = yo[tt % 2], yoB[tt % 2]
                        if tt + 1 < NT:
                            P.dma("sp", xb[(tt + 1) % 2][:], XIN[(tt + 1) * 128:(tt + 2) * 128, :], reads=[XINB], writes=[xbB[(tt + 1) % 2]])
                        for nh in range(2):
                            pt, pB = bank()
                            for hh in range(8):
                                P.op("pe", lambda e, pt=pt, hh=hh, tsl=tsl, nh=nh: e.matmul(pt[:], lhsT=OT[:, hh, tsl], rhs=wo[:, hh, nh * 512:(nh + 1) * 512], start=(hh == 0), stop=(hh == 7)),
                                     [OTB, woB], [pB])
                            sl = slice(nh * 512, (nh + 1) * 512)
                            P.op("dve", lambda e, pt=pt, sl=sl, y=y: e.tensor_tensor(out=y[:, sl], in0=pt[:], in1=G[:, sl], op=ALU.mult), [pB, GB], [yB])
                        P.op("pool", lambda e, y=y, xt=xt: e.tensor_tensor(out=y[:], in0=y[:], in1=xt[:], op=ALU.add), [yB, xtB], [yB])
                        P.dma("sp", XOUT[tsl, :], y[:], reads=[yB], writes=[XOUTB])
                    P.barrier()

        def copy_to_out(src, srcB):
            with ExitStack() as st:
                cb = [sb(st, f"cpy{i}", [128, D], F32) for i in range(2)]; cbB = [Buf(), Buf()]
                for tt in range(NT):
                    t_, tB = cb[tt % 2], cbB[tt % 2]
                    P.dma("sp", t_[:], src[tt * 128:(tt + 1) * 128, :], reads=[srcB], writes=[tB])
                    P.dma("sp", OUT[tt * 128:(tt + 1) * 128, :], t_[:], reads=[tB], writes=[OUTB])
                P.barrier()

        OUTB = Buf()

        phase_mod()
        phase_filter()
        phase_hyena_in()
        phase_hyena_fwd()
        phase_hyena_out()
        if stage == "hyena":
            copy_to_out(T["XA"], XAB)
        else:
            XBB = Buf()
            phase_moe(0, T["XA"], XAB, T["XB"], XBB, False)
            if stage == "moe0":
                copy_to_out(T["XB"], XBB)
            else:
                XCB = Buf()
                phase_mla_proj(T["XB"], XBB)
                phase_mla_attn(T["XB"], XBB, T["XC"], XCB)
                if stage == "mla":
                    copy_to_out(T["XC"], XCB)
                else:
                    phase_moe(1, T["XC"], XCB, OUT, OUTB, True)
        P.barrier()
    return nc


def _prep_inputs(inputs):
    global _CONSTS
    if _CONSTS is None:
        _CONSTS = _consts()
    shared = {}
    for k, v in inputs.items():
        if k in ("x", "c", "positions"):
            continue
        shared[k] = np.ascontiguousarray(np.asarray(v))
    shared.update(_CONSTS)
    in_maps = []
    x = np.asarray(inputs["x"]); c = np.asarray(inputs["c"]); pos = np.asarray(inputs["positions"])
    for b in range(x.shape[0]):
        m = dict(shared)
        m["x"] = np.ascontiguousarray(x[b])
        m["c"] = np.ascontiguousarray(c[b:b + 1])
        m["pos"] = np.ascontiguousarray(pos[b].astype(np.int32))
        in_maps.append(m)
    return in_maps


def kernel(**inputs):
    in_maps = _prep_inputs(inputs)
    nc = build("all")
    res = run_bass_kernel_spmd(nc, in_maps, core_ids=list(range(len(in_maps))))
    return np.stack([r["out"] for r in res.results], axis=0).astype(np.float32)
```

```python
import math
from contextlib import ExitStack

import ml_dtypes
import numpy as np

import concourse.bass as bass
import concourse.mybir as mybir
from concourse.bass_utils import run_bass_kernel_spmd

F32 = mybir.dt.float32
BF16 = mybir.dt.bfloat16
I32 = mybir.dt.int32
U32 = mybir.dt.uint32
AF = mybir.ActivationFunctionType
ALU = mybir.AluOpType
AX = mybir.AxisListType

S = 4096
D = 1024
NT = S // 128
EPS = 1e-6
NEXP = 16
CAP = 512
TWO_PI = 2.0 * math.pi


class Buf:
    __slots__ = ("w", "r")

    def __init__(self):
        self.w = None
        self.r = []


class Prog:
    NQ = 6

    def __init__(self, nc, es):
        self.nc = nc
        self.es = es
        self.eng = {"pe": nc.tensor, "act": nc.scalar, "dve": nc.vector,
                    "pool": nc.gpsimd, "sp": nc.sync}
        self.sem = {}
        self.cnt = {}
        self.nsem = 0
        for e in ("pe", "act", "dve", "pool"):
            self.sem[e] = self._newsem()
            self.cnt[e] = 0
        self.waited = {e: {} for e in self.eng}
        self.dq = {}
        self.dqi = {}
        for q in ("sp", "act", "pool"):
            self.dq[q] = [[self._newsem(), 0] for _ in range(16 if q == "pool" else self.NQ)]
            self.dqi[q] = 0

    def _newsem(self):
        self.nsem += 1
        return self.es.enter_context(self.nc.semaphore(f"sm{self.nsem}"))

    def _wait(self, e, tok):
        sem, val, src = tok
        if src == "pe" and e == "pe":
            return
        key = id(sem)
        if self.waited[e].get(key, 0) >= val:
            return
        self.eng[e].wait_ge(sem, val)
        self.waited[e][key] = val

    def _deps(self, e, reads, writes):
        for b in reads:
            if b.w is not None:
                self._wait(e, b.w)
        for b in writes:
            if b.w is not None:
                self._wait(e, b.w)
            for t in b.r:
                self._wait(e, t)

    def _record(self, tok, reads, writes):
        for b in reads:
            b.r = [t for t in b.r if t[0] is not tok[0]]
            b.r.append(tok)
        for b in writes:
            b.w = tok
            b.r = []

    def op(self, e, fn, reads=(), writes=()):
        self._deps(e, reads, writes)
        ins = fn(self.eng[e])
        self.cnt[e] += 1
        ins.then_inc(self.sem[e], 1)
        tok = (self.sem[e], self.cnt[e], e)
        self._record(tok, reads, writes)
        return tok

    def dma(self, q, out, in_, reads=(), writes=(), fn=None, **kw):
        slot = self.dq[q][self.dqi[q] % len(self.dq[q])]
        self.dqi[q] += 1
        sem, c = slot
        if c > 0:
            self._wait(q, (sem, c, None))
        self._deps(q, reads, writes)
        if fn is None:
            ins = self.eng[q].dma_start(out=out, in_=in_, **kw)
        else:
            ins = fn(self.eng[q])
        ins.then_inc(sem, 16)
        slot[1] = c + 16
        tok = (sem, c + 16, None)
        self._record(tok, reads, writes)
        return tok

    def barrier(self):
        toks = [(self.sem[e], self.cnt[e], None) for e in self.sem if self.cnt[e] > 0]
        for q in self.dq:
            for sem, c in self.dq[q]:
                if c > 0:
                    toks.append((sem, c, None))
        for e in self.eng:
            for t in toks:
                self._wait(e, t)
        for e in self.sem:
            if self.cnt[e] > 6000:
                self.sem[e] = self._newsem()
                self.cnt[e] = 0
        for q in self.dq:
            for slot in self.dq[q]:
                if slot[1] > 6000:
                    slot[0] = self._newsem()
                    slot[1] = 0


def _consts():
    c = {}
    c["ident"] = np.eye(128, dtype=np.float32)
    L = S
    t = np.linspace(0.0, 1.0, L, dtype=np.float32)[:, None]
    w = (2.0 * math.pi * np.arange(L, dtype=np.float32)[:, None] / L).astype(np.float32)
    f = np.linspace(1e-4, 15.0, 16, dtype=np.float32)[None, :]
    ang = (f * w).astype(np.float32)
    z = np.concatenate([t, np.cos(ang), -np.sin(ang)], axis=-1).astype(np.float32)
    c["zfT"] = np.ascontiguousarray(z.T)
    c["tneg"] = np.ascontiguousarray((-t[:, 0]).reshape(NT, 128).T)
    mind = math.log(1e-2) / 0.3
    maxd = math.log(1e-2) / 1.5
    deltas = np.linspace(mind, maxd, D, dtype=np.float32)
    c["absd"] = np.abs(deltas).astype(np.float32)
    fi = np.arange(4096, dtype=np.int64)
    ti = np.arange(4096, dtype=np.int64)
    m = ((2 * fi[None, :] + 1) * ti[:, None]) % 16384
    angm = m.astype(np.float64) * (math.pi / 8192.0)
    cosm = np.cos(angm)
    sinm = np.sin(angm)
    fw = np.stack([cosm, -sinm], axis=0)
    fw = fw.reshape(2, NT, 128, 32, 128)
    c["FWD"] = np.ascontiguousarray(fw.transpose(3, 2, 0, 1, 4)).astype(ml_dtypes.bfloat16)
    sc = 2.0 / 8192.0
    iv = np.stack([cosm * sc, -sinm * sc], axis=0)
    iv = iv.reshape(2, NT, 128, 32, 128)
    c["INV"] = np.ascontiguousarray(iv.transpose(1, 4, 3, 0, 2)).astype(ml_dtypes.bfloat16)
    invf = (10000.0 ** (-np.arange(0, 64, 2, dtype=np.float32) / 64)).astype(np.float32)
    c["invf"] = invf
    return c


_CONSTS = None


def build(stage="all", debug=False):
    nc = bass.Bass("TRN2", target_bir_lowering=False)
    T = {}

    def din(name, shape, dt=F32):
        T[name] = nc.dram_tensor(name, list(shape), dt, kind="ExternalInput").ap()
        return T[name]

    def dscr(name, shape, dt=F32):
        T[name] = nc.dram_tensor(name, list(shape), dt).ap()
        return T[name]

    din("x", [S, D]); din("c", [1, D]); din("pos", [S], I32)
    din("ada_w", [2, D, 6 * D]); din("ada_b", [2, 6 * D])
    din("norm_mix_g", [2, D]); din("norm_ffn_g", [2, D])
    din("hy_w_in", [1, D, 3 * D]); din("hy_b_in", [1, 3 * D]); din("hy_conv_w", [1, 3, 3 * D])
    din("hy_conv_b", [1, 3 * D]); din("hy_f_w1", [1, 33, 64]); din("hy_f_b1", [1, 64])
    din("hy_f_w2", [1, 64, 64]); din("hy_f_b2", [1, 64]); din("hy_f_w3", [1, 64, 2 * D])
    din("hy_f_freq", [1, 64]); din("hy_f_bias", [1, 1, D]); din("hy_w_out", [1, D, D]); din("hy_b_out", [1, D])
    din("mla_w_in", [1, D, 448]); din("mla_q_norm_g", [1, 256]); din("mla_w_qb", [1, 256, 1536])
    din("mla_kv_norm_g", [1, 128]); din("mla_w_kvb", [1, 128, 2048]); din("mla_w_out", [1, D, D])
    din("moe_w_router", [2, D, NEXP]); din("moe_w_gate", [2, NEXP, D, D]); din("moe_w_up", [2, NEXP, D, D])
    din("moe_w_down", [2, NEXP, D, D]); din("final_norm_g", [D])
    din("ident", [128, 128]); din("zfT", [33, S]); din("tneg", [128, NT]); din("absd", [D])
    din("FWD", [32, 128, 2, 32, 128], BF16); din("INV", [32, 128, 32, 2, 128], BF16); din("invf", [32])
    OUT = nc.dram_tensor("out", [S, D], F32, kind="ExternalOutput").ap()

    dscr("MOD", [2, 6 * D])
    dscr("ZT", [S, D], BF16)
    dscr("X0C", [D, S], BF16)
    dscr("KS", [2, S, D])
    dscr("KT", [2, S, D], BF16)
    dscr("YS", [32, 128, 2, D], BF16)
    dscr("XA", [S, D]); dscr("XB", [S, D]); dscr("XC", [S, D])
    dscr("HF", [S, D], BF16); dscr("ACC", [S, D])
    dscr("QN", [8, 128, S], BF16); dscr("KN", [8, 128, S], BF16); dscr("QP", [8, 64, S], BF16)
    dscr("KP", [64, S], BF16); dscr("V", [S, D], BF16)

    es = ExitStack()
    with es:
        P = Prog(nc, es)
        ps = [es.enter_context(nc.psum_tensor(f"ps{i}", [128, 512], F32)) for i in range(8)]
        psB = [Buf() for _ in range(8)]
        psi = [0]

        def bank():
            i = psi[0] % 8
            psi[0] += 1
            return ps[i], psB[i]

        sbn = [0]

        def sb(st, name, shape, dt):
            sbn[0] += 1
            return st.enter_context(nc.sbuf_tensor(f"{name}_{sbn[0]}", list(shape), dt))

        identf = sb(es, "identf", [128, 128], F32); identfB = Buf()
        identb = sb(es, "identb", [128, 128], BF16); identbB = Buf()
        P.dma("sp", identf[:], T["ident"][:, :], writes=[identfB])
        P.dma("pool", identb[:], T["ident"][:, :], writes=[identbB])

        def bcast_load(q, dst, dstB, src1d):
            return P.dma(q, dst, src1d.partition_broadcast(128), writes=[dstB])

        def phase_mod():
            with ExitStack() as st:
                ccol = sb(st, "ccol", [128, 8], F32); ccolB = Buf()
                cs = sb(st, "cs", [128, 8], F32); csB_ = Buf()
                csb = sb(st, "csb", [128, 8, 128], F32); csbB = Buf()
                adb = sb(st, "adb", [128, 6 * D], F32); adbB = Buf()
                modt = sb(st, "modt", [128, 6 * D], F32); modtB = Buf()
                wb = [sb(st, f"adw{i}", [128, 8, 512], F32) for i in range(2)]
                wbB = [Buf(), Buf()]
                P.dma("sp", ccol[:], T["c"].rearrange("o (kc p) -> p (o kc)", p=128), writes=[ccolB],
                      allow_slow_non_contiguous=True)
                P.op("act", lambda e: e.activation(out=cs[:], in_=ccol[:], func=AF.Silu), [ccolB], [csB_])
                for kc in range(8):
                    P.op("dve", lambda e, kc=kc: e.tensor_copy(out=csb[:, kc, :], in_=cs[:, kc:kc + 1].to_broadcast([128, 128])),
                         [csB_], [csbB])
                n = 0
                for i in range(2):
                    bcast_load("sp", adb[:], adbB, T["ada_b"][i, :])
                    wv = T["ada_w"][i].rearrange("(kc p) n -> p kc n", p=128)
                    for q in range(12):
                        w, wB = wb[n % 2], wbB[n % 2]
                        n += 1
                        P.dma("sp", w[:], wv[:, :, q * 512:(q + 1) * 512], writes=[wB])
                        pt, pB = bank()
                        for kc in range(8):
                            P.op("pe", lambda e, kc=kc, w=w, pt=pt: e.matmul(pt[:], lhsT=csb[:, kc, :], rhs=w[:, kc, :],
                                                                              start=(kc == 0), stop=(kc == 7)),
                                 [csbB, wB], [pB])
                        P.op("dve", lambda e, q=q, pt=pt: e.tensor_tensor(out=modt[:, q * 512:(q + 1) * 512], in0=pt[:],
                                                                         in1=adb[:, q * 512:(q + 1) * 512], op=ALU.add),
                             [pB, adbB], [modtB])
                    P.dma("sp", T["MOD"][i:i + 1, :], modt[0:1, :], reads=[modtB], writes=[MODB])
                P.barrier()

        MODB = Buf()

        def load_mod_tiles(st, layer, which, gname):
            base = 0 if which == 0 else 3
            A = sb(st, f"A{layer}{which}", [128, D], F32); AB = Buf()
            Bt = sb(st, f"B{layer}{which}", [128, D], F32); BB = Buf()
            G = sb(st, f"G{layer}{which}", [128, D], F32); GB = Buf()
            gt = sb(st, f"g{layer}{which}", [128, D], F32); gB = Buf()
            P.dma("sp", Bt[:], T["MOD"][layer, (base + 0) * D:(base + 1) * D].partition_broadcast(128), reads=[MODB], writes=[BB])
            P.dma("sp", A[:], T["MOD"][layer, (base + 1) * D:(base + 2) * D].partition_broadcast(128), reads=[MODB], writes=[AB])
            P.dma("sp", G[:], T["MOD"][layer, (base + 2) * D:(base + 3) * D].partition_broadcast(128), reads=[MODB], writes=[GB])
            bcast_load("sp", gt[:], gB, T[gname][layer, :])
            P.op("dve", lambda e: e.scalar_tensor_tensor(out=A[:], in0=A[:], scalar=1.0, in1=gt[:], op0=ALU.add, op1=ALU.mult),
                 [gB, AB], [AB])
            return (A, AB), (Bt, BB), (G, GB)

        class NormCtx:
            def __init__(self, st, tag):
                self.junk = sb(st, f"junk{tag}", [128, D], F32); self.junkB = Buf()
                self.ss = [sb(st, f"ss{tag}{i}", [128, 4], F32) for i in range(2)]; self.ssB = [Buf(), Buf()]
                self.tmp = [sb(st, f"nt{tag}{i}", [128, D], F32) for i in range(2)]; self.tmpB = [Buf(), Buf()]
                self.n = 0

        def norm_mod_g(nx, xt, xtB, A, AB, Bt, BB, out, outB):
            k = nx.n % 2
            nx.n += 1
            ss, ssB = nx.ss[k], nx.ssB[k]
            tmp, tmpB = nx.tmp[k][:], nx.tmpB[k]
            P.op("act", lambda e: e.activation(out=nx.junk[:], in_=xt, func=AF.Square, accum_out=ss[:, 0:1]),
                 [xtB], [nx.junkB, ssB])
            yield
            P.op("dve", lambda e: e.tensor_scalar(out=ss[:, 1:2], in0=ss[:, 0:1], scalar1=1.0 / D, scalar2=EPS,
                                                  op0=ALU.mult, op1=ALU.add), [ssB], [ssB])
            yield
            P.op("act", lambda e: e.activation(out=ss[:, 2:3], in_=ss[:, 1:2], func=AF.Sqrt), [ssB], [ssB])
            yield
            P.op("dve", lambda e: e.reciprocal(out=ss[:, 3:4], in_=ss[:, 2:3]), [ssB], [ssB])
            yield
            if Bt is None:
                P.op("dve", lambda e: e.scalar_tensor_tensor(out=out, in0=xt, scalar=ss[:, 3:4], in1=A[:],
                                                             op0=ALU.mult, op1=ALU.mult), [xtB, ssB, AB], [outB])
                return
            P.op("dve", lambda e: e.scalar_tensor_tensor(out=tmp, in0=xt, scalar=ss[:, 3:4], in1=A[:],
                                                         op0=ALU.mult, op1=ALU.mult), [xtB, ssB, AB], [tmpB])
            yield
            P.op("pool", lambda e: e.tensor_tensor(out=out, in0=tmp, in1=Bt[:], op=ALU.add), [tmpB, BB], [outB])

        def norm_mod(nx, xt, xtB, A, AB, Bt, BB, out, outB, tmp_unused=None, tmpB_unused=None):
            for _ in norm_mod_g(nx, xt, xtB, A, AB, Bt, BB, out, outB):
                pass

        def run_pipelined(n, make, depth=2, on_start=None):
            active = []
            nxt = 0
            while nxt < n or active:
                while len(active) < depth and nxt < n:
                    if on_start is not None:
                        on_start(nxt)
                    active.append(make(nxt))
                    nxt += 1
                for g in list(active):
                    try:
                        next(g)
                    except StopIteration:
                        active.remove(g)

        def fwd_dft(st, A, AB_, Bm, BB_, consume):
            fwb = [sb(st, f"fw{i}", [128, 2, 32, 128], BF16) for i in range(2)]
            fwB = [Buf(), Buf()]
            def load_fw(fc):
                fw, fB = fwb[fc % 2], fwB[fc % 2]
                P.dma("sp", fw[:, 0], T["FWD"][fc, :, 0], writes=[fB])
                P.dma("sp", fw[:, 1], T["FWD"][fc, :, 1], writes=[fB])

            load_fw(0)
            for fc in range(32):
                fw, fB = fwb[fc % 2], fwB[fc % 2]
                if fc + 1 < 32:
                    load_fw(fc + 1)
                res = []
                for h in range(2):
                    for cs_ in range(2):
                        src, sB = (A, AB_) if cs_ == 0 else (Bm, BB_)
                        pt, pB = bank()
                        for tt in range(32):
                            P.op("pe", lambda e, pt=pt, fw=fw, cs_=cs_, tt=tt, src=src, h=h:
                                 e.matmul(pt[:], lhsT=fw[:, cs_, tt, :], rhs=src[:, tt, h * 512:(h + 1) * 512],
                                          start=(tt == 0), stop=(tt == 31)), [fB, sB], [pB])
                        res.append((pt, pB))
                consume(fc, res)

        def phase_filter():
            with ExitStack() as st:
                KTB = Buf()
                with ExitStack() as s2:
                    kab = [sb(s2, f"kab{i}", [128, 2, D], BF16) for i in range(2)]; kabB = [Buf(), Buf()]
                    zf = sb(s2, "zf", [33, S], F32); zfB = Buf()
                    h1 = sb(s2, "h1", [64, S], F32); h1B = Buf()
                    h2 = sb(s2, "h2", [64, S], F32); h2B = Buf()
                    w1 = sb(s2, "fw1", [33, 64], F32); w1B = Buf()
                    w2 = sb(s2, "fw2", [64, 64], F32); w2B = Buf()
                    w3 = sb(s2, "fw3", [64, 2 * D], F32); w3B = Buf()
                    col = sb(s2, "fcol", [64, 8], F32); colB = Buf()
                    pre = sb(s2, "fpre", [64, 512], F32); preB = Buf()
                    ki = sb(s2, "fki", [64, 512], I32); kiB = Buf()
                    kf = sb(s2, "fkf", [64, 512], F32); kfB = Buf()
                    absd = sb(s2, "absd", [128, D], F32); absdB = Buf()
                    tneg = sb(s2, "tneg", [128, NT], F32); tnegB = Buf()
                    dec = sb(s2, "dec", [128, D], F32); decB = Buf()
                    kfw = sb(s2, "kfw", [128, D], F32); kfwB = Buf()
                    kbw = sb(s2, "kbw", [128, D], F32); kbwB = Buf()
                    fbias = sb(s2, "fbias", [1, D], F32); fbiasB = Buf()
                    P.dma("sp", zf[:], T["zfT"][:, :], writes=[zfB])
                    P.dma("sp", w1[:], T["hy_f_w1"][0], writes=[w1B])
                    P.dma("sp", w2[:], T["hy_f_w2"][0], writes=[w2B])
                    P.dma("sp", w3[:], T["hy_f_w3"][0], writes=[w3B])
                    P.dma("sp", col[:, 0:1], T["hy_f_b1"][0].rearrange("(p o) -> p o", o=1), writes=[colB])
                    P.dma("sp", col[:, 1:2], T["hy_f_b2"][0].rearrange("(p o) -> p o", o=1), writes=[colB])
                    P.dma("sp", col[:, 2:3], T["hy_f_freq"][0].rearrange("(p o) -> p o", o=1), writes=[colB])
                    P.dma("sp", tneg[:], T["tneg"][:, :], writes=[tnegB])
                    P.dma("sp", fbias[:], T["hy_f_bias"][0], writes=[fbiasB])
                    bcast_load("sp", absd[:], absdB, T["absd"])
                    P.op("dve", lambda e: e.tensor_tensor(out=col[:, 3:4], in0=col[:, 0:1], in1=col[:, 2:3], op=ALU.mult), [colB], [colB])
                    P.op("dve", lambda e: e.tensor_tensor(out=col[:, 4:5], in0=col[:, 1:2], in1=col[:, 2:3], op=ALU.mult), [colB], [colB])

                    def sin_layer(w, wB, src, srcB, K, bcol, dst, dstB):
                        for tch in range(8):
                            pt, pB = bank()
                            P.op("pe", lambda e, pt=pt, tch=tch: e.matmul(pt[0:64, :], lhsT=w[0:K, :], rhs=src[0:K, tch * 512:(tch + 1) * 512],
                                                                          start=True, stop=True), [wB, srcB], [pB])
                            P.op("dve", lambda e, pt=pt: e.tensor_scalar(out=pre[:], in0=pt[0:64, :], scalar1=col[:, 2:3], scalar2=col[:, bcol:bcol + 1],
                                                                         op0=ALU.mult, op1=ALU.add), [pB, colB], [preB])
                            P.op("dve", lambda e: e.tensor_scalar(out=ki[:], in0=pre[:], scalar1=1.0 / TWO_PI, scalar2=None, op0=ALU.mult), [preB], [kiB])
                            P.op("dve", lambda e: e.tensor_copy(out=kf[:], in_=ki[:]), [kiB], [kfB])
                            P.op("dve", lambda e: e.scalar_tensor_tensor(out=pre[:], in0=kf[:], scalar=-TWO_PI, in1=pre[:], op0=ALU.mult, op1=ALU.add),
                                 [kfB, preB], [preB])
                            P.op("dve", lambda e: e.tensor_scalar(out=pre[:], in0=pre[:], scalar1=math.pi, scalar2=-math.pi, op0=ALU.min, op1=ALU.max), [preB], [preB])
                            P.op("act", lambda e, tch=tch: e.activation(out=dst[:, tch * 512:(tch + 1) * 512], in_=pre[:], func=AF.Sin), [preB], [dstB])

                    sin_layer(w1, w1B, zf, zfB, 33, 3, h1, h1B)
                    sin_layer(w2, w2B, h1, h1B, 64, 4, h2, h2B)
                    for tt in range(NT):
                        P.op("act", lambda e, tt=tt: e.activation(out=dec[:], in_=absd[:], func=AF.Exp, scale=tneg[:, tt:tt + 1]), [absdB, tnegB], [decB])
                        for q in range(4):
                            pt, pB = bank()
                            P.op("pe", lambda e, pt=pt, tt=tt, q=q: e.matmul(pt[:], lhsT=h2[:, tt * 128:(tt + 1) * 128], rhs=w3[:, q * 512:(q + 1) * 512],
                                                                             start=True, stop=True), [h2B, w3B], [pB])
                            dst, dB = (kfw, kfwB) if q < 2 else (kbw, kbwB)
                            P.op("dve", lambda e, pt=pt, q=q, dst=dst: e.tensor_tensor(out=dst[:, (q % 2) * 512:(q % 2 + 1) * 512], in0=pt[:],
                                                                                       in1=dec[:, (q % 2) * 512:(q % 2 + 1) * 512], op=ALU.mult),
                                 [pB, decB], [dB])
                        if tt == 0:
                            P.op("dve", lambda e: e.tensor_tensor(out=kfw[0:1, :], in0=kfw[0:1, :], in1=fbias[0:1, :], op=ALU.add), [kfwB, fbiasB], [kfwB])
                            P.op("dve", lambda e: e.memset(kbw[0:1, :], 0.0), [], [kbwB])
                        ka, kaB = kab[tt % 2], kabB[tt % 2]
                        P.op("pool", lambda e, ka=ka: e.tensor_tensor(out=ka[:, 0, :], in0=kfw[:], in1=kbw[:], op=ALU.add), [kfwB, kbwB], [kaB])
                        P.op("dve", lambda e, ka=ka: e.tensor_tensor(out=ka[:, 1, :], in0=kfw[:], in1=kbw[:], op=ALU.subtract), [kfwB, kbwB], [kaB])
                        for j in range(2):
                            P.dma("sp", T["KT"][j, tt * 128:(tt + 1) * 128, :], ka[:, j, :], reads=[kaB], writes=[KTB])
                    P.barrier()
                with ExitStack() as s3:
                    KA = sb(s3, "KA", [128, NT, D], BF16); KAB = Buf()
                    KBm = sb(s3, "KBm", [128, NT, D], BF16); KBB = Buf()
                    for j, (kt, ktB) in enumerate(((KA, KAB), (KBm, KBB))):
                        kv = T["KT"][j].rearrange("(tt p) c -> p tt c", p=128)
                        for q in range(4):
                            P.dma("sp", kt[:, q * 8:(q + 1) * 8, :], kv[:, q * 8:(q + 1) * 8, :], reads=[KTB], writes=[ktB])
                    stg = [sb(s3, f"kst{i}", [128, 2, D], F32) for i in range(2)]
                    stgB = [Buf(), Buf()]

                    def store(fc, res):
                        sg, sB = stg[fc % 2], stgB[fc % 2]
                        for h in range(2):
                            for cs_ in range(2):
                                pt, pB = res[h * 2 + cs_]
                                P.op("act", lambda e, pt=pt, h=h, cs_=cs_, sg=sg: e.activation(out=sg[:, cs_, h * 512:(h + 1) * 512], in_=pt[:], func=AF.Identity),
                                     [pB], [sB])
                        for cs_ in range(2):
                            P.dma("sp", T["KS"][cs_, fc * 128:(fc + 1) * 128, :], sg[:, cs_, :], reads=[sB], writes=[KSB])

                    fwd_dft(s3, KA, KAB, KBm, KBB, store)
                    P.barrier()

        KSB = Buf()
        ZTB = Buf(); X0CB = Buf(); YSB = Buf(); XAB = Buf()

        def phase_hyena_in():
            with ExitStack() as st:
                hmT = sb(st, "hmT", [128, 8, S], BF16); hmTB = Buf()
                with ExitStack() as s2:
                    (A, AB), (Bt, BB), _ = load_mod_tiles(s2, 0, 0, "norm_mix_g")
                    nx = NormCtx(s2, "h")
                    xb = [sb(s2, f"hx{i}", [128, D], F32) for i in range(3)]; xbB = [Buf(), Buf(), Buf()]
                    hb = [sb(s2, f"hb{i}", [128, D], BF16) for i in range(2)]; hbB = [Buf(), Buf()]

                    def ld_hx(tt):
                        if tt < NT:
                            P.dma("sp", xb[tt % 3][:], T["x"][tt * 128:(tt + 1) * 128, :], writes=[xbB[tt % 3]])

                    ld_hx(0)

                    def h_tile(tt):
                        xt, xtB = xb[tt % 3], xbB[tt % 3]
                        h_, hB_ = hb[tt % 2], hbB[tt % 2]
                        yield from norm_mod_g(nx, xt[:], xtB, A, AB, Bt, BB, h_[:], hB_)
                        yield
                        pt, pB = bank()
                        ptb = pt[:].bitcast(BF16)
                        for kc in range(8):
                            P.op("pe", lambda e, kc=kc: e.transpose(out=ptb[:, kc * 128:(kc + 1) * 128], in_=h_[:, kc * 128:(kc + 1) * 128], identity=identb[:]),
                                 [hB_, identbB], [pB])
                        yield
                        P.op("act", lambda e: e.activation(out=hmT[:, :, tt * 128:(tt + 1) * 128], in_=ptb.rearrange("p (k t) -> p k t", k=8), func=AF.Identity),
                             [pB], [hmTB])

                    run_pipelined(NT, h_tile, depth=2, on_start=lambda tt: ld_hx(tt + 1))
                    P.barrier()
                with ExitStack() as s2:
                    bcol = sb(s2, "bcol", [128, 24], F32); bcolB = Buf()
                    cw = sb(s2, "cw", [128, 3, 24], F32); cwB = Buf()
                    cbc = sb(s2, "cbc", [128, 24], F32); cbcB = Buf()
                    P.dma("sp", bcol[:], T["hy_b_in"][0].rearrange("(cc p) -> p cc", p=128), writes=[bcolB], allow_slow_non_contiguous=True)
                    P.dma("sp", cbc[:], T["hy_conv_b"][0].rearrange("(cc p) -> p cc", p=128), writes=[cbcB], allow_slow_non_contiguous=True)
                    for j in range(3):
                        P.dma("sp", cw[:, j, :], T["hy_conv_w"][0, j].rearrange("(cc p) -> p cc", p=128), writes=[cwB], allow_slow_non_contiguous=True)
                    wch = [sb(s2, f"wch{i}", [128, 8, 128], BF16) for i in range(2)]; wchB = [Buf(), Buf()]
                    us = [sb(s2, f"u{i}", [128, S + 2], F32) for i in range(2)]; usB = [Buf(), Buf()]
                    ob = [sb(s2, f"ob{i}", [128, S], F32) for i in range(2)]; obB = [Buf(), Buf()]
                    zb = sb(s2, "zb", [128, S], BF16); zbB = Buf()
                    zst = sb(s2, "zst", [128, NT, 128], BF16); zstB = Buf()
                    for u, uB in zip(us, usB):
                        P.op("dve", lambda e, u=u: e.memset(u[:, 0:1], 0.0), [], [uB])
                        P.op("dve", lambda e, u=u: e.memset(u[:, S + 1:S + 2], 0.0), [], [uB])
                    wv = T["hy_w_in"][0].rearrange("(kc p) n -> p kc n", p=128)
                    order = []
                    for j in range(8):
                        order += [8 + j, 16 + j]
                    order += list(range(8))
                    for n, cc in enumerate(order):
                        w, wB = wch[n % 2], wchB[n % 2]
                        u, uB = us[n % 2], usB[n % 2]
                        P.dma("pool", w[:], wv[:, :, cc * 128:(cc + 1) * 128], writes=[wB])
                        for tch in range(8):
                            pt, pB = bank()
                            for kc in range(8):
                                P.op("pe", lambda e, pt=pt, kc=kc, w=w, tch=tch: e.matmul(pt[:], lhsT=w[:, kc, :], rhs=hmT[:, kc, tch * 512:(tch + 1) * 512],
                                                                                         start=(kc == 0), stop=(kc == 7)), [wB, hmTB], [pB])
                            P.op("act", lambda e, pt=pt, tch=tch, cc=cc, u=u: e.activation(out=u[:, 1 + tch * 512:1 + (tch + 1) * 512], in_=pt[:], func=AF.Identity,
                                                                                    bias=bcol[:, cc:cc + 1]), [pB, bcolB], [uB])
                        o, oB = ob[n % 2], obB[n % 2]
                        P.op("act", lambda e, o=o, cc=cc, u=u: e.activation(out=o[:], in_=u[:, 1:S + 1], func=AF.Identity, scale=cw[:, 1, cc:cc + 1], bias=cbc[:, cc:cc + 1]),
                             [uB, cwB, cbcB], [oB])
                        P.op("dve", lambda e, o=o, cc=cc, u=u: e.scalar_tensor_tensor(out=o[:], in0=u[:, 0:S], scalar=cw[:, 0, cc:cc + 1], in1=o[:], op0=ALU.mult, op1=ALU.add),
                             [uB, cwB, oB], [oB])
                        if cc >= 8:
                            P.op("dve", lambda e, o=o, cc=cc, u=u: e.scalar_tensor_tensor(out=o[:], in0=u[:, 2:S + 2], scalar=cw[:, 2, cc:cc + 1], in1=o[:], op0=ALU.mult, op1=ALU.add),
                                 [uB, cwB, oB], [oB])
                        else:
                            P.op("dve", lambda e, o=o, cc=cc, u=u: e.scalar_tensor_tensor(out=zb[:], in0=u[:, 2:S + 2], scalar=cw[:, 2, cc:cc + 1], in1=o[:], op0=ALU.mult, op1=ALU.add),
                                 [uB, cwB, oB], [zbB])
                            P.dma("sp", T["X0C"][cc * 128:(cc + 1) * 128, :], zb[:], reads=[zbB], writes=[X0CB])
                        if cc >= 16:
                            j = cc - 16
                            o1, o1B = ob[(n - 1) % 2], obB[(n - 1) % 2]
                            P.op("pool", lambda e, o=o, o1=o1: e.tensor_tensor(out=zb[:], in0=o[:], in1=o1[:], op=ALU.mult), [oB, o1B], [zbB])
                            for g4 in range(4):
                                pt, pB = bank()
                                ptb = pt[:].bitcast(BF16)
                                for k in range(8):
                                    tt = g4 * 8 + k
                                    P.op("pe", lambda e, ptb=ptb, k=k, tt=tt: e.transpose(out=ptb[:, k * 128:(k + 1) * 128], in_=zb[:, tt * 128:(tt + 1) * 128], identity=identb[:]),
                                         [zbB, identbB], [pB])
                                P.op("act", lambda e, ptb=ptb, g4=g4: e.activation(out=zst[:, g4 * 8:(g4 + 1) * 8, :], in_=ptb.rearrange("p (k c) -> p k c", k=8), func=AF.Identity),
                                     [pB], [zstB])
                            P.dma("sp", T["ZT"].rearrange("(tt p) c -> p tt c", p=128)[:, :, j * 128:(j + 1) * 128], zst[:], reads=[zstB], writes=[ZTB])
                    P.barrier()

        def phase_hyena_fwd():
            with ExitStack() as st:
                zt = sb(st, "zt", [128, NT, D], BF16); ztB = Buf()
                zv = T["ZT"].rearrange("(tt p) c -> p tt c", p=128)
                for q in range(4):
                    P.dma("sp", zt[:, q * 8:(q + 1) * 8, :], zv[:, q * 8:(q + 1) * 8, :], reads=[ZTB], writes=[ztB])
                kk = [sb(st, f"kk{i}", [128, 2, D], F32) for i in range(2)]; kkB = [Buf(), Buf()]
                yt = [sb(st, f"yt{i}", [128, 2, D], BF16) for i in range(2)]; ytB = [Buf(), Buf()]
                t1 = sb(st, "yt1", [128, 512], F32); t1B = Buf()
                t2 = sb(st, "yt2", [128, 512], F32); t2B = Buf()
                t3 = sb(st, "yt3", [128, 512], F32); t3B = Buf()
                t4 = sb(st, "yt4", [128, 512], F32); t4B = Buf()

                def mulk(fc, res):
                    k, kB = kk[fc % 2], kkB[fc % 2]
                    y, yB = yt[fc % 2], ytB[fc % 2]
                    for cs_ in range(2):
                        P.dma("sp", k[:, cs_, :], T["KS"][cs_, fc * 128:(fc + 1) * 128, :], reads=[KSB], writes=[kB])
                    for h in range(2):
                        zr, zrB = res[h * 2]
                        zi, ziB = res[h * 2 + 1]
                        sl = slice(h * 512, (h + 1) * 512)
                        P.op("dve", lambda e, zr=zr, sl=sl: e.tensor_tensor(out=t1[:], in0=zr[:], in1=k[:, 0, sl], op=ALU.mult), [zrB, kB], [t1B])
                        P.op("dve", lambda e, zi=zi, sl=sl: e.tensor_tensor(out=t2[:], in0=zi[:], in1=k[:, 1, sl], op=ALU.mult), [ziB, kB], [t2B])
                        P.op("dve", lambda e, zr=zr, sl=sl: e.tensor_tensor(out=t3[:], in0=zr[:], in1=k[:, 1, sl], op=ALU.mult), [zrB, kB], [t3B])
                        P.op("dve", lambda e, zi=zi, sl=sl: e.tensor_tensor(out=t4[:], in0=zi[:], in1=k[:, 0, sl], op=ALU.mult), [ziB, kB], [t4B])
                        P.op("pool", lambda e, sl=sl, y=y: e.tensor_tensor(out=y[:, 0, sl], in0=t1[:], in1=t2[:], op=ALU.subtract), [t1B, t2B], [yB])
                        P.op("pool", lambda e, sl=sl, y=y: e.tensor_tensor(out=y[:, 1, sl], in0=t3[:], in1=t4[:], op=ALU.add), [t3B, t4B], [yB])
                    P.dma("sp", T["YS"][fc], y[:], reads=[yB], writes=[YSB])

                fwd_dft(st, zt, ztB, zt, ztB, mulk)
                P.barrier()

        def phase_hyena_out():
            with ExitStack() as st:
                X0 = sb(st, "X0", [128, 8, S], BF16); X0B = Buf()
                xv = T["X0C"].rearrange("(cc p) t -> p cc t", p=128)
                for cc in range(8):
                    P.dma("sp", X0[:, cc, :], xv[:, cc, :], reads=[X0CB], writes=[X0B])
                with ExitStack() as s2:
                    Yh = sb(s2, "Yh", [128, 32, 2, 512], BF16); YhB = Buf()
                    gvb = [sb(s2, f"gv{i}", [128, 32, 2, 128], BF16) for i in range(2)]; gvB = [Buf(), Buf()]
                    ytm = [sb(s2, f"ytm{i}", [128, 512], BF16) for i in range(2)]; ytmB = [Buf(), Buf()]
                    yv = T["YS"].rearrange("fc p cs c -> p fc cs c")
                    prev = None

                    def xpose(h, to, ym, ymB):
                        p2, p2B = bank()
                        p2b = p2[:].bitcast(BF16)
                        for j in range(4):
                            P.op("pe", lambda e, j=j: e.transpose(out=p2b[:, j * 128:(j + 1) * 128], in_=ym[:, j * 128:(j + 1) * 128], identity=identb[:]),
                                 [ymB, identbB], [p2B])
                        P.op("dve", lambda e: e.tensor_tensor(out=X0[:, h * 4:(h + 1) * 4, to * 128:(to + 1) * 128],
                                                              in0=p2b[:, 0:512].rearrange("p (j t) -> p j t", j=4),
                                                              in1=X0[:, h * 4:(h + 1) * 4, to * 128:(to + 1) * 128], op=ALU.mult),
                             [p2B, X0B], [X0B])

                    for h in range(2):
                        for q in range(4):
                            for cs_ in range(2):
                                P.dma("sp", Yh[:, q * 8:(q + 1) * 8, cs_, :], yv[:, q * 8:(q + 1) * 8, cs_, h * 512:(h + 1) * 512], reads=[YSB], writes=[YhB])
                        for to in range(NT):
                            gv, gB = gvb[to % 2], gvB[to % 2]
                            for q in range(2):
                                P.dma("sp", gv[:, q * 16:(q + 1) * 16], T["INV"][to, :, q * 16:(q + 1) * 16], writes=[gB])
                            pt, pB = bank()
                            n = 0
                            for fc in range(32):
                                for cs_ in range(2):
                                    P.op("pe", lambda e, pt=pt, gv=gv, fc=fc, cs_=cs_, n=n: e.matmul(pt[:], lhsT=gv[:, fc, cs_, :], rhs=Yh[:, fc, cs_, :],
                                                                                               start=(n == 0), stop=(n == 63)), [gB, YhB], [pB])
                                    n += 1
                            ym, ymB = ytm[to % 2], ytmB[to % 2]
                            P.op("act", lambda e, pt=pt, ym=ym: e.activation(out=ym[:], in_=pt[:], func=AF.Identity), [pB], [ymB])
                            if prev is not None:
                                xpose(*prev)
                            prev = (h, to, ym, ymB)
                    xpose(*prev)
                    P.barrier()
                with ExitStack() as s2:
                    wo = sb(s2, "wo", [128, 8, D], BF16); woB = Buf()
                    wv = T["hy_w_out"][0].rearrange("(kc p) n -> p kc n", p=128)
                    for q in range(2):
                        P.dma("pool", wo[:, q * 4:(q + 1) * 4, :], wv[:, q * 4:(q + 1) * 4, :], writes=[woB])
                    _, _, (G, GB) = load_mod_tiles(s2, 0, 0, "norm_mix_g")
                    bo = sb(s2, "bo", [128, D], F32); boB = Buf()
                    bcast_load("sp", bo[:], boB, T["hy_b_out"][0, :])
                    xb = [sb(s2, f"ox{i}", [128, D], F32) for i in range(2)]; xbB = [Buf(), Buf()]
                    yo = [sb(s2, f"oy{i}", [128, D], F32) for i in range(2)]; yoB = [Buf(), Buf()]
                    P.dma("sp", xb[0][:], T["x"][0:128, :], writes=[xbB[0]])
                    for tt in range(NT):
                        xt, xtB = xb[tt % 2], xbB[tt % 2]
                        y, yB = yo[tt % 2], yoB[tt % 2]
                        if tt + 1 < NT:
                            P.dma("sp", xb[(tt + 1) % 2][:], T["x"][(tt + 1) * 128:(tt + 2) * 128, :], writes=[xbB[(tt + 1) % 2]])
                        for nh in range(2):
                            pt, pB = bank()
                            for cc in range(8):
                                P.op("pe", lambda e, pt=pt, cc=cc, tt=tt, nh=nh: e.matmul(pt[:], lhsT=X0[:, cc, tt * 128:(tt + 1) * 128], rhs=wo[:, cc, nh * 512:(nh + 1) * 512],
                                                                                         start=(cc == 0), stop=(cc == 7)), [X0B, woB], [pB])
                            sl = slice(nh * 512, (nh + 1) * 512)
                            P.op("dve", lambda e, pt=pt, sl=sl, y=y: e.tensor_tensor(out=y[:, sl], in0=pt[:], in1=bo[:, sl], op=ALU.add), [pB, boB], [yB])
                        P.op("pool", lambda e, y=y: e.tensor_tensor(out=y[:], in0=y[:], in1=G[:], op=ALU.mult), [yB, GB], [yB])
                        P.op("dve", lambda e, y=y, xt=xt: e.tensor_tensor(out=y[:], in0=y[:], in1=xt[:], op=ALU.add), [yB, xtB], [yB])
                        P.dma("sp", T["XA"][tt * 128:(tt + 1) * 128, :], y[:], reads=[yB], writes=[XAB])
                    P.barrier()

        HFB = Buf(); ACCB = Buf()

        def phase_moe(layer, XIN, XINB, XOUT, XOUTB, final):
            with ExitStack() as st:
                IDX = sb(st, "IDX", [128, 4, NEXP], U32); IDXB = Buf()
                GVt = sb(st, "GVt", [128, 4, NEXP], F32); GVB = Buf()
                (A, AB), (Bt, BB), (G, GB) = load_mod_tiles(st, layer, 1, "norm_ffn_g")
                wts = [[sb(st, f"w{n}{i}", [128, 8, D], BF16) for n in "gud"] for i in range(2)]
                wtsB = [[Buf() for _ in range(3)] for i in range(2)]
                wnames = ("moe_w_gate", "moe_w_up", "moe_w_down")

                def issue_wloads(e_):
                    for n in range(3):
                        wv = T[wnames[n]][layer, e_].rearrange("(kc p) n -> p kc n", p=128)
                        w, wB = wts[e_ % 2][n], wtsB[e_ % 2][n]
                        P.dma("pool", w[:], wv[:, :, :], writes=[wB])

                issue_wloads(0)
                issue_wloads(1)
                with ExitStack() as s1:
                    AFFT = sb(s1, "AFFT", [NEXP, S], F32); AFFTB = Buf()
                    with ExitStack() as s2:
                        nx = NormCtx(s2, "m")
                        wr = sb(s2, "wr", [128, 8, NEXP], F32); wrB = Buf()
                        P.dma("sp", wr[:], T["moe_w_router"][layer].rearrange("(kc p) e -> p kc e", p=128), writes=[wrB])
                        zt_ = sb(s2, "zero", [128, D], F32); ztB_ = Buf()
                        P.op("pool", lambda e: e.memset(zt_[:], 0.0), [], [ztB_])
                        for tt in range(NT):
                            P.dma("sp", T["ACC"][tt * 128:(tt + 1) * 128, :], zt_[:], reads=[ztB_], writes=[ACCB])
                        xb = [sb(s2, f"mx{i}", [128, D], F32) for i in range(3)]; xbB = [Buf(), Buf(), Buf()]
                        hf = [sb(s2, f"hf{i}", [128, D], F32) for i in range(2)]; hfB = [Buf(), Buf()]
                        hfb = [sb(s2, f"hfb{i}", [128, D], BF16) for i in range(2)]; hfbB = [Buf(), Buf()]
                        hfT = [sb(s2, f"hfT{i}", [128, 8, 128], F32) for i in range(2)]; hfTB = [Buf(), Buf()]
                        sm = [sb(s2, f"sm{i}", [128, 8], F32) for i in range(2)]; smB = [Buf(), Buf()]
                        ex = [sb(s2, f"ex{i}", [128, NEXP], F32) for i in range(2)]; exB = [Buf(), Buf()]
                        aff = [sb(s2, f"aff{i}", [128, NEXP], F32) for i in range(2)]; affB = [Buf(), Buf()]

                        def ld_x(tt):
                            if tt < NT:
                                P.dma("sp", xb[tt % 3][:], XIN[tt * 128:(tt + 1) * 128, :], reads=[XINB], writes=[xbB[tt % 3]])

                        ld_x(0)

                        def m1_tile(tt):
                            k = tt % 2
                            xt, xtB = xb[tt % 3], xbB[tt % 3]
                            h_, hB_ = hf[k], hfB[k]
                            hb_, hbB_ = hfb[k], hfbB[k]
                            hT, hTB = hfT[k], hfTB[k]
                            sm_, smB_ = sm[k], smB[k]
                            ex_, exB_ = ex[k], exB[k]
                            af_, afB_ = aff[k], affB[k]
                            yield from norm_mod_g(nx, xt[:], xtB, A, AB, Bt, BB, h_[:], hB_)
                            yield
                            P.op("act", lambda e: e.activation(out=hb_[:], in_=h_[:], func=AF.Identity), [hB_], [hbB_])
                            for half in range(2):
                                pt, pB = bank()
                                for k4 in range(4):
                                    kc = half * 4 + k4
                                    P.op("pe", lambda e, pt=pt, k4=k4, kc=kc: e.transpose(out=pt[:, k4 * 128:(k4 + 1) * 128], in_=h_[:, kc * 128:(kc + 1) * 128], identity=identf[:]),
                                         [hB_, identfB], [pB])
                                yield
                                P.op("act", lambda e, pt=pt, half=half: e.activation(out=hT[:, half * 4:(half + 1) * 4, :], in_=pt[:].rearrange("p (k t) -> p k t", k=4), func=AF.Identity),
                                     [pB], [hTB])
                            P.dma("sp", T["HF"][tt * 128:(tt + 1) * 128, :], hb_[:], reads=[hbB_], writes=[HFB])
                            yield
                            pt, pB = bank()
                            for kc in range(8):
                                P.op("pe", lambda e, pt=pt, kc=kc: e.matmul(pt[:, 0:NEXP], lhsT=hT[:, kc, :], rhs=wr[:, kc, :], start=(kc == 0), stop=(kc == 7)),
                                     [hTB, wrB], [pB])
                            yield
                            P.op("dve", lambda e: e.tensor_reduce(out=sm_[:, 0:1], in_=pt[:, 0:NEXP], axis=AX.X, op=ALU.max, negate=True), [pB], [smB_])
                            yield
                            P.op("act", lambda e: e.activation(out=ex_[:], in_=pt[:, 0:NEXP], func=AF.Exp, bias=sm_[:, 0:1], accum_out=sm_[:, 1:2]), [pB, smB_], [exB_, smB_])
                            yield
                            P.op("dve", lambda e: e.reciprocal(out=sm_[:, 2:3], in_=sm_[:, 1:2]), [smB_], [smB_])
                            yield
                            P.op("dve", lambda e: e.tensor_scalar(out=af_[:], in0=ex_[:], scalar1=sm_[:, 2:3], scalar2=None, op0=ALU.mult), [exB_, smB_], [afB_])
                            yield
                            p2, p2B = bank()
                            P.op("pe", lambda e: e.transpose(out=p2[0:NEXP, 0:128], in_=af_[:, 0:NEXP], identity=identf[:]), [afB_, identfB], [p2B])
                            yield
                            P.op("act", lambda e: e.activation(out=AFFT[:, tt * 128:(tt + 1) * 128], in_=p2[0:NEXP, 0:128], func=AF.Identity), [p2B], [AFFTB])

                        run_pipelined(NT, m1_tile, depth=2, on_start=lambda tt: ld_x(tt + 1))
                        P.barrier()
                    with ExitStack() as s2:
                        work = sb(s2, "work", [NEXP, S], F32); workB = Buf()
                        vals = sb(s2, "vals", [NEXP, CAP], F32); valsB = Buf()
                        idxu = sb(s2, "idxu", [NEXP, CAP], U32); idxuB = Buf()
                        idxf = sb(s2, "idxf", [NEXP, CAP], F32); idxfB = Buf()
                        idt = sb(s2, "idt", [128, 4, NEXP], F32); idtB = Buf()
                        P.op("dve", lambda e: e.tensor_copy(out=work[:], in_=AFFT[:]), [AFFTB], [workB])
                        for r in range(CAP // 8):
                            sl = slice(8 * r, 8 * r + 8)
                            P.op("dve", lambda e, sl=sl: e.max(out=vals[:, sl], in_=work[:]), [workB], [valsB])
                            P.op("dve", lambda e, sl=sl: e.max_index(out=idxu[:, sl], in_max=vals[:, sl], in_values=work[:]), [workB, valsB], [idxuB])
                            P.op("dve", lambda e, sl=sl: e.match_replace(out=work[:], in_to_replace=vals[:, sl], in_values=work[:], imm_value=-1.0), [valsB, workB], [workB])
                        P.op("dve", lambda e: e.tensor_copy(out=idxf[:], in_=idxu[:]), [idxuB], [idxfB])
                        for s_ in range(4):
                            pt, pB = bank()
                            P.op("pe", lambda e, pt=pt, s_=s_: e.transpose(out=pt[:, 0:NEXP], in_=idxf[0:NEXP, s_ * 128:(s_ + 1) * 128], identity=identf[0:NEXP, 0:NEXP]),
                                 [idxfB, identfB], [pB])
                            P.op("dve", lambda e, pt=pt, s_=s_: e.tensor_copy(out=idt[:, s_, :], in_=pt[:, 0:NEXP]), [pB], [idtB])
                            P.op("dve", lambda e, s_=s_: e.tensor_copy(out=IDX[:, s_, :], in_=idt[:, s_, :]), [idtB], [IDXB])
                            p2, p2B = bank()
                            P.op("pe", lambda e, p2=p2, s_=s_: e.transpose(out=p2[:, 0:NEXP], in_=vals[0:NEXP, s_ * 128:(s_ + 1) * 128], identity=identf[0:NEXP, 0:NEXP]),
                                 [valsB, identfB], [p2B])
                            P.op("dve", lambda e, p2=p2, s_=s_: e.tensor_copy(out=GVt[:, s_, :], in_=p2[:, 0:NEXP]), [p2B], [GVB])
                        P.barrier()
                with ExitStack() as s1:
                    xs = [sb(s1, f"xs{i}", [128, 4, D], BF16) for i in range(2)]; xsB = [[Buf() for _ in range(4)] for _ in range(2)]
                    xsT = sb(s1, "xsT", [128, 8, CAP], BF16); xsTB = Buf()
                    hid = sb(s1, "hid", [128, 8, CAP], BF16); hidB = Buf()
                    sg = [sb(s1, f"sg{i}", [128, 512], F32) for i in range(2)]; sgB = [Buf(), Buf()]
                    ye = [sb(s1, f"ye{i}", [128, D], F32) for i in range(4)]; yeB = [Buf() for _ in range(4)]

                    def issue_gathers(e_):
                        x_ = xs[e_ % 2]
                        for s_ in range(4):
                            P.dma("pool", None, None, reads=[HFB, IDXB], writes=[xsB[e_ % 2][s_]],
                                  fn=lambda g, s_=s_, x_=x_, e_=e_: g.indirect_dma_start(
                                      out=x_[:, s_, :], out_offset=None, in_=T["HF"][:, :],
                                      in_offset=bass.IndirectOffsetOnAxis(ap=IDX[:, s_, e_:e_ + 1], axis=0)))

                    issue_gathers(0)
                    issue_gathers(1)
                    prev_sc = []
                    for e_ in range(NEXP):
                        (wg, wu, wd), (wgB, wuB, wdB) = wts[e_ % 2], wtsB[e_ % 2]
                        x_ = xs[e_ % 2]
                        for s_ in range(4):
                            pt, pB = bank()
                            ptb = pt[:].bitcast(BF16)
                            for kc in range(8):
                                P.op("pe", lambda e, ptb=ptb, kc=kc, s_=s_, x_=x_: e.transpose(out=ptb[:, kc * 128:(kc + 1) * 128], in_=x_[:, s_, kc * 128:(kc + 1) * 128], identity=identb[:]),
                                     [xsB[e_ % 2][s_], identbB], [pB])
                            P.op("act", lambda e, ptb=ptb, s_=s_: e.activation(out=xsT[:, :, s_ * 128:(s_ + 1) * 128], in_=ptb.rearrange("p (k t) -> p k t", k=8), func=AF.Identity),
                                 [pB], [xsTB])
                        for fcn in range(8):
                            pg, pgB = bank()
                            pu, puB = bank()
                            for kc in range(8):
                                P.op("pe", lambda e, pg=pg, kc=kc, fcn=fcn, wg=wg: e.matmul(pg[:], lhsT=wg[:, kc, fcn * 128:(fcn + 1) * 128], rhs=xsT[:, kc, :], start=(kc == 0), stop=(kc == 7)),
                                     [wgB, xsTB], [pgB])
                            for kc in range(8):
                                P.op("pe", lambda e, pu=pu, kc=kc, fcn=fcn, wu=wu: e.matmul(pu[:], lhsT=wu[:, kc, fcn * 128:(fcn + 1) * 128], rhs=xsT[:, kc, :], start=(kc == 0), stop=(kc == 7)),
                                     [wuB, xsTB], [puB])
                            s__, sB__ = sg[fcn % 2], sgB[fcn % 2]
                            P.op("act", lambda e, pg=pg, s__=s__: e.activation(out=s__[:], in_=pg[:], func=AF.Silu), [pgB], [sB__])
                            P.op("dve", lambda e, pu=pu, s__=s__, fcn=fcn: e.tensor_tensor(out=hid[:, fcn, :], in0=pu[:], in1=s__[:], op=ALU.mult), [puB, sB__], [hidB])
                        for s_ in range(4):
                            y, yB = ye[s_], yeB[s_]
                            for nh in range(2):
                                pt, pB = bank()
                                for fcn in range(8):
                                    P.op("pe", lambda e, pt=pt, fcn=fcn, s_=s_, nh=nh, wd=wd: e.matmul(pt[:], lhsT=hid[:, fcn, s_ * 128:(s_ + 1) * 128], rhs=wd[:, fcn, nh * 512:(nh + 1) * 512],
                                                                                                 start=(fcn == 0), stop=(fcn == 7)), [hidB, wdB], [pB])
                                P.op("act", lambda e, pt=pt, y=y, nh=nh, s_=s_, e_=e_: e.activation(out=y[:, nh * 512:(nh + 1) * 512], in_=pt[:], func=AF.Identity, scale=GVt[:, s_, e_:e_ + 1]),
                                     [pB, GVB], [yB])
                        if e_ + 2 < NEXP:
                            issue_wloads(e_ + 2)
                        if ACCB.w is not None:
                            P._wait("pool", ACCB.w)
                        for t_ in prev_sc:
                            P._wait("pool", t_)
                        cur_sc = []
                        for s_ in range(4):
                            y, yB = ye[s_], yeB[s_]
                            cur_sc.append(P.dma("pool", None, None, reads=[yB, IDXB], writes=[ACCB],
                                                fn=lambda g, s_=s_, y=y, e_=e_: g.indirect_dma_start(
                                                    out=T["ACC"][:, :], out_offset=bass.IndirectOffsetOnAxis(ap=IDX[:, s_, e_:e_ + 1], axis=0),
                                                    in_=y[:], in_offset=None, compute_op=ALU.add)))
                        prev_sc[:] = cur_sc
                        if e_ + 2 < NEXP:
                            issue_gathers(e_ + 2)
                    P.barrier()
                with ExitStack() as s1:
                    xb = [sb(s1, f"cx{i}", [128, D], F32) for i in range(2)]; xbB = [Buf(), Buf()]
                    ab = [sb(s1, f"ca{i}", [128, D], F32) for i in range(2)]; abB = [Buf(), Buf()]
                    ob_ = [sb(s1, f"co{i}", [128, D], F32) for i in range(2)]; obB_ = [Buf(), Buf()]
                    if final:
                        nx = NormCtx(s1, "f")
                        fg = sb(s1, "fg", [128, D], F32); fgB = Buf()
                        bcast_load("sp", fg[:], fgB, T["final_norm_g"])
                    def ld_c(tt):
                        P.dma("sp", xb[tt % 2][:], XIN[tt * 128:(tt + 1) * 128, :], reads=[XINB], writes=[xbB[tt % 2]])
                        P.dma("sp", ab[tt % 2][:], T["ACC"][tt * 128:(tt + 1) * 128, :], reads=[ACCB], writes=[abB[tt % 2]])

                    ld_c(0)
                    for tt in range(NT):
                        xt, xtB = xb[tt % 2], xbB[tt % 2]
                        a_, aB_ = ab[tt % 2], abB[tt % 2]
                        if tt + 1 < NT:
                            ld_c(tt + 1)
                        P.op("pool", lambda e, a_=a_: e.tensor_tensor(out=a_[:], in0=a_[:], in1=G[:], op=ALU.mult), [aB_, GB], [aB_])
                        P.op("dve", lambda e, a_=a_, xt=xt: e.tensor_tensor(out=a_[:], in0=a_[:], in1=xt[:], op=ALU.add), [aB_, xtB], [aB_])
                        if final:
                            o_, oB_ = ob_[tt % 2], obB_[tt % 2]
                            norm_mod(nx, a_[:], aB_, fg, fgB, None, None, o_[:], oB_, None, None)
                            P.dma("sp", XOUT[tt * 128:(tt + 1) * 128, :], o_[:], reads=[oB_], writes=[XOUTB])
                        else:
                            P.dma("sp", XOUT[tt * 128:(tt + 1) * 128, :], a_[:], reads=[aB_], writes=[XOUTB])
                    P.barrier()

        def bank_fixed(i):
            return ps[i], psB[i]

        SCALE = 1.0 / math.sqrt(192.0)
        C1 = 6.28125
        C2 = TWO_PI - 6.28125
        QNB = Buf(); QPB = Buf(); KNB = Buf(); KPB = Buf(); VB = Buf()

        def phase_mla_proj(XIN, XINB):
            with ExitStack() as st:
                cqnT = sb(st, "cqnT", [128, 2, S], BF16); cqnTB = Buf()
                ckvT = sb(st, "ckvT", [128, S], BF16); ckvTB = Buf()
                kpT = sb(st, "kpT", [64, S], BF16); kpTB = Buf()
                cosT = sb(st, "cosT", [64, S], F32); cosTB = Buf()
                sinT = sb(st, "sinT", [64, S], F32); sinTB = Buf()
                with ExitStack() as s2:
                    posi = sb(s2, "posi", [64, S], I32); posiB = Buf()
                    ang = sb(s2, "ang", [64, S], F32); angB = Buf()
                    a2 = sb(s2, "a2", [64, S], F32); a2B = Buf()
                    kq = sb(s2, "kq", [64, S], I32); kqB = Buf()
                    kqf = sb(s2, "kqf", [64, S], F32); kqfB = Buf()
                    ivf = sb(s2, "ivf", [64, 1], F32); ivfB = Buf()
                    P.dma("sp", posi[:], T["pos"].partition_broadcast(64), writes=[posiB])
                    P.dma("sp", ivf[0:32, :], T["invf"].rearrange("(p o) -> p o", o=1), writes=[ivfB])
                    P.dma("sp", ivf[32:64, :], T["invf"].rearrange("(p o) -> p o", o=1), writes=[ivfB])
                    P.op("dve", lambda e: e.tensor_copy(out=ang[:], in_=posi[:]), [posiB], [angB])
                    P.op("dve", lambda e: e.tensor_scalar(out=ang[:], in0=ang[:], scalar1=ivf[:, 0:1], scalar2=None, op0=ALU.mult), [angB, ivfB], [angB])
                    for shift, dst, dB in ((0.0, sinT, sinTB), (math.pi / 2.0, cosT, cosTB)):
                        P.op("dve", lambda e, shift=shift: e.tensor_scalar(out=a2[:], in0=ang[:], scalar1=shift, scalar2=None, op0=ALU.add), [angB], [a2B])
                        P.op("dve", lambda e: e.tensor_scalar(out=kq[:], in0=a2[:], scalar1=1.0 / TWO_PI, scalar2=None, op0=ALU.mult), [a2B], [kqB])
                        P.op("dve", lambda e: e.tensor_copy(out=kqf[:], in_=kq[:]), [kqB], [kqfB])
                        P.op("dve", lambda e: e.scalar_tensor_tensor(out=a2[:], in0=kqf[:], scalar=-C1, in1=a2[:], op0=ALU.mult, op1=ALU.add), [kqfB, a2B], [a2B])
                        P.op("dve", lambda e: e.scalar_tensor_tensor(out=a2[:], in0=kqf[:], scalar=-C2, in1=a2[:], op0=ALU.mult, op1=ALU.add), [kqfB, a2B], [a2B])
                        P.op("dve", lambda e: e.tensor_scalar(out=a2[:], in0=a2[:], scalar1=math.pi, scalar2=-math.pi, op0=ALU.min, op1=ALU.max), [a2B], [a2B])
                        P.op("act", lambda e, dst=dst: e.activation(out=dst[:], in_=a2[:], func=AF.Sin), [a2B], [dB])
                    P.barrier()
                with ExitStack() as s2:
                    (A, AB), (Bt, BB), _ = load_mod_tiles(s2, 1, 0, "norm_mix_g")
                    nx = NormCtx(s2, "a")
                    win = sb(s2, "win", [128, 8, 448], BF16); winB = Buf()
                    wrot = sb(s2, "wrot", [128, 8, 64], BF16); wrotB = Buf()
                    P.dma("pool", win[:], T["mla_w_in"][0].rearrange("(kc p) n -> p kc n", p=128), writes=[winB])
                    P.op("dve", lambda e: e.tensor_scalar(out=wrot[:, :, 0:32], in0=win[:, :, 416:448], scalar1=-1.0, scalar2=None, op0=ALU.mult), [winB], [wrotB])
                    P.op("dve", lambda e: e.tensor_copy(out=wrot[:, :, 32:64], in_=win[:, :, 384:416]), [winB], [wrotB])
                    qg = sb(s2, "qg", [128, 256], F32); qgB = Buf()
                    kg = sb(s2, "kg", [128, 128], F32); kgB = Buf()
                    bcast_load("sp", qg[:], qgB, T["mla_q_norm_g"][0, :])
                    bcast_load("sp", kg[:], kgB, T["mla_kv_norm_g"][0, :])
                    xb = [sb(s2, f"ax{i}", [128, D], F32) for i in range(2)]; xbB = [Buf(), Buf()]
                    tmp = None; tmpB = None
                    hb = [sb(s2, f"ahb{i}", [128, D], BF16) for i in range(2)]; hbB = [Buf(), Buf()]
                    hT = [sb(s2, f"ahT{i}", [128, 8, 128], BF16) for i in range(2)]; hTB = [Buf(), Buf()]
                    jq = sb(s2, "jq", [128, 256], F32); jqB = Buf()
                    sq = sb(s2, "sq", [128, 8], F32); sqB = Buf()
                    cn = [sb(s2, f"cn{i}", [128, 384], BF16) for i in range(2)]; cnB = [Buf(), Buf()]
                    r1 = sb(s2, "r1", [64, 128], F32); r1B = Buf()
                    r2 = sb(s2, "r2", [64, 128], F32); r2B = Buf()
                    for tt in range(NT):
                        tsl = slice(tt * 128, (tt + 1) * 128)
                        xt, xtB = xb[tt % 2], xbB[tt % 2]
                        P.dma("sp", xt[:], XIN[tsl, :], reads=[XINB], writes=[xtB])
                        h_, hB_ = hb[tt % 2], hbB[tt % 2]
                        norm_mod(nx, xt[:], xtB, A, AB, Bt, BB, h_[:], hB_)
                        pt, pB = bank()
                        ptb = pt[:].bitcast(BF16)
                        for kc in range(8):
                            P.op("pe", lambda e, kc=kc, ptb=ptb, h_=h_: e.transpose(out=ptb[:, kc * 128:(kc + 1) * 128], in_=h_[:, kc * 128:(kc + 1) * 128], identity=identb[:]),
                                 [hB_, identbB], [pB])
                        ht, htB = hT[tt % 2], hTB[tt % 2]
                        P.op("act", lambda e, ptb=ptb, ht=ht: e.activation(out=ht[:], in_=ptb.rearrange("p (k t) -> p k t", k=8), func=AF.Identity), [pB], [htB])
                        pa, paB = bank()
                        for kc in range(8):
                            P.op("pe", lambda e, pa=pa, kc=kc, ht=ht: e.matmul(pa[:, 0:384], lhsT=ht[:, kc, :], rhs=win[:, kc, 0:384], start=(kc == 0), stop=(kc == 7)),
                                 [htB, winB], [paB])
                        pk1, pk1B = bank()
                        for kc in range(8):
                            P.op("pe", lambda e, pk1=pk1, kc=kc, ht=ht: e.matmul(pk1[0:64, 0:128], lhsT=win[:, kc, 384:448], rhs=ht[:, kc, :], start=(kc == 0), stop=(kc == 7)),
                                 [htB, winB], [pk1B])
                        pk2, pk2B = bank()
                        for kc in range(8):
                            P.op("pe", lambda e, pk2=pk2, kc=kc, ht=ht: e.matmul(pk2[0:64, 0:128], lhsT=wrot[:, kc, :], rhs=ht[:, kc, :], start=(kc == 0), stop=(kc == 7)),
                                 [htB, wrotB], [pk2B])
                        P.op("dve", lambda e, pk1=pk1, tsl=tsl: e.tensor_tensor(out=r1[:], in0=pk1[0:64, 0:128], in1=cosT[:, tsl], op=ALU.mult), [pk1B, cosTB], [r1B])
                        P.op("dve", lambda e, pk2=pk2, tsl=tsl: e.tensor_tensor(out=r2[:], in0=pk2[0:64, 0:128], in1=sinT[:, tsl], op=ALU.mult), [pk2B, sinTB], [r2B])
                        P.op("pool", lambda e, tsl=tsl: e.tensor_tensor(out=kpT[:, tsl], in0=r1[:], in1=r2[:], op=ALU.add), [r1B, r2B], [kpTB])
                        c_, cB_ = cn[tt % 2], cnB[tt % 2]
                        for (lo, hi, gt_, gB_, n_) in ((0, 256, qg, qgB, 256.0), (256, 384, kg, kgB, 128.0)):
                            o4 = 0 if lo == 0 else 4
                            P.op("act", lambda e, pa=pa, lo=lo, hi=hi, o4=o4: e.activation(out=jq[:, 0:hi - lo], in_=pa[:, lo:hi], func=AF.Square, accum_out=sq[:, o4:o4 + 1]),
                                 [paB], [jqB, sqB])
                            P.op("dve", lambda e, o4=o4, n_=n_: e.tensor_scalar(out=sq[:, o4 + 1:o4 + 2], in0=sq[:, o4:o4 + 1], scalar1=1.0 / n_, scalar2=EPS, op0=ALU.mult, op1=ALU.add), [sqB], [sqB])
                            P.op("act", lambda e, o4=o4: e.activation(out=sq[:, o4 + 2:o4 + 3], in_=sq[:, o4 + 1:o4 + 2], func=AF.Sqrt), [sqB], [sqB])
                            P.op("dve", lambda e, o4=o4: e.reciprocal(out=sq[:, o4 + 3:o4 + 4], in_=sq[:, o4 + 2:o4 + 3]), [sqB], [sqB])
                            P.op("dve", lambda e, pa=pa, lo=lo, hi=hi, o4=o4, gt_=gt_, c_=c_: e.scalar_tensor_tensor(out=c_[:, lo:hi], in0=pa[:, lo:hi], scalar=sq[:, o4 + 3:o4 + 4], in1=gt_[:],
                                                                                                              op0=ALU.mult, op1=ALU.mult), [paB, sqB, gB_], [cB_])
                        p3, p3B = bank()
                        p3b = p3[:].bitcast(BF16)
                        for j in range(3):
                            P.op("pe", lambda e, p3b=p3b, j=j, c_=c_: e.transpose(out=p3b[:, j * 128:(j + 1) * 128], in_=c_[:, j * 128:(j + 1) * 128], identity=identb[:]),
                                 [cB_, identbB], [p3B])
                        P.op("act", lambda e, p3b=p3b, tsl=tsl: e.activation(out=cqnT[:, :, tsl], in_=p3b[:, 0:256].rearrange("p (k t) -> p k t", k=2), func=AF.Identity), [p3B], [cqnTB])
                        P.op("act", lambda e, p3b=p3b, tsl=tsl: e.activation(out=ckvT[:, tsl], in_=p3b[:, 256:384], func=AF.Identity), [p3B], [ckvTB])
                    P.dma("sp", T["KP"][:, :], kpT[:], reads=[kpTB], writes=[KPB])
                    P.barrier()
                with ExitStack() as s2:
                    wqn = sb(s2, "wqn", [128, 2, 8, 128], BF16); wqnB = Buf()
                    wqp = sb(s2, "wqp", [128, 2, 8, 64], BF16); wqpB = Buf()
                    wqr = sb(s2, "wqr", [128, 2, 8, 64], BF16); wqrB = Buf()
                    wkk = sb(s2, "wkk", [128, 8, 128], BF16); wkkB = Buf()
                    wkv = sb(s2, "wkv", [128, 8, 128], BF16); wkvB = Buf()
                    qv = T["mla_w_qb"][0].rearrange("(k2 p) (h c) -> p k2 h c", p=128, c=192)
                    for k2 in range(2):
                        P.dma("pool", wqn[:, k2], qv[:, k2, :, 0:128], writes=[wqnB])
                        P.dma("pool", wqp[:, k2], qv[:, k2, :, 128:192], writes=[wqpB])
                    kvv = T["mla_w_kvb"][0].rearrange("k (h c) -> k h c", c=256)
                    P.dma("pool", wkk[:], kvv[:, :, 0:128], writes=[wkkB])
                    P.dma("pool", wkv[:], kvv[:, :, 128:256], writes=[wkvB])
                    P.op("dve", lambda e: e.tensor_scalar(out=wqr[:, :, :, 0:32], in0=wqp[:, :, :, 32:64], scalar1=-1.0, scalar2=None, op0=ALU.mult), [wqpB], [wqrB])
                    P.op("dve", lambda e: e.tensor_copy(out=wqr[:, :, :, 32:64], in_=wqp[:, :, :, 0:32]), [wqpB], [wqrB])
                    qn = [sb(s2, f"qn{i}", [128, S], BF16) for i in range(2)]; qnB = [Buf(), Buf()]
                    kn = [sb(s2, f"kn{i}", [128, S], BF16) for i in range(2)]; knB = [Buf(), Buf()]
                    qp = [sb(s2, f"qp{i}", [64, S], BF16) for i in range(2)]; qpB = [Buf(), Buf()]
                    r1 = sb(s2, "q1", [64, 512], F32); r1B = Buf()
                    r2 = sb(s2, "q2", [64, 512], F32); r2B = Buf()
                    vt = [sb(s2, f"vt{i}", [128, D], BF16) for i in range(2)]; vtB = [Buf(), Buf()]
                    for tt in range(NT):
                        tsl = slice(tt * 128, (tt + 1) * 128)
                        v_, vB_ = vt[tt % 2], vtB[tt % 2]
                        for nh in range(2):
                            pt, pB = bank()
                            P.op("pe", lambda e, pt=pt, tsl=tsl, nh=nh: e.matmul(pt[:], lhsT=ckvT[:, tsl], rhs=wkv[:, nh * 4:(nh + 1) * 4, :], start=True, stop=True),
                                 [ckvTB, wkvB], [pB])
                            P.op("act", lambda e, pt=pt, nh=nh, v_=v_: e.activation(out=v_[:, nh * 512:(nh + 1) * 512], in_=pt[:], func=AF.Identity), [pB], [vB_])
                        P.dma("sp", T["V"][tsl, :], v_[:], reads=[vB_], writes=[VB])
                    for h in range(8):
                        q_, qB_ = qn[h % 2], qnB[h % 2]
                        k_, kB_ = kn[h % 2], knB[h % 2]
                        p_, pB_ = qp[h % 2], qpB[h % 2]
                        for tch in range(8):
                            csl = slice(tch * 512, (tch + 1) * 512)
                            pt, pB = bank()
                            for k2 in range(2):
                                P.op("pe", lambda e, pt=pt, k2=k2, h=h, csl=csl: e.matmul(pt[:], lhsT=wqn[:, k2, h, :], rhs=cqnT[:, k2, csl], start=(k2 == 0), stop=(k2 == 1)),
                                     [wqnB, cqnTB], [pB])
                            P.op("act", lambda e, pt=pt, q_=q_, csl=csl: e.activation(out=q_[:, csl], in_=pt[:], func=AF.Identity), [pB], [qB_])
                            pt2, pB2 = bank()
                            P.op("pe", lambda e, pt2=pt2, h=h, csl=csl: e.matmul(pt2[:], lhsT=wkk[:, h, :], rhs=ckvT[:, csl], start=True, stop=True), [wkkB, ckvTB], [pB2])
                            P.op("act", lambda e, pt2=pt2, k_=k_, csl=csl: e.activation(out=k_[:, csl], in_=pt2[:], func=AF.Identity), [pB2], [kB_])
                            pa, paB = bank()
                            for k2 in range(2):
                                P.op("pe", lambda e, pa=pa, k2=k2, h=h, csl=csl: e.matmul(pa[0:64, :], lhsT=wqp[:, k2, h, :], rhs=cqnT[:, k2, csl], start=(k2 == 0), stop=(k2 == 1)),
                                     [wqpB, cqnTB], [paB])
                            pb_, pbB = bank()
                            for k2 in range(2):
                                P.op("pe", lambda e, pb_=pb_, k2=k2, h=h, csl=csl: e.matmul(pb_[0:64, :], lhsT=wqr[:, k2, h, :], rhs=cqnT[:, k2, csl], start=(k2 == 0), stop=(k2 == 1)),
                                     [wqrB, cqnTB], [pbB])
                            P.op("dve", lambda e, pa=pa, csl=csl: e.tensor_tensor(out=r1[:], in0=pa[0:64, :], in1=cosT[:, csl], op=ALU.mult), [paB, cosTB], [r1B])
                            P.op("dve", lambda e, pb_=pb_, csl=csl: e.tensor_tensor(out=r2[:], in0=pb_[0:64, :], in1=sinT[:, csl], op=ALU.mult), [pbB, sinTB], [r2B])
                            P.op("pool", lambda e, p_=p_, csl=csl: e.tensor_tensor(out=p_[:, csl], in0=r1[:], in1=r2[:], op=ALU.add), [r1B, r2B], [pB_])
                        P.dma("sp", T["QN"][h], q_[:], reads=[qB_], writes=[QNB])
                        P.dma("sp", T["KN"][h], k_[:], reads=[kB_], writes=[KNB])
                        P.dma("sp", T["QP"][h], p_[:], reads=[pB_], writes=[QPB])
                    P.barrier()

        def phase_mla_attn(XIN, XINB, XOUT, XOUTB):
            with ExitStack() as st:
                OT = sb(st, "OT", [128, 8, S], BF16); OTB = Buf()
                with ExitStack() as s2:
                    kp = sb(s2, "kp", [64, S], BF16); kpB = Buf()
                    P.dma("sp", kp[:], T["KP"][:, :], reads=[KPB], writes=[kpB])
                    ones = sb(s2, "ones", [128, 128], BF16); onesB = Buf()
                    P.op("dve", lambda e: e.memset(ones[:], 1.0), [], [onesB])
                    qn = [sb(s2, f"aq{i}", [128, S], BF16) for i in range(2)]; qnB = [Buf(), Buf()]
                    kn = [sb(s2, f"ak{i}", [128, S], BF16) for i in range(2)]; knB = [Buf(), Buf()]
                    qp = [sb(s2, f"ap{i}", [64, S], BF16) for i in range(2)]; qpB = [Buf(), Buf()]
                    vh = [sb(s2, f"av{i}", [128, NT, 128], BF16) for i in range(2)]; vhB = [Buf(), Buf()]
                    pT = [sb(s2, f"pT{i}", [128, 512], BF16) for i in range(3)]; pTB = [Buf(), Buf(), Buf()]
                    rs = sb(s2, "rs", [128, 512], F32); rsB = Buf()
                    npt = 0
                    nst = 0
                    vv = T["V"].rearrange("(tt p) c -> p tt c", p=128)

                    def load_head(h):
                        P.dma("sp", qn[h % 2][:], T["QN"][h], reads=[QNB], writes=[qnB[h % 2]])
                        P.dma("sp", kn[h % 2][:], T["KN"][h], reads=[KNB], writes=[knB[h % 2]])
                        P.dma("sp", qp[h % 2][:], T["QP"][h], reads=[QPB], writes=[qpB[h % 2]])
                        P.dma("sp", vh[h % 2][:], vv[:, :, h * 128:(h + 1) * 128], reads=[VB], writes=[vhB[h % 2]])

                    load_head(0)
                    for h in range(8):
                        if h + 1 < 8:
                            load_head(h + 1)
                        q_, qB_ = qn[h % 2], qnB[h % 2]
                        k_, kB_ = kn[h % 2], knB[h % 2]
                        p_, pB_ = qp[h % 2], qpB[h % 2]
                        v_, vB_ = vh[h % 2], vhB[h % 2]
                        for qc in range(8):
                            csl = slice(qc * 512, (qc + 1) * 512)
                            po, poB = bank_fixed(4 + qc % 2)
                            pz, pzB = bank_fixed(6 + qc % 2)
                            def qk(kt, k_=k_, kB_=kB_, q_=q_, qB_=qB_, p_=p_, pB_=pB_, csl=csl):
                                ksl = slice(kt * 128, (kt + 1) * 128)
                                pst, pstB = bank_fixed(kt % 4)
                                P.op("pe", lambda e: e.matmul(pst[:], lhsT=k_[:, ksl], rhs=q_[:, csl], start=True, stop=False),
                                     [kB_, qB_], [pstB])
                                P.op("pe", lambda e: e.matmul(pst[:], lhsT=kp[:, ksl], rhs=p_[:, csl], start=False, stop=True),
                                     [kpB, pB_], [pstB])
                                return pst, pstB

                            pend = {0: qk(0), 1: qk(1), 2: qk(2)}
                            for kt in range(NT):
                                pst, pstB = pend.pop(kt)
                                t_, tB_ = pT[npt % 3], pTB[npt % 3]
                                npt += 1
                                P.op("act", lambda e, pst=pst, t_=t_: e.activation(out=t_[:], in_=pst[:], func=AF.Exp, scale=SCALE), [pstB], [tB_])
                                if kt + 3 < NT:
                                    pend[kt + 3] = qk(kt + 3)
                                P.op("pe", lambda e, po=po, kt=kt, t_=t_, v_=v_: e.matmul(po[:], lhsT=v_[:, kt, :], rhs=t_[:], start=(kt == 0), stop=(kt == NT - 1)),
                                     [vB_, tB_], [poB])
                                P.op("pe", lambda e, pz=pz, kt=kt, t_=t_: e.matmul(pz[:], lhsT=ones[:], rhs=t_[:], start=(kt == 0), stop=(kt == NT - 1)),
                                     [onesB, tB_], [pzB])
                            P.op("dve", lambda e, pz=pz: e.reciprocal(out=rs[:], in_=pz[:]), [pzB], [rsB])
                            P.op("dve", lambda e, po=po, h=h, csl=csl: e.tensor_tensor(out=OT[:, h, csl], in0=po[:], in1=rs[:], op=ALU.mult), [poB, rsB], [OTB])
                    P.barrier()
                with ExitStack() as s2:
                    wo = sb(s2, "mwo", [128, 8, D], BF16); woB = Buf()
                    wv = T["mla_w_out"][0].rearrange("(kc p) n -> p kc n", p=128)
                    for q in range(2):
                        P.dma("pool", wo[:, q * 4:(q + 1) * 4, :], wv[:, q * 4:(q + 1) * 4, :], writes=[woB])
                    _, _, (G, GB) = load_mod_tiles(s2, 1, 0, "norm_mix_g")
                    xb = [sb(s2, f"bx{i}", [128, D], F32) for i in range(2)]; xbB = [Buf(), Buf()]
                    yo = [sb(s2, f"by{i}", [128, D], F32) for i in range(2)]; yoB = [Buf(), Buf()]
                    P.dma("sp", xb[0][:], XIN[0:128, :], reads=[XINB], writes=[xbB[0]])
                    for tt in range(NT):
                        tsl = slice(tt * 128, (tt + 1) * 128)
                        xt, xtB = xb[tt % 2], xbB[tt % 2]
                        y, yB = yo[tt % 2], yoB[tt % 2]
                        if tt + 1 < NT:
                            P.dma("sp", xb[(tt + 1) % 2][:], XIN[(tt + 1) * 128:(tt + 2) * 128, :], reads=[XINB], writes=[xbB[(tt + 1) % 2]])
                        for nh in range(2):
                            pt, pB = bank()
                            for hh in range(8):
                                P.op("pe", lambda e, pt=pt, hh=hh, tsl=tsl, nh=nh: e.matmul(pt[:], lhsT=OT[:, hh, tsl], rhs=wo[:, hh, nh * 512:(nh + 1) * 512], start=(hh == 0), stop=(hh == 7)),
                                     [OTB, woB], [pB])
                            sl = slice(nh * 512, (nh + 1) * 512)
                            P.op("dve", lambda e, pt=pt, sl=sl, y=y: e.tensor_tensor(out=y[:, sl], in0=pt[:], in1=G[:, sl], op=ALU.mult), [pB, GB], [yB])
                        P.op("pool", lambda e, y=y, xt=xt: e.tensor_tensor(out=y[:], in0=y[:], in1=xt[:], op=ALU.add), [yB, xtB], [yB])
                        P.dma("sp", XOUT[tsl, :], y[:], reads=[yB], writes=[XOUTB])
                    P.barrier()

        def copy_to_out(src, srcB):
            with ExitStack() as st:
                cb = [sb(st, f"cpy{i}", [128, D], F32) for i in range(2)]; cbB = [Buf(), Buf()]
                for tt in range(NT):
                    t_, tB = cb[tt % 2], cbB[tt % 2]
                    P.dma("sp", t_[:], src[tt * 128:(tt + 1) * 128, :], reads=[srcB], writes=[tB])
                    P.dma("sp", OUT[tt * 128:(tt + 1) * 128, :], t_[:], reads=[tB], writes=[OUTB])
                P.barrier()

        OUTB = Buf()

        phase_mod()
        phase_filter()
        phase_hyena_in()
        phase_hyena_fwd()
        phase_hyena_out()
        if stage == "hyena":
            copy_to_out(T["XA"], XAB)
        else:
            XBB = Buf()
            phase_moe(0, T["XA"], XAB, T["XB"], XBB, False)
            if stage == "moe0":
                copy_to_out(T["XB"], XBB)
            else:
                XCB = Buf()
                phase_mla_proj(T["XB"], XBB)
                phase_mla_attn(T["XB"], XBB, T["XC"], XCB)
                if stage == "mla":
                    copy_to_out(T["XC"], XCB)
                else:
                    phase_moe(1, T["XC"], XCB, OUT, OUTB, True)
        P.barrier()
    return nc


def _prep_inputs(inputs):
    global _CONSTS
    if _CONSTS is None:
        _CONSTS = _consts()
    shared = {}
    for k, v in inputs.items():
        if k in ("x", "c", "positions"):
            continue
        shared[k] = np.ascontiguousarray(np.asarray(v))
    shared.update(_CONSTS)
    in_maps = []
    x = np.asarray(inputs["x"]); c = np.asarray(inputs["c"]); pos = np.asarray(inputs["positions"])
    for b in range(x.shape[0]):
        m = dict(shared)
        m["x"] = np.ascontiguousarray(x[b])
        m["c"] = np.ascontiguousarray(c[b:b + 1])
        m["pos"] = np.ascontiguousarray(pos[b].astype(np.int32))
        in_maps.append(m)
    return in_maps


def kernel(**inputs):
    in_maps = _prep_inputs(inputs)
    nc = build("all")
    res = run_bass_kernel_spmd(nc, in_maps, core_ids=list(range(len(in_maps))))
    return np.stack([r["out"] for r in res.results], axis=0).astype(np.float32)
```

```python
import math
from contextlib import ExitStack

import ml_dtypes
import numpy as np

import concourse.bass as bass
import concourse.mybir as mybir
from concourse.bass_utils import run_bass_kernel_spmd

F32 = mybir.dt.float32
BF16 = mybir.dt.bfloat16
I32 = mybir.dt.int32
U32 = mybir.dt.uint32
AF = mybir.ActivationFunctionType
ALU = mybir.AluOpType
AX = mybir.AxisListType

S = 4096
D = 1024
NT = S // 128
EPS = 1e-6
NEXP = 16
CAP = 512
TWO_PI = 2.0 * math.pi


class Buf:
    __slots__ = ("w", "r")

    def __init__(self):
        self.w = None
        self.r = []


class Prog:
    NQ = 6

    def __init__(self, nc, es):
        self.nc = nc
        self.es = es
        self.eng = {"pe": nc.tensor, "act": nc.scalar, "dve": nc.vector,
                    "pool": nc.gpsimd, "sp": nc.sync}
        self.sem = {}
        self.cnt = {}
        self.nsem = 0
        for e in ("pe", "act", "dve", "pool"):
            self.sem[e] = self._newsem()
            self.cnt[e] = 0
        self.waited = {e: {} for e in self.eng}
        self.dq = {}
        self.dqi = {}
        for q in ("sp", "act", "pool"):
            self.dq[q] = [[self._newsem(), 0] for _ in range(16 if q == "pool" else self.NQ)]
            self.dqi[q] = 0

    def _newsem(self):
        self.nsem += 1
        return self.es.enter_context(self.nc.semaphore(f"sm{self.nsem}"))

    def _wait(self, e, tok):
        sem, val, src = tok
        if src == "pe" and e == "pe":
            return
        key = id(sem)
        if self.waited[e].get(key, 0) >= val:
            return
        self.eng[e].wait_ge(sem, val)
        self.waited[e][key] = val

    def _deps(self, e, reads, writes):
        for b in reads:
            if b.w is not None:
                self._wait(e, b.w)
        for b in writes:
            if b.w is not None:
                self._wait(e, b.w)
            for t in b.r:
                self._wait(e, t)

    def _record(self, tok, reads, writes):
        for b in reads:
            b.r = [t for t in b.r if t[0] is not tok[0]]
            b.r.append(tok)
        for b in writes:
            b.w = tok
            b.r = []

    def op(self, e, fn, reads=(), writes=()):
        self._deps(e, reads, writes)
        ins = fn(self.eng[e])
        self.cnt[e] += 1
        ins.then_inc(self.sem[e], 1)
        tok = (self.sem[e], self.cnt[e], e)
        self._record(tok, reads, writes)
        return tok

    def dma(self, q, out, in_, reads=(), writes=(), fn=None, **kw):
        slot = self.dq[q][self.dqi[q] % len(self.dq[q])]
        self.dqi[q] += 1
        sem, c = slot
        if c > 0:
            self._wait(q, (sem, c, None))
        self._deps(q, reads, writes)
        if fn is None:
            ins = self.eng[q].dma_start(out=out, in_=in_, **kw)
        else:
            ins = fn(self.eng[q])
        ins.then_inc(sem, 16)
        slot[1] = c + 16
        tok = (sem, c + 16, None)
        self._record(tok, reads, writes)
        return tok

    def barrier(self):
        toks = [(self.sem[e], self.cnt[e], None) for e in self.sem if self.cnt[e] > 0]
        for q in self.dq:
            for sem, c in self.dq[q]:
                if c > 0:
                    toks.append((sem, c, None))
        for e in self.eng:
            for t in toks:
                self._wait(e, t)
        for e in self.sem:
            if self.cnt[e] > 6000:
                self.sem[e] = self._newsem()
                self.cnt[e] = 0
        for q in self.dq:
            for slot in self.dq[q]:
                if slot[1] > 6000:
                    slot[0] = self._newsem()
                    slot[1] = 0


def _consts():
    c = {}
    c["ident"] = np.eye(128, dtype=np.float32)
    L = S
    t = np.linspace(0.0, 1.0, L, dtype=np.float32)[:, None]
    w = (2.0 * math.pi * np.arange(L, dtype=np.float32)[:, None] / L).astype(np.float32)
    f = np.linspace(1e-4, 15.0, 16, dtype=np.float32)[None, :]
    ang = (f * w).astype(np.float32)
    z = np.concatenate([t, np.cos(ang), -np.sin(ang)], axis=-1).astype(np.float32)
    c["zfT"] = np.ascontiguousarray(z.T)
    c["tneg"] = np.ascontiguousarray((-t[:, 0]).reshape(NT, 128).T)
    mind = math.log(1e-2) / 0.3
    maxd = math.log(1e-2) / 1.5
    deltas = np.linspace(mind, maxd, D, dtype=np.float32)
    c["absd"] = np.abs(deltas).astype(np.float32)
    fi = np.arange(4096, dtype=np.int64)
    ti = np.arange(4096, dtype=np.int64)
    m = ((2 * fi[None, :] + 1) * ti[:, None]) % 16384
    angm = m.astype(np.float64) * (math.pi / 8192.0)
    cosm = np.cos(angm)
    sinm = np.sin(angm)
    fw = np.stack([cosm, -sinm], axis=0)
    fw = fw.reshape(2, NT, 128, 32, 128)
    c["FWD"] = np.ascontiguousarray(fw.transpose(3, 2, 0, 1, 4)).astype(ml_dtypes.bfloat16)
    sc = 2.0 / 8192.0
    iv = np.stack([cosm * sc, -sinm * sc], axis=0)
    iv = iv.reshape(2, NT, 128, 32, 128)
    c["INV"] = np.ascontiguousarray(iv.transpose(1, 4, 3, 0, 2)).astype(ml_dtypes.bfloat16)
    invf = (10000.0 ** (-np.arange(0, 64, 2, dtype=np.float32) / 64)).astype(np.float32)
    c["invf"] = invf
    return c


_CONSTS = None


def build(stage="all", debug=False):
    nc = bass.Bass("TRN2", target_bir_lowering=False)
    T = {}

    def din(name, shape, dt=F32):
        T[name] = nc.dram_tensor(name, list(shape), dt, kind="ExternalInput").ap()
        return T[name]

    def dscr(name, shape, dt=F32):
        T[name] = nc.dram_tensor(name, list(shape), dt).ap()
        return T[name]

    din("x", [S, D]); din("c", [1, D]); din("pos", [S], I32)
    din("ada_w", [2, D, 6 * D]); din("ada_b", [2, 6 * D])
    din("norm_mix_g", [2, D]); din("norm_ffn_g", [2, D])
    din("hy_w_in", [1, D, 3 * D]); din("hy_b_in", [1, 3 * D]); din("hy_conv_w", [1, 3, 3 * D])
    din("hy_conv_b", [1, 3 * D]); din("hy_f_w1", [1, 33, 64]); din("hy_f_b1", [1, 64])
    din("hy_f_w2", [1, 64, 64]); din("hy_f_b2", [1, 64]); din("hy_f_w3", [1, 64, 2 * D])
    din("hy_f_freq", [1, 64]); din("hy_f_bias", [1, 1, D]); din("hy_w_out", [1, D, D]); din("hy_b_out", [1, D])
    din("mla_w_in", [1, D, 448]); din("mla_q_norm_g", [1, 256]); din("mla_w_qb", [1, 256, 1536])
    din("mla_kv_norm_g", [1, 128]); din("mla_w_kvb", [1, 128, 2048]); din("mla_w_out", [1, D, D])
    din("moe_w_router", [2, D, NEXP]); din("moe_w_gate", [2, NEXP, D, D]); din("moe_w_up", [2, NEXP, D, D])
    din("moe_w_down", [2, NEXP, D, D]); din("final_norm_g", [D])
    din("ident", [128, 128]); din("zfT", [33, S]); din("tneg", [128, NT]); din("absd", [D])
    din("FWD", [32, 128, 2, 32, 128], BF16); din("INV", [32, 128, 32, 2, 128], BF16); din("invf", [32])
    OUT = nc.dram_tensor("out", [S, D], F32, kind="ExternalOutput").ap()

    dscr("MOD", [2, 6 * D])
    dscr("ZT", [S, D], BF16)
    dscr("X0C", [D, S], BF16)
    dscr("KS", [2, S, D])
    dscr("KT", [2, S, D], BF16)
    dscr("YS", [32, 128, 2, D], BF16)
    dscr("XA", [S, D]); dscr("XB", [S, D]); dscr("XC", [S, D])
    dscr("HF", [S, D], BF16); dscr("ACC", [S, D])
    dscr("QN", [8, 128, S], BF16); dscr("KN", [8, 128, S], BF16); dscr("QP", [8, 64, S], BF16)
    dscr("KP", [64, S], BF16); dscr("V", [S, D], BF16)

    es = ExitStack()
    with es:
        P = Prog(nc, es)
        ps = [es.enter_context(nc.psum_tensor(f"ps{i}", [128, 512], F32)) for i in range(8)]
        psB = [Buf() for _ in range(8)]
        psi = [0]

        def bank():
            i = psi[0] % 8
            psi[0] += 1
            return ps[i], psB[i]

        sbn = [0]

        def sb(st, name, shape, dt):
            sbn[0] += 1
            return st.enter_context(nc.sbuf_tensor(f"{name}_{sbn[0]}", list(shape), dt))

        identf = sb(es, "identf", [128, 128], F32); identfB = Buf()
        identb = sb(es, "identb", [128, 128], BF16); identbB = Buf()
        P.dma("sp", identf[:], T["ident"][:, :], writes=[identfB])
        P.dma("pool", identb[:], T["ident"][:, :], writes=[identbB])

        def bcast_load(q, dst, dstB, src1d):
            return P.dma(q, dst, src1d.partition_broadcast(128), writes=[dstB])

        def phase_mod():
            with ExitStack() as st:
                ccol = sb(st, "ccol", [128, 8], F32); ccolB = Buf()
                cs = sb(st, "cs", [128, 8], F32); csB_ = Buf()
                csb = sb(st, "csb", [128, 8, 128], F32); csbB = Buf()
                adb = sb(st, "adb", [128, 6 * D], F32); adbB = Buf()
                modt = sb(st, "modt", [128, 6 * D], F32); modtB = Buf()
                wb = [sb(st, f"adw{i}", [128, 8, 512], F32) for i in range(2)]
                wbB = [Buf(), Buf()]
                P.dma("sp", ccol[:], T["c"].rearrange("o (kc p) -> p (o kc)", p=128), writes=[ccolB],
                      allow_slow_non_contiguous=True)
                P.op("act", lambda e: e.activation(out=cs[:], in_=ccol[:], func=AF.Silu), [ccolB], [csB_])
                for kc in range(8):
                    P.op("dve", lambda e, kc=kc: e.tensor_copy(out=csb[:, kc, :], in_=cs[:, kc:kc + 1].to_broadcast([128, 128])),
                         [csB_], [csbB])
                n = 0
                for i in range(2):
                    bcast_load("sp", adb[:], adbB, T["ada_b"][i, :])
                    wv = T["ada_w"][i].rearrange("(kc p) n -> p kc n", p=128)
                    for q in range(12):
                        w, wB = wb[n % 2], wbB[n % 2]
                        n += 1
                        P.dma("sp", w[:], wv[:, :, q * 512:(q + 1) * 512], writes=[wB])
                        pt, pB = bank()
                        for kc in range(8):
                            P.op("pe", lambda e, kc=kc, w=w, pt=pt: e.matmul(pt[:], lhsT=csb[:, kc, :], rhs=w[:, kc, :],
                                                                              start=(kc == 0), stop=(kc == 7)),
                                 [csbB, wB], [pB])
                        P.op("dve", lambda e, q=q, pt=pt: e.tensor_tensor(out=modt[:, q * 512:(q + 1) * 512], in0=pt[:],
                                                                         in1=adb[:, q * 512:(q + 1) * 512], op=ALU.add),
                             [pB, adbB], [modtB])
                    P.dma("sp", T["MOD"][i:i + 1, :], modt[0:1, :], reads=[modtB], writes=[MODB])
                P.barrier()

        MODB = Buf()

        def load_mod_tiles(st, layer, which, gname):
            base = 0 if which == 0 else 3
            A = sb(st, f"A{layer}{which}", [128, D], F32); AB = Buf()
            Bt = sb(st, f"B{layer}{which}", [128, D], F32); BB = Buf()
            G = sb(st, f"G{layer}{which}", [128, D], F32); GB = Buf()
            gt = sb(st, f"g{layer}{which}", [128, D], F32); gB = Buf()
            P.dma("sp", Bt[:], T["MOD"][layer, (base + 0) * D:(base + 1) * D].partition_broadcast(128), reads=[MODB], writes=[BB])
            P.dma("sp", A[:], T["MOD"][layer, (base + 1) * D:(base + 2) * D].partition_broadcast(128), reads=[MODB], writes=[AB])
            P.dma("sp", G[:], T["MOD"][layer, (base + 2) * D:(base + 3) * D].partition_broadcast(128), reads=[MODB], writes=[GB])
            bcast_load("sp", gt[:], gB, T[gname][layer, :])
            P.op("dve", lambda e: e.scalar_tensor_tensor(out=A[:], in0=A[:], scalar=1.0, in1=gt[:], op0=ALU.add, op1=ALU.mult),
                 [gB, AB], [AB])
            return (A, AB), (Bt, BB), (G, GB)

        class NormCtx:
            def __init__(self, st, tag):
                self.junk = sb(st, f"junk{tag}", [128, D], F32); self.junkB = Buf()
                self.ss = [sb(st, f"ss{tag}{i}", [128, 4], F32) for i in range(2)]; self.ssB = [Buf(), Buf()]
                self.tmp = [sb(st, f"nt{tag}{i}", [128, D], F32) for i in range(2)]; self.tmpB = [Buf(), Buf()]
                self.n = 0

        def norm_mod_g(nx, xt, xtB, A, AB, Bt, BB, out, outB):
            k = nx.n % 2
            nx.n += 1
            ss, ssB = nx.ss[k], nx.ssB[k]
            tmp, tmpB = nx.tmp[k][:], nx.tmpB[k]
            P.op("act", lambda e: e.activation(out=nx.junk[:], in_=xt, func=AF.Square, accum_out=ss[:, 0:1]),
                 [xtB], [nx.junkB, ssB])
            yield
            P.op("dve", lambda e: e.tensor_scalar(out=ss[:, 1:2], in0=ss[:, 0:1], scalar1=1.0 / D, scalar2=EPS,
                                                  op0=ALU.mult, op1=ALU.add), [ssB], [ssB])
            yield
            P.op("act", lambda e: e.activation(out=ss[:, 2:3], in_=ss[:, 1:2], func=AF.Sqrt), [ssB], [ssB])
            yield
            P.op("dve", lambda e: e.reciprocal(out=ss[:, 3:4], in_=ss[:, 2:3]), [ssB], [ssB])
            yield
            if Bt is None:
                P.op("dve", lambda e: e.scalar_tensor_tensor(out=out, in0=xt, scalar=ss[:, 3:4], in1=A[:],
                                                             op0=ALU.mult, op1=ALU.mult), [xtB, ssB, AB], [outB])
                return
            P.op("dve", lambda e: e.scalar_tensor_tensor(out=tmp, in0=xt, scalar=ss[:, 3:4], in1=A[:],
                                                         op0=ALU.mult, op1=ALU.mult), [xtB, ssB, AB], [tmpB])
            yield
            P.op("pool", lambda e: e.tensor_tensor(out=out, in0=tmp, in1=Bt[:], op=ALU.add), [tmpB, BB], [outB])

        def norm_mod(nx, xt, xtB, A, AB, Bt, BB, out, outB, tmp_unused=None, tmpB_unused=None):
            for _ in norm_mod_g(nx, xt, xtB, A, AB, Bt, BB, out, outB):
                pass

        def run_pipelined(n, make, depth=2, on_start=None):
            active = []
            nxt = 0
            while nxt < n or active:
                while len(active) < depth and nxt < n:
                    if on_start is not None:
                        on_start(nxt)
                    active.append(make(nxt))
                    nxt += 1
                for g in list(active):
                    try:
                        next(g)
                    except StopIteration:
                        active.remove(g)

        def fwd_dft(st, A, AB_, Bm, BB_, consume):
            fwb = [sb(st, f"fw{i}", [128, 2, 32, 128], BF16) for i in range(2)]
            fwB = [Buf(), Buf()]
            def load_fw(fc):
                fw, fB = fwb[fc % 2], fwB[fc % 2]
                P.dma("sp", fw[:, 0], T["FWD"][fc, :, 0], writes=[fB])
                P.dma("sp", fw[:, 1], T["FWD"][fc, :, 1], writes=[fB])

            load_fw(0)
            for fc in range(32):
                fw, fB = fwb[fc % 2], fwB[fc % 2]
                if fc + 1 < 32:
                    load_fw(fc + 1)
                res = []
                for h in range(2):
                    for cs_ in range(2):
                        src, sB = (A, AB_) if cs_ == 0 else (Bm, BB_)
                        pt, pB = bank()
                        for tt in range(32):
                            P.op("pe", lambda e, pt=pt, fw=fw, cs_=cs_, tt=tt, src=src, h=h:
                                 e.matmul(pt[:], lhsT=fw[:, cs_, tt, :], rhs=src[:, tt, h * 512:(h + 1) * 512],
                                          start=(tt == 0), stop=(tt == 31)), [fB, sB], [pB])
                        res.append((pt, pB))
                consume(fc, res)

        def phase_filter():
            with ExitStack() as st:
                KTB = Buf()
                with ExitStack() as s2:
                    kab = [sb(s2, f"kab{i}", [128, 2, D], BF16) for i in range(2)]; kabB = [Buf(), Buf()]
                    zf = sb(s2, "zf", [33, S], F32); zfB = Buf()
                    h1 = sb(s2, "h1", [64, S], F32); h1B = Buf()
                    h2 = sb(s2, "h2", [64, S], F32); h2B = Buf()
                    w1 = sb(s2, "fw1", [33, 64], F32); w1B = Buf()
                    w2 = sb(s2, "fw2", [64, 64], F32); w2B = Buf()
                    w3 = sb(s2, "fw3", [64, 2 * D], F32); w3B = Buf()
                    col = sb(s2, "fcol", [64, 8], F32); colB = Buf()
                    pre = sb(s2, "fpre", [64, 512], F32); preB = Buf()
                    ki = sb(s2, "fki", [64, 512], I32); kiB = Buf()
                    kf = sb(s2, "fkf", [64, 512], F32); kfB = Buf()
                    absd = sb(s2, "absd", [128, D], F32); absdB = Buf()
                    tneg = sb(s2, "tneg", [128, NT], F32); tnegB = Buf()
                    dec = sb(s2, "dec", [128, D], F32); decB = Buf()
                    kfw = sb(s2, "kfw", [128, D], F32); kfwB = Buf()
                    kbw = sb(s2, "kbw", [128, D], F32); kbwB = Buf()
                    fbias = sb(s2, "fbias", [1, D], F32); fbiasB = Buf()
                    P.dma("sp", zf[:], T["zfT"][:, :], writes=[zfB])
                    P.dma("sp", w1[:], T["hy_f_w1"][0], writes=[w1B])
                    P.dma("sp", w2[:], T["hy_f_w2"][0], writes=[w2B])
                    P.dma("sp", w3[:], T["hy_f_w3"][0], writes=[w3B])
                    P.dma("sp", col[:, 0:1], T["hy_f_b1"][0].rearrange("(p o) -> p o", o=1), writes=[colB])
                    P.dma("sp", col[:, 1:2], T["hy_f_b2"][0].rearrange("(p o) -> p o", o=1), writes=[colB])
                    P.dma("sp", col[:, 2:3], T["hy_f_freq"][0].rearrange("(p o) -> p o", o=1), writes=[colB])
                    P.dma("sp", tneg[:], T["tneg"][:, :], writes=[tnegB])
                    P.dma("sp", fbias[:], T["hy_f_bias"][0], writes=[fbiasB])
                    bcast_load("sp", absd[:], absdB, T["absd"])
                    P.op("dve", lambda e: e.tensor_tensor(out=col[:, 3:4], in0=col[:, 0:1], in1=col[:, 2:3], op=ALU.mult), [colB], [colB])
                    P.op("dve", lambda e: e.tensor_tensor(out=col[:, 4:5], in0=col[:, 1:2], in1=col[:, 2:3], op=ALU.mult), [colB], [colB])

                    def sin_layer(w, wB, src, srcB, K, bcol, dst, dstB):
                        for tch in range(8):
                            pt, pB = bank()
                            P.op("pe", lambda e, pt=pt, tch=tch: e.matmul(pt[0:64, :], lhsT=w[0:K, :], rhs=src[0:K, tch * 512:(tch + 1) * 512],
                                                                          start=True, stop=True), [wB, srcB], [pB])
                            P.op("dve", lambda e, pt=pt: e.tensor_scalar(out=pre[:], in0=pt[0:64, :], scalar1=col[:, 2:3], scalar2=col[:, bcol:bcol + 1],
                                                                         op0=ALU.mult, op1=ALU.add), [pB, colB], [preB])
                            P.op("dve", lambda e: e.tensor_scalar(out=ki[:], in0=pre[:], scalar1=1.0 / TWO_PI, scalar2=None, op0=ALU.mult), [preB], [kiB])
                            P.op("dve", lambda e: e.tensor_copy(out=kf[:], in_=ki[:]), [kiB], [kfB])
                            P.op("dve", lambda e: e.scalar_tensor_tensor(out=pre[:], in0=kf[:], scalar=-TWO_PI, in1=pre[:], op0=ALU.mult, op1=ALU.add),
                                 [kfB, preB], [preB])
                            P.op("dve", lambda e: e.tensor_scalar(out=pre[:], in0=pre[:], scalar1=math.pi, scalar2=-math.pi, op0=ALU.min, op1=ALU.max), [preB], [preB])
                            P.op("act", lambda e, tch=tch: e.activation(out=dst[:, tch * 512:(tch + 1) * 512], in_=pre[:], func=AF.Sin), [preB], [dstB])

                    sin_layer(w1, w1B, zf, zfB, 33, 3, h1, h1B)
                    sin_layer(w2, w2B, h1, h1B, 64, 4, h2, h2B)
                    for tt in range(NT):
                        P.op("act", lambda e, tt=tt: e.activation(out=dec[:], in_=absd[:], func=AF.Exp, scale=tneg[:, tt:tt + 1]), [absdB, tnegB], [decB])
                        for q in range(4):
                            pt, pB = bank()
                            P.op("pe", lambda e, pt=pt, tt=tt, q=q: e.matmul(pt[:], lhsT=h2[:, tt * 128:(tt + 1) * 128], rhs=w3[:, q * 512:(q + 1) * 512],
                                                                             start=True, stop=True), [h2B, w3B], [pB])
                            dst, dB = (kfw, kfwB) if q < 2 else (kbw, kbwB)
                            P.op("dve", lambda e, pt=pt, q=q, dst=dst: e.tensor_tensor(out=dst[:, (q % 2) * 512:(q % 2 + 1) * 512], in0=pt[:],
                                                                                       in1=dec[:, (q % 2) * 512:(q % 2 + 1) * 512], op=ALU.mult),
                                 [pB, decB], [dB])
                        if tt == 0:
                            P.op("dve", lambda e: e.tensor_tensor(out=kfw[0:1, :], in0=kfw[0:1, :], in1=fbias[0:1, :], op=ALU.add), [kfwB, fbiasB], [kfwB])
                            P.op("dve", lambda e: e.memset(kbw[0:1, :], 0.0), [], [kbwB])
                        ka, kaB = kab[tt % 2], kabB[tt % 2]
                        P.op("pool", lambda e, ka=ka: e.tensor_tensor(out=ka[:, 0, :], in0=kfw[:], in1=kbw[:], op=ALU.add), [kfwB, kbwB], [kaB])
                        P.op("dve", lambda e, ka=ka: e.tensor_tensor(out=ka[:, 1, :], in0=kfw[:], in1=kbw[:], op=ALU.subtract), [kfwB, kbwB], [kaB])
                        for j in range(2):
                            P.dma("sp", T["KT"][j, tt * 128:(tt + 1) * 128, :], ka[:, j, :], reads=[kaB], writes=[KTB])
                    P.barrier()
                with ExitStack() as s3:
                    KA = sb(s3, "KA", [128, NT, D], BF16); KAB = Buf()
                    KBm = sb(s3, "KBm", [128, NT, D], BF16); KBB = Buf()
                    for j, (kt, ktB) in enumerate(((KA, KAB), (KBm, KBB))):
                        kv = T["KT"][j].rearrange("(tt p) c -> p tt c", p=128)
                        for q in range(4):
                            P.dma("sp", kt[:, q * 8:(q + 1) * 8, :], kv[:, q * 8:(q + 1) * 8, :], reads=[KTB], writes=[ktB])
                    stg = [sb(s3, f"kst{i}", [128, 2, D], F32) for i in range(2)]
                    stgB = [Buf(), Buf()]

                    def store(fc, res):
                        sg, sB = stg[fc % 2], stgB[fc % 2]
                        for h in range(2):
                            for cs_ in range(2):
                                pt, pB = res[h * 2 + cs_]
                                P.op("act", lambda e, pt=pt, h=h, cs_=cs_, sg=sg: e.activation(out=sg[:, cs_, h * 512:(h + 1) * 512], in_=pt[:], func=AF.Identity),
                                     [pB], [sB])
                        for cs_ in range(2):
                            P.dma("sp", T["KS"][cs_, fc * 128:(fc + 1) * 128, :], sg[:, cs_, :], reads=[sB], writes=[KSB])

                    fwd_dft(s3, KA, KAB, KBm, KBB, store)
                    P.barrier()

        KSB = Buf()
        ZTB = Buf(); X0CB = Buf(); YSB = Buf(); XAB = Buf()

        def phase_hyena_in():
            with ExitStack() as st:
                hmT = sb(st, "hmT", [128, 8, S], BF16); hmTB = Buf()
                with ExitStack() as s2:
                    (A, AB), (Bt, BB), _ = load_mod_tiles(s2, 0, 0, "norm_mix_g")
                    nx = NormCtx(s2, "h")
                    xb = [sb(s2, f"hx{i}", [128, D], F32) for i in range(3)]; xbB = [Buf(), Buf(), Buf()]
                    hb = [sb(s2, f"hb{i}", [128, D], BF16) for i in range(2)]; hbB = [Buf(), Buf()]

                    def ld_hx(tt):
                        if tt < NT:
                            P.dma("sp", xb[tt % 3][:], T["x"][tt * 128:(tt + 1) * 128, :], writes=[xbB[tt % 3]])

                    ld_hx(0)

                    def h_tile(tt):
                        xt, xtB = xb[tt % 3], xbB[tt % 3]
                        h_, hB_ = hb[tt % 2], hbB[tt % 2]
                        yield from norm_mod_g(nx, xt[:], xtB, A, AB, Bt, BB, h_[:], hB_)
                        yield
                        pt, pB = bank()
                        ptb = pt[:].bitcast(BF16)
                        for kc in range(8):
                            P.op("pe", lambda e, kc=kc: e.transpose(out=ptb[:, kc * 128:(kc + 1) * 128], in_=h_[:, kc * 128:(kc + 1) * 128], identity=identb[:]),
                                 [hB_, identbB], [pB])
                        yield
                        P.op("act", lambda e: e.activation(out=hmT[:, :, tt * 128:(tt + 1) * 128], in_=ptb.rearrange("p (k t) -> p k t", k=8), func=AF.Identity),
                             [pB], [hmTB])

                    run_pipelined(NT, h_tile, depth=2, on_start=lambda tt: ld_hx(tt + 1))
                    P.barrier()
                with ExitStack() as s2:
                    bcol = sb(s2, "bcol", [128, 24], F32); bcolB = Buf()
                    cw = sb(s2, "cw", [128, 3, 24], F32); cwB = Buf()
                    cbc = sb(s2, "cbc", [128, 24], F32); cbcB = Buf()
                    P.dma("sp", bcol[:], T["hy_b_in"][0].rearrange("(cc p) -> p cc", p=128), writes=[bcolB], allow_slow_non_contiguous=True)
                    P.dma("sp", cbc[:], T["hy_conv_b"][0].rearrange("(cc p) -> p cc", p=128), writes=[cbcB], allow_slow_non_contiguous=True)
                    for j in range(3):
                        P.dma("sp", cw[:, j, :], T["hy_conv_w"][0, j].rearrange("(cc p) -> p cc", p=128), writes=[cwB], allow_slow_non_contiguous=True)
                    wch = [sb(s2, f"wch{i}", [128, 8, 128], BF16) for i in range(2)]; wchB = [Buf(), Buf()]
                    us = [sb(s2, f"u{i}", [128, S + 2], F32) for i in range(2)]; usB = [Buf(), Buf()]
                    ob = [sb(s2, f"ob{i}", [128, S], F32) for i in range(2)]; obB = [Buf(), Buf()]
                    zb = sb(s2, "zb", [128, S], BF16); zbB = Buf()
                    zst = sb(s2, "zst", [128, NT, 128], BF16); zstB = Buf()
                    for u, uB in zip(us, usB):
                        P.op("dve", lambda e, u=u: e.memset(u[:, 0:1], 0.0), [], [uB])
                        P.op("dve", lambda e, u=u: e.memset(u[:, S + 1:S + 2], 0.0), [], [uB])
                    wv = T["hy_w_in"][0].rearrange("(kc p) n -> p kc n", p=128)
                    order = []
                    for j in range(8):
                        order += [8 + j, 16 + j]
                    order += list(range(8))
                    for n, cc in enumerate(order):
                        w, wB = wch[n % 2], wchB[n % 2]
                        u, uB = us[n % 2], usB[n % 2]
                        P.dma("pool", w[:], wv[:, :, cc * 128:(cc + 1) * 128], writes=[wB])
                        for tch in range(8):
                            pt, pB = bank()
                            for kc in range(8):
                                P.op("pe", lambda e, pt=pt, kc=kc, w=w, tch=tch: e.matmul(pt[:], lhsT=w[:, kc, :], rhs=hmT[:, kc, tch * 512:(tch + 1) * 512],
                                                                                         start=(kc == 0), stop=(kc == 7)), [wB, hmTB], [pB])
                            P.op("act", lambda e, pt=pt, tch=tch, cc=cc, u=u: e.activation(out=u[:, 1 + tch * 512:1 + (tch + 1) * 512], in_=pt[:], func=AF.Identity,
                                                                                    bias=bcol[:, cc:cc + 1]), [pB, bcolB], [uB])
                        o, oB = ob[n % 2], obB[n % 2]
                        P.op("act", lambda e, o=o, cc=cc, u=u: e.activation(out=o[:], in_=u[:, 1:S + 1], func=AF.Identity, scale=cw[:, 1, cc:cc + 1], bias=cbc[:, cc:cc + 1]),
                             [uB, cwB, cbcB], [oB])
                        P.op("dve", lambda e, o=o, cc=cc, u=u: e.scalar_tensor_tensor(out=o[:], in0=u[:, 0:S], scalar=cw[:, 0, cc:cc + 1], in1=o[:], op0=ALU.mult, op1=ALU.add),
                             [uB, cwB, oB], [oB])
                        if cc >= 8:
                            P.op("dve", lambda e, o=o, cc=cc, u=u: e.scalar_tensor_tensor(out=o[:], in0=u[:, 2:S + 2], scalar=cw[:, 2, cc:cc + 1], in1=o[:], op0=ALU.mult, op1=ALU.add),
                                 [uB, cwB, oB], [oB])
                        else:
                            P.op("dve", lambda e, o=o, cc=cc, u=u: e.scalar_tensor_tensor(out=zb[:], in0=u[:, 2:S + 2], scalar=cw[:, 2, cc:cc + 1], in1=o[:], op0=ALU.mult, op1=ALU.add),
                                 [uB, cwB, oB], [zbB])
                            P.dma("sp", T["X0C"][cc * 128:(cc + 1) * 128, :], zb[:], reads=[zbB], writes=[X0CB])
                        if cc >= 16:
                            j = cc - 16
                            o1, o1B = ob[(n - 1) % 2], obB[(n - 1) % 2]
                            P.op("pool", lambda e, o=o, o1=o1: e.tensor_tensor(out=zb[:], in0=o[:], in1=o1[:], op=ALU.mult), [oB, o1B], [zbB])
                            for g4 in range(4):
                                pt, pB = bank()
                                ptb = pt[:].bitcast(BF16)
                                for k in range(8):
                                    tt = g4 * 8 + k
                                    P.op("pe", lambda e, ptb=ptb, k=k, tt=tt: e.transpose(out=ptb[:, k * 128:(k + 1) * 128], in_=zb[:, tt * 128:(tt + 1) * 128], identity=identb[:]),
                                         [zbB, identbB], [pB])
                                P.op("act", lambda e, ptb=ptb, g4=g4: e.activation(out=zst[:, g4 * 8:(g4 + 1) * 8, :], in_=ptb.rearrange("p (k c) -> p k c", k=8), func=AF.Identity),
                                     [pB], [zstB])
                            P.dma("sp", T["ZT"].rearrange("(tt p) c -> p tt c", p=128)[:, :, j * 128:(j + 1) * 128], zst[:], reads=[zstB], writes=[ZTB])
                    P.barrier()

        def phase_hyena_fwd():
            with ExitStack() as st:
                zt = sb(st, "zt", [128, NT, D], BF16); ztB = Buf()
                zv = T["ZT"].rearrange("(tt p) c -> p tt c", p=128)
                for q in range(4):
                    P.dma("sp", zt[:, q * 8:(q + 1) * 8, :], zv[:, q * 8:(q + 1) * 8, :], reads=[ZTB], writes=[ztB])
                kk = [sb(st, f"kk{i}", [128, 2, D], F32) for i in range(2)]; kkB = [Buf(), Buf()]
                yt = [sb(st, f"yt{i}", [128, 2, D], BF16) for i in range(2)]; ytB = [Buf(), Buf()]
                t1 = sb(st, "yt1", [128, 512], F32); t1B = Buf()
                t2 = sb(st, "yt2", [128, 512], F32); t2B = Buf()
                t3 = sb(st, "yt3", [128, 512], F32); t3B = Buf()
                t4 = sb(st, "yt4", [128, 512], F32); t4B = Buf()

                def mulk(fc, res):
                    k, kB = kk[fc % 2], kkB[fc % 2]
                    y, yB = yt[fc % 2], ytB[fc % 2]
                    for cs_ in range(2):
                        P.dma("sp", k[:, cs_, :], T["KS"][cs_, fc * 128:(fc + 1) * 128, :], reads=[KSB], writes=[kB])
                    for h in range(2):
                        zr, zrB = res[h * 2]
                        zi, ziB = res[h * 2 + 1]
                        sl = slice(h * 512, (h + 1) * 512)
                        P.op("dve", lambda e, zr=zr, sl=sl: e.tensor_tensor(out=t1[:], in0=zr[:], in1=k[:, 0, sl], op=ALU.mult), [zrB, kB], [t1B])
                        P.op("dve", lambda e, zi=zi, sl=sl: e.tensor_tensor(out=t2[:], in0=zi[:], in1=k[:, 1, sl], op=ALU.mult), [ziB, kB], [t2B])
                        P.op("dve", lambda e, zr=zr, sl=sl: e.tensor_tensor(out=t3[:], in0=zr[:], in1=k[:, 1, sl], op=ALU.mult), [zrB, kB], [t3B])
                        P.op("dve", lambda e, zi=zi, sl=sl: e.tensor_tensor(out=t4[:], in0=zi[:], in1=k[:, 0, sl], op=ALU.mult), [ziB, kB], [t4B])
                        P.op("pool", lambda e, sl=sl, y=y: e.tensor_tensor(out=y[:, 0, sl], in0=t1[:], in1=t2[:], op=ALU.subtract), [t1B, t2B], [yB])
                        P.op("pool", lambda e, sl=sl, y=y: e.tensor_tensor(out=y[:, 1, sl], in0=t3[:], in1=t4[:], op=ALU.add), [t3B, t4B], [yB])
                    P.dma("sp", T["YS"][fc], y[:], reads=[yB], writes=[YSB])

                fwd_dft(st, zt, ztB, zt, ztB, mulk)
                P.barrier()

        def phase_hyena_out():
            with ExitStack() as st:
                X0 = sb(st, "X0", [128, 8, S], BF16); X0B = Buf()
                xv = T["X0C"].rearrange("(cc p) t -> p cc t", p=128)
                for cc in range(8):
                    P.dma("sp", X0[:, cc, :], xv[:, cc, :], reads=[X0CB], writes=[X0B])
                with ExitStack() as s2:
                    Yh = sb(s2, "Yh", [128, 32, 2, 512], BF16); YhB = Buf()
                    gvb = [sb(s2, f"gv{i}", [128, 32, 2, 128], BF16) for i in range(2)]; gvB = [Buf(), Buf()]
                    ytm = [sb(s2, f"ytm{i}", [128, 512], BF16) for i in range(2)]; ytmB = [Buf(), Buf()]
                    yv = T["YS"].rearrange("fc p cs c -> p fc cs c")
                    prev = None

                    def xpose(h, to, ym, ymB):
                        p2, p2B = bank()
                        p2b = p2[:].bitcast(BF16)
                        for j in range(4):
                            P.op("pe", lambda e, j=j: e.transpose(out=p2b[:, j * 128:(j + 1) * 128], in_=ym[:, j * 128:(j + 1) * 128], identity=identb[:]),
                                 [ymB, identbB], [p2B])
                        P.op("dve", lambda e: e.tensor_tensor(out=X0[:, h * 4:(h + 1) * 4, to * 128:(to + 1) * 128],
                                                              in0=p2b[:, 0:512].rearrange("p (j t) -> p j t", j=4),
                                                              in1=X0[:, h * 4:(h + 1) * 4, to * 128:(to + 1) * 128], op=ALU.mult),
                             [p2B, X0B], [X0B])

                    for h in range(2):
                        for q in range(4):
                            for cs_ in range(2):
                                P.dma("sp", Yh[:, q * 8:(q + 1) * 8, cs_, :], yv[:, q * 8:(q + 1) * 8, cs_, h * 512:(h + 1) * 512], reads=[YSB], writes=[YhB])
                        for to in range(NT):
                            gv, gB = gvb[to % 2], gvB[to % 2]
                            for q in range(2):
                                P.dma("sp", gv[:, q * 16:(q + 1) * 16], T["INV"][to, :, q * 16:(q + 1) * 16], writes=[gB])
                            pt, pB = bank()
                            n = 0
                            for fc in range(32):
                                for cs_ in range(2):
                                    P.op("pe", lambda e, pt=pt, gv=gv, fc=fc, cs_=cs_, n=n: e.matmul(pt[:], lhsT=gv[:, fc, cs_, :], rhs=Yh[:, fc, cs_, :],
                                                                                               start=(n == 0), stop=(n == 63)), [gB, YhB], [pB])
                                    n += 1
                            ym, ymB = ytm[to % 2], ytmB[to % 2]
                            P.op("act", lambda e, pt=pt, ym=ym: e.activation(out=ym[:], in_=pt[:], func=AF.Identity), [pB], [ymB])
                            if prev is not None:
                                xpose(*prev)
                            prev = (h, to, ym, ymB)
                    xpose(*prev)
                    P.barrier()
                with ExitStack() as s2:
                    wo = sb(s2, "wo", [128, 8, D], BF16); woB = Buf()
                    wv = T["hy_w_out"][0].rearrange("(kc p) n -> p kc n", p=128)
                    for q in range(2):
                        P.dma("pool", wo[:, q * 4:(q + 1) * 4, :], wv[:, q * 4:(q + 1) * 4, :], writes=[woB])
                    _, _, (G, GB) = load_mod_tiles(s2, 0, 0, "norm_mix_g")
                    bo = sb(s2, "bo", [128, D], F32); boB = Buf()
                    bcast_load("sp", bo[:], boB, T["hy_b_out"][0, :])
                    xb = [sb(s2, f"ox{i}", [128, D], F32) for i in range(2)]; xbB = [Buf(), Buf()]
                    yo = [sb(s2, f"oy{i}", [128, D], F32) for i in range(2)]; yoB = [Buf(), Buf()]
                    P.dma("sp", xb[0][:], T["x"][0:128, :], writes=[xbB[0]])
                    for tt in range(NT):
                        xt, xtB = xb[tt % 2], xbB[tt % 2]
                        y, yB = yo[tt % 2], yoB[tt % 2]
                        if tt + 1 < NT:
                            P.dma("sp", xb[(tt + 1) % 2][:], T["x"][(tt + 1) * 128:(tt + 2) * 128, :], writes=[xbB[(tt + 1) % 2]])
                        for nh in range(2):
                            pt, pB = bank()
                            for cc in range(8):
                                P.op("pe", lambda e, pt=pt, cc=cc, tt=tt, nh=nh: e.matmul(pt[:], lhsT=X0[:, cc, tt * 128:(tt + 1) * 128], rhs=wo[:, cc, nh * 512:(nh + 1) * 512],
                                                                                         start=(cc == 0), stop=(cc == 7)), [X0B, woB], [pB])
                            sl = slice(nh * 512, (nh + 1) * 512)
                            P.op("dve", lambda e, pt=pt, sl=sl, y=y: e.tensor_tensor(out=y[:, sl], in0=pt[:], in1=bo[:, sl], op=ALU.add), [pB, boB], [yB])
                        P.op("pool", lambda e, y=y: e.tensor_tensor(out=y[:], in0=y[:], in1=G[:], op=ALU.mult), [yB, GB], [yB])
                        P.op("dve", lambda e, y=y, xt=xt: e.tensor_tensor(out=y[:], in0=y[:], in1=xt[:], op=ALU.add), [yB, xtB], [yB])
                        P.dma("sp", T["XA"][tt * 128:(tt + 1) * 128, :], y[:], reads=[yB], writes=[XAB])
                    P.barrier()

        HFB = Buf(); ACCB = Buf()

        def phase_moe(layer, XIN, XINB, XOUT, XOUTB, final):
            with ExitStack() as st:
                IDX = sb(st, "IDX", [128, 4, NEXP], U32); IDXB = Buf()
                GVt = sb(st, "GVt", [128, 4, NEXP], F32); GVB = Buf()
                (A, AB), (Bt, BB), (G, GB) = load_mod_tiles(st, layer, 1, "norm_ffn_g")
                wts = [[sb(st, f"w{n}{i}", [128, 8, D], BF16) for n in "gud"] for i in range(2)]
                wtsB = [[Buf() for _ in range(3)] for i in range(2)]
                wnames = ("moe_w_gate", "moe_w_up", "moe_w_down")

                def issue_wloads(e_):
                    for n in range(3):
                        wv = T[wnames[n]][layer, e_].rearrange("(kc p) n -> p kc n", p=128)
                        w, wB = wts[e_ % 2][n], wtsB[e_ % 2][n]
                        P.dma("pool", w[:], wv[:, :, :], writes=[wB])

                issue_wloads(0)
                issue_wloads(1)
                with ExitStack() as s1:
                    AFFT = sb(s1, "AFFT", [NEXP, S], F32); AFFTB = Buf()
                    with ExitStack() as s2:
                        nx = NormCtx(s2, "m")
                        wr = sb(s2, "wr", [128, 8, NEXP], F32); wrB = Buf()
                        P.dma("sp", wr[:], T["moe_w_router"][layer].rearrange("(kc p) e -> p kc e", p=128), writes=[wrB])
                        zt_ = sb(s2, "zero", [128, D], F32); ztB_ = Buf()
                        P.op("pool", lambda e: e.memset(zt_[:], 0.0), [], [ztB_])
                        for tt in range(NT):
                            P.dma("sp", T["ACC"][tt * 128:(tt + 1) * 128, :], zt_[:], reads=[ztB_], writes=[ACCB])
                        xb = [sb(s2, f"mx{i}", [128, D], F32) for i in range(3)]; xbB = [Buf(), Buf(), Buf()]
                        hf = [sb(s2, f"hf{i}", [128, D], F32) for i in range(2)]; hfB = [Buf(), Buf()]
                        hfb = [sb(s2, f"hfb{i}", [128, D], BF16) for i in range(2)]; hfbB = [Buf(), Buf()]
                        hfT = [sb(s2, f"hfT{i}", [128, 8, 128], F32) for i in range(2)]; hfTB = [Buf(), Buf()]
                        sm = [sb(s2, f"sm{i}", [128, 8], F32) for i in range(2)]; smB = [Buf(), Buf()]
                        ex = [sb(s2, f"ex{i}", [128, NEXP], F32) for i in range(2)]; exB = [Buf(), Buf()]
                        aff = [sb(s2, f"aff{i}", [128, NEXP], F32) for i in range(2)]; affB = [Buf(), Buf()]

                        def ld_x(tt):
                            if tt < NT:
                                P.dma("sp", xb[tt % 3][:], XIN[tt * 128:(tt + 1) * 128, :], reads=[XINB], writes=[xbB[tt % 3]])

                        ld_x(0)

                        def m1_tile(tt):
                            k = tt % 2
                            xt, xtB = xb[tt % 3], xbB[tt % 3]
                            h_, hB_ = hf[k], hfB[k]
                            hb_, hbB_ = hfb[k], hfbB[k]
                            hT, hTB = hfT[k], hfTB[k]
                            sm_, smB_ = sm[k], smB[k]
                            ex_, exB_ = ex[k], exB[k]
                            af_, afB_ = aff[k], affB[k]
                            yield from norm_mod_g(nx, xt[:], xtB, A, AB, Bt, BB, h_[:], hB_)
                            yield
                            P.op("act", lambda e: e.activation(out=hb_[:], in_=h_[:], func=AF.Identity), [hB_], [hbB_])
                            for half in range(2):
                                pt, pB = bank()
                                for k4 in range(4):
                                    kc = half * 4 + k4
                                    P.op("pe", lambda e, pt=pt, k4=k4, kc=kc: e.transpose(out=pt[:, k4 * 128:(k4 + 1) * 128], in_=h_[:, kc * 128:(kc + 1) * 128], identity=identf[:]),
                                         [hB_, identfB], [pB])
                                yield
                                P.op("act", lambda e, pt=pt, half=half: e.activation(out=hT[:, half * 4:(half + 1) * 4, :], in_=pt[:].rearrange("p (k t) -> p k t", k=4), func=AF.Identity),
                                     [pB], [hTB])
                            P.dma("sp", T["HF"][tt * 128:(tt + 1) * 128, :], hb_[:], reads=[hbB_], writes=[HFB])
                            yield
                            pt, pB = bank()
                            for kc in range(8):
                                P.op("pe", lambda e, pt=pt, kc=kc: e.matmul(pt[:, 0:NEXP], lhsT=hT[:, kc, :], rhs=wr[:, kc, :], start=(kc == 0), stop=(kc == 7)),
                                     [hTB, wrB], [pB])
                            yield
                            P.op("dve", lambda e: e.tensor_reduce(out=sm_[:, 0:1], in_=pt[:, 0:NEXP], axis=AX.X, op=ALU.max, negate=True), [pB], [smB_])
                            yield
                            P.op("act", lambda e: e.activation(out=ex_[:], in_=pt[:, 0:NEXP], func=AF.Exp, bias=sm_[:, 0:1], accum_out=sm_[:, 1:2]), [pB, smB_], [exB_, smB_])
                            yield
                            P.op("dve", lambda e: e.reciprocal(out=sm_[:, 2:3], in_=sm_[:, 1:2]), [smB_], [smB_])
                            yield
                            P.op("dve", lambda e: e.tensor_scalar(out=af_[:], in0=ex_[:], scalar1=sm_[:, 2:3], scalar2=None, op0=ALU.mult), [exB_, smB_], [afB_])
                            yield
                            p2, p2B = bank()
                            P.op("pe", lambda e: e.transpose(out=p2[0:NEXP, 0:128], in_=af_[:, 0:NEXP], identity=identf[:]), [afB_, identfB], [p2B])
                            yield
                            P.op("act", lambda e: e.activation(out=AFFT[:, tt * 128:(tt + 1) * 128], in_=p2[0:NEXP, 0:128], func=AF.Identity), [p2B], [AFFTB])

                        run_pipelined(NT, m1_tile, depth=2, on_start=lambda tt: ld_x(tt + 1))
                        P.barrier()
                    with ExitStack() as s2:
                        work = sb(s2, "work", [NEXP, S], F32); workB = Buf()
                        vals = sb(s2, "vals", [NEXP, CAP], F32); valsB = Buf()
                        idxu = sb(s2, "idxu", [NEXP, CAP], U32); idxuB = Buf()
                        idxf = sb(s2, "idxf", [NEXP, CAP], F32); idxfB = Buf()
                        idt = sb(s2, "idt", [128, 4, NEXP], F32); idtB = Buf()
                        P.op("dve", lambda e: e.tensor_copy(out=work[:], in_=AFFT[:]), [AFFTB], [workB])
                        for r in range(CAP // 8):
                            sl = slice(8 * r, 8 * r + 8)
                            P.op("dve", lambda e, sl=sl: e.max(out=vals[:, sl], in_=work[:]), [workB], [valsB])
                            P.op("dve", lambda e, sl=sl: e.max_index(out=idxu[:, sl], in_max=vals[:, sl], in_values=work[:]), [workB, valsB], [idxuB])
                            P.op("dve", lambda e, sl=sl: e.match_replace(out=work[:], in_to_replace=vals[:, sl], in_values=work[:], imm_value=-1.0), [valsB, workB], [workB])
                        P.op("dve", lambda e: e.tensor_copy(out=idxf[:], in_=idxu[:]), [idxuB], [idxfB])
                        for s_ in range(4):
                            pt, pB = bank()
                            P.op("pe", lambda e, pt=pt, s_=s_: e.transpose(out=pt[:, 0:NEXP], in_=idxf[0:NEXP, s_ * 128:(s_ + 1) * 128], identity=identf[0:NEXP, 0:NEXP]),
                                 [idxfB, identfB], [pB])
                            P.op("dve", lambda e, pt=pt, s_=s_: e.tensor_copy(out=idt[:, s_, :], in_=pt[:, 0:NEXP]), [pB], [idtB])
                            P.op("dve", lambda e, s_=s_: e.tensor_copy(out=IDX[:, s_, :], in_=idt[:, s_, :]), [idtB], [IDXB])
                            p2, p2B = bank()
                            P.op("pe", lambda e, p2=p2, s_=s_: e.transpose(out=p2[:, 0:NEXP], in_=vals[0:NEXP, s_ * 128:(s_ + 1) * 128], identity=identf[0:NEXP, 0:NEXP]),
                                 [valsB, identfB], [p2B])
                            P.op("dve", lambda e, p2=p2, s_=s_: e.tensor_copy(out=GVt[:, s_, :], in_=p2[:, 0:NEXP]), [p2B], [GVB])
                        P.barrier()
                with ExitStack() as s1:
                    xs = [sb(s1, f"xs{i}", [128, 4, D], BF16) for i in range(2)]; xsB = [[Buf() for _ in range(4)] for _ in range(2)]
                    xsT = sb(s1, "xsT", [128, 8, CAP], BF16); xsTB = Buf()
                    hid = sb(s1, "hid", [128, 8, CAP], BF16); hidB = Buf()
                    sg = [sb(s1, f"sg{i}", [128, 512], F32) for i in range(2)]; sgB = [Buf(), Buf()]
                    ye = [sb(s1, f"ye{i}", [128, D], F32) for i in range(4)]; yeB = [Buf() for _ in range(4)]

                    def issue_gathers(e_):
                        x_ = xs[e_ % 2]
                        for s_ in range(4):
                            P.dma("pool", None, None, reads=[HFB, IDXB], writes=[xsB[e_ % 2][s_]],
                                  fn=lambda g, s_=s_, x_=x_, e_=e_: g.indirect_dma_start(
                                      out=x_[:, s_, :], out_offset=None, in_=T["HF"][:, :],
                                      in_offset=bass.IndirectOffsetOnAxis(ap=IDX[:, s_, e_:e_ + 1], axis=0)))

                    issue_gathers(0)
                    issue_gathers(1)
                    prev_sc = []
                    for e_ in range(NEXP):
                        (wg, wu, wd), (wgB, wuB, wdB) = wts[e_ % 2], wtsB[e_ % 2]
                        x_ = xs[e_ % 2]
                        for s_ in range(4):
                            pt, pB = bank()
                            ptb = pt[:].bitcast(BF16)
                            for kc in range(8):
                                P.op("pe", lambda e, ptb=ptb, kc=kc, s_=s_, x_=x_: e.transpose(out=ptb[:, kc * 128:(kc + 1) * 128], in_=x_[:, s_, kc * 128:(kc + 1) * 128], identity=identb[:]),
                                     [xsB[e_ % 2][s_], identbB], [pB])
                            P.op("act", lambda e, ptb=ptb, s_=s_: e.activation(out=xsT[:, :, s_ * 128:(s_ + 1) * 128], in_=ptb.rearrange("p (k t) -> p k t", k=8), func=AF.Identity),
                                 [pB], [xsTB])
                        for fcn in range(8):
                            pg, pgB = bank()
                            pu, puB = bank()
                            for kc in range(8):
                                P.op("pe", lambda e, pg=pg, kc=kc, fcn=fcn, wg=wg: e.matmul(pg[:], lhsT=wg[:, kc, fcn * 128:(fcn + 1) * 128], rhs=xsT[:, kc, :], start=(kc == 0), stop=(kc == 7)),
                                     [wgB, xsTB], [pgB])
                            for kc in range(8):
                                P.op("pe", lambda e, pu=pu, kc=kc, fcn=fcn, wu=wu: e.matmul(pu[:], lhsT=wu[:, kc, fcn * 128:(fcn + 1) * 128], rhs=xsT[:, kc, :], start=(kc == 0), stop=(kc == 7)),
                                     [wuB, xsTB], [puB])
                            s__, sB__ = sg[fcn % 2], sgB[fcn % 2]
                            P.op("act", lambda e, pg=pg, s__=s__: e.activation(out=s__[:], in_=pg[:], func=AF.Silu), [pgB], [sB__])
                            P.op("dve", lambda e, pu=pu, s__=s__, fcn=fcn: e.tensor_tensor(out=hid[:, fcn, :], in0=pu[:], in1=s__[:], op=ALU.mult), [puB, sB__], [hidB])
                        for s_ in range(4):
                            y, yB = ye[s_], yeB[s_]
                            for nh in range(2):
                                pt, pB = bank()
                                for fcn in range(8):
                                    P.op("pe", lambda e, pt=pt, fcn=fcn, s_=s_, nh=nh, wd=wd: e.matmul(pt[:], lhsT=hid[:, fcn, s_ * 128:(s_ + 1) * 128], rhs=wd[:, fcn, nh * 512:(nh + 1) * 512],
                                                                                                 start=(fcn == 0), stop=(fcn == 7)), [hidB, wdB], [pB])
                                P.op("act", lambda e, pt=pt, y=y, nh=nh, s_=s_, e_=e_: e.activation(out=y[:, nh * 512:(nh + 1) * 512], in_=pt[:], func=AF.Identity, scale=GVt[:, s_, e_:e_ + 1]),
                                     [pB, GVB], [yB])
                        if e_ + 2 < NEXP:
                            issue_wloads(e_ + 2)
                        if ACCB.w is not None:
                            P._wait("pool", ACCB.w)
                        for t_ in prev_sc:
                            P._wait("pool", t_)
                        cur_sc = []
                        for s_ in range(4):
                            y, yB = ye[s_], yeB[s_]
                            cur_sc.append(P.dma("pool", None, None, reads=[yB, IDXB], writes=[ACCB],
                                                fn=lambda g, s_=s_, y=y, e_=e_: g.indirect_dma_start(
                                                    out=T["ACC"][:, :], out_offset=bass.IndirectOffsetOnAxis(ap=IDX[:, s_, e_:e_ + 1], axis=0),
                                                    in_=y[:], in_offset=None, compute_op=ALU.add)))
                        prev_sc[:] = cur_sc
                        if e_ + 2 < NEXP:
                            issue_gathers(e_ + 2)
                    P.barrier()
                with ExitStack() as s1:
                    xb = [sb(s1, f"cx{i}", [128, D], F32) for i in range(3)]; xbB = [Buf(), Buf(), Buf()]
                    ab = [sb(s1, f"ca{i}", [128, D], F32) for i in range(3)]; abB = [Buf(), Buf(), Buf()]
                    ob_ = [sb(s1, f"co{i}", [128, D], F32) for i in range(2)]; obB_ = [Buf(), Buf()]
                    if final:
                        nx = NormCtx(s1, "f")
                        fg = sb(s1, "fg", [128, D], F32); fgB = Buf()
                        bcast_load("sp", fg[:], fgB, T["final_norm_g"])

                    def ld_c(tt):
                        if tt < NT:
                            P.dma("sp", xb[tt % 3][:], XIN[tt * 128:(tt + 1) * 128, :], reads=[XINB], writes=[xbB[tt % 3]])
                            P.dma("sp", ab[tt % 3][:], T["ACC"][tt * 128:(tt + 1) * 128, :], reads=[ACCB], writes=[abB[tt % 3]])

                    ld_c(0)

                    def c_tile(tt):
                        xt, xtB = xb[tt % 3], xbB[tt % 3]
                        a_, aB_ = ab[tt % 3], abB[tt % 3]
                        P.op("pool", lambda e: e.tensor_tensor(out=a_[:], in0=a_[:], in1=G[:], op=ALU.mult), [aB_, GB], [aB_])
                        yield
                        P.op("dve", lambda e: e.tensor_tensor(out=a_[:], in0=a_[:], in1=xt[:], op=ALU.add), [aB_, xtB], [aB_])
                        yield
                        if final:
                            o_, oB_ = ob_[tt % 2], obB_[tt % 2]
                            yield from norm_mod_g(nx, a_[:], aB_, fg, fgB, None, None, o_[:], oB_)
                            yield
                            P.dma("sp", XOUT[tt * 128:(tt + 1) * 128, :], o_[:], reads=[oB_], writes=[XOUTB])
                        else:
                            P.dma("sp", XOUT[tt * 128:(tt + 1) * 128, :], a_[:], reads=[aB_], writes=[XOUTB])

                    run_pipelined(NT, c_tile, depth=2, on_start=lambda tt: ld_c(tt + 1))
                    P.barrier()

        def bank_fixed(i):
            return ps[i], psB[i]

        SCALE = 1.0 / math.sqrt(192.0)
        C1 = 6.28125
        C2 = TWO_PI - 6.28125
        QNB = Buf(); QPB = Buf(); KNB = Buf(); KPB = Buf(); VB = Buf()

        def phase_mla_proj(XIN, XINB):
            with ExitStack() as st:
                cqnT = sb(st, "cqnT", [128, 2, S], BF16); cqnTB = Buf()
                ckvT = sb(st, "ckvT", [128, S], BF16); ckvTB = Buf()
                kpT = sb(st, "kpT", [64, S], BF16); kpTB = Buf()
                cosT = sb(st, "cosT", [64, S], F32); cosTB = Buf()
                sinT = sb(st, "sinT", [64, S], F32); sinTB = Buf()
                with ExitStack() as s2:
                    posi = sb(s2, "posi", [64, S], I32); posiB = Buf()
                    ang = sb(s2, "ang", [64, S], F32); angB = Buf()
                    a2 = sb(s2, "a2", [64, S], F32); a2B = Buf()
                    kq = sb(s2, "kq", [64, S], I32); kqB = Buf()
                    kqf = sb(s2, "kqf", [64, S], F32); kqfB = Buf()
                    ivf = sb(s2, "ivf", [64, 1], F32); ivfB = Buf()
                    P.dma("sp", posi[:], T["pos"].partition_broadcast(64), writes=[posiB])
                    P.dma("sp", ivf[0:32, :], T["invf"].rearrange("(p o) -> p o", o=1), writes=[ivfB])
                    P.dma("sp", ivf[32:64, :], T["invf"].rearrange("(p o) -> p o", o=1), writes=[ivfB])
                    P.op("dve", lambda e: e.tensor_copy(out=ang[:], in_=posi[:]), [posiB], [angB])
                    P.op("dve", lambda e: e.tensor_scalar(out=ang[:], in0=ang[:], scalar1=ivf[:, 0:1], scalar2=None, op0=ALU.mult), [angB, ivfB], [angB])
                    for shift, dst, dB in ((0.0, sinT, sinTB), (math.pi / 2.0, cosT, cosTB)):
                        P.op("dve", lambda e, shift=shift: e.tensor_scalar(out=a2[:], in0=ang[:], scalar1=shift, scalar2=None, op0=ALU.add), [angB], [a2B])
                        P.op("dve", lambda e: e.tensor_scalar(out=kq[:], in0=a2[:], scalar1=1.0 / TWO_PI, scalar2=None, op0=ALU.mult), [a2B], [kqB])
                        P.op("dve", lambda e: e.tensor_copy(out=kqf[:], in_=kq[:]), [kqB], [kqfB])
                        P.op("dve", lambda e: e.scalar_tensor_tensor(out=a2[:], in0=kqf[:], scalar=-C1, in1=a2[:], op0=ALU.mult, op1=ALU.add), [kqfB, a2B], [a2B])
                        P.op("dve", lambda e: e.scalar_tensor_tensor(out=a2[:], in0=kqf[:], scalar=-C2, in1=a2[:], op0=ALU.mult, op1=ALU.add), [kqfB, a2B], [a2B])
                        P.op("dve", lambda e: e.tensor_scalar(out=a2[:], in0=a2[:], scalar1=math.pi, scalar2=-math.pi, op0=ALU.min, op1=ALU.max), [a2B], [a2B])
                        P.op("act", lambda e, dst=dst: e.activation(out=dst[:], in_=a2[:], func=AF.Sin), [a2B], [dB])
                    P.barrier()
                with ExitStack() as s2:
                    (A, AB), (Bt, BB), _ = load_mod_tiles(s2, 1, 0, "norm_mix_g")
                    nx = NormCtx(s2, "a")
                    win = sb(s2, "win", [128, 8, 448], BF16); winB = Buf()
                    wrot = sb(s2, "wrot", [128, 8, 64], BF16); wrotB = Buf()
                    P.dma("pool", win[:], T["mla_w_in"][0].rearrange("(kc p) n -> p kc n", p=128), writes=[winB])
                    P.op("dve", lambda e: e.tensor_scalar(out=wrot[:, :, 0:32], in0=win[:, :, 416:448], scalar1=-1.0, scalar2=None, op0=ALU.mult), [winB], [wrotB])
                    P.op("dve", lambda e: e.tensor_copy(out=wrot[:, :, 32:64], in_=win[:, :, 384:416]), [winB], [wrotB])
                    qg = sb(s2, "qg", [128, 256], F32); qgB = Buf()
                    kg = sb(s2, "kg", [128, 128], F32); kgB = Buf()
                    bcast_load("sp", qg[:], qgB, T["mla_q_norm_g"][0, :])
                    bcast_load("sp", kg[:], kgB, T["mla_kv_norm_g"][0, :])
                    xb = [sb(s2, f"ax{i}", [128, D], F32) for i in range(3)]; xbB = [Buf(), Buf(), Buf()]
                    hb = [sb(s2, f"ahb{i}", [128, D], BF16) for i in range(2)]; hbB = [Buf(), Buf()]
                    hT = [sb(s2, f"ahT{i}", [128, 8, 128], BF16) for i in range(2)]; hTB = [Buf(), Buf()]
                    jq = sb(s2, "jq", [128, 256], F32); jqB = Buf()
                    sqs = [sb(s2, f"sq{i}", [128, 8], F32) for i in range(2)]; sqsB = [Buf(), Buf()]
                    cn = [sb(s2, f"cn{i}", [128, 384], BF16) for i in range(2)]; cnB = [Buf(), Buf()]
                    r1s = [sb(s2, f"r1{i}", [64, 128], F32) for i in range(2)]; r1sB = [Buf(), Buf()]
                    r2s = [sb(s2, f"r2{i}", [64, 128], F32) for i in range(2)]; r2sB = [Buf(), Buf()]

                    def ld_ax(tt):
                        if tt < NT:
                            P.dma("sp", xb[tt % 3][:], XIN[tt * 128:(tt + 1) * 128, :], reads=[XINB], writes=[xbB[tt % 3]])

                    ld_ax(0)

                    def a_tile(tt):
                        k = tt % 2
                        tsl = slice(tt * 128, (tt + 1) * 128)
                        xt, xtB = xb[tt % 3], xbB[tt % 3]
                        h_, hB_ = hb[k], hbB[k]
                        ht, htB = hT[k], hTB[k]
                        sq, sqB = sqs[k], sqsB[k]
                        c_, cB_ = cn[k], cnB[k]
                        r1, r1B = r1s[k], r1sB[k]
                        r2, r2B = r2s[k], r2sB[k]
                        yield from norm_mod_g(nx, xt[:], xtB, A, AB, Bt, BB, h_[:], hB_)
                        yield
                        pt, pB = bank()
                        ptb = pt[:].bitcast(BF16)
                        for kc in range(8):
                            P.op("pe", lambda e, kc=kc: e.transpose(out=ptb[:, kc * 128:(kc + 1) * 128], in_=h_[:, kc * 128:(kc + 1) * 128], identity=identb[:]),
                                 [hB_, identbB], [pB])
                        yield
                        P.op("act", lambda e: e.activation(out=ht[:], in_=ptb.rearrange("p (k t) -> p k t", k=8), func=AF.Identity), [pB], [htB])
                        yield
                        pa, paB = bank()
                        for kc in range(8):
                            P.op("pe", lambda e, kc=kc: e.matmul(pa[:, 0:384], lhsT=ht[:, kc, :], rhs=win[:, kc, 0:384], start=(kc == 0), stop=(kc == 7)),
                                 [htB, winB], [paB])
                        pk1, pk1B = bank()
                        for kc in range(8):
                            P.op("pe", lambda e, kc=kc: e.matmul(pk1[0:64, 0:128], lhsT=win[:, kc, 384:448], rhs=ht[:, kc, :], start=(kc == 0), stop=(kc == 7)),
                                 [htB, winB], [pk1B])
                        pk2, pk2B = bank()
                        for kc in range(8):
                            P.op("pe", lambda e, kc=kc: e.matmul(pk2[0:64, 0:128], lhsT=wrot[:, kc, :], rhs=ht[:, kc, :], start=(kc == 0), stop=(kc == 7)),
                                 [htB, wrotB], [pk2B])
                        yield
                        P.op("dve", lambda e: e.tensor_tensor(out=r1[:], in0=pk1[0:64, 0:128], in1=cosT[:, tsl], op=ALU.mult), [pk1B, cosTB], [r1B])
                        P.op("dve", lambda e: e.tensor_tensor(out=r2[:], in0=pk2[0:64, 0:128], in1=sinT[:, tsl], op=ALU.mult), [pk2B, sinTB], [r2B])
                        yield
                        P.op("pool", lambda e: e.tensor_tensor(out=kpT[:, tsl], in0=r1[:], in1=r2[:], op=ALU.add), [r1B, r2B], [kpTB])
                        groups = ((0, 256, qg, qgB, 256.0, 0), (256, 384, kg, kgB, 128.0, 4))
                        for (lo, hi, gt_, gB_, n_, o4) in groups:
                            P.op("act", lambda e, lo=lo, hi=hi, o4=o4: e.activation(out=jq[:, 0:hi - lo], in_=pa[:, lo:hi], func=AF.Square, accum_out=sq[:, o4:o4 + 1]),
                                 [paB], [jqB, sqB])
                        yield
                        for (lo, hi, gt_, gB_, n_, o4) in groups:
                            P.op("dve", lambda e, o4=o4, n_=n_: e.tensor_scalar(out=sq[:, o4 + 1:o4 + 2], in0=sq[:, o4:o4 + 1], scalar1=1.0 / n_, scalar2=EPS, op0=ALU.mult, op1=ALU.add), [sqB], [sqB])
                        yield
                        for (lo, hi, gt_, gB_, n_, o4) in groups:
                            P.op("act", lambda e, o4=o4: e.activation(out=sq[:, o4 + 2:o4 + 3], in_=sq[:, o4 + 1:o4 + 2], func=AF.Sqrt), [sqB], [sqB])
                        yield
                        for (lo, hi, gt_, gB_, n_, o4) in groups:
                            P.op("dve", lambda e, o4=o4: e.reciprocal(out=sq[:, o4 + 3:o4 + 4], in_=sq[:, o4 + 2:o4 + 3]), [sqB], [sqB])
                        yield
                        for (lo, hi, gt_, gB_, n_, o4) in groups:
                            P.op("dve", lambda e, lo=lo, hi=hi, o4=o4, gt_=gt_: e.scalar_tensor_tensor(out=c_[:, lo:hi], in0=pa[:, lo:hi], scalar=sq[:, o4 + 3:o4 + 4], in1=gt_[:],
                                                                                                op0=ALU.mult, op1=ALU.mult), [paB, sqB, gB_], [cB_])
                        yield
                        p3, p3B = bank()
                        p3b = p3[:].bitcast(BF16)
                        for j in range(3):
                            P.op("pe", lambda e, j=j: e.transpose(out=p3b[:, j * 128:(j + 1) * 128], in_=c_[:, j * 128:(j + 1) * 128], identity=identb[:]),
                                 [cB_, identbB], [p3B])
                        yield
                        P.op("act", lambda e: e.activation(out=cqnT[:, :, tsl], in_=p3b[:, 0:256].rearrange("p (k t) -> p k t", k=2), func=AF.Identity), [p3B], [cqnTB])
                        P.op("act", lambda e: e.activation(out=ckvT[:, tsl], in_=p3b[:, 256:384], func=AF.Identity), [p3B], [ckvTB])

                    run_pipelined(NT, a_tile, depth=2, on_start=lambda tt: ld_ax(tt + 1))
                    P.dma("sp", T["KP"][:, :], kpT[:], reads=[kpTB], writes=[KPB])
                    P.barrier()
                with ExitStack() as s2:
                    wqn = sb(s2, "wqn", [128, 2, 8, 128], BF16); wqnB = Buf()
                    wqp = sb(s2, "wqp", [128, 2, 8, 64], BF16); wqpB = Buf()
                    wqr = sb(s2, "wqr", [128, 2, 8, 64], BF16); wqrB = Buf()
                    wkk = sb(s2, "wkk", [128, 8, 128], BF16); wkkB = Buf()
                    wkv = sb(s2, "wkv", [128, 8, 128], BF16); wkvB = Buf()
                    qv = T["mla_w_qb"][0].rearrange("(k2 p) (h c) -> p k2 h c", p=128, c=192)
                    for k2 in range(2):
                        P.dma("pool", wqn[:, k2], qv[:, k2, :, 0:128], writes=[wqnB])
                        P.dma("pool", wqp[:, k2], qv[:, k2, :, 128:192], writes=[wqpB])
                    kvv = T["mla_w_kvb"][0].rearrange("k (h c) -> k h c", c=256)
                    P.dma("pool", wkk[:], kvv[:, :, 0:128], writes=[wkkB])
                    P.dma("pool", wkv[:], kvv[:, :, 128:256], writes=[wkvB])
                    P.op("dve", lambda e: e.tensor_scalar(out=wqr[:, :, :, 0:32], in0=wqp[:, :, :, 32:64], scalar1=-1.0, scalar2=None, op0=ALU.mult), [wqpB], [wqrB])
                    P.op("dve", lambda e: e.tensor_copy(out=wqr[:, :, :, 32:64], in_=wqp[:, :, :, 0:32]), [wqpB], [wqrB])
                    qn = [sb(s2, f"qn{i}", [128, S], BF16) for i in range(2)]; qnB = [Buf(), Buf()]
                    kn = [sb(s2, f"kn{i}", [128, S], BF16) for i in range(2)]; knB = [Buf(), Buf()]
                    qp = [sb(s2, f"qp{i}", [64, S], BF16) for i in range(2)]; qpB = [Buf(), Buf()]
                    r1 = sb(s2, "q1", [64, 512], F32); r1B = Buf()
                    r2 = sb(s2, "q2", [64, 512], F32); r2B = Buf()
                    vt = [sb(s2, f"vt{i}", [128, D], BF16) for i in range(2)]; vtB = [Buf(), Buf()]
                    for tt in range(NT):
                        tsl = slice(tt * 128, (tt + 1) * 128)
                        v_, vB_ = vt[tt % 2], vtB[tt % 2]
                        for nh in range(2):
                            pt, pB = bank()
                            P.op("pe", lambda e, pt=pt, tsl=tsl, nh=nh: e.matmul(pt[:], lhsT=ckvT[:, tsl], rhs=wkv[:, nh * 4:(nh + 1) * 4, :], start=True, stop=True),
                                 [ckvTB, wkvB], [pB])
                            P.op("act", lambda e, pt=pt, nh=nh, v_=v_: e.activation(out=v_[:, nh * 512:(nh + 1) * 512], in_=pt[:], func=AF.Identity), [pB], [vB_])
                        P.dma("sp", T["V"][tsl, :], v_[:], reads=[vB_], writes=[VB])
                    for h in range(8):
                        q_, qB_ = qn[h % 2], qnB[h % 2]
                        k_, kB_ = kn[h % 2], knB[h % 2]
                        p_, pB_ = qp[h % 2], qpB[h % 2]
                        for tch in range(8):
                            csl = slice(tch * 512, (tch + 1) * 512)
                            pt, pB = bank()
                            for k2 in range(2):
                                P.op("pe", lambda e, pt=pt, k2=k2, h=h, csl=csl: e.matmul(pt[:], lhsT=wqn[:, k2, h, :], rhs=cqnT[:, k2, csl], start=(k2 == 0), stop=(k2 == 1)),
                                     [wqnB, cqnTB], [pB])
                            P.op("act", lambda e, pt=pt, q_=q_, csl=csl: e.activation(out=q_[:, csl], in_=pt[:], func=AF.Identity), [pB], [qB_])
                            pt2, pB2 = bank()
                            P.op("pe", lambda e, pt2=pt2, h=h, csl=csl: e.matmul(pt2[:], lhsT=wkk[:, h, :], rhs=ckvT[:, csl], start=True, stop=True), [wkkB, ckvTB], [pB2])
                            P.op("act", lambda e, pt2=pt2, k_=k_, csl=csl: e.activation(out=k_[:, csl], in_=pt2[:], func=AF.Identity), [pB2], [kB_])
                            pa, paB = bank()
                            for k2 in range(2):
                                P.op("pe", lambda e, pa=pa, k2=k2, h=h, csl=csl: e.matmul(pa[0:64, :], lhsT=wqp[:, k2, h, :], rhs=cqnT[:, k2, csl], start=(k2 == 0), stop=(k2 == 1)),
                                     [wqpB, cqnTB], [paB])
                            pb_, pbB = bank()
                            for k2 in range(2):
                                P.op("pe", lambda e, pb_=pb_, k2=k2, h=h, csl=csl: e.matmul(pb_[0:64, :], lhsT=wqr[:, k2, h, :], rhs=cqnT[:, k2, csl], start=(k2 == 0), stop=(k2 == 1)),
                                     [wqrB, cqnTB], [pbB])
                            P.op("dve", lambda e, pa=pa, csl=csl: e.tensor_tensor(out=r1[:], in0=pa[0:64, :], in1=cosT[:, csl], op=ALU.mult), [paB, cosTB], [r1B])
                            P.op("dve", lambda e, pb_=pb_, csl=csl: e.tensor_tensor(out=r2[:], in0=pb_[0:64, :], in1=sinT[:, csl], op=ALU.mult), [pbB, sinTB], [r2B])
                            P.op("pool", lambda e, p_=p_, csl=csl: e.tensor_tensor(out=p_[:, csl], in0=r1[:], in1=r2[:], op=ALU.add), [r1B, r2B], [pB_])
                        P.dma("sp", T["QN"][h], q_[:], reads=[qB_], writes=[QNB])
                        P.dma("sp", T["KN"][h], k_[:], reads=[kB_], writes=[KNB])
                        P.dma("sp", T["QP"][h], p_[:], reads=[pB_], writes=[QPB])
                    P.barrier()

        def phase_mla_attn(XIN, XINB, XOUT, XOUTB):
            with ExitStack() as st:
                OT = sb(st, "OT", [128, 8, S], BF16); OTB = Buf()
                with ExitStack() as s2:
                    kp = sb(s2, "kp", [64, S], BF16); kpB = Buf()
                    P.dma("sp", kp[:], T["KP"][:, :], reads=[KPB], writes=[kpB])
                    ones = sb(s2, "ones", [128, 128], BF16); onesB = Buf()
                    P.op("dve", lambda e: e.memset(ones[:], 1.0), [], [onesB])
                    qn = [sb(s2, f"aq{i}", [128, S], BF16) for i in range(2)]; qnB = [Buf(), Buf()]
                    kn = [sb(s2, f"ak{i}", [128, S], BF16) for i in range(2)]; knB = [Buf(), Buf()]
                    qp = [sb(s2, f"ap{i}", [64, S], BF16) for i in range(2)]; qpB = [Buf(), Buf()]
                    vh = [sb(s2, f"av{i}", [128, NT, 128], BF16) for i in range(2)]; vhB = [Buf(), Buf()]
                    pT = [sb(s2, f"pT{i}", [128, 512], BF16) for i in range(3)]; pTB = [Buf(), Buf(), Buf()]
                    rs = sb(s2, "rs", [128, 512], F32); rsB = Buf()
                    npt = 0
                    nst = 0
                    vv = T["V"].rearrange("(tt p) c -> p tt c", p=128)

                    def load_head(h):
                        P.dma("sp", qn[h % 2][:], T["QN"][h], reads=[QNB], writes=[qnB[h % 2]])
                        P.dma("sp", kn[h % 2][:], T["KN"][h], reads=[KNB], writes=[knB[h % 2]])
                        P.dma("sp", qp[h % 2][:], T["QP"][h], reads=[QPB], writes=[qpB[h % 2]])
                        P.dma("sp", vh[h % 2][:], vv[:, :, h * 128:(h + 1) * 128], reads=[VB], writes=[vhB[h % 2]])

                    load_head(0)
                    for h in range(8):
                        if h + 1 < 8:
                            load_head(h + 1)
                        q_, qB_ = qn[h % 2], qnB[h % 2]
                        k_, kB_ = kn[h % 2], knB[h % 2]
                        p_, pB_ = qp[h % 2], qpB[h % 2]
                        v_, vB_ = vh[h % 2], vhB[h % 2]
                        for qc in range(8):
                            csl = slice(qc * 512, (qc + 1) * 512)
                            po, poB = bank_fixed(4 + qc % 2)
                            pz, pzB = bank_fixed(6 + qc % 2)
                            def qk(kt, k_=k_, kB_=kB_, q_=q_, qB_=qB_, p_=p_, pB_=pB_, csl=csl):
                                ksl = slice(kt * 128, (kt + 1) * 128)
                                pst, pstB = bank_fixed(kt % 4)
                                P.op("pe", lambda e: e.matmul(pst[:], lhsT=k_[:, ksl], rhs=q_[:, csl], start=True, stop=False),
                                     [kB_, qB_], [pstB])
                                P.op("pe", lambda e: e.matmul(pst[:], lhsT=kp[:, ksl], rhs=p_[:, csl], start=False, stop=True),
                                     [kpB, pB_], [pstB])
                                return pst, pstB

                            pend = {0: qk(0), 1: qk(1), 2: qk(2)}
                            for kt in range(NT):
                                pst, pstB = pend.pop(kt)
                                t_, tB_ = pT[npt % 3], pTB[npt % 3]
                                npt += 1
                                P.op("act", lambda e, pst=pst, t_=t_: e.activation(out=t_[:], in_=pst[:], func=AF.Exp, scale=SCALE), [pstB], [tB_])
                                if kt + 3 < NT:
                                    pend[kt + 3] = qk(kt + 3)
                                P.op("pe", lambda e, po=po, kt=kt, t_=t_, v_=v_: e.matmul(po[:], lhsT=v_[:, kt, :], rhs=t_[:], start=(kt == 0), stop=(kt == NT - 1)),
                                     [vB_, tB_], [poB])
                                P.op("pe", lambda e, pz=pz, kt=kt, t_=t_: e.matmul(pz[:], lhsT=ones[:], rhs=t_[:], start=(kt == 0), stop=(kt == NT - 1)),
                                     [onesB, tB_], [pzB])
                            P.op("dve", lambda e, pz=pz: e.reciprocal(out=rs[:], in_=pz[:]), [pzB], [rsB])
                            P.op("dve", lambda e, po=po, h=h, csl=csl: e.tensor_tensor(out=OT[:, h, csl], in0=po[:], in1=rs[:], op=ALU.mult), [poB, rsB], [OTB])
                    P.barrier()
                with ExitStack() as s2:
                    wo = sb(s2, "mwo", [128, 8, D], BF16); woB = Buf()
                    wv = T["mla_w_out"][0].rearrange("(kc p) n -> p kc n", p=128)
                    for q in range(2):
                        P.dma("pool", wo[:, q * 4:(q + 1) * 4, :], wv[:, q * 4:(q + 1) * 4, :], writes=[woB])
                    _, _, (G, GB) = load_mod_tiles(s2, 1, 0, "norm_mix_g")
                    xb = [sb(s2, f"bx{i}", [128, D], F32) for i in range(2)]; xbB = [Buf(), Buf()]
                    yo = [sb(s2, f"by{i}", [128, D], F32) for i in range(2)]; yoB = [Buf(), Buf()]
                    P.dma("sp", xb[0][:], XIN[0:128, :], reads=[XINB], writes=[xbB[0]])
                    for tt in range(NT):
                        tsl = slice(tt * 128, (tt + 1) * 128)
                        xt, xtB = xb[tt % 2], xbB[tt % 2]
                        y, yB = yo[tt % 2], yoB[tt % 2]
                        if tt + 1 < NT:
                            P.dma("sp", xb[(tt + 1) % 2][:], XIN[(tt + 1) * 128:(tt + 2) * 128, :], reads=[XINB], writes=[xbB[(tt + 1) % 2]])
                        for nh in range(2):
                            pt, pB = bank()
                            for hh in range(8):
                                P.op("pe", lambda e, pt=pt, hh=hh, tsl=tsl, nh=nh: e.matmul(pt[:], lhsT=OT[:, hh, tsl], rhs=wo[:, hh, nh * 512:(nh + 1) * 512], start=(hh == 0), stop=(hh == 7)),
                                     [OTB, woB], [pB])
                            sl = slice(nh * 512, (nh + 1) * 512)
                            P.op("dve", lambda e, pt=pt, sl=sl, y=y: e.tensor_tensor(out=y[:, sl], in0=pt[:], in1=G[:, sl], op=ALU.mult), [pB, GB], [yB])
                        P.op("pool", lambda e, y=y, xt=xt: e.tensor_tensor(out=y[:], in0=y[:], in1=xt[:], op=ALU.add), [yB, xtB], [yB])
                        P.dma("sp", XOUT[tsl, :], y[:], reads=[yB], writes=[XOUTB])
                    P.barrier()

        def copy_to_out(src, srcB):
            with ExitStack() as st:
                cb = [sb(st, f"cpy{i}", [128, D], F32) for i in range(2)]; cbB = [Buf(), Buf()]
                for tt in range(NT):
                    t_, tB = cb[tt % 2], cbB[tt % 2]
                    P.dma("sp", t_[:], src[tt * 128:(tt + 1) * 128, :], reads=[srcB], writes=[tB])
                    P.dma("sp", OUT[tt * 128:(tt + 1) * 128, :], t_[:], reads=[tB], writes=[OUTB])
                P.barrier()

        OUTB = Buf()

        phase_mod()
        phase_filter()
        phase_hyena_in()
        phase_hyena_fwd()
        phase_hyena_out()
        if stage == "hyena":
            copy_to_out(T["XA"], XAB)
        else:
            XBB = Buf()
            phase_moe(0, T["XA"], XAB, T["XB"], XBB, False)
            if stage == "moe0":
                copy_to_out(T["XB"], XBB)
            else:
                XCB = Buf()
                phase_mla_proj(T["XB"], XBB)
                phase_mla_attn(T["XB"], XBB, T["XC"], XCB)
                if stage == "mla":
                    copy_to_out(T["XC"], XCB)
                else:
                    phase_moe(1, T["XC"], XCB, OUT, OUTB, True)
        P.barrier()
    return nc


def _prep_inputs(inputs):
    global _CONSTS
    if _CONSTS is None:
        _CONSTS = _consts()
    shared = {}
    for k, v in inputs.items():
        if k in ("x", "c", "positions"):
            continue
        shared[k] = np.ascontiguousarray(np.asarray(v))
    shared.update(_CONSTS)
    in_maps = []
    x = np.asarray(inputs["x"]); c = np.asarray(inputs["c"]); pos = np.asarray(inputs["positions"])
    for b in range(x.shape[0]):
        m = dict(shared)
        m["x"] = np.ascontiguousarray(x[b])
        m["c"] = np.ascontiguousarray(c[b:b + 1])
        m["pos"] = np.ascontiguousarray(pos[b].astype(np.int32))
        in_maps.append(m)
    return in_maps


def kernel(**inputs):
    in_maps = _prep_inputs(inputs)
    nc = build("all")
    res = run_bass_kernel_spmd(nc, in_maps, core_ids=list(range(len(in_maps))))
    return np.stack([r["out"] for r in res.results], axis=0).astype(np.float32)
```

```python
import math
from contextlib import ExitStack

import ml_dtypes
import numpy as np

import concourse.bass as bass
import concourse.mybir as mybir
from concourse.bass_utils import run_bass_kernel_spmd

F32 = mybir.dt.float32
BF16 = mybir.dt.bfloat16
I32 = mybir.dt.int32
U32 = mybir.dt.uint32
AF = mybir.ActivationFunctionType
ALU = mybir.AluOpType
AX = mybir.AxisListType

S = 4096
D = 1024
NT = S // 128
EPS = 1e-6
NEXP = 16
CAP = 512
TWO_PI = 2.0 * math.pi


class Buf:
    __slots__ = ("w", "r")

    def __init__(self):
        self.w = None
        self.r = []


class Prog:
    NQ = 6

    def __init__(self, nc, es):
        self.nc = nc
        self.es = es
        self.eng = {"pe": nc.tensor, "act": nc.scalar, "dve": nc.vector,
                    "pool": nc.gpsimd, "sp": nc.sync}
        self.sem = {}
        self.cnt = {}
        self.nsem = 0
        for e in ("pe", "act", "dve", "pool"):
            self.sem[e] = self._newsem()
            self.cnt[e] = 0
        self.waited = {e: {} for e in self.eng}
        self.dq = {}
        self.dqi = {}
        for q in ("sp", "act", "pool"):
            self.dq[q] = [[self._newsem(), 0] for _ in range(16 if q == "pool" else self.NQ)]
            self.dqi[q] = 0

    def _newsem(self):
        self.nsem += 1
        return self.es.enter_context(self.nc.semaphore(f"sm{self.nsem}"))

    def _wait(self, e, tok):
        sem, val, src = tok
        if src == "pe" and e == "pe":
            return
        key = id(sem)
        if self.waited[e].get(key, 0) >= val:
            return
        self.eng[e].wait_ge(sem, val)
        self.waited[e][key] = val

    def _deps(self, e, reads, writes):
        for b in reads:
            if b.w is not None:
                self._wait(e, b.w)
        for b in writes:
            if b.w is not None:
                self._wait(e, b.w)
            for t in b.r:
                self._wait(e, t)

    def _record(self, tok, reads, writes):
        for b in reads:
            b.r = [t for t in b.r if t[0] is not tok[0]]
            b.r.append(tok)
        for b in writes:
            b.w = tok
            b.r = []

    def op(self, e, fn, reads=(), writes=()):
        self._deps(e, reads, writes)
        ins = fn(self.eng[e])
        self.cnt[e] += 1
        ins.then_inc(self.sem[e], 1)
        tok = (self.sem[e], self.cnt[e], e)
        self._record(tok, reads, writes)
        return tok

    def dma(self, q, out, in_, reads=(), writes=(), fn=None, **kw):
        slot = self.dq[q][self.dqi[q] % len(self.dq[q])]
        self.dqi[q] += 1
        sem, c = slot
        if c > 0:
            self._wait(q, (sem, c, None))
        self._deps(q, reads, writes)
        if fn is None:
            ins = self.eng[q].dma_start(out=out, in_=in_, **kw)
        else:
            ins = fn(self.eng[q])
        ins.then_inc(sem, 16)
        slot[1] = c + 16
        tok = (sem, c + 16, None)
        self._record(tok, reads, writes)
        return tok

    def barrier(self):
        toks = [(self.sem[e], self.cnt[e], None) for e in self.sem if self.cnt[e] > 0]
        for q in self.dq:
            for sem, c in self.dq[q]:
                if c > 0:
                    toks.append((sem, c, None))
        for e in self.eng:
            for t in toks:
                self._wait(e, t)
        for e in self.sem:
            if self.cnt[e] > 6000:
                self.sem[e] = self._newsem()
                self.cnt[e] = 0
        for q in self.dq:
            for slot in self.dq[q]:
                if slot[1] > 6000:
                    slot[0] = self._newsem()
                    slot[1] = 0


def _consts():
    c = {}
    c["ident"] = np.eye(128, dtype=np.float32)
    L = S
    t = np.linspace(0.0, 1.0, L, dtype=np.float32)[:, None]
    w = (2.0 * math.pi * np.arange(L, dtype=np.float32)[:, None] / L).astype(np.float32)
    f = np.linspace(1e-4, 15.0, 16, dtype=np.float32)[None, :]
    ang = (f * w).astype(np.float32)
    z = np.concatenate([t, np.cos(ang), -np.sin(ang)], axis=-1).astype(np.float32)
    c["zfT"] = np.ascontiguousarray(z.T)
    c["tneg"] = np.ascontiguousarray((-t[:, 0]).reshape(NT, 128).T)
    mind = math.log(1e-2) / 0.3
    maxd = math.log(1e-2) / 1.5
    deltas = np.linspace(mind, maxd, D, dtype=np.float32)
    c["absd"] = np.abs(deltas).astype(np.float32)
    fi = np.arange(4096, dtype=np.int64)
    ti = np.arange(4096, dtype=np.int64)
    m = ((2 * fi[None, :] + 1) * ti[:, None]) % 16384
    angm = m.astype(np.float64) * (math.pi / 8192.0)
    cosm = np.cos(angm)
    sinm = np.sin(angm)
    fw = np.stack([cosm, -sinm], axis=0)
    fw = fw.reshape(2, NT, 128, 32, 128)
    c["FWD"] = np.ascontiguousarray(fw.transpose(3, 2, 0, 1, 4)).astype(ml_dtypes.bfloat16)
    sc = 2.0 / 8192.0
    iv = np.stack([cosm * sc, -sinm * sc], axis=0)
    iv = iv.reshape(2, NT, 128, 32, 128)
    c["INV"] = np.ascontiguousarray(iv.transpose(1, 4, 3, 0, 2)).astype(ml_dtypes.bfloat16)
    invf = (10000.0 ** (-np.arange(0, 64, 2, dtype=np.float32) / 64)).astype(np.float32)
    c["invf"] = invf
    return c


_CONSTS = None


def build(stage="all", debug=False):
    nc = bass.Bass("TRN2", target_bir_lowering=False)
    T = {}

    def din(name, shape, dt=F32):
        T[name] = nc.dram_tensor(name, list(shape), dt, kind="ExternalInput").ap()
        return T[name]

    def dscr(name, shape, dt=F32):
        T[name] = nc.dram_tensor(name, list(shape), dt).ap()
        return T[name]

    din("x", [S, D]); din("c", [1, D]); din("pos", [S], I32)
    din("ada_w", [2, D, 6 * D]); din("ada_b", [2, 6 * D])
    din("norm_mix_g", [2, D]); din("norm_ffn_g", [2, D])
    din("hy_w_in", [1, D, 3 * D]); din("hy_b_in", [1, 3 * D]); din("hy_conv_w", [1, 3, 3 * D])
    din("hy_conv_b", [1, 3 * D]); din("hy_f_w1", [1, 33, 64]); din("hy_f_b1", [1, 64])
    din("hy_f_w2", [1, 64, 64]); din("hy_f_b2", [1, 64]); din("hy_f_w3", [1, 64, 2 * D])
    din("hy_f_freq", [1, 64]); din("hy_f_bias", [1, 1, D]); din("hy_w_out", [1, D, D]); din("hy_b_out", [1, D])
    din("mla_w_in", [1, D, 448]); din("mla_q_norm_g", [1, 256]); din("mla_w_qb", [1, 256, 1536])
    din("mla_kv_norm_g", [1, 128]); din("mla_w_kvb", [1, 128, 2048]); din("mla_w_out", [1, D, D])
    din("moe_w_router", [2, D, NEXP]); din("moe_w_gate", [2, NEXP, D, D]); din("moe_w_up", [2, NEXP, D, D])
    din("moe_w_down", [2, NEXP, D, D]); din("final_norm_g", [D])
    din("ident", [128, 128]); din("zfT", [33, S]); din("tneg", [128, NT]); din("absd", [D])
    din("FWD", [32, 128, 2, 32, 128], BF16); din("INV", [32, 128, 32, 2, 128], BF16); din("invf", [32])
    OUT = nc.dram_tensor("out", [S, D], F32, kind="ExternalOutput").ap()

    dscr("MOD", [2, 6 * D])
    dscr("ZT", [S, D], BF16)
    dscr("X0C", [D, S], BF16)
    dscr("KS", [2, S, D])
    dscr("KT", [2, S, D], BF16)
    dscr("YS", [32, 128, 2, D], BF16)
    dscr("XA", [S, D]); dscr("XB", [S, D]); dscr("XC", [S, D])
    dscr("HF", [S, D], BF16); dscr("ACC", [S, D])
    dscr("QN", [8, 128, S], BF16); dscr("KN", [8, 128, S], BF16); dscr("QP", [8, 64, S], BF16)
    dscr("KP", [64, S], BF16); dscr("V", [S, D], BF16)

    es = ExitStack()
    with es:
        P = Prog(nc, es)
        ps = [es.enter_context(nc.psum_tensor(f"ps{i}", [128, 512], F32)) for i in range(8)]
        psB = [Buf() for _ in range(8)]
        psi = [0]

        def bank():
            i = psi[0] % 8
            psi[0] += 1
            return ps[i], psB[i]

        sbn = [0]

        def sb(st, name, shape, dt):
            sbn[0] += 1
            return st.enter_context(nc.sbuf_tensor(f"{name}_{sbn[0]}", list(shape), dt))

        identf = sb(es, "identf", [128, 128], F32); identfB = Buf()
        identb = sb(es, "identb", [128, 128], BF16); identbB = Buf()
        P.dma("sp", identf[:], T["ident"][:, :], writes=[identfB])
        P.dma("pool", identb[:], T["ident"][:, :], writes=[identbB])

        def bcast_load(q, dst, dstB, src1d):
            return P.dma(q, dst, src1d.partition_broadcast(128), writes=[dstB])

        def phase_mod():
            with ExitStack() as st:
                ccol = sb(st, "ccol", [128, 8], F32); ccolB = Buf()
                cs = sb(st, "cs", [128, 8], F32); csB_ = Buf()
                csb = sb(st, "csb", [128, 8, 128], F32); csbB = Buf()
                adb = sb(st, "adb", [128, 6 * D], F32); adbB = Buf()
                modt = sb(st, "modt", [128, 6 * D], F32); modtB = Buf()
                wb = [sb(st, f"adw{i}", [128, 8, 512], F32) for i in range(2)]
                wbB = [Buf(), Buf()]
                P.dma("sp", ccol[:], T["c"].rearrange("o (kc p) -> p (o kc)", p=128), writes=[ccolB],
                      allow_slow_non_contiguous=True)
                P.op("act", lambda e: e.activation(out=cs[:], in_=ccol[:], func=AF.Silu), [ccolB], [csB_])
                for kc in range(8):
                    P.op("dve", lambda e, kc=kc: e.tensor_copy(out=csb[:, kc, :], in_=cs[:, kc:kc + 1].to_broadcast([128, 128])),
                         [csB_], [csbB])
                n = 0
                for i in range(2):
                    bcast_load("sp", adb[:], adbB, T["ada_b"][i, :])
                    wv = T["ada_w"][i].rearrange("(kc p) n -> p kc n", p=128)
                    for q in range(12):
                        w, wB = wb[n % 2], wbB[n % 2]
                        n += 1
                        P.dma("sp", w[:], wv[:, :, q * 512:(q + 1) * 512], writes=[wB])
                        pt, pB = bank()
                        for kc in range(8):
                            P.op("pe", lambda e, kc=kc, w=w, pt=pt: e.matmul(pt[:], lhsT=csb[:, kc, :], rhs=w[:, kc, :],
                                                                              start=(kc == 0), stop=(kc == 7)),
                                 [csbB, wB], [pB])
                        P.op("dve", lambda e, q=q, pt=pt: e.tensor_tensor(out=modt[:, q * 512:(q + 1) * 512], in0=pt[:],
                                                                         in1=adb[:, q * 512:(q + 1) * 512], op=ALU.add),
                             [pB, adbB], [modtB])
                    P.dma("sp", T["MOD"][i:i + 1, :], modt[0:1, :], reads=[modtB], writes=[MODB])
                P.barrier()

        MODB = Buf()

        def load_mod_tiles(st, layer, which, gname):
            base = 0 if which == 0 else 3
            A = sb(st, f"A{layer}{which}", [128, D], F32); AB = Buf()
            Bt = sb(st, f"B{layer}{which}", [128, D], F32); BB = Buf()
            G = sb(st, f"G{layer}{which}", [128, D], F32); GB = Buf()
            gt = sb(st, f"g{layer}{which}", [128, D], F32); gB = Buf()
            P.dma("sp", Bt[:], T["MOD"][layer, (base + 0) * D:(base + 1) * D].partition_broadcast(128), reads=[MODB], writes=[BB])
            P.dma("sp", A[:], T["MOD"][layer, (base + 1) * D:(base + 2) * D].partition_broadcast(128), reads=[MODB], writes=[AB])
            P.dma("sp", G[:], T["MOD"][layer, (base + 2) * D:(base + 3) * D].partition_broadcast(128), reads=[MODB], writes=[GB])
            bcast_load("sp", gt[:], gB, T[gname][layer, :])
            P.op("dve", lambda e: e.scalar_tensor_tensor(out=A[:], in0=A[:], scalar=1.0, in1=gt[:], op0=ALU.add, op1=ALU.mult),
                 [gB, AB], [AB])
            return (A, AB), (Bt, BB), (G, GB)

        class NormCtx:
            def __init__(self, st, tag):
                self.junk = sb(st, f"junk{tag}", [128, D], F32); self.junkB = Buf()
                self.ss = [sb(st, f"ss{tag}{i}", [128, 4], F32) for i in range(2)]; self.ssB = [Buf(), Buf()]
                self.tmp = [sb(st, f"nt{tag}{i}", [128, D], F32) for i in range(2)]; self.tmpB = [Buf(), Buf()]
                self.n = 0

        def norm_mod_g(nx, xt, xtB, A, AB, Bt, BB, out, outB):
            k = nx.n % 2
            nx.n += 1
            ss, ssB = nx.ss[k], nx.ssB[k]
            tmp, tmpB = nx.tmp[k][:], nx.tmpB[k]
            P.op("act", lambda e: e.activation(out=nx.junk[:], in_=xt, func=AF.Square, accum_out=ss[:, 0:1]),
                 [xtB], [nx.junkB, ssB])
            yield
            P.op("dve", lambda e: e.tensor_scalar(out=ss[:, 1:2], in0=ss[:, 0:1], scalar1=1.0 / D, scalar2=EPS,
                                                  op0=ALU.mult, op1=ALU.add), [ssB], [ssB])
            yield
            P.op("act", lambda e: e.activation(out=ss[:, 2:3], in_=ss[:, 1:2], func=AF.Sqrt), [ssB], [ssB])
            yield
            P.op("dve", lambda e: e.reciprocal(out=ss[:, 3:4], in_=ss[:, 2:3]), [ssB], [ssB])
            yield
            if Bt is None:
                P.op("dve", lambda e: e.scalar_tensor_tensor(out=out, in0=xt, scalar=ss[:, 3:4], in1=A[:],
                                                             op0=ALU.mult, op1=ALU.mult), [xtB, ssB, AB], [outB])
                return
            P.op("dve", lambda e: e.scalar_tensor_tensor(out=tmp, in0=xt, scalar=ss[:, 3:4], in1=A[:],
                                                         op0=ALU.mult, op1=ALU.mult), [xtB, ssB, AB], [tmpB])
            yield
            P.op("pool", lambda e: e.tensor_tensor(out=out, in0=tmp, in1=Bt[:], op=ALU.add), [tmpB, BB], [outB])

        def norm_mod(nx, xt, xtB, A, AB, Bt, BB, out, outB, tmp_unused=None, tmpB_unused=None):
            for _ in norm_mod_g(nx, xt, xtB, A, AB, Bt, BB, out, outB):
                pass

        def run_pipelined(n, make, depth=2, on_start=None):
            active = []
            nxt = 0
            while nxt < n or active:
                while len(active) < depth and nxt < n:
                    if on_start is not None:
                        on_start(nxt)
                    active.append(make(nxt))
                    nxt += 1
                for g in list(active):
                    try:
                        next(g)
                    except StopIteration:
                        active.remove(g)

        def fwd_dft(st, A, AB_, Bm, BB_, consume):
            fwb = [sb(st, f"fw{i}", [128, 2, 32, 128], BF16) for i in range(2)]
            fwB = [Buf(), Buf()]
            def load_fw(fc):
                fw, fB = fwb[fc % 2], fwB[fc % 2]
                P.dma("sp", fw[:, 0], T["FWD"][fc, :, 0], writes=[fB])
                P.dma("sp", fw[:, 1], T["FWD"][fc, :, 1], writes=[fB])

            load_fw(0)
            for fc in range(32):
                fw, fB = fwb[fc % 2], fwB[fc % 2]
                if fc + 1 < 32:
                    load_fw(fc + 1)
                res = []
                for h in range(2):
                    for cs_ in range(2):
                        src, sB = (A, AB_) if cs_ == 0 else (Bm, BB_)
                        pt, pB = bank()
                        for tt in range(32):
                            P.op("pe", lambda e, pt=pt, fw=fw, cs_=cs_, tt=tt, src=src, h=h:
                                 e.matmul(pt[:], lhsT=fw[:, cs_, tt, :], rhs=src[:, tt, h * 512:(h + 1) * 512],
                                          start=(tt == 0), stop=(tt == 31)), [fB, sB], [pB])
                        res.append((pt, pB))
                consume(fc, res)

        def phase_filter():
            with ExitStack() as st:
                KTB = Buf()
                with ExitStack() as s2:
                    kab = [sb(s2, f"kab{i}", [128, 2, D], BF16) for i in range(2)]; kabB = [Buf(), Buf()]
                    zf = sb(s2, "zf", [33, S], F32); zfB = Buf()
                    h1 = sb(s2, "h1", [64, S], F32); h1B = Buf()
                    h2 = sb(s2, "h2", [64, S], F32); h2B = Buf()
                    w1 = sb(s2, "fw1", [33, 64], F32); w1B = Buf()
                    w2 = sb(s2, "fw2", [64, 64], F32); w2B = Buf()
                    w3 = sb(s2, "fw3", [64, 2 * D], F32); w3B = Buf()
                    col = sb(s2, "fcol", [64, 8], F32); colB = Buf()
                    pre = sb(s2, "fpre", [64, 512], F32); preB = Buf()
                    ki = sb(s2, "fki", [64, 512], I32); kiB = Buf()
                    kf = sb(s2, "fkf", [64, 512], F32); kfB = Buf()
                    absd = sb(s2, "absd", [128, D], F32); absdB = Buf()
                    tneg = sb(s2, "tneg", [128, NT], F32); tnegB = Buf()
                    dec = sb(s2, "dec", [128, D], F32); decB = Buf()
                    kfw = sb(s2, "kfw", [128, D], F32); kfwB = Buf()
                    kbw = sb(s2, "kbw", [128, D], F32); kbwB = Buf()
                    fbias = sb(s2, "fbias", [1, D], F32); fbiasB = Buf()
                    P.dma("sp", zf[:], T["zfT"][:, :], writes=[zfB])
                    P.dma("sp", w1[:], T["hy_f_w1"][0], writes=[w1B])
                    P.dma("sp", w2[:], T["hy_f_w2"][0], writes=[w2B])
                    P.dma("sp", w3[:], T["hy_f_w3"][0], writes=[w3B])
                    P.dma("sp", col[:, 0:1], T["hy_f_b1"][0].rearrange("(p o) -> p o", o=1), writes=[colB])
                    P.dma("sp", col[:, 1:2], T["hy_f_b2"][0].rearrange("(p o) -> p o", o=1), writes=[colB])
                    P.dma("sp", col[:, 2:3], T["hy_f_freq"][0].rearrange("(p o) -> p o", o=1), writes=[colB])
                    P.dma("sp", tneg[:], T["tneg"][:, :], writes=[tnegB])
                    P.dma("sp", fbias[:], T["hy_f_bias"][0], writes=[fbiasB])
                    bcast_load("sp", absd[:], absdB, T["absd"])
                    P.op("dve", lambda e: e.tensor_tensor(out=col[:, 3:4], in0=col[:, 0:1], in1=col[:, 2:3], op=ALU.mult), [colB], [colB])
                    P.op("dve", lambda e: e.tensor_tensor(out=col[:, 4:5], in0=col[:, 1:2], in1=col[:, 2:3], op=ALU.mult), [colB], [colB])

                    def sin_layer(w, wB, src, srcB, K, bcol, dst, dstB):
                        for tch in range(8):
                            pt, pB = bank()
                            P.op("pe", lambda e, pt=pt, tch=tch: e.matmul(pt[0:64, :], lhsT=w[0:K, :], rhs=src[0:K, tch * 512:(tch + 1) * 512],
                                                                          start=True, stop=True), [wB, srcB], [pB])
                            P.op("dve", lambda e, pt=pt: e.tensor_scalar(out=pre[:], in0=pt[0:64, :], scalar1=col[:, 2:3], scalar2=col[:, bcol:bcol + 1],
                                                                         op0=ALU.mult, op1=ALU.add), [pB, colB], [preB])
                            P.op("dve", lambda e: e.tensor_scalar(out=ki[:], in0=pre[:], scalar1=1.0 / TWO_PI, scalar2=None, op0=ALU.mult), [preB], [kiB])
                            P.op("dve", lambda e: e.tensor_copy(out=kf[:], in_=ki[:]), [kiB], [kfB])
                            P.op("dve", lambda e: e.scalar_tensor_tensor(out=pre[:], in0=kf[:], scalar=-TWO_PI, in1=pre[:], op0=ALU.mult, op1=ALU.add),
                                 [kfB, preB], [preB])
                            P.op("dve", lambda e: e.tensor_scalar(out=pre[:], in0=pre[:], scalar1=math.pi, scalar2=-math.pi, op0=ALU.min, op1=ALU.max), [preB], [preB])
                            P.op("act", lambda e, tch=tch: e.activation(out=dst[:, tch * 512:(tch + 1) * 512], in_=pre[:], func=AF.Sin), [preB], [dstB])

                    sin_layer(w1, w1B, zf, zfB, 33, 3, h1, h1B)
                    sin_layer(w2, w2B, h1, h1B, 64, 4, h2, h2B)
                    for tt in range(NT):
                        P.op("act", lambda e, tt=tt: e.activation(out=dec[:], in_=absd[:], func=AF.Exp, scale=tneg[:, tt:tt + 1]), [absdB, tnegB], [decB])
                        for q in range(4):
                            pt, pB = bank()
                            P.op("pe", lambda e, pt=pt, tt=tt, q=q: e.matmul(pt[:], lhsT=h2[:, tt * 128:(tt + 1) * 128], rhs=w3[:, q * 512:(q + 1) * 512],
                                                                             start=True, stop=True), [h2B, w3B], [pB])
                            dst, dB = (kfw, kfwB) if q < 2 else (kbw, kbwB)
                            P.op("dve", lambda e, pt=pt, q=q, dst=dst: e.tensor_tensor(out=dst[:, (q % 2) * 512:(q % 2 + 1) * 512], in0=pt[:],
                                                                                       in1=dec[:, (q % 2) * 512:(q % 2 + 1) * 512], op=ALU.mult),
                                 [pB, decB], [dB])
                        if tt == 0:
                            P.op("dve", lambda e: e.tensor_tensor(out=kfw[0:1, :], in0=kfw[0:1, :], in1=fbias[0:1, :], op=ALU.add), [kfwB, fbiasB], [kfwB])
                            P.op("dve", lambda e: e.memset(kbw[0:1, :], 0.0), [], [kbwB])
                        ka, kaB = kab[tt % 2], kabB[tt % 2]
                        P.op("pool", lambda e, ka=ka: e.tensor_tensor(out=ka[:, 0, :], in0=kfw[:], in1=kbw[:], op=ALU.add), [kfwB, kbwB], [kaB])
                        P.op("dve", lambda e, ka=ka: e.tensor_tensor(out=ka[:, 1, :], in0=kfw[:], in1=kbw[:], op=ALU.subtract), [kfwB, kbwB], [kaB])
                        for j in range(2):
                            P.dma("sp", T["KT"][j, tt * 128:(tt + 1) * 128, :], ka[:, j, :], reads=[kaB], writes=[KTB])
                    P.barrier()
                with ExitStack() as s3:
                    KA = sb(s3, "KA", [128, NT, D], BF16); KAB = Buf()
                    KBm = sb(s3, "KBm", [128, NT, D], BF16); KBB = Buf()
                    for j, (kt, ktB) in enumerate(((KA, KAB), (KBm, KBB))):
                        kv = T["KT"][j].rearrange("(tt p) c -> p tt c", p=128)
                        for q in range(4):
                            P.dma("sp", kt[:, q * 8:(q + 1) * 8, :], kv[:, q * 8:(q + 1) * 8, :], reads=[KTB], writes=[ktB])
                    stg = [sb(s3, f"kst{i}", [128, 2, D], F32) for i in range(2)]
                    stgB = [Buf(), Buf()]

                    def store(fc, res):
                        sg, sB = stg[fc % 2], stgB[fc % 2]
                        for h in range(2):
                            for cs_ in range(2):
                                pt, pB = res[h * 2 + cs_]
                                P.op("act", lambda e, pt=pt, h=h, cs_=cs_, sg=sg: e.activation(out=sg[:, cs_, h * 512:(h + 1) * 512], in_=pt[:], func=AF.Identity),
                                     [pB], [sB])
                        for cs_ in range(2):
                            P.dma("sp", T["KS"][cs_, fc * 128:(fc + 1) * 128, :], sg[:, cs_, :], reads=[sB], writes=[KSB])

                    fwd_dft(s3, KA, KAB, KBm, KBB, store)
                    P.barrier()

        KSB = Buf()
        ZTB = Buf(); X0CB = Buf(); YSB = Buf(); XAB = Buf()

        def phase_hyena_in():
            with ExitStack() as st:
                hmT = sb(st, "hmT", [128, 8, S], BF16); hmTB = Buf()
                with ExitStack() as s2:
                    (A, AB), (Bt, BB), _ = load_mod_tiles(s2, 0, 0, "norm_mix_g")
                    nx = NormCtx(s2, "h")
                    xb = [sb(s2, f"hx{i}", [128, D], F32) for i in range(3)]; xbB = [Buf(), Buf(), Buf()]
                    hb = [sb(s2, f"hb{i}", [128, D], BF16) for i in range(2)]; hbB = [Buf(), Buf()]

                    def ld_hx(tt):
                        if tt < NT:
                            P.dma("sp", xb[tt % 3][:], T["x"][tt * 128:(tt + 1) * 128, :], writes=[xbB[tt % 3]])

                    ld_hx(0)

                    def h_tile(tt):
                        xt, xtB = xb[tt % 3], xbB[tt % 3]
                        h_, hB_ = hb[tt % 2], hbB[tt % 2]
                        yield from norm_mod_g(nx, xt[:], xtB, A, AB, Bt, BB, h_[:], hB_)
                        yield
                        pt, pB = bank()
                        ptb = pt[:].bitcast(BF16)
                        for kc in range(8):
                            P.op("pe", lambda e, kc=kc: e.transpose(out=ptb[:, kc * 128:(kc + 1) * 128], in_=h_[:, kc * 128:(kc + 1) * 128], identity=identb[:]),
                                 [hB_, identbB], [pB])
                        yield
                        P.op("act", lambda e: e.activation(out=hmT[:, :, tt * 128:(tt + 1) * 128], in_=ptb.rearrange("p (k t) -> p k t", k=8), func=AF.Identity),
                             [pB], [hmTB])

                    run_pipelined(NT, h_tile, depth=2, on_start=lambda tt: ld_hx(tt + 1))
                    P.barrier()
                with ExitStack() as s2:
                    bcol = sb(s2, "bcol", [128, 24], F32); bcolB = Buf()
                    cw = sb(s2, "cw", [128, 3, 24], F32); cwB = Buf()
                    cbc = sb(s2, "cbc", [128, 24], F32); cbcB = Buf()
                    P.dma("sp", bcol[:], T["hy_b_in"][0].rearrange("(cc p) -> p cc", p=128), writes=[bcolB], allow_slow_non_contiguous=True)
                    P.dma("sp", cbc[:], T["hy_conv_b"][0].rearrange("(cc p) -> p cc", p=128), writes=[cbcB], allow_slow_non_contiguous=True)
                    for j in range(3):
                        P.dma("sp", cw[:, j, :], T["hy_conv_w"][0, j].rearrange("(cc p) -> p cc", p=128), writes=[cwB], allow_slow_non_contiguous=True)
                    wch = [sb(s2, f"wch{i}", [128, 8, 128], BF16) for i in range(2)]; wchB = [Buf(), Buf()]
                    us = [sb(s2, f"u{i}", [128, S + 2], F32) for i in range(2)]; usB = [Buf(), Buf()]
                    ob = [sb(s2, f"ob{i}", [128, S], F32) for i in range(2)]; obB = [Buf(), Buf()]
                    zb = sb(s2, "zb", [128, S], BF16); zbB = Buf()
                    zst = sb(s2, "zst", [128, NT, 128], BF16); zstB = Buf()
                    for u, uB in zip(us, usB):
                        P.op("dve", lambda e, u=u: e.memset(u[:, 0:1], 0.0), [], [uB])
                        P.op("dve", lambda e, u=u: e.memset(u[:, S + 1:S + 2], 0.0), [], [uB])
                    wv = T["hy_w_in"][0].rearrange("(kc p) n -> p kc n", p=128)
                    order = []
                    for j in range(8):
                        order += [8 + j, 16 + j]
                    order += list(range(8))
                    for n, cc in enumerate(order):
                        w, wB = wch[n % 2], wchB[n % 2]
                        u, uB = us[n % 2], usB[n % 2]
                        P.dma("pool", w[:], wv[:, :, cc * 128:(cc + 1) * 128], writes=[wB])
                        for tch in range(8):
                            pt, pB = bank()
                            for kc in range(8):
                                P.op("pe", lambda e, pt=pt, kc=kc, w=w, tch=tch: e.matmul(pt[:], lhsT=w[:, kc, :], rhs=hmT[:, kc, tch * 512:(tch + 1) * 512],
                                                                                         start=(kc == 0), stop=(kc == 7)), [wB, hmTB], [pB])
                            P.op("act", lambda e, pt=pt, tch=tch, cc=cc, u=u: e.activation(out=u[:, 1 + tch * 512:1 + (tch + 1) * 512], in_=pt[:], func=AF.Identity,
                                                                                    bias=bcol[:, cc:cc + 1]), [pB, bcolB], [uB])
                        o, oB = ob[n % 2], obB[n % 2]
                        P.op("act", lambda e, o=o, cc=cc, u=u: e.activation(out=o[:], in_=u[:, 1:S + 1], func=AF.Identity, scale=cw[:, 1, cc:cc + 1], bias=cbc[:, cc:cc + 1]),
                             [uB, cwB, cbcB], [oB])
                        P.op("dve", lambda e, o=o, cc=cc, u=u: e.scalar_tensor_tensor(out=o[:], in0=u[:, 0:S], scalar=cw[:, 0, cc:cc + 1], in1=o[:], op0=ALU.mult, op1=ALU.add),
                             [uB, cwB, oB], [oB])
                        if cc >= 8:
                            P.op("dve", lambda e, o=o, cc=cc, u=u: e.scalar_tensor_tensor(out=o[:], in0=u[:, 2:S + 2], scalar=cw[:, 2, cc:cc + 1], in1=o[:], op0=ALU.mult, op1=ALU.add),
                                 [uB, cwB, oB], [oB])
                        else:
                            P.op("dve", lambda e, o=o, cc=cc, u=u: e.scalar_tensor_tensor(out=zb[:], in0=u[:, 2:S + 2], scalar=cw[:, 2, cc:cc + 1], in1=o[:], op0=ALU.mult, op1=ALU.add),
                                 [uB, cwB, oB], [zbB])
                            P.dma("sp", T["X0C"][cc * 128:(cc + 1) * 128, :], zb[:], reads=[zbB], writes=[X0CB])
                        if cc >= 16:
                            j = cc - 16
                            o1, o1B = ob[(n - 1) % 2], obB[(n - 1) % 2]
                            P.op("pool", lambda e, o=o, o1=o1: e.tensor_tensor(out=zb[:], in0=o[:], in1=o1[:], op=ALU.mult), [oB, o1B], [zbB])
                            for g4 in range(4):
                                pt, pB = bank()
                                ptb = pt[:].bitcast(BF16)
                                for k in range(8):
                                    tt = g4 * 8 + k
                                    P.op("pe", lambda e, ptb=ptb, k=k, tt=tt: e.transpose(out=ptb[:, k * 128:(k + 1) * 128], in_=zb[:, tt * 128:(tt + 1) * 128], identity=identb[:]),
                                         [zbB, identbB], [pB])
                                P.op("act", lambda e, ptb=ptb, g4=g4: e.activation(out=zst[:, g4 * 8:(g4 + 1) * 8, :], in_=ptb.rearrange("p (k c) -> p k c", k=8), func=AF.Identity),
                                     [pB], [zstB])
                            P.dma("sp", T["ZT"].rearrange("(tt p) c -> p tt c", p=128)[:, :, j * 128:(j + 1) * 128], zst[:], reads=[zstB], writes=[ZTB])
                    P.barrier()

        def phase_hyena_fwd():
            with ExitStack() as st:
                zt = sb(st, "zt", [128, NT, D], BF16); ztB = Buf()
                zv = T["ZT"].rearrange("(tt p) c -> p tt c", p=128)
                for q in range(4):
                    P.dma("sp", zt[:, q * 8:(q + 1) * 8, :], zv[:, q * 8:(q + 1) * 8, :], reads=[ZTB], writes=[ztB])
                kk = [sb(st, f"kk{i}", [128, 2, D], F32) for i in range(2)]; kkB = [Buf(), Buf()]
                yt = [sb(st, f"yt{i}", [128, 2, D], BF16) for i in range(2)]; ytB = [Buf(), Buf()]
                t1 = sb(st, "yt1", [128, 512], F32); t1B = Buf()
                t2 = sb(st, "yt2", [128, 512], F32); t2B = Buf()
                t3 = sb(st, "yt3", [128, 512], F32); t3B = Buf()
                t4 = sb(st, "yt4", [128, 512], F32); t4B = Buf()

                def mulk(fc, res):
                    k, kB = kk[fc % 2], kkB[fc % 2]
                    y, yB = yt[fc % 2], ytB[fc % 2]
                    for cs_ in range(2):
                        P.dma("sp", k[:, cs_, :], T["KS"][cs_, fc * 128:(fc + 1) * 128, :], reads=[KSB], writes=[kB])
                    for h in range(2):
                        zr, zrB = res[h * 2]
                        zi, ziB = res[h * 2 + 1]
                        sl = slice(h * 512, (h + 1) * 512)
                        P.op("dve", lambda e, zr=zr, sl=sl: e.tensor_tensor(out=t1[:], in0=zr[:], in1=k[:, 0, sl], op=ALU.mult), [zrB, kB], [t1B])
                        P.op("dve", lambda e, zi=zi, sl=sl: e.tensor_tensor(out=t2[:], in0=zi[:], in1=k[:, 1, sl], op=ALU.mult), [ziB, kB], [t2B])
                        P.op("dve", lambda e, zr=zr, sl=sl: e.tensor_tensor(out=t3[:], in0=zr[:], in1=k[:, 1, sl], op=ALU.mult), [zrB, kB], [t3B])
                        P.op("dve", lambda e, zi=zi, sl=sl: e.tensor_tensor(out=t4[:], in0=zi[:], in1=k[:, 0, sl], op=ALU.mult), [ziB, kB], [t4B])
                        P.op("pool", lambda e, sl=sl, y=y: e.tensor_tensor(out=y[:, 0, sl], in0=t1[:], in1=t2[:], op=ALU.subtract), [t1B, t2B], [yB])
                        P.op("pool", lambda e, sl=sl, y=y: e.tensor_tensor(out=y[:, 1, sl], in0=t3[:], in1=t4[:], op=ALU.add), [t3B, t4B], [yB])
                    P.dma("sp", T["YS"][fc], y[:], reads=[yB], writes=[YSB])

                fwd_dft(st, zt, ztB, zt, ztB, mulk)
                P.barrier()

        def phase_hyena_out():
            with ExitStack() as st:
                X0 = sb(st, "X0", [128, 8, S], BF16); X0B = Buf()
                xv = T["X0C"].rearrange("(cc p) t -> p cc t", p=128)
                for cc in range(8):
                    P.dma("sp", X0[:, cc, :], xv[:, cc, :], reads=[X0CB], writes=[X0B])
                with ExitStack() as s2:
                    Yh = sb(s2, "Yh", [128, 32, 2, 512], BF16); YhB = Buf()
                    gvb = [sb(s2, f"gv{i}", [128, 32, 2, 128], BF16) for i in range(2)]; gvB = [Buf(), Buf()]
                    ytm = [sb(s2, f"ytm{i}", [128, 512], BF16) for i in range(2)]; ytmB = [Buf(), Buf()]
                    yv = T["YS"].rearrange("fc p cs c -> p fc cs c")
                    prev = None

                    def xpose(h, to, ym, ymB):
                        p2, p2B = bank()
                        p2b = p2[:].bitcast(BF16)
                        for j in range(4):
                            P.op("pe", lambda e, j=j: e.transpose(out=p2b[:, j * 128:(j + 1) * 128], in_=ym[:, j * 128:(j + 1) * 128], identity=identb[:]),
                                 [ymB, identbB], [p2B])
                        P.op("dve", lambda e: e.tensor_tensor(out=X0[:, h * 4:(h + 1) * 4, to * 128:(to + 1) * 128],
                                                              in0=p2b[:, 0:512].rearrange("p (j t) -> p j t", j=4),
                                                              in1=X0[:, h * 4:(h + 1) * 4, to * 128:(to + 1) * 128], op=ALU.mult),
                             [p2B, X0B], [X0B])

                    for h in range(2):
                        for q in range(4):
                            for cs_ in range(2):
                                P.dma("sp", Yh[:, q * 8:(q + 1) * 8, cs_, :], yv[:, q * 8:(q + 1) * 8, cs_, h * 512:(h + 1) * 512], reads=[YSB], writes=[YhB])
                        for to in range(NT):
                            gv, gB = gvb[to % 2], gvB[to % 2]
                            for q in range(2):
                                P.dma("sp", gv[:, q * 16:(q + 1) * 16], T["INV"][to, :, q * 16:(q + 1) * 16], writes=[gB])
                            pt, pB = bank()
                            n = 0
                            for fc in range(32):
                                for cs_ in range(2):
                                    P.op("pe", lambda e, pt=pt, gv=gv, fc=fc, cs_=cs_, n=n: e.matmul(pt[:], lhsT=gv[:, fc, cs_, :], rhs=Yh[:, fc, cs_, :],
                                                                                               start=(n == 0), stop=(n == 63)), [gB, YhB], [pB])
                                    n += 1
                            ym, ymB = ytm[to % 2], ytmB[to % 2]
                            P.op("act", lambda e, pt=pt, ym=ym: e.activation(out=ym[:], in_=pt[:], func=AF.Identity), [pB], [ymB])
                            if prev is not None:
                                xpose(*prev)
                            prev = (h, to, ym, ymB)
                    xpose(*prev)
                    P.barrier()
                with ExitStack() as s2:
                    wo = sb(s2, "wo", [128, 8, D], BF16); woB = Buf()
                    wv = T["hy_w_out"][0].rearrange("(kc p) n -> p kc n", p=128)
                    for q in range(2):
                        P.dma("pool", wo[:, q * 4:(q + 1) * 4, :], wv[:, q * 4:(q + 1) * 4, :], writes=[woB])
                    _, _, (G, GB) = load_mod_tiles(s2, 0, 0, "norm_mix_g")
                    bo = sb(s2, "bo", [128, D], F32); boB = Buf()
                    bcast_load("sp", bo[:], boB, T["hy_b_out"][0, :])
                    xb = [sb(s2, f"ox{i}", [128, D], F32) for i in range(2)]; xbB = [Buf(), Buf()]
                    yo = [sb(s2, f"oy{i}", [128, D], F32) for i in range(2)]; yoB = [Buf(), Buf()]
                    P.dma("sp", xb[0][:], T["x"][0:128, :], writes=[xbB[0]])
                    for tt in range(NT):
                        xt, xtB = xb[tt % 2], xbB[tt % 2]
                        y, yB = yo[tt % 2], yoB[tt % 2]
                        if tt + 1 < NT:
                            P.dma("sp", xb[(tt + 1) % 2][:], T["x"][(tt + 1) * 128:(tt + 2) * 128, :], writes=[xbB[(tt + 1) % 2]])
                        for nh in range(2):
                            pt, pB = bank()
                            for cc in range(8):
                                P.op("pe", lambda e, pt=pt, cc=cc, tt=tt, nh=nh: e.matmul(pt[:], lhsT=X0[:, cc, tt * 128:(tt + 1) * 128], rhs=wo[:, cc, nh * 512:(nh + 1) * 512],
                                                                                         start=(cc == 0), stop=(cc == 7)), [X0B, woB], [pB])
                            sl = slice(nh * 512, (nh + 1) * 512)
                            P.op("dve", lambda e, pt=pt, sl=sl, y=y: e.tensor_tensor(out=y[:, sl], in0=pt[:], in1=bo[:, sl], op=ALU.add), [pB, boB], [yB])
                        P.op("pool", lambda e, y=y: e.tensor_tensor(out=y[:], in0=y[:], in1=G[:], op=ALU.mult), [yB, GB], [yB])
                        P.op("dve", lambda e, y=y, xt=xt: e.tensor_tensor(out=y[:], in0=y[:], in1=xt[:], op=ALU.add), [yB, xtB], [yB])
                        P.dma("sp", T["XA"][tt * 128:(tt + 1) * 128, :], y[:], reads=[yB], writes=[XAB])
                    P.barrier()

        HFB = Buf(); ACCB = Buf()

        def phase_moe(layer, XIN, XINB, XOUT, XOUTB, final):
            with ExitStack() as st:
                IDX = sb(st, "IDX", [128, 4, NEXP], U32); IDXB = Buf()
                GVt = sb(st, "GVt", [128, 4, NEXP], F32); GVB = Buf()
                (A, AB), (Bt, BB), (G, GB) = load_mod_tiles(st, layer, 1, "norm_ffn_g")
                wts = [[sb(st, f"w{n}{i}", [128, 8, D], BF16) for n in "gud"] for i in range(2)]
                wtsB = [[Buf() for _ in range(3)] for i in range(2)]
                wnames = ("moe_w_gate", "moe_w_up", "moe_w_down")

                def issue_wloads(e_):
                    for n in range(3):
                        wv = T[wnames[n]][layer, e_].rearrange("(kc p) n -> p kc n", p=128)
                        w, wB = wts[e_ % 2][n], wtsB[e_ % 2][n]
                        P.dma("pool", w[:], wv[:, :, :], writes=[wB])

                issue_wloads(0)
                issue_wloads(1)
                with ExitStack() as s1:
                    AFFT = sb(s1, "AFFT", [NEXP, S], F32); AFFTB = Buf()
                    with ExitStack() as s2:
                        nx = NormCtx(s2, "m")
                        wr = sb(s2, "wr", [128, 8, NEXP], F32); wrB = Buf()
                        P.dma("sp", wr[:], T["moe_w_router"][layer].rearrange("(kc p) e -> p kc e", p=128), writes=[wrB])
                        zt_ = sb(s2, "zero", [128, D], F32); ztB_ = Buf()
                        P.op("pool", lambda e: e.memset(zt_[:], 0.0), [], [ztB_])
                        for tt in range(NT):
                            P.dma("sp", T["ACC"][tt * 128:(tt + 1) * 128, :], zt_[:], reads=[ztB_], writes=[ACCB])
                        xb = [sb(s2, f"mx{i}", [128, D], F32) for i in range(3)]; xbB = [Buf(), Buf(), Buf()]
                        hf = [sb(s2, f"hf{i}", [128, D], F32) for i in range(2)]; hfB = [Buf(), Buf()]
                        hfb = [sb(s2, f"hfb{i}", [128, D], BF16) for i in range(2)]; hfbB = [Buf(), Buf()]
                        hfT = [sb(s2, f"hfT{i}", [128, 8, 128], F32) for i in range(2)]; hfTB = [Buf(), Buf()]
                        sm = [sb(s2, f"sm{i}", [128, 8], F32) for i in range(2)]; smB = [Buf(), Buf()]
                        ex = [sb(s2, f"ex{i}", [128, NEXP], F32) for i in range(2)]; exB = [Buf(), Buf()]
                        aff = [sb(s2, f"aff{i}", [128, NEXP], F32) for i in range(2)]; affB = [Buf(), Buf()]

                        def ld_x(tt):
                            if tt < NT:
                                P.dma("sp", xb[tt % 3][:], XIN[tt * 128:(tt + 1) * 128, :], reads=[XINB], writes=[xbB[tt % 3]])

                        ld_x(0)

                        def m1_tile(tt):
                            k = tt % 2
                            xt, xtB = xb[tt % 3], xbB[tt % 3]
                            h_, hB_ = hf[k], hfB[k]
                            hb_, hbB_ = hfb[k], hfbB[k]
                            hT, hTB = hfT[k], hfTB[k]
                            sm_, smB_ = sm[k], smB[k]
                            ex_, exB_ = ex[k], exB[k]
                            af_, afB_ = aff[k], affB[k]
                            yield from norm_mod_g(nx, xt[:], xtB, A, AB, Bt, BB, h_[:], hB_)
                            yield
                            P.op("act", lambda e: e.activation(out=hb_[:], in_=h_[:], func=AF.Identity), [hB_], [hbB_])
                            for half in range(2):
                                pt, pB = bank()
                                for k4 in range(4):
                                    kc = half * 4 + k4
                                    P.op("pe", lambda e, pt=pt, k4=k4, kc=kc: e.transpose(out=pt[:, k4 * 128:(k4 + 1) * 128], in_=h_[:, kc * 128:(kc + 1) * 128], identity=identf[:]),
                                         [hB_, identfB], [pB])
                                yield
                                P.op("act", lambda e, pt=pt, half=half: e.activation(out=hT[:, half * 4:(half + 1) * 4, :], in_=pt[:].rearrange("p (k t) -> p k t", k=4), func=AF.Identity),
                                     [pB], [hTB])
                            P.dma("sp", T["HF"][tt * 128:(tt + 1) * 128, :], hb_[:], reads=[hbB_], writes=[HFB])
                            yield
                            pt, pB = bank()
                            for kc in range(8):
                                P.op("pe", lambda e, pt=pt, kc=kc: e.matmul(pt[:, 0:NEXP], lhsT=hT[:, kc, :], rhs=wr[:, kc, :], start=(kc == 0), stop=(kc == 7)),
                                     [hTB, wrB], [pB])
                            yield
                            P.op("dve", lambda e: e.tensor_reduce(out=sm_[:, 0:1], in_=pt[:, 0:NEXP], axis=AX.X, op=ALU.max, negate=True), [pB], [smB_])
                            yield
                            P.op("act", lambda e: e.activation(out=ex_[:], in_=pt[:, 0:NEXP], func=AF.Exp, bias=sm_[:, 0:1], accum_out=sm_[:, 1:2]), [pB, smB_], [exB_, smB_])
                            yield
                            P.op("dve", lambda e: e.reciprocal(out=sm_[:, 2:3], in_=sm_[:, 1:2]), [smB_], [smB_])
                            yield
                            P.op("dve", lambda e: e.tensor_scalar(out=af_[:], in0=ex_[:], scalar1=sm_[:, 2:3], scalar2=None, op0=ALU.mult), [exB_, smB_], [afB_])
                            yield
                            p2, p2B = bank()
                            P.op("pe", lambda e: e.transpose(out=p2[0:NEXP, 0:128], in_=af_[:, 0:NEXP], identity=identf[:]), [afB_, identfB], [p2B])
                            yield
                            P.op("act", lambda e: e.activation(out=AFFT[:, tt * 128:(tt + 1) * 128], in_=p2[0:NEXP, 0:128], func=AF.Identity), [p2B], [AFFTB])

                        run_pipelined(NT, m1_tile, depth=2, on_start=lambda tt: ld_x(tt + 1))
                        P.barrier()
                    with ExitStack() as s2:
                        work = sb(s2, "work", [NEXP, S], F32); workB = Buf()
                        vals = sb(s2, "vals", [NEXP, CAP], F32); valsB = Buf()
                        idxu = sb(s2, "idxu", [NEXP, CAP], U32); idxuB = Buf()
                        idxf = sb(s2, "idxf", [NEXP, CAP], F32); idxfB = Buf()
                        idt = sb(s2, "idt", [128, 4, NEXP], F32); idtB = Buf()
                        P.op("dve", lambda e: e.tensor_copy(out=work[:], in_=AFFT[:]), [AFFTB], [workB])
                        for r in range(CAP // 8):
                            sl = slice(8 * r, 8 * r + 8)
                            P.op("dve", lambda e, sl=sl: e.max(out=vals[:, sl], in_=work[:]), [workB], [valsB])
                            P.op("dve", lambda e, sl=sl: e.max_index(out=idxu[:, sl], in_max=vals[:, sl], in_values=work[:]), [workB, valsB], [idxuB])
                            P.op("dve", lambda e, sl=sl: e.match_replace(out=work[:], in_to_replace=vals[:, sl], in_values=work[:], imm_value=-1.0), [valsB, workB], [workB])
                        P.op("dve", lambda e: e.tensor_copy(out=idxf[:], in_=idxu[:]), [idxuB], [idxfB])
                        for s_ in range(4):
                            pt, pB = bank()
                            P.op("pe", lambda e, pt=pt, s_=s_: e.transpose(out=pt[:, 0:NEXP], in_=idxf[0:NEXP, s_ * 128:(s_ + 1) * 128], identity=identf[0:NEXP, 0:NEXP]),
                                 [idxfB, identfB], [pB])
                            P.op("dve", lambda e, pt=pt, s_=s_: e.tensor_copy(out=idt[:, s_, :], in_=pt[:, 0:NEXP]), [pB], [idtB])
                            P.op("dve", lambda e, s_=s_: e.tensor_copy(out=IDX[:, s_, :], in_=idt[:, s_, :]), [idtB], [IDXB])
                            p2, p2B = bank()
                            P.op("pe", lambda e, p2=p2, s_=s_: e.transpose(out=p2[:, 0:NEXP], in_=vals[0:NEXP, s_ * 128:(s_ + 1) * 128], identity=identf[0:NEXP, 0:NEXP]),
                                 [valsB, identfB], [p2B])
                            P.op("dve", lambda e, p2=p2, s_=s_: e.tensor_copy(out=GVt[:, s_, :], in_=p2[:, 0:NEXP]), [p2B], [GVB])
                        P.barrier()
                with ExitStack() as s1:
                    xs = [sb(s1, f"xs{i}", [128, 4, D], BF16) for i in range(2)]; xsB = [[Buf() for _ in range(4)] for _ in range(2)]
                    xsT = sb(s1, "xsT", [128, 8, CAP], BF16); xsTB = Buf()
                    hid = sb(s1, "hid", [128, 8, CAP], BF16); hidB = Buf()
                    sg = [sb(s1, f"sg{i}", [128, 512], F32) for i in range(2)]; sgB = [Buf(), Buf()]
                    ye = [sb(s1, f"ye{i}", [128, D], F32) for i in range(4)]; yeB = [Buf() for _ in range(4)]

                    def issue_gathers(e_):
                        x_ = xs[e_ % 2]
                        for s_ in range(4):
                            P.dma("pool", None, None, reads=[HFB, IDXB], writes=[xsB[e_ % 2][s_]],
                                  fn=lambda g, s_=s_, x_=x_, e_=e_: g.indirect_dma_start(
                                      out=x_[:, s_, :], out_offset=None, in_=T["HF"][:, :],
                                      in_offset=bass.IndirectOffsetOnAxis(ap=IDX[:, s_, e_:e_ + 1], axis=0)))

                    issue_gathers(0)
                    issue_gathers(1)
                    prev_sc = []
                    for e_ in range(NEXP):
                        (wg, wu, wd), (wgB, wuB, wdB) = wts[e_ % 2], wtsB[e_ % 2]
                        x_ = xs[e_ % 2]
                        for s_ in range(4):
                            pt, pB = bank()
                            ptb = pt[:].bitcast(BF16)
                            for kc in range(8):
                                P.op("pe", lambda e, ptb=ptb, kc=kc, s_=s_, x_=x_: e.transpose(out=ptb[:, kc * 128:(kc + 1) * 128], in_=x_[:, s_, kc * 128:(kc + 1) * 128], identity=identb[:]),
                                     [xsB[e_ % 2][s_], identbB], [pB])
                            P.op("act", lambda e, ptb=ptb, s_=s_: e.activation(out=xsT[:, :, s_ * 128:(s_ + 1) * 128], in_=ptb.rearrange("p (k t) -> p k t", k=8), func=AF.Identity),
                                 [pB], [xsTB])
                        for fcn in range(8):
                            pg, pgB = bank()
                            pu, puB = bank()
                            for kc in range(8):
                                P.op("pe", lambda e, pg=pg, kc=kc, fcn=fcn, wg=wg: e.matmul(pg[:], lhsT=wg[:, kc, fcn * 128:(fcn + 1) * 128], rhs=xsT[:, kc, :], start=(kc == 0), stop=(kc == 7)),
                                     [wgB, xsTB], [pgB])
                            for kc in range(8):
                                P.op("pe", lambda e, pu=pu, kc=kc, fcn=fcn, wu=wu: e.matmul(pu[:], lhsT=wu[:, kc, fcn * 128:(fcn + 1) * 128], rhs=xsT[:, kc, :], start=(kc == 0), stop=(kc == 7)),
                                     [wuB, xsTB], [puB])
                            s__, sB__ = sg[fcn % 2], sgB[fcn % 2]
                            P.op("act", lambda e, pg=pg, s__=s__: e.activation(out=s__[:], in_=pg[:], func=AF.Silu), [pgB], [sB__])
                            P.op("dve", lambda e, pu=pu, s__=s__, fcn=fcn: e.tensor_tensor(out=hid[:, fcn, :], in0=pu[:], in1=s__[:], op=ALU.mult), [puB, sB__], [hidB])
                        for s_ in range(4):
                            y, yB = ye[s_], yeB[s_]
                            for nh in range(2):
                                pt, pB = bank()
                                for fcn in range(8):
                                    P.op("pe", lambda e, pt=pt, fcn=fcn, s_=s_, nh=nh, wd=wd: e.matmul(pt[:], lhsT=hid[:, fcn, s_ * 128:(s_ + 1) * 128], rhs=wd[:, fcn, nh * 512:(nh + 1) * 512],
                                                                                                 start=(fcn == 0), stop=(fcn == 7)), [hidB, wdB], [pB])
                                P.op("act", lambda e, pt=pt, y=y, nh=nh, s_=s_, e_=e_: e.activation(out=y[:, nh * 512:(nh + 1) * 512], in_=pt[:], func=AF.Identity, scale=GVt[:, s_, e_:e_ + 1]),
                                     [pB, GVB], [yB])
                        if e_ + 2 < NEXP:
                            issue_wloads(e_ + 2)
                        if ACCB.w is not None:
                            P._wait("pool", ACCB.w)
                        for t_ in prev_sc:
                            P._wait("pool", t_)
                        cur_sc = []
                        for s_ in range(4):
                            y, yB = ye[s_], yeB[s_]
                            cur_sc.append(P.dma("pool", None, None, reads=[yB, IDXB], writes=[ACCB],
                                                fn=lambda g, s_=s_, y=y, e_=e_: g.indirect_dma_start(
                                                    out=T["ACC"][:, :], out_offset=bass.IndirectOffsetOnAxis(ap=IDX[:, s_, e_:e_ + 1], axis=0),
                                                    in_=y[:], in_offset=None, compute_op=ALU.add)))
                        prev_sc[:] = cur_sc
                        if e_ + 2 < NEXP:
                            issue_gathers(e_ + 2)
                    P.barrier()
                with ExitStack() as s1:
                    xb = [sb(s1, f"cx{i}", [128, D], F32) for i in range(3)]; xbB = [Buf(), Buf(), Buf()]
                    ab = [sb(s1, f"ca{i}", [128, D], F32) for i in range(3)]; abB = [Buf(), Buf(), Buf()]
                    ob_ = [sb(s1, f"co{i}", [128, D], F32) for i in range(2)]; obB_ = [Buf(), Buf()]
                    if final:
                        nx = NormCtx(s1, "f")
                        fg = sb(s1, "fg", [128, D], F32); fgB = Buf()
                        bcast_load("sp", fg[:], fgB, T["final_norm_g"])

                    def ld_c(tt):
                        if tt < NT:
                            P.dma("sp", xb[tt % 3][:], XIN[tt * 128:(tt + 1) * 128, :], reads=[XINB], writes=[xbB[tt % 3]])
                            P.dma("sp", ab[tt % 3][:], T["ACC"][tt * 128:(tt + 1) * 128, :], reads=[ACCB], writes=[abB[tt % 3]])

                    ld_c(0)

                    def c_tile(tt):
                        xt, xtB = xb[tt % 3], xbB[tt % 3]
                        a_, aB_ = ab[tt % 3], abB[tt % 3]
                        P.op("pool", lambda e: e.tensor_tensor(out=a_[:], in0=a_[:], in1=G[:], op=ALU.mult), [aB_, GB], [aB_])
                        yield
                        P.op("dve", lambda e: e.tensor_tensor(out=a_[:], in0=a_[:], in1=xt[:], op=ALU.add), [aB_, xtB], [aB_])
                        yield
                        if final:
                            o_, oB_ = ob_[tt % 2], obB_[tt % 2]
                            yield from norm_mod_g(nx, a_[:], aB_, fg, fgB, None, None, o_[:], oB_)
                            yield
                            P.dma("sp", XOUT[tt * 128:(tt + 1) * 128, :], o_[:], reads=[oB_], writes=[XOUTB])
                        else:
                            P.dma("sp", XOUT[tt * 128:(tt + 1) * 128, :], a_[:], reads=[aB_], writes=[XOUTB])

                    run_pipelined(NT, c_tile, depth=2, on_start=lambda tt: ld_c(tt + 1))
                    P.barrier()

        def bank_fixed(i):
            return ps[i], psB[i]

        SCALE = 1.0 / math.sqrt(192.0)
        C1 = 6.28125
        C2 = TWO_PI - 6.28125
        QNB = Buf(); QPB = Buf(); KNB = Buf(); KPB = Buf(); VB = Buf()

        def phase_mla_proj(XIN, XINB):
            with ExitStack() as st:
                cqnT = sb(st, "cqnT", [128, 2, S], BF16); cqnTB = Buf()
                ckvT = sb(st, "ckvT", [128, S], BF16); ckvTB = Buf()
                kpT = sb(st, "kpT", [64, S], BF16); kpTB = Buf()
                cosT = sb(st, "cosT", [64, S], F32); cosTB = Buf()
                sinT = sb(st, "sinT", [64, S], F32); sinTB = Buf()
                with ExitStack() as s2:
                    posi = sb(s2, "posi", [64, S], I32); posiB = Buf()
                    ang = sb(s2, "ang", [64, S], F32); angB = Buf()
                    a2 = sb(s2, "a2", [64, S], F32); a2B = Buf()
                    kq = sb(s2, "kq", [64, S], I32); kqB = Buf()
                    kqf = sb(s2, "kqf", [64, S], F32); kqfB = Buf()
                    ivf = sb(s2, "ivf", [64, 1], F32); ivfB = Buf()
                    P.dma("sp", posi[:], T["pos"].partition_broadcast(64), writes=[posiB])
                    P.dma("sp", ivf[0:32, :], T["invf"].rearrange("(p o) -> p o", o=1), writes=[ivfB])
                    P.dma("sp", ivf[32:64, :], T["invf"].rearrange("(p o) -> p o", o=1), writes=[ivfB])
                    P.op("dve", lambda e: e.tensor_copy(out=ang[:], in_=posi[:]), [posiB], [angB])
                    P.op("dve", lambda e: e.tensor_scalar(out=ang[:], in0=ang[:], scalar1=ivf[:, 0:1], scalar2=None, op0=ALU.mult), [angB, ivfB], [angB])
                    for shift, dst, dB in ((0.0, sinT, sinTB), (math.pi / 2.0, cosT, cosTB)):
                        P.op("dve", lambda e, shift=shift: e.tensor_scalar(out=a2[:], in0=ang[:], scalar1=shift, scalar2=None, op0=ALU.add), [angB], [a2B])
                        P.op("dve", lambda e: e.tensor_scalar(out=kq[:], in0=a2[:], scalar1=1.0 / TWO_PI, scalar2=None, op0=ALU.mult), [a2B], [kqB])
                        P.op("dve", lambda e: e.tensor_copy(out=kqf[:], in_=kq[:]), [kqB], [kqfB])
                        P.op("dve", lambda e: e.scalar_tensor_tensor(out=a2[:], in0=kqf[:], scalar=-C1, in1=a2[:], op0=ALU.mult, op1=ALU.add), [kqfB, a2B], [a2B])
                        P.op("dve", lambda e: e.scalar_tensor_tensor(out=a2[:], in0=kqf[:], scalar=-C2, in1=a2[:], op0=ALU.mult, op1=ALU.add), [kqfB, a2B], [a2B])
                        P.op("dve", lambda e: e.tensor_scalar(out=a2[:], in0=a2[:], scalar1=math.pi, scalar2=-math.pi, op0=ALU.min, op1=ALU.max), [a2B], [a2B])
                        P.op("act", lambda e, dst=dst: e.activation(out=dst[:], in_=a2[:], func=AF.Sin), [a2B], [dB])
                    P.barrier()
                with ExitStack() as s2:
                    (A, AB), (Bt, BB), _ = load_mod_tiles(s2, 1, 0, "norm_mix_g")
                    nx = NormCtx(s2, "a")
                    win = sb(s2, "win", [128, 8, 448], BF16); winB = Buf()
                    wrot = sb(s2, "wrot", [128, 8, 64], BF16); wrotB = Buf()
                    P.dma("pool", win[:], T["mla_w_in"][0].rearrange("(kc p) n -> p kc n", p=128), writes=[winB])
                    P.op("dve", lambda e: e.tensor_scalar(out=wrot[:, :, 0:32], in0=win[:, :, 416:448], scalar1=-1.0, scalar2=None, op0=ALU.mult), [winB], [wrotB])
                    P.op("dve", lambda e: e.tensor_copy(out=wrot[:, :, 32:64], in_=win[:, :, 384:416]), [winB], [wrotB])
                    qg = sb(s2, "qg", [128, 256], F32); qgB = Buf()
                    kg = sb(s2, "kg", [128, 128], F32); kgB = Buf()
                    bcast_load("sp", qg[:], qgB, T["mla_q_norm_g"][0, :])
                    bcast_load("sp", kg[:], kgB, T["mla_kv_norm_g"][0, :])
                    xb = [sb(s2, f"ax{i}", [128, D], F32) for i in range(3)]; xbB = [Buf(), Buf(), Buf()]
                    hb = [sb(s2, f"ahb{i}", [128, D], BF16) for i in range(2)]; hbB = [Buf(), Buf()]
                    hT = [sb(s2, f"ahT{i}", [128, 8, 128], BF16) for i in range(2)]; hTB = [Buf(), Buf()]
                    jq = sb(s2, "jq", [128, 256], F32); jqB = Buf()
                    sqs = [sb(s2, f"sq{i}", [128, 8], F32) for i in range(2)]; sqsB = [Buf(), Buf()]
                    cn = [sb(s2, f"cn{i}", [128, 384], BF16) for i in range(2)]; cnB = [Buf(), Buf()]
                    r1s = [sb(s2, f"r1{i}", [64, 128], F32) for i in range(2)]; r1sB = [Buf(), Buf()]
                    r2s = [sb(s2, f"r2{i}", [64, 128], F32) for i in range(2)]; r2sB = [Buf(), Buf()]

                    def ld_ax(tt):
                        if tt < NT:
                            P.dma("sp", xb[tt % 3][:], XIN[tt * 128:(tt + 1) * 128, :], reads=[XINB], writes=[xbB[tt % 3]])

                    ld_ax(0)

                    def a_tile(tt):
                        k = tt % 2
                        tsl = slice(tt * 128, (tt + 1) * 128)
                        xt, xtB = xb[tt % 3], xbB[tt % 3]
                        h_, hB_ = hb[k], hbB[k]
                        ht, htB = hT[k], hTB[k]
                        sq, sqB = sqs[k], sqsB[k]
                        c_, cB_ = cn[k], cnB[k]
                        r1, r1B = r1s[k], r1sB[k]
                        r2, r2B = r2s[k], r2sB[k]
                        yield from norm_mod_g(nx, xt[:], xtB, A, AB, Bt, BB, h_[:], hB_)
                        yield
                        pt, pB = bank()
                        ptb = pt[:].bitcast(BF16)
                        for kc in range(8):
                            P.op("pe", lambda e, kc=kc: e.transpose(out=ptb[:, kc * 128:(kc + 1) * 128], in_=h_[:, kc * 128:(kc + 1) * 128], identity=identb[:]),
                                 [hB_, identbB], [pB])
                        yield
                        P.op("act", lambda e: e.activation(out=ht[:], in_=ptb.rearrange("p (k t) -> p k t", k=8), func=AF.Identity), [pB], [htB])
                        yield
                        pa, paB = bank()
                        for kc in range(8):
                            P.op("pe", lambda e, kc=kc: e.matmul(pa[:, 0:384], lhsT=ht[:, kc, :], rhs=win[:, kc, 0:384], start=(kc == 0), stop=(kc == 7)),
                                 [htB, winB], [paB])
                        pk1, pk1B = bank()
                        for kc in range(8):
                            P.op("pe", lambda e, kc=kc: e.matmul(pk1[0:64, 0:128], lhsT=win[:, kc, 384:448], rhs=ht[:, kc, :], start=(kc == 0), stop=(kc == 7)),
                                 [htB, winB], [pk1B])
                        pk2, pk2B = bank()
                        for kc in range(8):
                            P.op("pe", lambda e, kc=kc: e.matmul(pk2[0:64, 0:128], lhsT=wrot[:, kc, :], rhs=ht[:, kc, :], start=(kc == 0), stop=(kc == 7)),
                                 [htB, wrotB], [pk2B])
                        yield
                        P.op("dve", lambda e: e.tensor_tensor(out=r1[:], in0=pk1[0:64, 0:128], in1=cosT[:, tsl], op=ALU.mult), [pk1B, cosTB], [r1B])
                        P.op("dve", lambda e: e.tensor_tensor(out=r2[:], in0=pk2[0:64, 0:128], in1=sinT[:, tsl], op=ALU.mult), [pk2B, sinTB], [r2B])
                        yield
                        P.op("pool", lambda e: e.tensor_tensor(out=kpT[:, tsl], in0=r1[:], in1=r2[:], op=ALU.add), [r1B, r2B], [kpTB])
                        groups = ((0, 256, qg, qgB, 256.0, 0), (256, 384, kg, kgB, 128.0, 4))
                        for (lo, hi, gt_, gB_, n_, o4) in groups:
                            P.op("act", lambda e, lo=lo, hi=hi, o4=o4: e.activation(out=jq[:, 0:hi - lo], in_=pa[:, lo:hi], func=AF.Square, accum_out=sq[:, o4:o4 + 1]),
                                 [paB], [jqB, sqB])
                        yield
                        for (lo, hi, gt_, gB_, n_, o4) in groups:
                            P.op("dve", lambda e, o4=o4, n_=n_: e.tensor_scalar(out=sq[:, o4 + 1:o4 + 2], in0=sq[:, o4:o4 + 1], scalar1=1.0 / n_, scalar2=EPS, op0=ALU.mult, op1=ALU.add), [sqB], [sqB])
                        yield
                        for (lo, hi, gt_, gB_, n_, o4) in groups:
                            P.op("act", lambda e, o4=o4: e.activation(out=sq[:, o4 + 2:o4 + 3], in_=sq[:, o4 + 1:o4 + 2], func=AF.Sqrt), [sqB], [sqB])
                        yield
                        for (lo, hi, gt_, gB_, n_, o4) in groups:
                            P.op("dve", lambda e, o4=o4: e.reciprocal(out=sq[:, o4 + 3:o4 + 4], in_=sq[:, o4 + 2:o4 + 3]), [sqB], [sqB])
                        yield
                        for (lo, hi, gt_, gB_, n_, o4) in groups:
                            P.op("dve", lambda e, lo=lo, hi=hi, o4=o4, gt_=gt_: e.scalar_tensor_tensor(out=c_[:, lo:hi], in0=pa[:, lo:hi], scalar=sq[:, o4 + 3:o4 + 4], in1=gt_[:],
                                                                                                op0=ALU.mult, op1=ALU.mult), [paB, sqB, gB_], [cB_])
                        yield
                        p3, p3B = bank()
                        p3b = p3[:].bitcast(BF16)
                        for j in range(3):
                            P.op("pe", lambda e, j=j: e.transpose(out=p3b[:, j * 128:(j + 1) * 128], in_=c_[:, j * 128:(j + 1) * 128], identity=identb[:]),
                                 [cB_, identbB], [p3B])
                        yield
                        P.op("act", lambda e: e.activation(out=cqnT[:, :, tsl], in_=p3b[:, 0:256].rearrange("p (k t) -> p k t", k=2), func=AF.Identity), [p3B], [cqnTB])
                        P.op("act", lambda e: e.activation(out=ckvT[:, tsl], in_=p3b[:, 256:384], func=AF.Identity), [p3B], [ckvTB])

                    run_pipelined(NT, a_tile, depth=2, on_start=lambda tt: ld_ax(tt + 1))
                    P.dma("sp", T["KP"][:, :], kpT[:], reads=[kpTB], writes=[KPB])
                    P.barrier()
                with ExitStack() as s2:
                    wqn = sb(s2, "wqn", [128, 2, 8, 128], BF16); wqnB = Buf()
                    wqp = sb(s2, "wqp", [128, 2, 8, 64], BF16); wqpB = Buf()
                    wqr = sb(s2, "wqr", [128, 2, 8, 64], BF16); wqrB = Buf()
                    wkk = sb(s2, "wkk", [128, 8, 128], BF16); wkkB = Buf()
                    wkv = sb(s2, "wkv", [128, 8, 128], BF16); wkvB = Buf()
                    qv = T["mla_w_qb"][0].rearrange("(k2 p) (h c) -> p k2 h c", p=128, c=192)
                    for k2 in range(2):
                        P.dma("pool", wqn[:, k2], qv[:, k2, :, 0:128], writes=[wqnB])
                        P.dma("pool", wqp[:, k2], qv[:, k2, :, 128:192], writes=[wqpB])
                    kvv = T["mla_w_kvb"][0].rearrange("k (h c) -> k h c", c=256)
                    P.dma("pool", wkk[:], kvv[:, :, 0:128], writes=[wkkB])
                    P.dma("pool", wkv[:], kvv[:, :, 128:256], writes=[wkvB])
                    P.op("dve", lambda e: e.tensor_scalar(out=wqr[:, :, :, 0:32], in0=wqp[:, :, :, 32:64], scalar1=-1.0, scalar2=None, op0=ALU.mult), [wqpB], [wqrB])
                    P.op("dve", lambda e: e.tensor_copy(out=wqr[:, :, :, 32:64], in_=wqp[:, :, :, 0:32]), [wqpB], [wqrB])
                    qn = [sb(s2, f"qn{i}", [128, S], BF16) for i in range(2)]; qnB = [Buf(), Buf()]
                    kn = [sb(s2, f"kn{i}", [128, S], BF16) for i in range(2)]; knB = [Buf(), Buf()]
                    qp = [sb(s2, f"qp{i}", [64, S], BF16) for i in range(2)]; qpB = [Buf(), Buf()]
                    r1 = sb(s2, "q1", [64, 512], F32); r1B = Buf()
                    r2 = sb(s2, "q2", [64, 512], F32); r2B = Buf()
                    vt = [sb(s2, f"vt{i}", [128, D], BF16) for i in range(2)]; vtB = [Buf(), Buf()]
                    for tt in range(NT):
                        tsl = slice(tt * 128, (tt + 1) * 128)
                        v_, vB_ = vt[tt % 2], vtB[tt % 2]
                        for nh in range(2):
                            pt, pB = bank()
                            P.op("pe", lambda e, pt=pt, tsl=tsl, nh=nh: e.matmul(pt[:], lhsT=ckvT[:, tsl], rhs=wkv[:, nh * 4:(nh + 1) * 4, :], start=True, stop=True),
                                 [ckvTB, wkvB], [pB])
                            P.op("act", lambda e, pt=pt, nh=nh, v_=v_: e.activation(out=v_[:, nh * 512:(nh + 1) * 512], in_=pt[:], func=AF.Identity), [pB], [vB_])
                        P.dma("sp", T["V"][tsl, :], v_[:], reads=[vB_], writes=[VB])
                    for h in range(8):
                        q_, qB_ = qn[h % 2], qnB[h % 2]
                        k_, kB_ = kn[h % 2], knB[h % 2]
                        p_, pB_ = qp[h % 2], qpB[h % 2]
                        for tch in range(8):
                            csl = slice(tch * 512, (tch + 1) * 512)
                            pt, pB = bank()
                            for k2 in range(2):
                                P.op("pe", lambda e, pt=pt, k2=k2, h=h, csl=csl: e.matmul(pt[:], lhsT=wqn[:, k2, h, :], rhs=cqnT[:, k2, csl], start=(k2 == 0), stop=(k2 == 1)),
                                     [wqnB, cqnTB], [pB])
                            P.op("act", lambda e, pt=pt, q_=q_, csl=csl: e.activation(out=q_[:, csl], in_=pt[:], func=AF.Identity), [pB], [qB_])
                            pt2, pB2 = bank()
                            P.op("pe", lambda e, pt2=pt2, h=h, csl=csl: e.matmul(pt2[:], lhsT=wkk[:, h, :], rhs=ckvT[:, csl], start=True, stop=True), [wkkB, ckvTB], [pB2])
                            P.op("act", lambda e, pt2=pt2, k_=k_, csl=csl: e.activation(out=k_[:, csl], in_=pt2[:], func=AF.Identity), [pB2], [kB_])
                            pa, paB = bank()
                            for k2 in range(2):
                                P.op("pe", lambda e, pa=pa, k2=k2, h=h, csl=csl: e.matmul(pa[0:64, :], lhsT=wqp[:, k2, h, :], rhs=cqnT[:, k2, csl], start=(k2 == 0), stop=(k2 == 1)),
                                     [wqpB, cqnTB], [paB])
                            pb_, pbB = bank()
                            for k2 in range(2):
                                P.op("pe", lambda e, pb_=pb_, k2=k2, h=h, csl=csl: e.matmul(pb_[0:64, :], lhsT=wqr[:, k2, h, :], rhs=cqnT[:, k2, csl], start=(k2 == 0), stop=(k2 == 1)),
                                     [wqrB, cqnTB], [pbB])
                            P.op("dve", lambda e, pa=pa, csl=csl: e.tensor_tensor(out=r1[:], in0=pa[0:64, :], in1=cosT[:, csl], op=ALU.mult), [paB, cosTB], [r1B])
                            P.op("dve", lambda e, pb_=pb_, csl=csl: e.tensor_tensor(out=r2[:], in0=pb_[0:64, :], in1=sinT[:, csl], op=ALU.mult), [pbB, sinTB], [r2B])
                            P.op("pool", lambda e, p_=p_, csl=csl: e.tensor_tensor(out=p_[:, csl], in0=r1[:], in1=r2[:], op=ALU.add), [r1B, r2B], [pB_])
                        P.dma("sp", T["QN"][h], q_[:], reads=[qB_], writes=[QNB])
                        P.dma("sp", T["KN"][h], k_[:], reads=[kB_], writes=[KNB])
                        P.dma("sp", T["QP"][h], p_[:], reads=[pB_], writes=[QPB])
                    P.barrier()

        def phase_mla_attn(XIN, XINB, XOUT, XOUTB):
            with ExitStack() as st:
                OT = sb(st, "OT", [128, 8, S], BF16); OTB = Buf()
                with ExitStack() as s2:
                    kp = sb(s2, "kp", [128, S], BF16); kpB = Buf()
                    P.dma("sp", kp[0:64, :], T["KP"][:, :], reads=[KPB], writes=[kpB])
                    P.dma("sp", kp[64:128, :], T["KP"][:, :], reads=[KPB], writes=[kpB])
                    ones = sb(s2, "ones", [128, 128], BF16); onesB = Buf()
                    P.op("dve", lambda e: e.memset(ones[:], 1.0), [], [onesB])
                    qn = [sb(s2, f"aq{i}", [128, S], BF16) for i in range(2)]; qnB = [Buf(), Buf()]
                    kn = [sb(s2, f"ak{i}", [128, S], BF16) for i in range(2)]; knB = [Buf(), Buf()]
                    qp = [sb(s2, f"ap{i}", [128, S], BF16) for i in range(2)]; qpB = [Buf(), Buf()]
                    vh = [sb(s2, f"av{i}", [128, NT, 128], BF16) for i in range(2)]; vhB = [Buf(), Buf()]
                    pT = [sb(s2, f"pT{i}", [128, 512], BF16) for i in range(3)]; pTB = [Buf(), Buf(), Buf()]
                    rs = sb(s2, "rs", [128, 512], F32); rsB = Buf()
                    npt = 0
                    nst = 0
                    vv = T["V"].rearrange("(tt p) c -> p tt c", p=128)

                    def load_head(h):
                        P.dma("sp", qn[h % 2][:], T["QN"][h], reads=[QNB], writes=[qnB[h % 2]])
                        P.dma("sp", kn[h % 2][:], T["KN"][h], reads=[KNB], writes=[knB[h % 2]])
                        P.dma("sp", qp[h % 2][0:64, :], T["QP"][h], reads=[QPB], writes=[qpB[h % 2]])
                        P.dma("sp", qp[h % 2][64:128, :], T["QP"][h], reads=[QPB], writes=[qpB[h % 2]])
                        P.dma("sp", vh[h % 2][:], vv[:, :, h * 128:(h + 1) * 128], reads=[VB], writes=[vhB[h % 2]])

                    load_head(0)
                    for h in range(8):
                        if h + 1 < 8:
                            load_head(h + 1)
                        q_, qB_ = qn[h % 2], qnB[h % 2]
                        k_, kB_ = kn[h % 2], knB[h % 2]
                        p_, pB_ = qp[h % 2], qpB[h % 2]
                        v_, vB_ = vh[h % 2], vhB[h % 2]
                        for qc in range(8):
                            csl = slice(qc * 512, (qc + 1) * 512)
                            po, poB = bank_fixed(4 + qc % 2)
                            pz, pzB = bank_fixed(6 + qc % 2)
                            def qk_pair(kt0, k_=k_, kB_=kB_, q_=q_, qB_=qB_, p_=p_, pB_=pB_, csl=csl):
                                outs = []
                                for j in range(2):
                                    ksl = slice((kt0 + j) * 128, (kt0 + j + 1) * 128)
                                    pst, pstB = bank_fixed((kt0 + j) % 4)
                                    P.op("pe", lambda e, pst=pst, ksl=ksl: e.matmul(pst[:], lhsT=k_[:, ksl], rhs=q_[:, csl], start=True, stop=False),
                                         [kB_, qB_], [pstB])
                                    outs.append((pst, pstB, ksl))
                                for j, (pst, pstB, ksl) in enumerate(outs):
                                    lo = 64 * j
                                    P.op("pe", lambda e, pst=pst, ksl=ksl, lo=lo: e.matmul(pst[:], lhsT=kp[lo:lo + 64, ksl], rhs=p_[lo:lo + 64, csl], start=False, stop=True),
                                         [kpB, pB_], [pstB])
                                return [(o[0], o[1]) for o in outs]

                            pend = {}
                            for kt0 in (0, 2):
                                r_ = qk_pair(kt0)
                                pend[kt0], pend[kt0 + 1] = r_
                            for kt in range(NT):
                                pst, pstB = pend.pop(kt)
                                t_, tB_ = pT[npt % 3], pTB[npt % 3]
                                npt += 1
                                P.op("act", lambda e, pst=pst, t_=t_: e.activation(out=t_[:], in_=pst[:], func=AF.Exp, scale=SCALE), [pstB], [tB_])
                                if kt % 2 == 1 and kt + 3 < NT:
                                    r_ = qk_pair(kt + 3)
                                    pend[kt + 3], pend[kt + 4] = r_
                                P.op("pe", lambda e, po=po, kt=kt, t_=t_, v_=v_: e.matmul(po[:], lhsT=v_[:, kt, :], rhs=t_[:], start=(kt == 0), stop=(kt == NT - 1)),
                                     [vB_, tB_], [poB])
                                P.op("pe", lambda e, pz=pz, kt=kt, t_=t_: e.matmul(pz[:], lhsT=ones[:], rhs=t_[:], start=(kt == 0), stop=(kt == NT - 1)),
                                     [onesB, tB_], [pzB])
                            P.op("dve", lambda e, pz=pz: e.reciprocal(out=rs[:], in_=pz[:]), [pzB], [rsB])
                            P.op("dve", lambda e, po=po, h=h, csl=csl: e.tensor_tensor(out=OT[:, h, csl], in0=po[:], in1=rs[:], op=ALU.mult), [poB, rsB], [OTB])
                    P.barrier()
                with ExitStack() as s2:
                    wo = sb(s2, "mwo", [128, 8, D], BF16); woB = Buf()
                    wv = T["mla_w_out"][0].rearrange("(kc p) n -> p kc n", p=128)
                    for q in range(2):
                        P.dma("pool", wo[:, q * 4:(q + 1) * 4, :], wv[:, q * 4:(q + 1) * 4, :], writes=[woB])
                    _, _, (G, GB) = load_mod_tiles(s2, 1, 0, "norm_mix_g")
                    xb = [sb(s2, f"bx{i}", [128, D], F32) for i in range(2)]; xbB = [Buf(), Buf()]
                    yo = [sb(s2, f"by{i}", [128, D], F32) for i in range(2)]; yoB = [Buf(), Buf()]
                    P.dma("sp", xb[0][:], XIN[0:128, :], reads=[XINB], writes=[xbB[0]])
                    for tt in range(NT):
                        tsl = slice(tt * 128, (tt + 1) * 128)
                        xt, xtB = xb[tt % 2], xbB[tt % 2]
                        y, yB = yo[tt % 2], yoB[tt % 2]
                        if tt + 1 < NT:
                            P.dma("sp", xb[(tt + 1) % 2][:], XIN[(tt + 1) * 128:(tt + 2) * 128, :], reads=[XINB], writes=[xbB[(tt + 1) % 2]])
                        for nh in range(2):
                            pt, pB = bank()
                            for hh in range(8):
                                P.op("pe", lambda e, pt=pt, hh=hh, tsl=tsl, nh=nh: e.matmul(pt[:], lhsT=OT[:, hh, tsl], rhs=wo[:, hh, nh * 512:(nh + 1) * 512], start=(hh == 0), stop=(hh == 7)),
                                     [OTB, woB], [pB])
                            sl = slice(nh * 512, (nh + 1) * 512)
                            P.op("dve", lambda e, pt=pt, sl=sl, y=y: e.tensor_tensor(out=y[:, sl], in0=pt[:], in1=G[:, sl], op=ALU.mult), [pB, GB], [yB])
                        P.op("pool", lambda e, y=y, xt=xt: e.tensor_tensor(out=y[:], in0=y[:], in1=xt[:], op=ALU.add), [yB, xtB], [yB])
                        P.dma("sp", XOUT[tsl, :], y[:], reads=[yB], writes=[XOUTB])
                    P.barrier()

        def copy_to_out(src, srcB):
            with ExitStack() as st:
                cb = [sb(st, f"cpy{i}", [128, D], F32) for i in range(2)]; cbB = [Buf(), Buf()]
                for tt in range(NT):
                    t_, tB = cb[tt % 2], cbB[tt % 2]
                    P.dma("sp", t_[:], src[tt * 128:(tt + 1) * 128, :], reads=[srcB], writes=[tB])
                    P.dma("sp", OUT[tt * 128:(tt + 1) * 128, :], t_[:], reads=[tB], writes=[OUTB])
                P.barrier()

        OUTB = Buf()

        phase_mod()
        phase_filter()
        phase_hyena_in()
        phase_hyena_fwd()
        phase_hyena_out()
        if stage == "hyena":
            copy_to_out(T["XA"], XAB)
        else:
            XBB = Buf()
            phase_moe(0, T["XA"], XAB, T["XB"], XBB, False)
            if stage == "moe0":
                copy_to_out(T["XB"], XBB)
            else:
                XCB = Buf()
                phase_mla_proj(T["XB"], XBB)
                phase_mla_attn(T["XB"], XBB, T["XC"], XCB)
                if stage == "mla":
                    copy_to_out(T["XC"], XCB)
                else:
                    phase_moe(1, T["XC"], XCB, OUT, OUTB, True)
        P.barrier()
    return nc


def _prep_inputs(inputs):
    global _CONSTS
    if _CONSTS is None:
        _CONSTS = _consts()
    shared = {}
    for k, v in inputs.items():
        if k in ("x", "c", "positions"):
            continue
        shared[k] = np.ascontiguousarray(np.asarray(v))
    shared.update(_CONSTS)
    in_maps = []
    x = np.asarray(inputs["x"]); c = np.asarray(inputs["c"]); pos = np.asarray(inputs["positions"])
    for b in range(x.shape[0]):
        m = dict(shared)
        m["x"] = np.ascontiguousarray(x[b])
        m["c"] = np.ascontiguousarray(c[b:b + 1])
        m["pos"] = np.ascontiguousarray(pos[b].astype(np.int32))
        in_maps.append(m)
    return in_maps


def kernel(**inputs):
    in_maps = _prep_inputs(inputs)
    nc = build("all")
    res = run_bass_kernel_spmd(nc, in_maps, core_ids=list(range(len(in_maps))))
    return np.stack([r["out"] for r in res.results], axis=0).astype(np.float32)
```
